# Optimizing a Trainium2 kernel written in Bass

```python
import math
import jax
import jax.numpy as jnp
from jax import lax
import numpy as np

D_MODEL = 1024
BATCH = 16
SEQ = 2048
DEPTH = 2

F32 = jnp.float32
CTX_LEN = 256
GRID_W = 64
N_EVEN = (DEPTH + 1) // 2
N_ODD = DEPTH // 2
N_MOD = 6
NORM_EPS = 1e-6

ML_HEADS = 4
ML_DH = 128
ML_W = ML_HEADS * ML_DH
ML_CHUNK = 64

AT_HEADS = 8
AT_KV = 2
AT_G = AT_HEADS // AT_KV
AT_DH = 64
AT_W = AT_HEADS * AT_DH
AT_KVW = AT_KV * AT_DH
Q_BLOCK = 128
ROPE_THETA = 10000.0
ROPE_NF = AT_DH // 4

EV_SPLITS = (ML_W, 2 * ML_W, 3 * ML_W, 4 * ML_W, 4 * ML_W + 4 * ML_HEADS,
             4 * ML_W + 4 * ML_HEADS + AT_W, 4 * ML_W + 4 * ML_HEADS + AT_W + AT_KVW)
EV_IN = 4 * ML_W + 4 * ML_HEADS + AT_W + 2 * AT_KVW
EV_MIX = ML_W + AT_W

S5_W = 512
S5_GSIZE = 16
S5_GROUPS = S5_W // S5_GSIZE
S5_N = 64
S5_CHUNK = 128

SSD_HEADS = 8
SSD_P = 64
SSD_W = SSD_HEADS * SSD_P
SSD_G = 2
SSD_N = 128
SSD_CONV = 3
SSD_CHUNK = 128
SSD_CONV_CH = SSD_W + 2 * SSD_G * SSD_N

OD_SPLITS = (S5_W, S5_W + SSD_W, S5_W + SSD_W + SSD_CONV_CH)
OD_IN = S5_W + SSD_W + SSD_CONV_CH + 2 * SSD_HEADS
OD_MIX = S5_W + SSD_W

MOE_GROUPS = 4
MOE_PER_GROUP = 4
N_EXPERTS = MOE_GROUPS * MOE_PER_GROUP
MOE_TOPK = 2
EXPERT_HIDDEN = 512

kernel_name = "hybrid_mlstm_gqa_s5_ssd_hmoe"


def rms_norm(x, g, eps=NORM_EPS):
    xf = x.astype(F32)
    y = xf * lax.rsqrt(jnp.mean(xf * xf, axis=-1, keepdims=True) + eps)
    return (y * g.astype(F32)).astype(x.dtype)


def adaln(cond, w, b):
    m = jax.nn.silu(cond) @ w + b
    return jnp.split(m[..., None, :], N_MOD, axis=-1)


def _flip(a, rev, axis):
    return jnp.flip(a, axis=axis) if rev else a


def axial_rope(rows):
    row = jnp.repeat(jnp.arange(rows, dtype=F32), GRID_W)
    col = jnp.tile(jnp.arange(GRID_W, dtype=F32), rows)
    inv = 1.0 / (ROPE_THETA ** (jnp.arange(ROPE_NF, dtype=F32) / ROPE_NF))
    ar = row[:, None] * inv
    ac = col[:, None] * inv
    return (jnp.cos(ar), jnp.sin(ar), jnp.cos(ac), jnp.sin(ac))


def _rotate(x, cos, sin):
    x1, x2 = jnp.split(x, 2, axis=-1)
    return jnp.concatenate([x1 * cos - x2 * sin, x2 * cos + x1 * sin], axis=-1)


def apply_axial_rope(x, rope):
    cr, sr, cc, sc = [t.astype(x.dtype) for t in rope]
    xr, xc = jnp.split(x, 2, axis=-1)
    return jnp.concatenate([_rotate(xr, cr, sr), _rotate(xc, cc, sc)], axis=-1)


def mlstm_scan(q, k, v, li, lf, state0):
    Bsz, H, L, dh = q.shape
    nc = L // ML_CHUNK

    def blocks(a):
        return jnp.moveaxis(a.reshape(Bsz, H, nc, ML_CHUNK, *a.shape[3:]), 2, 0)

    mask = jnp.tril(jnp.ones((ML_CHUNK, ML_CHUNK), dtype=bool))

    def step(carry, inp):
        C0, n0, m0 = carry
        qc, kc, vc, lic, lfc = inp
        b = jnp.cumsum(lfc, axis=-1)
        logw = jnp.where(mask, b[..., :, None] - b[..., None, :] + lic[..., None, :], -jnp.inf)
        inter = b + m0[..., None]
        m = jnp.maximum(inter, jnp.max(logw, axis=-1))
        w = jnp.exp(logw - m[..., None])
        g = jnp.exp(inter - m)
        s = jnp.einsum('bhjd,bhsd->bhjs', qc, kc) * w
        num = jnp.einsum('bhjs,bhsd->bhjd', s, vc) + g[..., None] * jnp.einsum('bhvk,bhjk->bhjv', C0, qc)
        den = jnp.sum(s, axis=-1) + g * jnp.einsum('bhk,bhjk->bhj', n0, qc)
        hout = num / jnp.maximum(jnp.abs(den), jnp.exp(-m))[..., None]
        b_end = b[..., -1:]
        logw_end = b_end - b + lic
        m_new = jnp.maximum(b_end[..., 0] + m0, jnp.max(logw_end, axis=-1))
        w_end = jnp.exp(logw_end - m_new[..., None])
        decay = jnp.exp(b_end[..., 0] + m0 - m_new)
        C_new = decay[..., None, None] * C0 + jnp.einsum('bhs,bhsv,bhsk->bhvk', w_end, vc, kc)
        n_new = decay[..., None] * n0 + jnp.einsum('bhs,bhsk->bhk', w_end, kc)
        return (C_new, n_new, m_new), hout

    state, hs = lax.scan(step, state0, (blocks(q), blocks(k), blocks(v), blocks(li), blocks(lf)))
    return state, jnp.moveaxis(hs, 0, 2).reshape(Bsz, H, L, dh)


def mlstm_prep(q, k, v, g, gate_b):
    Bsz, L = q.shape[0], q.shape[1]

    def hd(a):
        return a.astype(F32).reshape(Bsz, L, ML_HEADS, ML_DH).transpose(0, 2, 1, 3)

    gates = (g.astype(F32) + gate_b.astype(F32)).reshape(Bsz, L, 2, 2, ML_HEADS).transpose(2, 3, 0, 4, 1)
    return (hd(q), hd(k) * (ML_DH ** -0.5), hd(v), gates)


def mlstm_bidir(ctx_in, lat_in):
    qc, kc, vc, gc = ctx_in
    ql, kl, vl, gl = lat_in
    Bsz = ql.shape[0]
    zero = (jnp.zeros((Bsz, ML_HEADS, ML_DH, ML_DH), F32),
            jnp.zeros((Bsz, ML_HEADS, ML_DH), F32),
            jnp.zeros((Bsz, ML_HEADS), F32))
    hc = jnp.zeros_like(qc)
    hl = jnp.zeros_like(ql)
    for d in range(2):
        li_c, lf_c = gc[d, 0], jax.nn.log_sigmoid(gc[d, 1])
        li_l, lf_l = gl[d, 0], jax.nn.log_sigmoid(gl[d, 1])
        st, hcd = mlstm_scan(_flip(qc, d, 2), _flip(kc, d, 2), _flip(vc, d, 2),
                             _flip(li_c, d, 2), _flip(lf_c, d, 2), zero)
        _, hld = mlstm_scan(_flip(ql, d, 2), _flip(kl, d, 2), _flip(vl, d, 2),
                            _flip(li_l, d, 2), _flip(lf_l, d, 2), st)
        hc = hc + _flip(hcd, d, 2)
        hl = hl + _flip(hld, d, 2)
    return hc, hl


def mlstm_output(hh, o, gain):
    Bsz, _, L, _ = hh.shape
    hh = rms_norm(hh, gain.reshape(ML_HEADS, 1, ML_DH))
    hh = hh.transpose(0, 2, 1, 3).reshape(Bsz, L, ML_W)
    return (hh * jax.nn.sigmoid(o.astype(F32))).astype(o.dtype)


def _attend(q, k, v):
    s = jnp.einsum('bkgqd,bknd->bkgqn', q, k).astype(F32) * (AT_DH ** -0.5)
    p = jax.nn.softmax(s, axis=-1).astype(v.dtype)
    return jnp.einsum('bkgqn,bknd->bkgqd', p, v)


def gqa_heads(q, k, v, q_norm, k_norm):
    Bsz, L = q.shape[0], q.shape[1]
    q = rms_norm(q.reshape(Bsz, L, AT_KV, AT_G, AT_DH), q_norm).transpose(0, 2, 3, 1, 4)
    k = rms_norm(k.reshape(Bsz, L, AT_KV, AT_DH), k_norm).transpose(0, 2, 1, 3)
    v = v.reshape(Bsz, L, AT_KV, AT_DH).transpose(0, 2, 1, 3)
    return q, k, v


def gqa_mixer(ctx_qkv, lat_qkv, q_norm, k_norm, rope, need_ctx):
    qc, kc, vc = gqa_heads(*ctx_qkv, q_norm, k_norm)
    ql, kl, vl = gqa_heads(*lat_qkv, q_norm, k_norm)
    ql = apply_axial_rope(ql, rope)
    kl = apply_axial_rope(kl, rope)
    Bsz, S = ql.shape[0], ql.shape[3]
    out_c = None
    if need_ctx:
        oc = _attend(qc, kc, vc)
        out_c = oc.transpose(0, 3, 1, 2, 4).reshape(Bsz, -1, AT_W)
    k_all = jnp.concatenate([kc, kl], axis=2)
    v_all = jnp.concatenate([vc, vl], axis=2)
    nb = S // Q_BLOCK
    qb = jnp.moveaxis(ql.reshape(Bsz, AT_KV, AT_G, nb, Q_BLOCK, AT_DH), 3, 0)
    ob = lax.map(lambda qq: _attend(qq, k_all, v_all), qb)
    out_l = ob.transpose(1, 0, 4, 2, 3, 5).reshape(Bsz, S, AT_W)
    return out_c, out_l


def even_mixer(h, hc, w_in, w_out, gate_b, ml_norm, q_norm, k_norm, rope, need_ctx):
    mq_c, mk_c, mv_c, mo_c, mg_c, aq_c, ak_c, av_c = jnp.split(hc @ w_in, EV_SPLITS, axis=-1)
    mq_l, mk_l, mv_l, mo_l, mg_l, aq_l, ak_l, av_l = jnp.split(h @ w_in, EV_SPLITS, axis=-1)
    ml_c, ml_l = mlstm_bidir(mlstm_prep(mq_c, mk_c, mv_c, mg_c, gate_b),
                             mlstm_prep(mq_l, mk_l, mv_l, mg_l, gate_b))
    at_c, at_l = gqa_mixer((aq_c, ak_c, av_c), (aq_l, ak_l, av_l), q_norm, k_norm, rope, need_ctx)
    y = jnp.concatenate([mlstm_output(ml_l, mo_l, ml_norm), at_l], axis=-1) @ w_out
    yc = None
    if need_ctx:
        yc = jnp.concatenate([mlstm_output(ml_c, mo_c, ml_norm), at_c], axis=-1) @ w_out
    return y, yc


def _complex_affine_combine(e1, e2):
    a1r, a1i, b1r, b1i = e1
    a2r, a2i, b2r, b2i = e2
    return (a2r * a1r - a2i * a1i, a2r * a1i + a2i * a1r,
            a2r * b1r - a2i * b1i + b2r, a2r * b1i + a2i * b1r + b2i)


def s5_scan(u, a_re, a_im, log_dt, b_re, b_im, c_re, c_im, h0):
    a_re, a_im, b_re, b_im, c_re, c_im = [p.astype(F32) for p in (a_re, a_im, b_re, b_im, c_re, c_im)]
    dt = jnp.exp(log_dt.astype(F32))[:, None]
    mag = jnp.exp(dt * a_re)
    ab_re = mag * jnp.cos(dt * a_im)
    ab_im = mag * jnp.sin(dt * a_im)
    den = a_re * a_re + a_im * a_im
    zr = ((ab_re - 1.0) * a_re + ab_im * a_im) / den
    zi = (ab_im * a_re - (ab_re - 1.0) * a_im) / den
    bb_re = zr[..., None] * b_re - zi[..., None] * b_im
    bb_im = zr[..., None] * b_im + zi[..., None] * b_re
    bu_re = jnp.einsum('gnc,blgc->blgn', bb_re, u)
    bu_im = jnp.einsum('gnc,blgc->blgn', bb_im, u)
    Bsz, L = u.shape[0], u.shape[1]
    nc = L // S5_CHUNK
    shape = (Bsz, S5_CHUNK, S5_GROUPS, S5_N)
    a_blk_re = jnp.broadcast_to(ab_re, shape)
    a_blk_im = jnp.broadcast_to(ab_im, shape)

    def blocks(a):
        return jnp.moveaxis(a.reshape(Bsz, nc, S5_CHUNK, S5_GROUPS, S5_N), 1, 0)

    def step(h, inp):
        h_re, h_im = h
        br, bi = inp
        p_re, p_im, s_re, s_im = lax.associative_scan(
            _complex_affine_combine, (a_blk_re, a_blk_im, br, bi), axis=1)
        hr = s_re + p_re * h_re[:, None] - p_im * h_im[:, None]
        hi = s_im + p_re * h_im[:, None] + p_im * h_re[:, None]
        y = jnp.einsum('gcn,blgn->blgc', c_re, hr) - jnp.einsum('gcn,blgn->blgc', c_im, hi)
        return (hr[:, -1], hi[:, -1]), y

    h_last, ys = lax.scan(step, h0, (blocks(bu_re), blocks(bu_im)))
    return h_last, jnp.moveaxis(ys, 0, 1).reshape(Bsz, L, S5_GROUPS, S5_GSIZE)


def s5_bidir(u_c, u_l, s5p):
    Bsz = u_l.shape[0]
    uc = u_c.astype(F32).reshape(Bsz, -1, S5_GROUPS, S5_GSIZE)
    ul = u_l.astype(F32).reshape(Bsz, -1, S5_GROUPS, S5_GSIZE)
    zero = (jnp.zeros((Bsz, S5_GROUPS, S5_N), F32), jnp.zeros((Bsz, S5_GROUPS, S5_N), F32))
    yc = jnp.zeros_like(uc)
    yl = jnp.zeros_like(ul)
    for d in range(2):
        pd = [p[d] for p in s5p]
        st, ycd = s5_scan(_flip(uc, d, 1), *pd, zero)
        _, yld = s5_scan(_flip(ul, d, 1), *pd, st)
        yc = yc + _flip(ycd, d, 1)
        yl = yl + _flip(yld, d, 1)
    return yc, yl


def s5_output(y, u, d_skip, glu_w, glu_b):
    Bsz, L = u.shape[0], u.shape[1]
    y = y.reshape(Bsz, L, S5_W) + d_skip.astype(F32) * u.astype(F32)
    g = jax.nn.gelu(y)
    return (g * jax.nn.sigmoid(g @ glu_w.astype(F32) + glu_b.astype(F32))).astype(u.dtype)


def dwconv_centered(x, w, b):
    ch = x.shape[-1]
    y = lax.conv_general_dilated(x, w[:, None, :].astype(x.dtype), window_strides=(1,),
                                 padding=[(SSD_CONV // 2, SSD_CONV // 2)],
                                 dimension_numbers=('NWC', 'WIO', 'NWC'), feature_group_count=ch)
    return y + b.astype(x.dtype)


def ssd_scan(x, dt, a, bm, cm, h0):
    Bsz, L = x.shape[0], x.shape[1]
    nc = L // SSD_CHUNK
    hg = SSD_HEADS // SSD_G
    x = x.reshape(Bsz, nc, SSD_CHUNK, SSD_G, hg, SSD_P)
    dt = dt.reshape(Bsz, nc, SSD_CHUNK, SSD_G, hg)
    bm = bm.reshape(Bsz, nc, SSD_CHUNK, SSD_G, SSD_N)
    cm = cm.reshape(Bsz, nc, SSD_CHUNK, SSD_G, SSD_N)
    acs = jnp.cumsum(dt * a.reshape(SSD_G, hg), axis=2)
    mask = jnp.tril(jnp.ones((SSD_CHUNK, SSD_CHUNK), dtype=bool))[:, :, None, None]
    seg = acs[:, :, :, None] - acs[:, :, None, :]
    decay = jnp.exp(jnp.where(mask, seg, -jnp.inf))
    cb = jnp.einsum('bclgn,bcsgn->bclsg', cm, bm)
    w = cb[..., None] * decay * dt[:, :, None]
    y_diag = jnp.einsum('bclsgh,bcsghp->bclghp', w, x)
    decay_end = jnp.exp(acs[:, :, -1:] - acs) * dt
    states = jnp.einsum('bcsgh,bcsgn,bcsghp->bcghpn', decay_end, bm, x)
    chunk_decay = jnp.exp(acs[:, :, -1])

    def step(h, inp):
        st, dec = inp
        return dec[..., None, None] * h + st, h

    h_last, h_in = lax.scan(step, h0.reshape(Bsz, SSD_G, hg, SSD_P, SSD_N),
                            (jnp.moveaxis(states, 1, 0), jnp.moveaxis(chunk_decay, 1, 0)))
    h_in = jnp.moveaxis(h_in, 0, 1)
    y_off = jnp.einsum('bclgn,bcghpn,bclgh->bclghp', cm, h_in, jnp.exp(acs))
    return (h_last.reshape(Bsz, SSD_HEADS, SSD_P, SSD_N),
            (y_diag + y_off).reshape(Bsz, L, SSD_HEADS, SSD_P))


def ssd_prep(xbc, dtr, conv_w, conv_b):
    Bsz, L = xbc.shape[0], xbc.shape[1]
    act = jax.nn.silu(dwconv_centered(xbc, conv_w, conv_b)).astype(F32)
    xs, bm, cm = jnp.split(act, (SSD_W, SSD_W + SSD_G * SSD_N), axis=-1)
    return (xs.reshape(Bsz, L, SSD_HEADS, SSD_P),
            dtr.astype(F32).reshape(Bsz, L, 2, SSD_HEADS),
            bm.reshape(Bsz, L, SSD_G, SSD_N),
            cm.reshape(Bsz, L, SSD_G, SSD_N))


def ssd_bidir(ctx_in, lat_in, dt_bias, a_log, d_skip):
    xs_c, dtr_c, b_c, c_c = ctx_in
    xs_l, dtr_l, b_l, c_l = lat_in
    Bsz = xs_l.shape[0]
    zero = jnp.zeros((Bsz, SSD_HEADS, SSD_P, SSD_N), F32)
    dsk = d_skip.astype(F32)[:, None]
    yc = dsk * xs_c
    yl = dsk * xs_l
    for d in range(2):
        a = -jnp.exp(a_log[d].astype(F32))
        dtb = dt_bias[d].astype(F32)
        dt_c = jax.nn.softplus(dtr_c[:, :, d] + dtb)
        dt_l = jax.nn.softplus(dtr_l[:, :, d] + dtb)
        st, ycd = ssd_scan(_flip(xs_c, d, 1), _flip(dt_c, d, 1), a, _flip(b_c, d, 1), _flip(c_c, d, 1), zero)
        _, yld = ssd_scan(_flip(xs_l, d, 1), _flip(dt_l, d, 1), a, _flip(b_l, d, 1), _flip(c_l, d, 1), st)
        yc = yc + _flip(ycd, d, 1)
        yl = yl + _flip(yld, d, 1)
    return yc, yl


def ssd_output(y, z, gain):
    Bsz, L = z.shape[0], z.shape[1]
    y = y.reshape(Bsz, L, SSD_W) * jax.nn.silu(z.astype(F32))
    return rms_norm(y, gain).astype(z.dtype)


def odd_mixer(h, hc, w_in, w_out, s5p, s5_d, glu_w, glu_b, conv_w, conv_b, dt_bias, a_log, ssd_d,
              ssd_norm, need_ctx):
    u_c, z_c, xbc_c, dt_c = jnp.split(hc @ w_in, OD_SPLITS, axis=-1)
    u_l, z_l, xbc_l, dt_l = jnp.split(h @ w_in, OD_SPLITS, axis=-1)
    s5c, s5l = s5_bidir(u_c, u_l, s5p)
    ssc, ssl = ssd_bidir(ssd_prep(xbc_c, dt_c, conv_w, conv_b), ssd_prep(xbc_l, dt_l, conv_w, conv_b),
                         dt_bias, a_log, ssd_d)
    y = jnp.concatenate([s5_output(s5l, u_l, s5_d, glu_w, glu_b), ssd_output(ssl, z_l, ssd_norm)], axis=-1) @ w_out
    yc = None
    if need_ctx:
        yc = jnp.concatenate([s5_output(s5c, u_c, s5_d, glu_w, glu_b), ssd_output(ssc, z_c, ssd_norm)],
                             axis=-1) @ w_out
    return y, yc


def hier_moe(h, gr_w, gr_b, er_w, er_b, w_gate, w_up, w_down):
    Bsz, L, D = h.shape
    t = h.reshape(Bsz * L, D)
    g_probs = jax.nn.softmax((t @ gr_w).astype(F32) + gr_b.astype(F32), axis=-1)
    g_p, g_idx = lax.top_k(g_probs, 1)
    e_logits = ((t @ er_w).astype(F32) + er_b.astype(F32)).reshape(-1, MOE_GROUPS, MOE_PER_GROUP)
    e_in = e_logits[jnp.arange(t.shape[0]), g_idx[:, 0]]
    top_v, top_i = lax.top_k(e_in, MOE_TOPK)
    w = jax.nn.softmax(top_v, axis=-1) * g_p
    eid = g_idx * MOE_PER_GROUP + top_i
    combine = jnp.sum(jax.nn.one_hot(eid, N_EXPERTS, dtype=F32) * w[..., None], axis=1).astype(t.dtype)
    out = jnp.zeros_like(t)
    for e in range(N_EXPERTS):
        act = jax.nn.silu(t @ w_gate[e]) * (t @ w_up[e])
        out = out + combine[:, e:e + 1] * (act @ w_down[e])
    return out.reshape(Bsz, L, D)


def setup_inputs(seed: int = 0) -> dict:
    key = jax.random.key(seed)
    ks = iter(jax.random.split(key, 48))

    def nrm(shape, scale):
        return jax.random.normal(next(ks), shape, F32) * scale

    def unif(shape, lo, hi):
        return jax.random.uniform(next(ks), shape, F32, lo, hi)

    D = D_MODEL
    x = nrm((BATCH, SEQ, D), 1.0)
    c = nrm((BATCH, D), 1.0)
    ctx = nrm((BATCH, CTX_LEN, D), 1.0)
    c_ctx = nrm((D,), 1.0)
    ada_w = nrm((DEPTH, D, N_MOD * D), 0.5 * D ** -0.5)
    ada_b = nrm((DEPTH, N_MOD * D), 0.02)
    norm_mix = 1.0 + nrm((DEPTH, D), 0.05)
    norm_ffn = 1.0 + nrm((DEPTH, D), 0.05)
    ev_w_in = nrm((N_EVEN, D, EV_IN), D ** -0.5)
    ev_w_out = nrm((N_EVEN, EV_MIX, D), EV_MIX ** -0.5)
    ig = nrm((N_EVEN, 2, ML_HEADS), 0.1)
    fg = unif((N_EVEN, 2, ML_HEADS), 3.0, 6.0)
    ml_gate_b = jnp.stack([ig, fg], axis=2).reshape(N_EVEN, 4 * ML_HEADS)
    ml_norm = 1.0 + nrm((N_EVEN, ML_W), 0.05)
    at_q_norm = 1.0 + nrm((N_EVEN, AT_DH), 0.05)
    at_k_norm = 1.0 + nrm((N_EVEN, AT_DH), 0.05)
    od_w_in = nrm((N_ODD, D, OD_IN), D ** -0.5)
    od_w_out = nrm((N_ODD, OD_MIX, D), OD_MIX ** -0.5)
    s5_a_re = -0.5 + nrm((N_ODD, 2, S5_GROUPS, S5_N), 0.01)
    s5_a_im = math.pi * jnp.arange(S5_N, dtype=F32) + nrm((N_ODD, 2, S5_GROUPS, S5_N), 0.01)
    s5_log_dt = unif((N_ODD, 2, S5_GROUPS), math.log(1e-3), math.log(1e-1))
    s5_b_re = nrm((N_ODD, 2, S5_GROUPS, S5_N, S5_GSIZE), (2 * S5_GSIZE) ** -0.5)
    s5_b_im = nrm((N_ODD, 2, S5_GROUPS, S5_N, S5_GSIZE), (2 * S5_GSIZE) ** -0.5)
    s5_c_re = nrm((N_ODD, 2, S5_GROUPS, S5_GSIZE, S5_N), S5_N ** -0.5)
    s5_c_im = nrm((N_ODD, 2, S5_GROUPS, S5_GSIZE, S5_N), S5_N ** -0.5)
    s5_d = nrm((N_ODD, S5_W), 0.5)
    s5_glu_w = nrm((N_ODD, S5_W, S5_W), S5_W ** -0.5)
    s5_glu_b = nrm((N_ODD, S5_W), 0.02)
    ssd_conv_w = nrm((N_ODD, SSD_CONV, SSD_CONV_CH), SSD_CONV ** -0.5)
    ssd_conv_b = nrm((N_ODD, SSD_CONV_CH), 0.02)
    dt0 = jnp.exp(unif((N_ODD, 2, SSD_HEADS), math.log(1e-3), math.log(1e-1)))
    ssd_dt_bias = dt0 + jnp.log(-jnp.expm1(-dt0))
    ssd_a_log = jnp.log(unif((N_ODD, 2, SSD_HEADS), 1.0, 16.0))
    ssd_d = 1.0 + nrm((N_ODD, SSD_HEADS), 0.1)
    ssd_norm = 1.0 + nrm((N_ODD, SSD_W), 0.05)
    moe_gr_w = nrm((DEPTH, D, MOE_GROUPS), D ** -0.5)
    moe_gr_b = nrm((DEPTH, MOE_GROUPS), 0.01)
    moe_er_w = nrm((DEPTH, D, N_EXPERTS), D ** -0.5)
    moe_er_b = nrm((DEPTH, N_EXPERTS), 0.01)
    moe_w_gate = nrm((DEPTH, N_EXPERTS, D, EXPERT_HIDDEN), D ** -0.5)
    moe_w_up = nrm((DEPTH, N_EXPERTS, D, EXPERT_HIDDEN), D ** -0.5)
    moe_w_down = nrm((DEPTH, N_EXPERTS, EXPERT_HIDDEN, D), EXPERT_HIDDEN ** -0.5)
    return {
        "x": x, "c": c, "ctx": ctx, "c_ctx": c_ctx,
        "ada_w": ada_w, "ada_b": ada_b, "norm_mix": norm_mix, "norm_ffn": norm_ffn,
        "ev_w_in": ev_w_in, "ev_w_out": ev_w_out, "ml_gate_b": ml_gate_b, "ml_norm": ml_norm,
        "at_q_norm": at_q_norm, "at_k_norm": at_k_norm,
        "od_w_in": od_w_in, "od_w_out": od_w_out,
        "s5_a_re": s5_a_re, "s5_a_im": s5_a_im, "s5_log_dt": s5_log_dt,
        "s5_b_re": s5_b_re, "s5_b_im": s5_b_im, "s5_c_re": s5_c_re, "s5_c_im": s5_c_im,
        "s5_d": s5_d, "s5_glu_w": s5_glu_w, "s5_glu_b": s5_glu_b,
        "ssd_conv_w": ssd_conv_w, "ssd_conv_b": ssd_conv_b, "ssd_dt_bias": ssd_dt_bias,
        "ssd_a_log": ssd_a_log, "ssd_d": ssd_d, "ssd_norm": ssd_norm,
        "moe_gr_w": moe_gr_w, "moe_gr_b": moe_gr_b, "moe_er_w": moe_er_w, "moe_er_b": moe_er_b,
        "moe_w_gate": moe_w_gate, "moe_w_up": moe_w_up, "moe_w_down": moe_w_down,
    }


def reference(x, c, ctx, c_ctx, ada_w, ada_b, norm_mix, norm_ffn,
              ev_w_in, ev_w_out, ml_gate_b, ml_norm, at_q_norm, at_k_norm,
              od_w_in, od_w_out, s5_a_re, s5_a_im, s5_log_dt, s5_b_re, s5_b_im, s5_c_re, s5_c_im,
              s5_d, s5_glu_w, s5_glu_b,
              ssd_conv_w, ssd_conv_b, ssd_dt_bias, ssd_a_log, ssd_d, ssd_norm,
              moe_gr_w, moe_gr_b, moe_er_w, moe_er_b, moe_w_gate, moe_w_up, moe_w_down):
    rows = x.shape[1] // GRID_W
    rope = axial_rope(rows)
    xc = ctx
    for i in range(DEPTH):
        need_ctx = i < DEPTH - 1
        j = i // 2
        sh1, sc1, g1, sh2, sc2, g2 = adaln(c, ada_w[i], ada_b[i])
        csh1, csc1, cg1, csh2, csc2, cg2 = adaln(c_ctx, ada_w[i], ada_b[i])
        h = rms_norm(x, norm_mix[i]) * (1.0 + sc1) + sh1
        hc = rms_norm(xc, norm_mix[i]) * (1.0 + csc1) + csh1
        if i % 2 == 0:
            y, yc = even_mixer(h, hc, ev_w_in[j], ev_w_out[j], ml_gate_b[j], ml_norm[j],
                               at_q_norm[j], at_k_norm[j], rope, need_ctx)
        else:
            s5p = (s5_a_re[j], s5_a_im[j], s5_log_dt[j], s5_b_re[j], s5_b_im[j], s5_c_re[j], s5_c_im[j])
            y, yc = odd_mixer(h, hc, od_w_in[j], od_w_out[j], s5p, s5_d[j], s5_glu_w[j], s5_glu_b[j],
                              ssd_conv_w[j], ssd_conv_b[j], ssd_dt_bias[j], ssd_a_log[j], ssd_d[j],
                              ssd_norm[j], need_ctx)
        moe_p = (moe_gr_w[i], moe_gr_b[i], moe_er_w[i], moe_er_b[i], moe_w_gate[i], moe_w_up[i], moe_w_down[i])
        x = x + g1 * y
        x = x + g2 * hier_moe(rms_norm(x, norm_ffn[i]) * (1.0 + sc2) + sh2, *moe_p)
        if need_ctx:
            xc = xc + cg1 * yc
            xc = xc + cg2 * hier_moe(rms_norm(xc, norm_ffn[i]) * (1.0 + csc2) + csh2, *moe_p)
    return x
```

```python
import math
import os
import numpy as np
import concourse.bass as bass
import concourse.mybir as mybir
from concourse.bass_utils import run_bass_kernel_spmd
from contextlib import ExitStack

F32 = mybir.dt.float32
BF16 = mybir.dt.bfloat16
I32 = mybir.dt.int32
AF = mybir.ActivationFunctionType
ALU = mybir.AluOpType
AX = mybir.AxisListType

ENGS = ['pe', 'act', 'dve', 'pool', 'sp']
NDMASEM = 40
NCORES = 8
NB = 2
NT = 2304
NCH = 18
EPS = 1e-6
TILES = [(0, 256), (256, 512), (768, 512), (1280, 512), (1792, 512)]


class _Op:
    __slots__ = ('eng', 'fn', 'waits', 'dwaits', 'idx', 'signal', 'isdma', 'dsem', 'dval', 'know')


class Prog:
    def __init__(self, nc):
        self.nc = nc
        self.ops = {e: [] for e in ENGS}
        self.lastw = {}
        self.readers = {}
        self.know = {e: {f: -1 for f in ENGS} for e in ENGS}
        self.dknow = {e: {} for e in ENGS}
        self.dsem_last = [None] * NDMASEM
        self.dsem_val = [0] * NDMASEM
        self.dsem_rr = 0
        self.dsem_rr_sw = 0

    def _dep_tokens(self, reads, writes):
        toks = []
        for r in reads:
            t = self.lastw.get(r)
            if t is not None:
                toks.append(t)
        for w in writes:
            t = self.lastw.get(w)
            if t is not None:
                toks.append(t)
            toks.extend(self.readers.get(w, ()))
        return toks

    def _record(self, op, reads, writes):
        for r in reads:
            self.readers.setdefault(r, []).append(op)
        for w in writes:
            self.lastw[w] = op
            self.readers[w] = []

    def _resolve(self, eng, toks, op, same_ok=False):
        need = {}
        dneed = {}
        for t in toks:
            if t.isdma:
                if self.dknow[eng].get(t.dsem, 0) >= t.dval:
                    continue
                dneed[t.dsem] = max(dneed.get(t.dsem, 0), t.dval)
            else:
                if t.eng == eng and same_ok:
                    continue
                if self.know[eng][t.eng] >= t.idx:
                    continue
                need[t.eng] = max(need.get(t.eng, -1), t.idx)
        for f, j in need.items():
            src = self.ops[f][j]
            src.signal = True
            kn = src.know
            for g in ENGS:
                if kn[g] > self.know[eng][g]:
                    self.know[eng][g] = kn[g]
            if j > self.know[eng][f]:
                self.know[eng][f] = j
        for s, v in dneed.items():
            self.dknow[eng][s] = v
        op.waits = list(need.items())
        op.dwaits = list(dneed.items())

    def op(self, eng, fn, reads=(), writes=(), same_ok=False):
        if eng != 'pe':
            psr = [r for r in reads if isinstance(r, tuple) and r[0] == 'ps']
            if psr:
                writes = list(writes) + psr
        o = _Op()
        o.eng = eng
        o.fn = fn
        o.isdma = False
        o.signal = False
        o.idx = len(self.ops[eng])
        toks = self._dep_tokens(reads, writes)
        self._resolve(eng, toks, o, same_ok=same_ok)
        kn = dict(self.know[eng])
        kn[eng] = o.idx
        o.know = kn
        self.ops[eng].append(o)
        self._record(o, reads, writes)
        return o

    def dma(self, q, fns, reads=(), writes=()):
        if not isinstance(fns, (list, tuple)):
            fns = [fns]
        o = _Op()
        o.eng = q
        o.fn = list(fns)
        o.isdma = True
        o.signal = False
        o.idx = len(self.ops[q])
        half = NDMASEM // 2
        if q == 'pool':
            s = half + self.dsem_rr_sw
            self.dsem_rr_sw = (self.dsem_rr_sw + 1) % (NDMASEM - half)
        else:
            s = self.dsem_rr
            self.dsem_rr = (self.dsem_rr + 1) % half
        toks = self._dep_tokens(reads, writes)
        if self.dsem_last[s] is not None:
            toks.append(self.dsem_last[s])
        self._resolve(q, toks, o)
        o.dsem = s
        self.dsem_val[s] += 16 * len(fns)
        o.dval = self.dsem_val[s]
        self.dsem_last[s] = o
        o.know = dict(self.know[q])
        self.ops[q].append(o)
        self._record(o, reads, writes)
        return o

    def _waitop(self, eng, toks):
        o = _Op()
        o.eng = eng
        o.fn = None
        o.isdma = False
        o.signal = False
        o.idx = len(self.ops[eng])
        self._resolve(eng, toks, o)
        kn = dict(self.know[eng])
        kn[eng] = o.idx
        o.know = kn
        self.ops[eng].append(o)
        return o

    def barrier(self):
        last = {e: (self.ops[e][-1] if self.ops[e] else None) for e in ENGS}
        dtoks = [t for t in self.dsem_last if t is not None]
        for e in ENGS:
            toks = list(dtoks)
            for f in ENGS:
                t = last[f]
                if t is None:
                    continue
                if t.isdma or t.fn is None:
                    j = len(self.ops[f]) - 1
                    while j >= 0 and (self.ops[f][j].isdma or self.ops[f][j].fn is None):
                        j -= 1
                    if j < 0:
                        continue
                    t = self.ops[f][j]
                toks.append(t)
            self._waitop(e, toks)
        self.lastw = {}
        self.readers = {}

    def wait_all_dma(self, eng='sp'):
        toks = [t for t in self.dsem_last if t is not None]
        return self._waitop(eng, toks)

    def emit(self, stack):
        nc = self.nc
        esem = {e: stack.enter_context(nc.semaphore("s_" + e)) for e in ENGS}
        dsem = [stack.enter_context(nc.semaphore("d%d" % i)) for i in range(NDMASEM)]
        semval = {}
        for e in ENGS:
            c = 0
            vals = []
            for o in self.ops[e]:
                if o.signal and not o.isdma and o.fn is not None:
                    c += 1
                vals.append(c)
            semval[e] = vals
        engobj = {'pe': 'tensor', 'act': 'scalar', 'dve': 'vector', 'pool': 'gpsimd', 'sp': 'sync'}
        self.stats = {e: (len(self.ops[e]), sum(len(o.waits) + len(o.dwaits) for o in self.ops[e]),
                          semval[e][-1] if semval[e] else 0) for e in ENGS}
        block = stack.enter_context(nc.Block())

        def body_for(e):
            def body(engine):
                for o in self.ops[e]:
                    for f, j in o.waits:
                        engine.wait_ge(esem[f], semval[f][j])
                    for s, v in o.dwaits:
                        engine.wait_ge(dsem[s], v)
                    if o.fn is None:
                        continue
                    if o.isdma:
                        for fn in o.fn:
                            fn(engine).then_inc(dsem[o.dsem], 16)
                    else:
                        ins = o.fn(engine)
                        if o.signal:
                            ins.then_inc(esem[e], 1)
            return body

        for e in ENGS:
            if self.ops[e]:
                getattr(block, engobj[e])(body_for(e))


class ArenaScope:
    def __init__(self, bld):
        self.bld = bld

    def __enter__(self):
        self.mark = self.bld.alo
        return self

    def __exit__(self, *a):
        self.bld.alo = self.mark
        return False


def tk(name, t0, n):
    return [(name, c) for c in range(t0 // 128, (t0 + n + 127) // 128)]


class Builder:
    def __init__(self, taps=(), nb=NB, stop=None):
        self.taps = set(taps)
        self.nb = nb
        self.stop = stop
        self.tap_specs = {}
        self.nc = bass.Bass("TRN2", target_bir_lowering=False)
        self.p = Prog(self.nc)
        self.dram = {}

    def din(self, name, shape, dt=F32):
        t = self.nc.dram_tensor(name, list(shape), dt, kind="ExternalInput").ap()
        self.dram[name] = t
        return t

    def dout(self, name, shape, dt=F32):
        t = self.nc.dram_tensor(name, list(shape), dt, kind="ExternalOutput").ap()
        self.dram[name] = t
        return t

    def tap(self, name, src_ap, shape, dt, reads):
        if name not in self.taps:
            return
        d = self.dout("tap_" + name, shape, dt)
        self.tap_specs[name] = (shape, dt)
        self.p.dma('sp', lambda e: e.dma_start(out=d, in_=src_ap), reads=reads)

    def dmas(self, q, pairs, reads=(), writes=(), slow=False):
        kw = {'allow_slow_non_contiguous': True} if slow else {}
        fns = [(lambda e, o=o, i=i: e.dma_start(out=o, in_=i, **kw)) for (o, i) in pairs]
        self.p.dma(q, fns, reads=reads, writes=writes)

    def mm(self, out, lhsT, rhs, start, stop, reads, writes, **kw):
        self.p.op('pe', lambda e: e.matmul(out, lhsT=lhsT, rhs=rhs, start=start, stop=stop, **kw),
                  reads, writes, same_ok=True)

    def tr(self, out, in_, ident, reads, writes):
        self.p.op('pe', lambda e: e.transpose(out, in_, ident), reads, writes, same_ok=True)

    def act(self, out, in_, func, reads, writes, bias=None, scale=None, accum_out=None, eng='act'):
        kw = {}
        if bias is not None:
            kw['bias'] = bias
        if scale is not None:
            kw['scale'] = scale
        if accum_out is not None:
            kw['accum_out'] = accum_out
        self.p.op('act', lambda e: e.activation(out=out, in_=in_, func=func, **kw), reads, writes)

    def tt(self, out, in0, in1, op, reads, writes, eng='dve'):
        self.p.op(eng, lambda e: e.tensor_tensor(out=out, in0=in0, in1=in1, op=op), reads, writes)

    def ts(self, out, in0, s1, s2, op0, op1, reads, writes, eng='dve'):
        if op1 is None:
            self.p.op(eng, lambda e: e.tensor_scalar(out=out, in0=in0, scalar1=s1, scalar2=None, op0=op0), reads, writes)
        else:
            self.p.op(eng, lambda e: e.tensor_scalar(out=out, in0=in0, scalar1=s1, scalar2=s2, op0=op0, op1=op1), reads, writes)

    def stt(self, out, in0, scalar, in1, op0, op1, reads, writes):
        self.p.op('dve', lambda e: e.scalar_tensor_tensor(out=out, in0=in0, scalar=scalar, in1=in1, op0=op0, op1=op1),
                  reads, writes)

    def cp(self, out, in_, reads, writes, eng='dve'):
        self.p.op(eng, lambda e: e.tensor_copy(out=out, in_=in_), reads, writes)

    def recip(self, out, in_, reads, writes):
        self.p.op('dve', lambda e: e.reciprocal(out=out, in_=in_), reads, writes)

    def memset(self, ap, val, writes, eng='dve'):
        self.p.op(eng, lambda e: e.memset(ap, val), (), writes)

    def sb(self, st, name, shape, dt):
        if isinstance(st, ArenaScope):
            return self.carve(shape, dt)
        self._uid = getattr(self, '_uid', 0) + 1
        return st.enter_context(self.nc.sbuf_tensor("%s_%d" % (name, self._uid), list(shape), dt))

    def carve(self, shape, dt, high=False):
        nelem = 1
        for d in shape[1:]:
            nelem *= d
        n32 = nelem if dt != BF16 else (nelem + 1) // 2
        n32 = (n32 + 7) // 8 * 8
        if high:
            self.ahi -= n32
            off = self.ahi
        else:
            off = self.alo
            self.alo += n32
        assert self.alo <= self.ahi, "arena overflow: lo=%d hi=%d" % (self.alo, self.ahi)
        ap = self.ARENA[0:shape[0], off:off + n32]
        if dt == BF16:
            ap = ap.bitcast(BF16)
        ap = ap[:, 0:nelem]
        if len(shape) == 3:
            ap = ap.rearrange("p (a b) -> p a b", b=shape[2])
        elif len(shape) == 4:
            ap = ap.rearrange("p (a b c) -> p a b c", b=shape[2], c=shape[3])
        elif len(shape) == 5:
            ap = ap.rearrange("p (a b c d) -> p a b c d", b=shape[2], c=shape[3], d=shape[4])
        return ap

    def scope(self):
        return ArenaScope(self)

    def build(self):
        nc, p = self.nc, self.p
        D = self.din
        nb = self.nb
        x = D("x", [nb, 2048, 1024]); c = D("c", [nb, 1024]); ctx = D("ctx", [nb, 256, 1024]); c_ctx = D("c_ctx", [1024])
        ada_w = D("ada_w", [2, 1024, 6144]); ada_b = D("ada_b", [2, 6144])
        norm_mix = D("norm_mix", [2, 1024]); norm_ffn = D("norm_ffn", [2, 1024])
        self.ev_w_in = D("ev_w_in", [1, 1024, 2832]); self.ev_w_out = D("ev_w_out", [1, 1024, 1024])
        self.ml_gate_b = D("ml_gate_b", [1, 16]); self.ml_norm = D("ml_norm", [1, 512])
        self.at_q_norm = D("at_q_norm", [1, 64]); self.at_k_norm = D("at_k_norm", [1, 64])
        self.od_w_in = D("od_w_in", [1, 1024, 2064]); self.od_w_out = D("od_w_out", [1, 1024, 1024])
        for nm, shp in [("s5_a_re", [1, 2, 32, 64]), ("s5_a_im", [1, 2, 32, 64]), ("s5_log_dt", [1, 2, 32]),
                        ("s5_b_re", [1, 2, 32, 64, 16]), ("s5_b_im", [1, 2, 32, 64, 16]),
                        ("s5_c_re", [1, 2, 32, 16, 64]), ("s5_c_im", [1, 2, 32, 16, 64]),
                        ("s5_d", [1, 512]), ("s5_glu_w", [1, 512, 512]), ("s5_glu_b", [1, 512]),
                        ("ssd_conv_w", [1, 3, 1024]), ("ssd_conv_b", [1, 1024]), ("ssd_dt_bias", [1, 2, 8]),
                        ("ssd_a_log", [1, 2, 8]), ("ssd_d", [1, 8]), ("ssd_norm", [1, 512]),
                        ("moe_gr_w", [2, 1024, 4]), ("moe_gr_b", [2, 4]), ("moe_er_w", [2, 1024, 16]),
                        ("moe_er_b", [2, 16]), ("moe_w_gate", [2, 16, 1024, 512]), ("moe_w_up", [2, 16, 1024, 512]),
                        ("moe_w_down", [2, 16, 512, 1024])]:
            setattr(self, nm, D(nm, shp))
        k_ident = D("k_ident", [128, 128]); k_tri = D("k_tri", [6, 128, 128])
        k_rope = D("k_rope", [2, 2048, 64]); k_sel = D("k_sel", [16, 16, 128])
        self.k_tri = k_tri
        self.k_mask8 = D("k_mask8", [128, 8]); self.k_posc = D("k_posc", [128, 4]); self.k_posr = D("k_posr", [128, 2, 128])
        out = self.dout("out", [nb, 2048, 1024])

        with ExitStack() as st:
            S = lambda name, shape, dt=F32: self.sb(st, name, shape, dt)
            self.IDF = S("IDF", [128, 128]); self.IDB = S("IDB", [128, 128], BF16)
            self.ONESB = S("ONESB", [128, 128], BF16); self.ONESF = S("ONESF", [128, 128])
            self.TRI = S("TRI", [128, 6, 128])
            self.MOD = S("MOD", [128, 2, 48, 3])
            self.GSC = S("GSC", [128, 2, 2, 8, 3])
            self.EPSC = S("EPSC", [128, 1])
            self.memset(self.EPSC[:], EPS, ['EPSC'])
            ARENA_WORDS = 46080
            self.ARENA = S("ARENA", [128, ARENA_WORDS])
            self.alo, self.ahi = 0, ARENA_WORDS
            self.XS = [nc.dram_tensor("xscr%d" % i, [128, 8, NT], F32, kind="Internal").ap() for i in range(nb)]
            self.HT = self.carve([128, 8, NT], BF16)
            self.PS = [st.enter_context(nc.psum_tensor("ps%d" % i, [128, 512], F32)) for i in range(8)]
            PS = self.PS
            p.dma('sp', lambda e: e.dma_start(out=self.IDF[:], in_=k_ident), writes=['IDF'])
            p.dma('pool', lambda e: e.dma_start(out=self.IDB[:], in_=k_ident), writes=['IDB'])
            p.dma('sp', lambda e: e.dma_start(out=self.TRI[:], in_=k_tri.rearrange("a p n -> p a n")), writes=['TRI'])
            self.memset(self.ONESB[:], 1.0, ['ONESB'])
            self.memset(self.ONESF[:], 1.0, ['ONESF'])
            self.k_rope = k_rope; self.k_sel = k_sel

            with self.scope() as ph:
                P = lambda name, shape, dt=F32: self.sb(ph, name, shape, dt)
                STG = P("STG", [128, 128]); FM = P("FM", [128, 128])
                AW = [P("AW%d" % i, [128, 8, 512]) for i in range(2)]
                SC = P("SC", [128, 8, 3])
                self.memset(STG[:], 0.0, ['STG'])
                r_c = 32
                r_cc = 32 + 8 * nb
                p.dma('sp', [lambda e: e.dma_start(out=STG[0:16, :], in_=norm_mix.rearrange("l (k p) -> (l k) p", p=128)),
                             lambda e: e.dma_start(out=STG[16:32, :], in_=norm_ffn.rearrange("l (k p) -> (l k) p", p=128)),
                             lambda e: e.dma_start(out=STG[r_c:r_c + 8 * nb, :], in_=c.rearrange("b (k p) -> (b k) p", p=128)),
                             lambda e: e.dma_start(out=STG[r_cc:r_cc + 8, :], in_=c_ctx.rearrange("(k p) -> k p", p=128))],
                      reads=['STG'], writes=['STG'])
                self.tr(PS[0][:, 0:128], STG[:], self.IDF[:], ['STG', 'IDF'], [('ps', 0)])
                self.cp(FM[:], PS[0][:, 0:128], [('ps', 0)], ['FM'])
                for b in range(nb):
                    self.act(SC[:, :, b], FM[:, r_c + 8 * b:r_c + 8 * b + 8], AF.Silu, ['FM'], [('SC', b)])
                self.act(SC[:, :, 2], FM[:, r_cc:r_cc + 8], AF.Silu, ['FM'], [('SC', 2)])
                if nb == 1:
                    self.cp(SC[:, :, 1], SC[:, :, 0], [('SC', 0)], [('SC', 1)])
                STG2 = P("STG2", [128, 128]); ADAB = P("ADAB", [128, 96])
                self.memset(STG2[:], 0.0, ['STG2'])
                p.dma('sp', lambda e: e.dma_start(out=STG2[0:96, :], in_=ada_b.rearrange("l (j p) -> (l j) p", p=128)),
                      reads=['STG2'], writes=['STG2'])
                self.tr(PS[1][:, 0:128], STG2[:], self.IDF[:], ['STG2', 'IDF'], [('ps', 1)])
                self.cp(ADAB[:], PS[1][:, 0:96], [('ps', 1)], ['ADAB'])
                screads = [('SC', 0), ('SC', 1), ('SC', 2)]
                for l in range(2):
                    for pc in range(12):
                        slot = pc % 2
                        aw = AW[slot]
                        src = ada_w[l].rearrange("(k p) n -> p k n", p=128)
                        p.dma('sp' if slot == 0 else 'act',
                              [lambda e, src=src, aw=aw, pc=pc, h=h: e.dma_start(out=aw[:, h * 4:(h + 1) * 4, :], in_=src[:, h * 4:(h + 1) * 4, pc * 512:(pc + 1) * 512])
                               for h in range(2)], writes=[('AW', slot)])
                        for j in range(4):
                            col = (pc * 4 + j) * 3
                            for k in range(8):
                                self.mm(PS[2 + l][:, col:col + 3], aw[:, k, j * 128:(j + 1) * 128], SC[:, k, :],
                                        k == 0, k == 7, [('AW', slot)] + screads, [('ps', 2 + l)])
                    self.tt(self.MOD[:, l], PS[2 + l][:, 0:144].rearrange("p (j t) -> p j t", t=3),
                            ADAB[:, l * 48:(l + 1) * 48].unsqueeze(2).to_broadcast([128, 48, 3]), ALU.add,
                            [('ps', 2 + l), 'ADAB'], [('MOD', l)])
                    for w in range(2):
                        mi = 1 if w == 0 else 4
                        g_ap = FM[:, 16 * w + 8 * l:16 * w + 8 * l + 8]
                        gb = g_ap.unsqueeze(2).to_broadcast([128, 8, 3])
                        self.tt(self.GSC[:, l, w], self.MOD[:, l, mi * 8:(mi + 1) * 8, :], gb, ALU.mult,
                                [('MOD', l), 'FM'], [('GSC', l, w)])
                        self.tt(self.GSC[:, l, w], self.GSC[:, l, w], gb, ALU.add,
                                [('GSC', l, w), 'FM'], [('GSC', l, w)])
                self.tap("mod", self.MOD[:], [128, 2, 48, 3], F32, [('MOD', 0), ('MOD', 1)])
                p.barrier()
            if self.stop == 'mod':
                return self.finish(st)

            for b in range(nb):
                self.run_batch(st, b, x, ctx, out)
                if self.stop is not None:
                    break
            return self.finish(st)

    def finish(self, st):
        self.p.wait_all_dma('sp')
        self.p.emit(st)
        return self.nc

    def load_x(self, b, x, ctx):
        p, PS = self.p, self.PS
        with self.scope() as ph:
            XT = [self.sb(ph, "XT%d" % i, [128, 1024], F32) for i in range(3)]
            for ch in range(NCH):
                s = ch % 3
                src = ctx[b, ch * 128:(ch + 1) * 128, :] if ch < 2 else x[b, (ch - 2) * 128:(ch - 1) * 128, :]
                p.dma('sp' if ch % 2 == 0 else 'act', lambda e, s=s, src=src: e.dma_start(out=XT[s][:], in_=src), writes=[('XT', s)])
                for hf in range(2):
                    bank = (ch * 2 + hf) % 4
                    for kk in range(4):
                        k = hf * 4 + kk
                        self.tr(PS[bank][:, kk * 128:(kk + 1) * 128], XT[s][:, k * 128:(k + 1) * 128], self.IDF[:],
                                [('XT', s), 'IDF'], [('ps', bank)])
                    dst = self.X[:, hf * 4:(hf + 1) * 4, ch * 128:(ch + 1) * 128]
                    srcp = PS[bank][:].rearrange("p (k t) -> p k t", t=128)
                    if hf == 0:
                        self.act(dst, srcp, AF.Copy, [('ps', bank)], [('X', ch)])
                    else:
                        self.cp(dst, srcp, [('ps', bank)], [('X', ch)])
            p.barrier()

    def norm(self, b, l, w, f32_cb=None, skip_ctx=False):
        p, PS = self.p, self.PS
        mi = 0 if w == 0 else 3
        with self.scope() as ph:
            SQ = [self.sb(ph, "SQ%d" % i, [128, 512], BF16) for i in range(3)]
            SD = self.sb(ph, "SD", [128, 512], F32)
            RS = self.sb(ph, "RS", [128, 512], F32)
            TM = [self.sb(ph, "TM%d" % i, [128, 512], F32) for i in range(2)]
            H32 = self.sb(ph, "H32", [128, 8, 512], F32) if f32_cb is not None else None
            cnt = 0
            for ti, (t0, n) in enumerate(TILES):
                if skip_ctx and ti == 0:
                    continue
                j = 2 if ti == 0 else b
                xk = tk('X', t0, n)
                bank = ti % 2
                for k in range(8):
                    s = cnt % 3
                    cnt += 1
                    self.act(SQ[s][:, :n], self.X[:, k, t0:t0 + n], AF.Square, xk, [('SQ', s)])
                    self.mm(PS[bank][:, :n], self.ONESB[:], SQ[s][:, :n], k == 0, k == 7, [('SQ', s), 'ONESB'], [('ps', bank)])
                self.act(SD[:, :n], PS[bank][:, :n], AF.Sqrt, [('ps', bank)], ['SD'], bias=self.EPSC[:, 0:1], scale=1.0 / 1024)
                self.recip(RS[:, :n], SD[:, :n], ['SD'], ['RS'])
                for k in range(8):
                    s = k % 2
                    self.stt(TM[s][:, :n], self.X[:, k, t0:t0 + n], self.GSC[:, l, w, k, j:j + 1], RS[:, :n], ALU.mult, ALU.mult,
                             xk + ['RS', ('GSC', l, w)], [('TM', s)])
                    if f32_cb is not None:
                        self.act(H32[:, k, :n], TM[s][:, :n], AF.Identity, [('TM', s)], [('H32', k)],
                                 bias=self.MOD[:, l, mi * 8 + k, j:j + 1], scale=1.0)
                        self.cp(self.HT[:, k, t0:t0 + n], H32[:, k, :n], [('H32', k)], tk('HT', t0, n), eng='pool')
                    else:
                        self.act(self.HT[:, k, t0:t0 + n], TM[s][:, :n], AF.Identity, [('TM', s)], tk('HT', t0, n),
                                 bias=self.MOD[:, l, mi * 8 + k, j:j + 1], scale=1.0)
                if f32_cb is not None:
                    f32_cb(ti, t0, n, H32)
            p.barrier()

    def x_alloc(self):
        self._ahi_mark = self.ahi
        self.X = self.carve([128, 8, NT], F32, high=True)

    def x_free(self):
        self.ahi = self._ahi_mark

    def x_spill(self, b):
        p = self.p
        p.dma('sp', [lambda e, k=k: e.dma_start(out=self.XS[b][:, 2 * k:2 * k + 2, :], in_=self.X[:, 2 * k:2 * k + 2, :]) for k in range(4)],
              reads=tk('X', 0, NT), writes=['XS'])
        p.barrier()

    def x_reload(self, b):
        p = self.p
        p.dma('sp', [lambda e, k=k: e.dma_start(out=self.X[:, 2 * k:2 * k + 2, :], in_=self.XS[b][:, 2 * k:2 * k + 2, :]) for k in range(4)],
              reads=['XS'], writes=tk('X', 0, NT))

    def run_batch(self, st, b, x, ctx, out):
        p = self.p
        self.x_alloc()
        self.load_x(b, x, ctx)
        if self.stop == 'loadx':
            self.tap("X", self.X[:], [128, 8, NT], F32, tk('X', 0, NT))
            return
        skipl0 = bool(os.environ.get('SKIPL0'))
        if not skipl0:
            self.norm(b, 0, 0)
        if self.stop == 'h0':
            self.tap("h0", self.HT[:], [128, 8, NT], BF16, tk('HT', 0, NT))
            return
        if not skipl0:
            self.layer0(b)
            if self.stop is not None and self.stop in ('mlstm', 'attn', 'xmid0', 'xout0', 'mlgate', 'mlproj', 'mlloop'):
                return
        self.layer1(b, out)

    def layer0(self, b):
        p = self.p
        self.x_spill(b)
        self.x_free()
        with self.scope() as lay:
            self.MIXT = self.carve([128, 8, NT], BF16)
            if 'ml' not in os.environ.get('SKIPMIX', ''):
                self.mlstm(b)
            if self.stop == 'mlstm':
                self.tap("mix0", self.MIXT[:], [128, 8, NT], BF16, [])
                return
            self.attention(b)
            if self.stop == 'attn':
                self.tap("mix0", self.MIXT[:], [128, 8, NT], BF16, [])
                return
            self.tap("mix0", self.MIXT[:], [128, 8, NT], BF16, [])
            self.x_alloc()
            self.x_reload(b)
            self.outproj(b, 0, self.ev_w_out[0])
            self.tap("xmid0", self.X[:], [128, 8, NT], F32, [])
        if self.stop == 'xmid0':
            self.tap("X", self.X[:], [128, 8, NT], F32, tk('X', 0, NT))
            return
        if 'moe0' not in os.environ.get('SKIPMIX', ''):
            self.moe(b, 0)
        if self.stop == 'xout0':
            self.tap("X", self.X[:], [128, 8, NT], F32, tk('X', 0, NT))
            return

    def layer1(self, b, out):
        p = self.p
        self.norm(b, 1, 0)
        self.tap("h1", self.HT[:], [128, 8, NT], BF16, [])
        self.x_spill(b)
        self.x_free()
        with self.scope() as lay:
            self.MIXT = self.carve([128, 8, NT], BF16)
            if 's5' not in os.environ.get('SKIPMIX', ''):
                self.s5(b)
            if self.stop in ('s5', 's5tab'):
                if self.stop == 's5':
                    self.tap("s5y", self.S5_YT[:], [128, 4, NT], BF16, [])
                return
            self.ssd(b)
            if self.stop in ('ssdprep', 'ssdraw'):
                return
            self.tap("mix1", self.MIXT[:], [128, 8, NT], BF16, [])
            if self.stop == 'mix1':
                return
            self.x_alloc()
            self.x_reload(b)
            self.outproj(b, 1, self.od_w_out[0])
        self.tap("xmid1", self.X[:], [128, 8, NT], F32, [])
        if self.stop == 'xmid1':
            return
        self.moe(b, 1)
        self.tap("xout1", self.X[:], [128, 8, NT], F32, [])
        self.write_out(b, out)
        self.x_free()

    def mlstm(self, b):
        p, PS, nc = self.p, self.PS, self.nc
        w_in = self.ev_w_in[0].rearrange("(k p) n -> p k n", p=128)
        order = [list(range(NCH)), [1, 0] + list(range(NCH - 1, 1, -1))]
        with self.scope() as ph:
            P = lambda name, shape, dt=F32: self.sb(ph, name, shape, dt)
            GT = P("GT", [128, NCH, 16]); LF = P("LF", [128, NCH, 8]); GB = P("GB", [128, 16])
            GJ = P("GJ", [128, NCH, 2, 4]); BIAS = P("BIAS", [128, NCH, 2, 4]); WE = P("WE", [128, NCH, 2, 4]); DEC = P("DEC", [128, NCH, 2, 4])
            MLN = P("MLN", [128, 512]); WG = P("WG", [128, 8, 16], BF16); ONEC = P("ONEC", [128, 1])
            ET = P("ET", [128, 8]); T12 = P("T12", [128, 2, 4])
            self.memset(ONEC[:], 1.0, ['ONEC'])
            p.dma('sp', [lambda e: e.dma_start(out=GB[:], in_=self.ml_gate_b[0].partition_broadcast(128)),
                         lambda e: e.dma_start(out=MLN[:], in_=self.ml_norm[0].partition_broadcast(128))], writes=['GB', 'MLN'])
            p.dma('pool', lambda e: e.dma_start(out=WG[:], in_=w_in[:, :, 2048:2064]), writes=['WG'])
            for ch in range(NCH):
                bank = ch % 2
                hk = tk('HT', ch * 128, 128)
                for k in range(8):
                    self.mm(PS[bank][:, 0:16], self.HT[:, k, ch * 128:(ch + 1) * 128], WG[:, k, :], k == 0, k == 7,
                            hk + ['WG'], [('ps', bank)])
                self.tt(GT[:, ch, :], PS[bank][:, 0:16], GB[:], ALU.add, [('ps', bank), 'GB'], [('GT', ch)])
                gv = GT[:, ch, :].rearrange("p (d g h) -> p d g h", d=2, g=2)
                lfv = LF[:, ch, :].rearrange("p (d h) -> p d h", d=2)
                etv = ET[:].rearrange("p (d h) -> p d h", d=2)
                self.act(etv, gv[:, :, 1, :], AF.Exp, [('GT', ch)], ['ET'], scale=-1.0)
                self.act(etv, etv, AF.Ln, ['ET'], ['ET'], bias=ONEC[:, 0:1], scale=1.0)
                self.ts(lfv, etv, -1.0, None, ALU.mult, None, ['ET'], [('LF', ch)])
                for d in range(2):
                    bk = 2 + d
                    rhs = LF[:, ch, d * 4:(d + 1) * 4]
                    self.mm(PS[bk][:, 0:4], self.TRI[:, 3 * d + 0, :], rhs, True, True, [('LF', ch), 'TRI'], [('ps', bk)])
                    self.mm(PS[bk][:, 4:8], self.TRI[:, 3 * d + 1, :], rhs, True, True, [('LF', ch), 'TRI'], [('ps', bk)])
                    self.mm(PS[bk][:, 8:12], self.ONESF[:], rhs, True, True, [('LF', ch), 'ONESF'], [('ps', bk)])
                    li = gv[:, d, 0, :]
                    self.act(GJ[:, ch, d, :], PS[bk][:, 0:4], AF.Exp, [('ps', bk)], [('GJ', ch, d)])
                    self.tt(BIAS[:, ch, d, :], li, PS[bk][:, 0:4], ALU.subtract, [('ps', bk), ('GT', ch)], [('BIAS', ch, d)])
                    self.tt(T12[:, d, :], li, PS[bk][:, 4:8], ALU.add, [('ps', bk), ('GT', ch)], [('T12', d)])
                    self.act(WE[:, ch, d, :], T12[:, d, :], AF.Exp, [('T12', d)], [('WE', ch, d)])
                    self.act(DEC[:, ch, d, :], PS[bk][:, 8:12], AF.Exp, [('ps', bk)], [('DEC', ch, d)])
            p.barrier()
            if self.stop == 'mlgate':
                for nm, t in [('GJ', GJ), ('BIAS', BIAS), ('WE', WE), ('DEC', DEC)]:
                    self.tap(nm, t[:], [128, NCH, 2, 4], F32, [])
                return
            for hg in range(2):
                heads = [2 * hg, 2 * hg + 1]
                with self.scope() as hs:
                    H = lambda name, shape, dt=F32: self.sb(hs, name, shape, dt)
                    B_ = {}
                    for hd in heads:
                        B_[hd] = dict(
                            W4=H("W4", [128, 8, 4, 128], BF16), QT=H("QT", [128, NT], BF16), KT=H("KT", [128, NT], BF16),
                            KTOK=H("KTOK", [128, NCH, 128], BF16), VP=H("VP", [128, NCH, 129], BF16), SO=H("SO", [128, NCH, 128], BF16),
                            HS=H("HS", [128, NCH, 128]), SALL=H("SALL", [128, NCH, 128], BF16))
                    U_ = {}
                    for hd in heads:
                        for d in range(2):
                            U_[(hd, d)] = dict(CS=H("CS", [128, 129]), CSB=H("CSB", [128, 129], BF16), LFB=H("LFB", [128, 128]),
                                               DT=H("DT", [128, 128], BF16), PT=H("PT", [128, 128], BF16), TB=H("TB", [128, 129]),
                                               DEN=H("DEN", [128, 2]), VW=H("VW", [128, 129], BF16))
                            U_[(hd, d)]['NUM'] = U_[(hd, d)]['TB']
                    for hd in heads:
                        bb = B_[hd]
                        W4 = bb['W4']
                        self.dmas('pool', [(W4[:, :, i, :], w_in[:, :, i * 512 + hd * 128:i * 512 + (hd + 1) * 128]) for i in range(4)], writes=[('W4', hd)])
                        self.memset(bb['HS'][:], 0.0, [('HS', hd, c) for c in range(NCH)])
                        self.memset(bb['VP'][:, :, 128:129], 1.0, [('VP1', hd)], eng='pool')
                        for d in range(2):
                            self.memset(U_[(hd, d)]['CS'][:], 0.0, [('CS', hd, d)])
                            self.memset(U_[(hd, d)]['CSB'][:], 0.0, [('CSB', hd, d)], eng='pool')
                    cnt = 0
                    for hd in heads:
                        bb = B_[hd]
                        for ti, (t0, n) in enumerate(TILES):
                            hk = tk('HT', t0, n)
                            for i, nm in enumerate(['QT', 'KT']):
                                bank = cnt % 4
                                cnt += 1
                                for k in range(8):
                                    self.mm(PS[bank][:, :n], bb['W4'][:, k, i, :], self.HT[:, k, t0:t0 + n], k == 0, k == 7, hk + [('W4', hd)], [('ps', bank)])
                                self.act(bb[nm][:, t0:t0 + n], PS[bank][:, :n], AF.Copy, [('ps', bank)], [(nm, hd, c) for c in range(t0 // 128, (t0 + n) // 128)],
                                         scale=1.0 if i == 0 else 128.0 ** -0.5)
                    for hd in heads:
                        bb = B_[hd]
                        for ch in range(NCH):
                            bank = 4 + ch % 2
                            sbk = 6 + ch % 2
                            tsl = slice(ch * 128, (ch + 1) * 128)
                            hk = tk('HT', ch * 128, 128)
                            for k in range(8):
                                self.mm(PS[bank][:, 0:384], self.HT[:, k, tsl], bb['W4'][:, k, 1:4, :].rearrange("p a n -> p (a n)"),
                                        k == 0, k == 7, hk + [('W4', hd)], [('ps', bank)])
                            self.act(bb['KTOK'][:, ch, :], PS[bank][:, 0:128], AF.Copy, [('ps', bank)], [('KTOK', hd, ch)], scale=128.0 ** -0.5)
                            self.cp(bb['VP'][:, ch, 0:128], PS[bank][:, 128:256], [('ps', bank)], [('VP', hd, ch)])
                            self.act(bb['SO'][:, ch, :], PS[bank][:, 256:384], AF.Sigmoid, [('ps', bank)], [('SO', hd, ch)])
                            self.mm(PS[sbk][:, 0:128], bb['KT'][:, tsl], bb['QT'][:, tsl], True, True, [('KT', hd, ch), ('QT', hd, ch)], [('ps', sbk)])
                            self.cp(bb['SALL'][:, ch, :], PS[sbk][:, 0:128], [('ps', sbk)], [('SALL', hd, ch)])
                    units = [(hd, d) for hd in heads for d in range(2)]
                    for step in range(NCH):
                        ctxs = []
                        for ui, (hd, d) in enumerate(units):
                            ch = order[d][step]
                            ctxs.append((ui, hd, d, ch, slice(ch * 128, (ch + 1) * 128), B_[hd], U_[(hd, d)], 2 * ui, 2 * ui + 1))
                        for (ui, hd, d, ch, tsl, bb, uu, bx, by) in ctxs:
                            self.act(uu['LFB'][:], self.ONESF[:], AF.Identity, [('LF', ch), 'ONESF'], [('LFB', hd, d)], scale=LF[:, ch, d * 4 + hd:d * 4 + hd + 1])
                        for (ui, hd, d, ch, tsl, bb, uu, bx, by) in ctxs:
                            self.mm(PS[bx][:, 0:128], uu['LFB'][:], self.TRI[:, 3 * d + 0, :], True, False, [('LFB', hd, d), 'TRI'], [('ps', bx)])
                            self.mm(PS[bx][:, 0:128], self.IDF[:], self.TRI[:, 3 * d + 2, :], False, True, ['IDF', 'TRI'], [('ps', bx)])
                        for (ui, hd, d, ch, tsl, bb, uu, bx, by) in ctxs:
                            self.act(uu['DT'][:], PS[bx][:, 0:128], AF.Exp, [('ps', bx), ('BIAS', ch, d)], [('DT', hd, d)], bias=BIAS[:, ch, d, hd:hd + 1], scale=1.0)
                        for (ui, hd, d, ch, tsl, bb, uu, bx, by) in ctxs:
                            self.tt(uu['PT'][:], bb['SALL'][:, ch, :], uu['DT'][:], ALU.mult, [('SALL', hd, ch), ('DT', hd, d)], [('PT', hd, d)], eng='pool')
                            self.ts(uu['VW'][:], bb['VP'][:, ch, :], WE[:, ch, d, hd:hd + 1], None, ALU.mult, None,
                                    [('VP', hd, ch), ('VP1', hd), ('WE', ch, d)], [('VW', hd, d)])
                        for (ui, hd, d, ch, tsl, bb, uu, bx, by) in ctxs:
                            self.mm(PS[by][:, 0:129], uu['PT'][:], bb['VP'][:, ch, :], True, True, [('PT', hd, d), ('VP', hd, ch), ('VP1', hd)], [('ps', by)])
                            self.mm(PS[by][:, 129:258], bb['QT'][:, tsl], uu['CSB'][:], True, True, [('QT', hd, ch), ('CSB', hd, d)], [('ps', by)])
                            self.mm(PS[bx][:, 128:257], bb['KTOK'][:, ch, :], uu['VW'][:], True, True, [('KTOK', hd, ch), ('VW', hd, d)], [('ps', bx)])
                        for (ui, hd, d, ch, tsl, bb, uu, bx, by) in ctxs:
                            self.act(uu['TB'][:], PS[by][:, 129:258], AF.Identity, [('ps', by), ('GJ', ch, d)], [('TB', hd, d)], scale=GJ[:, ch, d, hd:hd + 1])
                        for (ui, hd, d, ch, tsl, bb, uu, bx, by) in ctxs:
                            self.tt(uu['NUM'][:], PS[by][:, 0:129], uu['TB'][:], ALU.add, [('ps', by), ('TB', hd, d)], [('NUM', hd, d), ('TB', hd, d)])
                            self.stt(uu['CS'][:], uu['CS'][:], DEC[:, ch, d, hd:hd + 1], PS[bx][:, 128:257], ALU.mult, ALU.add,
                                     [('CS', hd, d), ('DEC', ch, d), ('ps', bx)], [('CS', hd, d)])
                        for (ui, hd, d, ch, tsl, bb, uu, bx, by) in ctxs:
                            self.act(uu['CSB'][:], uu['CS'][:], AF.Copy, [('CS', hd, d)], [('CSB', hd, d)])
                        for (ui, hd, d, ch, tsl, bb, uu, bx, by) in ctxs:
                            DEN = uu['DEN']
                            self.stt(DEN[:, 0:1], uu['NUM'][:, 128:129], -1.0, uu['NUM'][:, 128:129], ALU.mult, ALU.max, [('NUM', hd, d), ('TB', hd, d)], [('DEN', hd, d, 0)])
                        for (ui, hd, d, ch, tsl, bb, uu, bx, by) in ctxs:
                            DEN = uu['DEN']
                            self.ts(DEN[:, 0:1], DEN[:, 0:1], 1.0, None, ALU.max, None, [('DEN', hd, d, 0)], [('DEN', hd, d, 0)])
                        for (ui, hd, d, ch, tsl, bb, uu, bx, by) in ctxs:
                            DEN = uu['DEN']
                            self.recip(DEN[:, 1:2], DEN[:, 0:1], [('DEN', hd, d, 0)], [('DEN', hd, d, 1)])
                        for (ui, hd, d, ch, tsl, bb, uu, bx, by) in ctxs:
                            DEN = uu['DEN']
                            self.stt(bb['HS'][:, ch, :], uu['NUM'][:, 0:128], DEN[:, 1:2], bb['HS'][:, ch, :], ALU.mult, ALU.add,
                                     [('NUM', hd, d), ('TB', hd, d), ('DEN', hd, d, 1), ('HS', hd, ch)], [('HS', hd, ch)])
                    SSQ = H("SSQ", [128, 4]); JK = [H("JK0", [128, 128])] * 2; T1 = [H("T10", [128, 128])] * 2
                    MT = [H("MT%d" % i, [128, 128], BF16) for i in range(2)]
                    for hi_, hd in enumerate(heads):
                        bb = B_[hd]
                        for ch in range(NCH):
                            bank = (hi_ * NCH + ch) % 4
                            s2 = ch % 2
                            o0 = 2 * s2
                            self.act(JK[s2][:], bb['HS'][:, ch, :], AF.Square, [('HS', hd, ch)], [('JK', 0)])
                            self.p.op('dve', lambda e, s2=s2, o0=o0, JK=JK, SSQ=SSQ: e.reduce_sum(out=SSQ[:, o0:o0 + 1], in_=JK[s2][:], axis=AX.X), [('JK', 0)], [('SSQ', o0)])
                            self.act(SSQ[:, o0:o0 + 1], SSQ[:, o0:o0 + 1], AF.Sqrt, [('SSQ', o0)], [('SSQ', o0)], bias=self.EPSC[:, 0:1], scale=1.0 / 128)
                            self.recip(SSQ[:, o0 + 1:o0 + 2], SSQ[:, o0:o0 + 1], [('SSQ', o0)], [('SSQ', o0 + 1)])
                            self.stt(T1[s2][:], bb['HS'][:, ch, :], SSQ[:, o0 + 1:o0 + 2], MLN[:, hd * 128:(hd + 1) * 128], ALU.mult, ALU.mult,
                                     [('HS', hd, ch), ('SSQ', o0 + 1), 'MLN'], [('T1', 0)])
                            self.tt(MT[s2][:], T1[s2][:], bb['SO'][:, ch, :], ALU.mult, [('T1', 0), ('SO', hd, ch)], [('MT', s2)])
                            pv = PS[bank][:].bitcast(BF16)
                            self.tr(pv[:, 0:128], MT[s2][:], self.IDB[:], [('MT', s2), 'IDB'], [('ps', bank)])
                            self.act(self.MIXT[:, hd, ch * 128:(ch + 1) * 128], pv[:, 0:128], AF.Copy, [('ps', bank)], [('MIXT', ch)])
                    p.barrier()

    def attention(self, b):
        p, PS, nc = self.p, self.PS, self.nc
        w_in = self.ev_w_in[0].rearrange("(k p) n -> p k n", p=128)
        with self.scope() as ph:
            P = lambda name, shape, dt=F32: self.sb(ph, name, shape, dt)
            WA = P("WA", [128, 8, 768], BF16)
            GQ = P("GQ", [128, 640]); RC = P("RC", [128, 16, 64]); RSN = P("RSN", [128, 16, 64])
            QT4 = P("QT4", [128, 4, NT], BF16); KT = P("KTa", [128, NT], BF16); VP = P("VPa", [128, NCH, 2, 65], BF16)
            ATT = P("ATT", [128, NCH, 512], BF16)
            QS = [P("QS%d" % i, [128, 768]) for i in range(2)]
            SQ = P("SQa", [128, 640]); SSQ = P("SSQa", [128, 10]); RSTD = P("RSTDa", [128, 10])
            QN1 = P("QN1", [128, 640]); T1 = P("T1a", [128, 640]); T2 = P("T2a", [128, 640])
            QRq = P("QRq", [128, 4, 2, 64], BF16); QRk = P("QRk", [128, 128], BF16)
            ET = [P("ET%d" % i, [128, 512], BF16) for i in range(3)]
            RD = [P("RD%d" % i, [128, 4]) for i in range(2)]
            p.dma('pool', [lambda e, i=i: e.dma_start(out=WA[:, i * 4:(i + 1) * 4, :], in_=w_in[:, i * 4:(i + 1) * 4, 2064:2832]) for i in range(2)], writes=['WA'])
            p.dma('sp', [lambda e, i=i: e.dma_start(out=GQ[:, i * 64:(i + 1) * 64], in_=self.at_q_norm[0].partition_broadcast(128)) for i in range(8)] +
                        [lambda e, i=i: e.dma_start(out=GQ[:, 512 + i * 64:512 + (i + 1) * 64], in_=self.at_k_norm[0].partition_broadcast(128)) for i in range(2)],
                  writes=['GQ'])
            p.dma('act', [lambda e: e.dma_start(out=RC[:], in_=self.k_rope[0].rearrange("(c p) d -> p c d", p=128)),
                          lambda e: e.dma_start(out=RSN[:], in_=self.k_rope[1].rearrange("(c p) d -> p c d", p=128))], writes=['ROPE'])
            self.ts(GQ[:, 0:512], GQ[:, 0:512], 0.125, None, ALU.mult, None, ['GQ'], ['GQ'])
            self.memset(VP[:, :, :, 64:65], 1.0, [('VP1',)], eng='pool')
            for ch in range(NCH):
                hk = tk('HT', ch * 128, 128)
                qs = QS[ch % 2]
                bA, bB, bT = (ch % 2) * 3, (ch % 2) * 3 + 1, (ch % 2) * 3 + 2
                for k in range(8):
                    self.mm(PS[bA][:, 0:512], self.HT[:, k, ch * 128:(ch + 1) * 128], WA[:, k, 0:512], k == 0, k == 7, hk + ['WA'], [('ps', bA)])
                for k in range(8):
                    self.mm(PS[bB][:, 0:256], self.HT[:, k, ch * 128:(ch + 1) * 128], WA[:, k, 512:768], k == 0, k == 7, hk + ['WA'], [('ps', bB)])
                self.act(qs[:, 0:512], PS[bA][:, 0:512], AF.Copy, [('ps', bA)], [('QS', ch % 2)])
                self.act(qs[:, 512:768], PS[bB][:, 0:256], AF.Copy, [('ps', bB)], [('QS', ch % 2)])
                self.act(SQ[:], qs[:, 0:640], AF.Square, [('QS', ch % 2)], ['SQ'])
                self.p.op('dve', lambda e: e.tensor_reduce(out=SSQ[:], in_=SQ[:].rearrange("p (h d) -> p h d", d=64), axis=AX.X, op=ALU.add), ['SQ'], ['SSQ'])
                self.act(SSQ[:], SSQ[:], AF.Sqrt, ['SSQ'], ['SSQ'], bias=self.EPSC[:, 0:1], scale=1.0 / 64)
                self.recip(RSTD[:], SSQ[:], ['SSQ'], ['RSTD'])
                self.tt(QN1[:].rearrange("p (h d) -> p h d", d=64), qs[:, 0:640].rearrange("p (h d) -> p h d", d=64),
                        RSTD[:].unsqueeze(2).to_broadcast([128, 10, 64]), ALU.mult, [('QS', ch % 2), 'RSTD'], ['QN1'])
                self.tt(QN1[:], QN1[:], GQ[:], ALU.mult, ['QN1', 'GQ'], ['QN1'])
                qdst = QRq[:].rearrange("p pr hl d -> p hl pr d")
                if ch >= 2:
                    lc = ch - 2
                    self.tt(T1[:].rearrange("p (h d) -> p h d", d=64), QN1[:].rearrange("p (h d) -> p h d", d=64),
                            RC[:, lc, :].unsqueeze(1).to_broadcast([128, 10, 64]), ALU.mult, ['QN1', 'ROPE'], ['T1'], eng='pool')
                    q3 = QN1[:].rearrange("p (h d) -> p h d", d=64)
                    t3 = T2[:].rearrange("p (h d) -> p h d", d=64)
                    for rc in range(2):
                        for f in range(2):
                            o0 = rc * 32 + f * 16
                            i0 = rc * 32 + (1 - f) * 16
                            self.tt(t3[:, :, o0:o0 + 16], q3[:, :, i0:i0 + 16], RSN[:, lc, o0:o0 + 16].unsqueeze(1).to_broadcast([128, 10, 16]),
                                    ALU.mult, ['QN1', 'ROPE'], ['T2'])
                    self.tt(qdst, T1[:, 0:512].rearrange("p (hl pr d) -> p hl pr d", hl=2, pr=4), T2[:, 0:512].rearrange("p (hl pr d) -> p hl pr d", hl=2, pr=4),
                            ALU.add, ['T1', 'T2'], ['QRq'])
                    self.tt(QRk[:], T1[:, 512:640], T2[:, 512:640], ALU.add, ['T1', 'T2'], ['QRk'])
                else:
                    self.cp(qdst, QN1[:, 0:512].rearrange("p (hl pr d) -> p hl pr d", hl=2, pr=4), ['QN1'], ['QRq'])
                    self.cp(QRk[:], QN1[:, 512:640], ['QN1'], ['QRk'])
                self.cp(VP[:, ch, :, 0:64], qs[:, 640:768].rearrange("p (k d) -> p k d", d=64), [('QS', ch % 2)], [('VP', ch)], eng='pool')
                pv = PS[bT][:].bitcast(BF16)
                for pr in range(4):
                    self.tr(pv[:, pr * 128:(pr + 1) * 128], QRq[:, pr, :, :].rearrange("p a d -> p (a d)"), self.IDB[:], ['QRq', 'IDB'], [('ps', bT)])
                self.tr(pv[:, 512:640], QRk[:], self.IDB[:], ['QRk', 'IDB'], [('ps', bT)])
                self.act(QT4[:, :, ch * 128:(ch + 1) * 128], pv[:, 0:512].rearrange("p (a t) -> p a t", t=128), AF.Copy, [('ps', bT)], [('QT4', ch)])
                self.cp(KT[:, ch * 128:(ch + 1) * 128], pv[:, 512:640], [('ps', bT)], [('KTa', ch)])
            p.barrier()
            jobs = []
            for h in range(8):
                jobs.append((h, 0, 256, [0, 1]))
                for i in range(4):
                    jobs.append((h, 256 + 512 * i, 512, list(range(NCH))))
            sct = 0
            for ji, (h, q0, n, kcs) in enumerate(jobs):
                hl, pr = h // 4, h % 4
                ob = 4 + ji % 4
                nsub = n // 128
                qk = [('QT4', c) for c in range(q0 // 128, (q0 + n) // 128)]

                def issue_s(ki, sct):
                    kc = kcs[ki]
                    sb_ = sct % 4
                    self.mm(PS[sb_][:, :n], KT[hl * 64:(hl + 1) * 64, kc * 128:(kc + 1) * 128], QT4[hl * 64:(hl + 1) * 64, pr, q0:q0 + n],
                            True, True, [('KTa', kc)] + qk, [('ps', sb_)])
                issue_s(0, sct)
                for ki, kc in enumerate(kcs):
                    if ki + 1 < len(kcs):
                        issue_s(ki + 1, sct + 1)
                    sb_ = sct % 4
                    et = ET[sct % 3]
                    self.act(et[:, :n], PS[sb_][:, :n], AF.Exp, [('ps', sb_)], [('ET', sct % 3)])
                    for j in range(nsub):
                        self.mm(PS[ob][:, j * 65:(j + 1) * 65], et[:, j * 128:(j + 1) * 128], VP[:, kc, hl, :], ki == 0 and j == 0, ki == len(kcs) - 1,
                                [('ET', sct % 3), ('VP', kc), ('VP1',)], [('ps', ob)], skip_group_check=True)
                    sct += 1
                rd = RD[ji % 2]
                ov = PS[ob][:, 0:nsub * 65].rearrange("p (j d) -> p j d", d=65)
                self.recip(rd[:, 0:nsub], ov[:, :, 64], [('ps', ob)], [('RD', ji % 2)])
                c0 = q0 // 128
                self.tt(ATT[:, c0:c0 + nsub, h * 64:(h + 1) * 64], ov[:, :, 0:64], rd[:, 0:nsub].unsqueeze(2).to_broadcast([128, nsub, 64]), ALU.mult,
                        [('ps', ob), ('RD', ji % 2)], [('ATT', c) for c in range(c0, c0 + nsub)])
            for ch in range(NCH):
                bank = ch % 4
                pv = PS[bank][:].bitcast(BF16)
                for j in range(4):
                    self.tr(pv[:, j * 128:(j + 1) * 128], ATT[:, ch, j * 128:(j + 1) * 128], self.IDB[:], [('ATT', ch), 'IDB'], [('ps', bank)])
                self.act(self.MIXT[:, 4:8, ch * 128:(ch + 1) * 128], pv[:, 0:512].rearrange("p (a t) -> p a t", t=128), AF.Copy, [('ps', bank)], [('MIXT', ch)])
            p.barrier()

    def outproj(self, b, l, w_out):
        p, PS = self.p, self.PS
        with self.scope() as ph:
            WO = self.sb(ph, "WO", [128, 8, 1024], BF16)
            src = w_out.rearrange("(k p) n -> p k n", p=128)
            p.dma('pool', [lambda e, i=i: e.dma_start(out=WO[:, i * 4:(i + 1) * 4, :], in_=src[:, i * 4:(i + 1) * 4, :]) for i in range(2)], writes=['WO'])
            cnt = 0
            for ti, (t0, n) in enumerate(TILES):
                if l == 1 and ti == 0:
                    continue
                j = 2 if ti == 0 else b
                mk = tk('MIXT', t0, n)
                xk = tk('X', t0, n)
                for c in range(8):
                    bank = cnt % 4
                    cnt += 1
                    for k in range(8):
                        self.mm(PS[bank][:, :n], WO[:, k, c * 128:(c + 1) * 128], self.MIXT[:, k, t0:t0 + n], k == 0, k == 7, mk + ['WO'], [('ps', bank)])
                    self.stt(self.X[:, c, t0:t0 + n], PS[bank][:, :n], self.MOD[:, l, 16 + c, j:j + 1], self.X[:, c, t0:t0 + n], ALU.mult, ALU.add,
                             [('ps', bank)] + xk, xk)
            p.barrier()

    def moe(self, b, l):
        p, PS = self.p, self.PS
        tiles = TILES[1:] if l == 1 else TILES
        with self.scope() as ph:
            P = lambda name, shape, dt=F32: self.sb(ph, name, shape, dt)
            COMBT = P("COMBT", [16, NT], BF16)
            SEL = P("SEL", [16, 16, 128], BF16)
            WGU = [None, None]
            WD = [None, None]
            p.dma('pool', lambda e: e.dma_start(out=SEL[:], in_=self.k_sel.rearrange("e r n -> r e n")), writes=['SEL'])

            def load_expert(e):
                slot = e % 2
                g = self.moe_w_gate[l, e].rearrange("(k p) n -> p k n", p=128)
                u = self.moe_w_up[l, e].rearrange("(k p) n -> p k n", p=128)
                dn = self.moe_w_down[l, e].rearrange("(k p) n -> p k n", p=128)
                p.dma('pool', [lambda en, i=i: en.dma_start(out=WGU[slot][:, i * 4:(i + 1) * 4, 0:512], in_=g[:, i * 4:(i + 1) * 4, :]) for i in range(2)] +
                              [lambda en, i=i: en.dma_start(out=WGU[slot][:, i * 4:(i + 1) * 4, 512:1024], in_=u[:, i * 4:(i + 1) * 4, :]) for i in range(2)],
                      writes=[('WGU', slot)])
                p.dma('pool', [lambda en, i=i: en.dma_start(out=WD[slot][:, i * 2:(i + 1) * 2, :], in_=dn[:, i * 2:(i + 1) * 2, :]) for i in range(2)],
                      writes=[('WD', slot)])
            with self.scope() as rs:
                R = lambda name, shape, dt=F32: self.sb(rs, name, shape, dt)
                WR = R("WR", [128, 8, 20]); RB = R("RB", [128, 20]); L = R("L", [128, 20])
                SM = R("SM", [128, 16]); GM = R("GM", [128, 4]); GE = R("GE", [128, 4]); PEN = R("PEN", [128, 4])
                EM = R("EM", [128, 16]); EM2 = R("EM2", [128, 16]); M1 = R("M1", [128, 16]); M2 = R("M2", [128, 16]); COMB = R("COMB", [128, 16])
                p.dma('sp', [lambda e: e.dma_start(out=WR[:, :, 0:4], in_=self.moe_gr_w[l].rearrange("(k p) n -> p k n", p=128)),
                             lambda e: e.dma_start(out=WR[:, :, 4:20], in_=self.moe_er_w[l].rearrange("(k p) n -> p k n", p=128)),
                             lambda e: e.dma_start(out=RB[:, 0:4], in_=self.moe_gr_b[l].partition_broadcast(128)),
                             lambda e: e.dma_start(out=RB[:, 4:20], in_=self.moe_er_b[l].partition_broadcast(128))], writes=['WR', 'RB'])

                def router(ti, t0, n, H32):
                    for sub in range(n // 128):
                        rb = 2 + sub % 2
                        tb = 4 + sub % 2
                        for k in range(8):
                            self.mm(PS[rb][:, 0:20], H32[:, k, sub * 128:(sub + 1) * 128], WR[:, k, :], k == 0, k == 7, [('H32', k), 'WR'], [('ps', rb)])
                        self.tt(L[:], PS[rb][:, 0:20], RB[:], ALU.add, [('ps', rb), 'RB'], ['L'])
                        sm = lambda i: SM[:, i:i + 1]
                        self.p.op('dve', lambda e: e.reduce_max(out=SM[:, 0:1], in_=L[:, 0:4], axis=AX.X), ['L'], [('SM', 0)])
                        self.ts(GM[:], L[:, 0:4], sm(0), None, ALU.is_equal, None, ['L', ('SM', 0)], ['GM'])
                        self.ts(sm(1), sm(0), -1.0, None, ALU.mult, None, [('SM', 0)], [('SM', 1)])
                        self.act(GE[:], L[:, 0:4], AF.Exp, ['L', ('SM', 1)], ['GE'], bias=sm(1), scale=1.0)
                        self.p.op('dve', lambda e: e.reduce_sum(out=SM[:, 2:3], in_=GE[:], axis=AX.X), ['GE'], [('SM', 2)])
                        self.recip(sm(3), sm(2), [('SM', 2)], [('SM', 3)])
                        self.ts(PEN[:], GM[:], 1e30, -1e30, ALU.mult, ALU.add, ['GM'], ['PEN'])
                        self.tt(EM[:].rearrange("p (g e) -> p g e", e=4), L[:, 4:20].rearrange("p (g e) -> p g e", e=4),
                                PEN[:].unsqueeze(2).to_broadcast([128, 4, 4]), ALU.add, ['L', 'PEN'], ['EM'])
                        self.p.op('dve', lambda e: e.reduce_max(out=SM[:, 4:5], in_=EM[:], axis=AX.X), ['EM'], [('SM', 4)])
                        self.ts(M1[:], EM[:], sm(4), None, ALU.is_equal, None, ['EM', ('SM', 4)], ['M1'])
                        self.stt(EM2[:], M1[:], -1e30, EM[:], ALU.mult, ALU.add, ['M1', 'EM'], ['EM2'])
                        self.p.op('dve', lambda e: e.reduce_max(out=SM[:, 5:6], in_=EM2[:], axis=AX.X), ['EM2'], [('SM', 5)])
                        self.ts(M2[:], EM2[:], sm(5), None, ALU.is_equal, None, ['EM2', ('SM', 5)], ['M2'])
                        self.tt(sm(6), sm(5), sm(4), ALU.subtract, [('SM', 5), ('SM', 4)], [('SM', 6)])
                        self.act(sm(7), sm(6), AF.Exp, [('SM', 6)], [('SM', 7)])
                        self.ts(sm(8), sm(7), 1.0, None, ALU.add, None, [('SM', 7)], [('SM', 8)])
                        self.recip(sm(9), sm(8), [('SM', 8)], [('SM', 9)])
                        self.tt(sm(10), sm(9), sm(3), ALU.mult, [('SM', 9), ('SM', 3)], [('SM', 10)])
                        self.tt(sm(11), sm(10), sm(7), ALU.mult, [('SM', 10), ('SM', 7)], [('SM', 11)])
                        self.ts(COMB[:], M1[:], sm(10), None, ALU.mult, None, ['M1', ('SM', 10)], ['COMB'])
                        self.stt(COMB[:], M2[:], sm(11), COMB[:], ALU.mult, ALU.add, ['M2', ('SM', 11), 'COMB'], ['COMB'])
                        self.tr(PS[tb][0:16, 0:128], COMB[:], self.IDF[:], ['COMB', 'IDF'], [('ps', tb)])
                        tt0 = t0 + sub * 128
                        self.act(COMBT[:, tt0:tt0 + 128], PS[tb][0:16, 0:128], AF.Copy, [('ps', tb)], [('COMBT', tt0 // 128)])
                self.norm(b, l, 1, f32_cb=router, skip_ctx=(l == 1))
            for i in range(2):
                WGU[i] = P("WGU%d" % i, [128, 8, 1024], BF16)
                WD[i] = P("WD%d" % i, [128, 4, 1024], BF16)
            load_expert(0)
            load_expert(1)
            CB = [P("CB%d" % i, [128, 512], BF16) for i in range(2)]
            SG = [P("SG%d" % i, [128, 512], BF16) for i in range(2)]
            ACTT = [P("ACTT%d" % i, [128, 4, 512], BF16) for i in range(2)]
            items = [(e, ti) for e in range(16) for ti in range(len(tiles))]

            def stage_a(ii):
                e, ti = items[ii]
                t0, n = tiles[ti]
                slot = e % 2
                hk = tk('HT', t0, n)
                cb = CB[ii % 2]
                self.mm(PS[0][:, :n], SEL[:, e, :], COMBT[:, t0:t0 + n], True, True, ['SEL'] + tk('COMBT', t0, n), [('ps', 0)])
                self.act(cb[:, :n], PS[0][:, :n], AF.Copy, [('ps', 0)], [('CB', ii % 2)])
                for j in range(4):
                    gb = 1 + j % 2
                    ub = 3 + j % 2
                    sg = SG[j % 2]
                    for k in range(8):
                        self.mm(PS[gb][:, :n], WGU[slot][:, k, j * 128:(j + 1) * 128], self.HT[:, k, t0:t0 + n], k == 0, k == 7,
                                hk + [('WGU', slot)], [('ps', gb)])
                    for k in range(8):
                        self.mm(PS[ub][:, :n], WGU[slot][:, k, 512 + j * 128:512 + (j + 1) * 128], self.HT[:, k, t0:t0 + n], k == 0, k == 7,
                                hk + [('WGU', slot)], [('ps', ub)])
                    self.act(sg[:, :n], PS[gb][:, :n], AF.Silu, [('ps', gb)], [('SG', j % 2)])
                    self.tt(sg[:, :n], sg[:, :n], cb[:, :n], ALU.mult, [('SG', j % 2), ('CB', ii % 2)], [('SG', j % 2)])
                    self.tt(ACTT[ii % 2][:, j, :n], sg[:, :n], PS[ub][:, :n], ALU.mult, [('SG', j % 2), ('ps', ub)], [('ACTT', ii % 2, j)])

            def stage_b(ii):
                e, ti = items[ii]
                t0, n = tiles[ti]
                slot = e % 2
                j_ = 2 if (l == 0 and ti == 0) else b
                xk = tk('X', t0, n)
                for c in range(8):
                    db = 5 + c % 3
                    for j in range(4):
                        self.mm(PS[db][:, :n], WD[slot][:, j, c * 128:(c + 1) * 128], ACTT[ii % 2][:, j, :n], j == 0, j == 3,
                                [('WD', slot), ('ACTT', ii % 2, j)], [('ps', db)])
                    self.stt(self.X[:, c, t0:t0 + n], PS[db][:, :n], self.MOD[:, l, 40 + c, j_:j_ + 1], self.X[:, c, t0:t0 + n], ALU.mult, ALU.add,
                             [('ps', db)] + xk, xk)
            stage_a(0)
            for ii in range(len(items)):
                if ii + 1 < len(items):
                    stage_a(ii + 1)
                stage_b(ii)
                e_, ti_ = items[ii]
                if ti_ == len(tiles) - 1 and e_ + 2 < 16:
                    load_expert(e_ + 2)
            p.barrier()

    def write_out(self, b, out):
        p, PS = self.p, self.PS
        with self.scope() as ph:
            OT = [self.sb(ph, "OT%d" % i, [128, 1024], F32) for i in range(2)]
            for ch in range(2, NCH):
                s = ch % 2
                for hf in range(2):
                    bank = (ch * 2 + hf) % 4
                    for kk in range(4):
                        k = hf * 4 + kk
                        self.tr(PS[bank][:, kk * 128:(kk + 1) * 128], self.X[:, k, ch * 128:(ch + 1) * 128], self.IDF[:], [('X', ch), 'IDF'], [('ps', bank)])
                    if hf == 0:
                        self.act(OT[s][:, 0:512], PS[bank][:], AF.Copy, [('ps', bank)], [('OT', s, 0)])
                    else:
                        self.cp(OT[s][:, 512:1024], PS[bank][:], [('ps', bank)], [('OT', s, 1)])
                p.dma('sp' if ch % 2 == 0 else 'act', lambda e, s=s, ch=ch: e.dma_start(out=out[b, (ch - 2) * 128:(ch - 1) * 128, :], in_=OT[s][:]),
                      reads=[('OT', s, 0), ('OT', s, 1)])
            p.barrier()

    def cis(self, R, MAG, ORE, OIM, I, F, G, S, key, neg_im=False):
        TWO_PI = 6.2831845
        MAGIC = 12582912.0
        k = lambda n: (key, n)
        self.ts(I, R, MAGIC, None, ALU.add, None, [k('R')], [k('I')])
        self.ts(I, I, MAGIC, None, ALU.subtract, None, [k('I')], [k('I')])
        self.tt(F, R, I, ALU.subtract, [k('R'), k('I')], [k('F')])
        self.act(S, F, AF.Sin, [k('F')], [k('S')], scale=TWO_PI)
        self.act(G, MAG, AF.Exp, [k('MAG'), k('G')], [k('G')])
        if neg_im:
            self.stt(OIM, G, -1.0, S, ALU.mult, ALU.mult, [k('G'), k('S')], [k('OIM')])
        else:
            self.tt(OIM, G, S, ALU.mult, [k('G'), k('S')], [k('OIM')])
        self.ts(F, F, 0.25, None, ALU.add, None, [k('F')], [k('F')])
        self.ts(I, F, MAGIC, None, ALU.add, None, [k('F')], [k('I')])
        self.ts(I, I, MAGIC, None, ALU.subtract, None, [k('I')], [k('I')])
        self.tt(F, F, I, ALU.subtract, [k('F'), k('I')], [k('F')])
        self.act(S, F, AF.Sin, [k('F')], [k('S')], scale=TWO_PI)
        self.tt(ORE, G, S, ALU.mult, [k('G'), k('S')], [k('ORE')])

    def s5(self, b):
        p, PS, nc = self.p, self.PS, self.nc
        w_in = self.od_w_in[0].rearrange("(k p) n -> p k n", p=128)
        order = [list(range(NCH)), [1, 0] + list(range(NCH - 1, 1, -1))]
        INV2PI = 1.0 / (2.0 * math.pi)
        with self.scope() as ph:
            P = lambda name, shape, dt=F32: self.sb(ph, name, shape, dt)
            UT = P("UT", [128, 4, NT], BF16); YT = self.MIXT[:, 0:4, :]
            self.S5_UT, self.S5_YT = UT, YT
            TRIB = P("TRIB", [128, 6, 128], BF16); MASK8 = P("MASK8", [128, 8]); POSC = P("POSC", [128, 4]); POSR = P("POSR", [128, 2, 128])
            self.dmas('pool', [(TRIB[:], self.k_tri.rearrange("a p n -> p a n"))], writes=['TRIB'])
            self.dmas('sp', [(MASK8[:], self.k_mask8), (POSC[:], self.k_posc),
                         (POSR[:], self.k_posr)], writes=['MASK8', 'POSC', 'POSR'])
            with self.scope() as s1:
                WU = self.sb(s1, "WU", [128, 8, 512], BF16)
                self.dmas('pool', [(WU[:, i * 4:(i + 1) * 4, :], w_in[:, i * 4:(i + 1) * 4, 0:512]) for i in range(2)], writes=['WU'])
                cnt = 0
                for ti, (t0, n) in enumerate(TILES):
                    hk = tk('HT', t0, n)
                    for c in range(4):
                        bank = cnt % 4
                        cnt += 1
                        for k in range(8):
                            self.mm(PS[bank][:, :n], WU[:, k, c * 128:(c + 1) * 128], self.HT[:, k, t0:t0 + n], k == 0, k == 7, hk + ['WU'], [('ps', bank)])
                        self.act(UT[:, c, t0:t0 + n], PS[bank][:, :n], AF.Copy, [('ps', bank)], tk('UT', t0, n))
                p.barrier()
            for d in range(2):
                with self.scope() as sd:
                    D = lambda name, shape, dt=F32: self.sb(sd, name, shape, dt)
                    KR = D("KR", [128, 2048], BF16); KI = D("KI", [128, 2048], BF16)
                    QR = D("QR", [128, 16, 128], BF16); QI = D("QI", [128, 16, 128], BF16)
                    BDR = D("BDR", [128, 4, 512], BF16); BDI = D("BDI", [128, 4, 512], BF16)
                    CPR = D("CPR", [128, 16, 128], BF16); CPI = D("CPI", [128, 16, 128], BF16)
                    with self.scope() as st_:
                        T = lambda name, shape, dt=F32: self.sb(st_, name, shape, dt)
                        AB = T("AB", [128, 512]); OM = T("OM", [128, 512]); LDT = T("LDT", [128, 32])
                        RR = T("RR", [128, 512]); MG = T("MG", [128, 512]); II = T("II", [128, 512])
                        FF = T("FF", [128, 512]); GG = T("GG", [128, 512]); SS = T("SS", [128, 512])
                        self.dmas('sp', [(LDT[:], self.s5_log_dt[0, d].partition_broadcast(128))], writes=['LDT'])
                        self.act(LDT[:], LDT[:], AF.Exp, ['LDT'], ['LDT'])
                        for q in range(4):
                            are_q = self.s5_a_re[0, d].rearrange("g n -> (g n)")[q * 512:(q + 1) * 512]
                            aim_q = self.s5_a_im[0, d].rearrange("g n -> (g n)")[q * 512:(q + 1) * 512]
                            self.dmas('sp', [(AB[:], are_q.partition_broadcast(128)),
                                         (OM[:], aim_q.partition_broadcast(128))],
                                  reads=[('kt', 'ORE'), ('kt', 'OIM')], writes=[('kt', 'ORE'), ('kt', 'OIM')])
                            dtb = LDT[:, q * 8:(q + 1) * 8].unsqueeze(2).to_broadcast([128, 8, 64])
                            v3 = lambda t: t[:].rearrange("p (g n) -> p g n", n=64)
                            self.tt(v3(AB), v3(AB), dtb, ALU.mult, [('kt', 'ORE'), 'LDT'], [('kt', 'ORE')])
                            self.tt(v3(OM), v3(OM), dtb, ALU.mult, [('kt', 'OIM'), 'LDT'], [('kt', 'OIM')])
                            self.ts(RR[:], OM[:], POSC[:, d:d + 1], INV2PI, ALU.mult, ALU.mult, [('kt', 'OIM'), 'POSC'], [('kt', 'R')])
                            self.ts(MG[:], AB[:], POSC[:, 2 + d:3 + d], None, ALU.mult, None, [('kt', 'ORE'), 'POSC'], [('kt', 'MAG')])
                            self.cis(RR[:], MG[:], AB[:], OM[:], II[:], FF[:], GG[:], SS[:], 'kt', neg_im=True)
                            self.cp(KR[:, q * 512:(q + 1) * 512], AB[:], [('kt', 'ORE')], ['KR'])
                            self.cp(KI[:, q * 512:(q + 1) * 512], OM[:], [('kt', 'OIM')], ['KI'])
                        p.barrier()
                    with self.scope() as st_:
                        T = lambda name, shape, dt=F32: self.sb(st_, name, shape, dt)
                        STG = T("STG", [128, 128]); PRM = T("PRM", [128, 32]); LD2 = T("LD2", [128, 16])
                        RHO = T("RHO", [128, 16]); OMT = T("OMT", [128, 16])
                        RR = T("RR", [128, 4, 128]); MG = T("MG", [128, 4, 128]); II = T("II", [128, 4, 128])
                        FF = T("FF", [128, 4, 128]); GG = T("GG", [128, 4, 128]); SS = T("SS", [128, 4, 128])
                        self.memset(STG[:], 0.0, ['STG'])
                        self.dmas('sp', [(STG[0:16, :], self.s5_a_re[0, d].rearrange("(pr g2) n -> pr (g2 n)", g2=2)),
                                     (STG[16:32, :], self.s5_a_im[0, d].rearrange("(pr g2) n -> pr (g2 n)", g2=2))],
                              reads=['STG'], writes=['STG'])
                        ld = self.s5_log_dt[0, d].rearrange("(pr g2) -> g2 pr", g2=2)
                        self.dmas('sp', [(LD2[g2 * 64:(g2 + 1) * 64, :], ld[g2].partition_broadcast(64))
                                     for g2 in range(2)], writes=['LD2'], slow=True)
                        self.tr(PS[0][:, 0:128], STG[:], self.IDF[:], ['STG', 'IDF'], [('ps', 0)])
                        self.cp(PRM[:], PS[0][:, 0:32], [('ps', 0)], ['PRM'])
                        self.act(LD2[:], LD2[:], AF.Exp, ['LD2'], ['LD2'])
                        self.tt(RHO[:], PRM[:, 0:16], LD2[:], ALU.mult, ['PRM', 'LD2'], ['RHO'])
                        self.stt(OMT[:], PRM[:, 16:32], INV2PI, LD2[:], ALU.mult, ALU.mult, ['PRM', 'LD2'], ['OMT'])
                        posb = POSR[:, d, :].unsqueeze(1).to_broadcast([128, 4, 128])
                        f2 = lambda t: t.rearrange("p a b -> p (a b)")
                        for q in range(4):
                            self.tt(RR[:], OMT[:, 4 * q:4 * q + 4].unsqueeze(2).to_broadcast([128, 4, 128]), posb, ALU.mult, ['OMT', 'POSR'], [('qt', 'R')])
                            self.tt(MG[:], RHO[:, 4 * q:4 * q + 4].unsqueeze(2).to_broadcast([128, 4, 128]), posb, ALU.mult, ['RHO', 'POSR'], [('qt', 'MAG')])
                            self.cis(f2(RR[:]), f2(MG[:]), f2(QR[:, 4 * q:4 * q + 4, :]), f2(QI[:, 4 * q:4 * q + 4, :]), f2(II[:]), f2(FF[:]), f2(GG[:]), f2(SS[:]), 'qt')
                        p.barrier()
                    with self.scope() as st_:
                        T = lambda name, shape, dt=F32: self.sb(st_, name, shape, dt)
                        STG = T("STG", [128, 64]); AT = T("AT", [64, 64]); DTB = T("DTB", [64, 32])
                        RHO = T("RHO", [64, 32]); OMT = T("OMT", [64, 32]); ABR = T("ABR", [64, 32]); ABI = T("ABI", [64, 32])
                        II = T("II", [64, 32]); FF = T("FF", [64, 32]); GG = T("GG", [64, 32]); SS = T("SS", [64, 32])
                        DEN = T("DEN", [64, 32]); ZR = T("ZR", [64, 32]); ZI = T("ZI", [64, 32]); TT1 = T("TT1", [64, 32]); TT2 = T("TT2", [64, 32])
                        BRE = T("BRE", [64, 32, 16]); BIM = T("BIM", [64, 32, 16]); BBR = T("BBR", [64, 32, 16]); BBI = T("BBI", [64, 32, 16]); TB3 = T("TB3", [64, 32, 16])
                        TRS = T("TRS", [128, 64]); CC = T("CC", [16, 2, 32, 64], BF16)
                        self.memset(STG[:], 0.0, ['STG'])
                        self.dmas('sp', [(STG[0:32, :], self.s5_a_re[0, d]),
                                     (STG[32:64, :], self.s5_a_im[0, d])], reads=['STG'], writes=['STG'])
                        self.dmas('sp', [(DTB[:], self.s5_log_dt[0, d].partition_broadcast(64)),
                                     (BRE[:], self.s5_b_re[0, d].rearrange("g n c -> n g c")),
                                     (BIM[:], self.s5_b_im[0, d].rearrange("g n c -> n g c"))],
                              writes=['DTB', 'BRE', 'BIM'])
                        self.dmas('pool', [(CC[:, 0], self.s5_c_re[0, d].rearrange("g c n -> c g n")),
                                       (CC[:, 1], self.s5_c_im[0, d].rearrange("g c n -> c g n"))], writes=['CC'])
                        self.tr(PS[0][0:64, 0:128], STG[:], self.IDF[:], ['STG', 'IDF'], [('ps', 0)])
                        self.cp(AT[:], PS[0][0:64, 0:64], [('ps', 0)], ['AT'])
                        self.act(DTB[:], DTB[:], AF.Exp, ['DTB'], ['DTB'])
                        self.tt(RHO[:], AT[:, 0:32], DTB[:], ALU.mult, ['AT', 'DTB'], [('zt', 'MAG')])
                        self.stt(OMT[:], AT[:, 32:64], INV2PI, DTB[:], ALU.mult, ALU.mult, ['AT', 'DTB'], [('zt', 'R')])
                        self.cis(OMT[:], RHO[:], ABR[:], ABI[:], II[:], FF[:], GG[:], SS[:], 'zt')
                        are, aim = AT[:, 0:32], AT[:, 32:64]
                        self.tt(DEN[:], are, are, ALU.mult, ['AT'], ['DEN'])
                        self.tt(TT1[:], aim, aim, ALU.mult, ['AT'], ['TT1'])
                        self.tt(DEN[:], DEN[:], TT1[:], ALU.add, ['DEN', 'TT1'], ['DEN'])
                        self.recip(DEN[:], DEN[:], ['DEN'], ['DEN'])
                        self.ts(ABR[:], ABR[:], -1.0, None, ALU.add, None, [('zt', 'ORE')], [('zt', 'ORE')])
                        self.tt(TT1[:], ABR[:], are, ALU.mult, [('zt', 'ORE'), 'AT'], ['TT1'])
                        self.tt(TT2[:], ABI[:], aim, ALU.mult, [('zt', 'OIM'), 'AT'], ['TT2'])
                        self.tt(TT1[:], TT1[:], TT2[:], ALU.add, ['TT1', 'TT2'], ['TT1'])
                        self.tt(ZR[:], TT1[:], DEN[:], ALU.mult, ['TT1', 'DEN'], ['ZR'])
                        self.tt(TT1[:], ABI[:], are, ALU.mult, [('zt', 'OIM'), 'AT'], ['TT1'])
                        self.tt(TT2[:], ABR[:], aim, ALU.mult, [('zt', 'ORE'), 'AT'], ['TT2'])
                        self.tt(TT1[:], TT1[:], TT2[:], ALU.subtract, ['TT1', 'TT2'], ['TT1'])
                        self.tt(ZI[:], TT1[:], DEN[:], ALU.mult, ['TT1', 'DEN'], ['ZI'])
                        zrb = ZR[:].unsqueeze(2).to_broadcast([64, 32, 16])
                        zib = ZI[:].unsqueeze(2).to_broadcast([64, 32, 16])
                        self.tt(BBR[:], BRE[:], zrb, ALU.mult, ['BRE', 'ZR'], ['BBR'])
                        self.tt(TB3[:], BIM[:], zib, ALU.mult, ['BIM', 'ZI'], ['TB3'])
                        self.tt(BBR[:], BBR[:], TB3[:], ALU.subtract, ['BBR', 'TB3'], ['BBR'])
                        self.tt(BBI[:], BIM[:], zrb, ALU.mult, ['BIM', 'ZR'], ['BBI'])
                        self.tt(TB3[:], BRE[:], zib, ALU.mult, ['BRE', 'ZI'], ['TB3'])
                        self.tt(BBI[:], BBI[:], TB3[:], ALU.add, ['BBI', 'TB3'], ['BBI'])
                        mk = MASK8[:].unsqueeze(2).to_broadcast([128, 8, 64])
                        for ri, (bb, bd) in enumerate([(BBR, BDR), (BBI, BDI)]):
                            for q in range(4):
                                bank = (ri * 4 + q) % 2
                                self.tr(PS[bank][:, 0:64], bb[:, q * 8:(q + 1) * 8, :].rearrange("p g c -> p (g c)"), self.IDF[0:64, 0:64],
                                        ['BBR', 'BBI', 'IDF'], [('ps', bank)])
                                self.cp(TRS[:], PS[bank][:, 0:64], [('ps', bank)], ['TRS'])
                                self.tt(bd[:, q, :].rearrange("p (g n) -> p g n", n=64), TRS[:].unsqueeze(1).to_broadcast([128, 8, 64]), mk, ALU.mult,
                                        ['TRS', 'MASK8'], [('BD', ri)])
                        self.memset(CPR[:], 0.0, [('CP', 0)], eng='pool')
                        self.memset(CPI[:], 0.0, [('CP', 1)], eng='pool')
                        for ri, cp_ in enumerate([CPR, CPI]):
                            for g2 in range(2):
                                bank = 2 + (ri * 2 + g2) % 2
                                for pr in range(16):
                                    g = 2 * pr + g2
                                    self.tr(PS[bank][:].bitcast(BF16)[g2 * 64:(g2 + 1) * 64, pr * 16:(pr + 1) * 16], CC[:, ri, g, :], self.IDB[0:16, 0:16], ['CC', 'IDB'], [('ps', bank)])
                                for pq in range(4):
                                    src = PS[bank][:].bitcast(BF16)[g2 * 64:(g2 + 1) * 64, 0:256].rearrange("p (q r c) -> p q r c", q=4, r=4)[:, :, pq, :]
                                    c0 = (2 * pq + g2) * 16
                                    dst = cp_[g2 * 64:(g2 + 1) * 64, :, c0:c0 + 16].rearrange("p (q r) c -> p q r c", r=4)[:, :, pq, :]
                                    self.act(dst, src, AF.Copy, [('ps', bank)], [('CP', ri)], scale=(1.0 if ri == 0 else -1.0))
                        p.barrier()
                    if self.stop == 's5tab' and d == 0:
                        self.tap("KR", KR[:], [128, 2048], BF16, []); self.tap("KI", KI[:], [128, 2048], BF16, [])
                        self.tap("QR", QR[:], [128, 16, 128], F32, []); self.tap("QI", QI[:], [128, 16, 128], F32, [])
                        self.tap("BDR", BDR[:], [128, 4, 512], BF16, []); self.tap("BDI", BDI[:], [128, 4, 512], BF16, [])
                        self.tap("CPR", CPR[:], [128, 16, 128], BF16, []); self.tap("CPI", CPI[:], [128, 16, 128], BF16, [])
                        return
                    BUR = [D("BUR%d" % i, [128, 512], BF16) for i in range(3)]; BUI = [D("BUI%d" % i, [128, 512], BF16) for i in range(3)]
                    P1 = [D("P1%d" % i, [128, 512], BF16) for i in range(4)]
                    XR = [D("XR%d" % i, [128, 512], BF16) for i in range(3)]; XI = [D("XI%d" % i, [128, 512], BF16) for i in range(3)]
                    HR = [D("HR%d" % i, [128, 4, 128], BF16) for i in range(2)]; HI = [D("HI%d" % i, [128, 4, 128], BF16) for i in range(2)]
                    P2 = [D("P2%d" % i, [128, 4, 128], BF16) for i in range(4)]
                    GFB = [D("GF%d" % i, [128, 2, 4, 128], BF16) for i in range(2)]
                    H0S = [[D("H0S%d%d" % (i, q), [128, 2, 4]) for q in range(4)] for i in range(2)]
                    GB = GFB
                    ZC = D("ZC", [128, 1])
                    self.memset(ZC[:], 0.0, ['ZC'])
                    tinc = 0 if d == 0 else 3
                    lastcol = 127 if d == 0 else 0
                    units = [(step, q) for step in range(NCH) for q in range(4)]

                    def stage_a(ui):
                        step, q = units[ui]
                        ch = order[d][step]
                        tsl = slice(ch * 128, (ch + 1) * 128)
                        s2 = ui % 3
                        ba, bb_ = (0, 1) if ui % 2 == 0 else (6, 7)
                        self.mm(PS[ba][:, :], UT[:, q, tsl], BDR[:, q, :], True, True, [('UT', ch), ('BD', 0)], [('ps', ba)])
                        self.mm(PS[bb_][:, :], UT[:, q, tsl], BDI[:, q, :], True, True, [('UT', ch), ('BD', 1)], [('ps', bb_)])
                        self.act(BUR[s2][:], PS[ba][:, :], AF.Copy, [('ps', ba)], [('BUR', s2)])
                        self.act(BUI[s2][:], PS[bb_][:, :], AF.Copy, [('ps', bb_)], [('BUI', s2)])
                        kr = KR[:, q * 512:(q + 1) * 512]
                        ki = KI[:, q * 512:(q + 1) * 512]
                        self.tt(P1[0][:], kr, BUR[s2][:], ALU.mult, ['KR', ('BUR', s2)], [('P1', 0)])
                        self.tt(P1[2][:], kr, BUI[s2][:], ALU.mult, ['KR', ('BUI', s2)], [('P1', 2)], eng='pool')
                        self.tt(P1[1][:], ki, BUI[s2][:], ALU.mult, ['KI', ('BUI', s2)], [('P1', 1)])
                        self.tt(XR[s2][:], P1[0][:], P1[1][:], ALU.subtract, [('P1', 0), ('P1', 1)], [('XR', s2)])
                        self.tt(P1[3][:], ki, BUR[s2][:], ALU.mult, ['KI', ('BUR', s2)], [('P1', 3)])
                        self.tt(XI[s2][:], P1[2][:], P1[3][:], ALU.add, [('P1', 2), ('P1', 3)], [('XI', s2)], eng='pool')

                    def stage_b(ui):
                        step, q = units[ui]
                        ch = order[d][step]
                        tsl = slice(ch * 128, (ch + 1) * 128)
                        s2 = ui % 2
                        s3 = ui % 3
                        par = step % 2
                        gf = GFB[s2]
                        h0p = H0S[1 - par][q]
                        for pq in range(4):
                            self.mm(PS[2][:, pq * 128:(pq + 1) * 128], XR[s3][:, pq * 128:(pq + 1) * 128], TRIB[:, tinc, :], True, True,
                                    [('XR', s3), 'TRIB'], [('ps', 2)])
                        for pq in range(4):
                            self.mm(PS[3][:, pq * 128:(pq + 1) * 128], XI[s3][:, pq * 128:(pq + 1) * 128], TRIB[:, tinc, :], True, True,
                                    [('XI', s3), 'TRIB'], [('ps', 3)])
                        for pq in range(4):
                            if step == 0:
                                br, bi = ZC[:, 0:1], ZC[:, 0:1]
                                rk = ['ZC']
                            else:
                                br, bi = h0p[:, 0, pq:pq + 1], h0p[:, 1, pq:pq + 1]
                                rk = [('H0S', 1 - par, q)]
                            self.act(HR[s2][:, pq, :], PS[2][:, pq * 128:(pq + 1) * 128], AF.Identity, [('ps', 2)] + rk, [('HR', s2)], bias=br, scale=1.0)
                            self.act(HI[s2][:, pq, :], PS[3][:, pq * 128:(pq + 1) * 128], AF.Identity, [('ps', 3)] + rk, [('HI', s2)], bias=bi, scale=1.0)
                        qr = QR[:, 4 * q:4 * q + 4, :]
                        qi = QI[:, 4 * q:4 * q + 4, :]
                        self.tt(P2[0][:], qr, HR[s2][:], ALU.mult, ['Q', ('HR', s2)], [('P2', 0)])
                        self.tt(P2[2][:], qr, HI[s2][:], ALU.mult, ['Q', ('HI', s2)], [('P2', 2)], eng='pool')
                        self.tt(P2[1][:], qi, HI[s2][:], ALU.mult, ['Q', ('HI', s2)], [('P2', 1)])
                        self.tt(P2[3][:], qi, HR[s2][:], ALU.mult, ['Q', ('HR', s2)], [('P2', 3)], eng='pool')
                        self.tt(gf[:, 0], P2[0][:], P2[1][:], ALU.subtract, [('P2', 0), ('P2', 1)], [('GF', s2, 0)])
                        self.tt(gf[:, 1], P2[2][:], P2[3][:], ALU.add, [('P2', 2), ('P2', 3)], [('GF', s2, 1)], eng='pool')
                        self.act(H0S[par][q][:], gf[:, :, :, lastcol], AF.Copy, [('GF', s2, 0), ('GF', s2, 1)], [('H0S', par, q)])
                        if ch >= 2:
                            yb_ = 4 + step % 2
                            for pq in range(4):
                                pr = 4 * q + pq
                                self.mm(PS[yb_][:, q * 128:(q + 1) * 128], CPR[:, pr, :], GB[s2][:, 0, pq, :], q == 0 and pq == 0, False,
                                        [('CP', 0), ('GF', s2, 0)], [('ps', yb_)], skip_group_check=True)
                                self.mm(PS[yb_][:, q * 128:(q + 1) * 128], CPI[:, pr, :], GB[s2][:, 1, pq, :], False, q == 3 and pq == 3,
                                        [('CP', 1), ('GF', s2, 1)], [('ps', yb_)], skip_group_check=True)
                            if q == 3:
                                yv = YT[:, :, tsl]
                                pv = PS[yb_][:, :].rearrange("p (q t) -> p q t", t=128)
                                if d == 0:
                                    self.cp(yv, pv, [('ps', yb_)], [('YT', ch)])
                                else:
                                    self.tt(yv, pv, yv, ALU.add, [('ps', yb_), ('YT', ch)], [('YT', ch)])

                    stage_a(0)
                    stage_a(1)
                    for ui in range(len(units)):
                        if ui + 2 < len(units):
                            stage_a(ui + 2)
                        stage_b(ui)
                    p.barrier()
            if self.stop == 's5':
                return
            with self.scope() as so:
                O = lambda name, shape, dt=F32: self.sb(so, name, shape, dt)
                GLW = O("GLW", [128, 4, 512], BF16); STG = O("STG", [128, 128]); PR1 = O("PR1", [128, 8])
                TT_ = [O("TTo%d" % i, [128, 512]) for i in range(2)]; T2_ = [O("T2o%d" % i, [128, 512]) for i in range(2)]
                GTt = O("GTt", [128, 4, 512], BF16); SGo = [O("SGo%d" % i, [128, 512], BF16) for i in range(2)]
                self.dmas('pool', [(GLW[:], self.s5_glu_w[0].rearrange("(k p) n -> p k n", p=128))], writes=['GLW'])
                self.memset(STG[:], 0.0, ['STG'])
                self.dmas('sp', [(STG[0:4, :], self.s5_d[0].rearrange("(k p) -> k p", p=128)),
                                 (STG[4:8, :], self.s5_glu_b[0].rearrange("(k p) -> k p", p=128))], reads=['STG'], writes=['STG'])
                self.tr(PS[0][:, 0:128], STG[:], self.IDF[:], ['STG', 'IDF'], [('ps', 0)])
                self.cp(PR1[:], PS[0][:, 0:8], [('ps', 0)], ['PR1'])
                for ti, (t0, n) in enumerate(TILES[1:]):
                    yk = tk('YT', t0, n)
                    for c in range(4):
                        s2 = c % 2
                        xg = TT_[s2]
                        self.stt(xg[:], UT[:, c, t0:t0 + n], PR1[:, c:c + 1], YT[:, c, t0:t0 + n], ALU.mult, ALU.add, tk('UT', t0, n) + yk + ['PR1'], [('TTo', s2)])
                        self.tt(T2_[s2][:], xg[:], xg[:], ALU.mult, [('TTo', s2)], [('T2o', s2)], eng='pool')
                        self.ts(T2_[s2][:], T2_[s2][:], 0.044715, 1.0, ALU.mult, ALU.add, [('T2o', s2)], [('T2o', s2)])
                        self.tt(T2_[s2][:], T2_[s2][:], xg[:], ALU.mult, [('T2o', s2), ('TTo', s2)], [('T2o', s2)], eng='pool')
                        self.act(T2_[s2][:], T2_[s2][:], AF.Sigmoid, [('T2o', s2)], [('T2o', s2)], scale=1.5957691)
                        self.tt(GTt[:, c, :], xg[:], T2_[s2][:], ALU.mult, [('TTo', s2), ('T2o', s2)], [('GTt', c)])
                    for c in range(4):
                        bank = 1 + c % 2
                        for k in range(4):
                            self.mm(PS[bank][:, :n], GLW[:, k, c * 128:(c + 1) * 128], GTt[:, k, :], k == 0, k == 3, ['GLW', ('GTt', k)], [('ps', bank)])
                        self.act(SGo[c % 2][:], PS[bank][:, :n], AF.Sigmoid, [('ps', bank), 'PR1'], [('SGo', c % 2)], bias=PR1[:, 4 + c:5 + c], scale=1.0)
                        self.tt(self.MIXT[:, c, t0:t0 + n], GTt[:, c, :], SGo[c % 2][:], ALU.mult, [('GTt', c), ('SGo', c % 2)], yk + tk('MIXT', t0, n))
                p.barrier()

    def ssd(self, b):
        p, PS, nc = self.p, self.PS, self.nc
        w_in = self.od_w_in[0].rearrange("(k p) n -> p k n", p=128)
        order = [list(range(NCH)), [1, 0] + list(range(NCH - 1, 1, -1))]
        with self.scope() as ph:
            P = lambda name, shape, dt=F32: self.sb(ph, name, shape, dt)
            XTOK = P("XTOK", [128, NCH, 768], BF16); YS = P("YS", [128, NCH, 512], BF16)
            DT = P("DTs", [128, NCH, 16]); DA = P("DAs", [128, NCH, 16]); LNDT = P("LNDT", [128, NCH, 16])
            GJ = P("GJs", [128, NCH, 2, 8]); BIASD = P("BIASD", [128, NCH, 2, 8]); WE = P("WEs", [128, NCH, 2, 8]); DEC = P("DECs", [128, NCH, 2, 8])
            PRM = P("PRMs", [128, 48]); DTB = P("DTBs", [128, 16]); AN = P("ANs", [128, 16]); DSK = P("DSK", [128, 8]); GN = P("GNs", [128, 512])
            ONEC = P("ONECs", [128, 1]); WDT = P("WDT", [128, 8, 16], BF16); TRIB = P("TRIBs", [128, 6, 128], BF16)
            markA = self.alo
            BCT = P("BCT", [128, 4, NT], BF16)
            self.memset(ONEC[:], 1.0, ['ONEC'])
            self.dmas('pool', [(WDT[:], w_in[:, :, 2048:2064]), (TRIB[:], self.k_tri.rearrange("a p n -> p a n"))], writes=['WDT', 'TRIB'])
            self.dmas('sp', [(DTB[:], self.ssd_dt_bias[0].rearrange("d h -> (d h)").partition_broadcast(128)),
                             (AN[:], self.ssd_a_log[0].rearrange("d h -> (d h)").partition_broadcast(128)),
                             (DSK[:], self.ssd_d[0].partition_broadcast(128)),
                             (GN[:], self.ssd_norm[0].partition_broadcast(128))], writes=['DTB', 'AN', 'DSK', 'GN'])
            self.act(AN[:], AN[:], AF.Exp, ['AN'], ['AN'])
            self.ts(AN[:], AN[:], -1.0, None, ALU.mult, None, ['AN'], ['AN'])
            with self.scope() as cs:
                C_ = lambda name, shape, dt=F32: self.sb(cs, name, shape, dt)
                STG = C_("STGc", [128, 128]); RAW = C_("RAW", [128, NT]); AC = C_("AC", [128, NT]); XFM = C_("XFM", [128, NT], BF16)
                WX = [C_("WX%d" % i, [128, 8, 128], BF16) for i in range(2)]
                self.memset(STG[:], 0.0, ['STG'])
                self.dmas('sp', [(STG[0:24, :], self.ssd_conv_w[0].rearrange("j (k p) -> (j k) p", p=128)),
                                 (STG[24:32, :], self.ssd_conv_b[0].rearrange("(k p) -> k p", p=128))], reads=['STG'], writes=['STG'])
                self.tr(PS[0][:, 0:128], STG[:], self.IDF[:], ['STG', 'IDF'], [('ps', 0)])
                self.cp(PRM[:, 0:32], PS[0][:, 0:32], [('ps', 0)], ['PRM'])
                for ch in range(NCH):
                    bank = 1 + ch % 2
                    hk = tk('HT', ch * 128, 128)
                    for k in range(8):
                        self.mm(PS[bank][:, 0:16], self.HT[:, k, ch * 128:(ch + 1) * 128], WDT[:, k, :], k == 0, k == 7, hk + ['WDT'], [('ps', bank)])
                    self.tt(DT[:, ch, :], PS[bank][:, 0:16], DTB[:], ALU.add, [('ps', bank), 'DTB'], [('DT', ch)])
                    self.act(DT[:, ch, :], DT[:, ch, :], AF.Exp, [('DT', ch)], [('DT', ch)])
                    self.act(DT[:, ch, :], DT[:, ch, :], AF.Ln, [('DT', ch)], [('DT', ch)], bias=ONEC[:, 0:1], scale=1.0)
                    self.act(LNDT[:, ch, :], DT[:, ch, :], AF.Ln, [('DT', ch)], [('LNDT', ch)])
                    self.tt(DA[:, ch, :], DT[:, ch, :], AN[:], ALU.mult, [('DT', ch), 'AN'], [('DA', ch)])
                    for d in range(2):
                        bk = 3 + d
                        rhs = DA[:, ch, d * 8:(d + 1) * 8]
                        self.mm(PS[bk][:, 0:8], self.TRI[:, 3 * d + 0, :], rhs, True, True, [('DA', ch), 'TRI'], [('ps', bk)])
                        self.mm(PS[bk][:, 8:16], self.TRI[:, 3 * d + 1, :], rhs, True, True, [('DA', ch), 'TRI'], [('ps', bk)])
                        self.mm(PS[bk][:, 16:24], self.ONESF[:], rhs, True, True, [('DA', ch), 'ONESF'], [('ps', bk)])
                        li = LNDT[:, ch, d * 8:(d + 1) * 8]
                        self.act(GJ[:, ch, d, :], PS[bk][:, 0:8], AF.Exp, [('ps', bk)], [('GJ', ch, d)])
                        self.tt(BIASD[:, ch, d, :], li, PS[bk][:, 0:8], ALU.subtract, [('ps', bk), ('LNDT', ch)], [('BIASD', ch, d)])
                        self.tt(WE[:, ch, d, :], li, PS[bk][:, 8:16], ALU.add, [('ps', bk), ('LNDT', ch)], [('WE', ch, d)])
                        self.act(WE[:, ch, d, :], WE[:, ch, d, :], AF.Exp, [('WE', ch, d)], [('WE', ch, d)])
                        self.act(DEC[:, ch, d, :], PS[bk][:, 16:24], AF.Exp, [('ps', bk)], [('DEC', ch, d)])
                for k8 in range(8):
                    wx = WX[k8 % 2]
                    self.dmas('pool', [(wx[:], w_in[:, :, 1024 + k8 * 128:1024 + (k8 + 1) * 128])], writes=[('WX', k8 % 2)])
                    for ti, (t0, n) in enumerate(TILES):
                        bank = 5 + ti % 3
                        hk = tk('HT', t0, n)
                        for k in range(8):
                            self.mm(PS[bank][:, :n], wx[:, k, :], self.HT[:, k, t0:t0 + n], k == 0, k == 7, hk + [('WX', k8 % 2)], [('ps', bank)])
                        self.act(RAW[:, t0:t0 + n], PS[bank][:, :n], AF.Copy, [('ps', bank)], ['RAW'])
                    w0, w1, w2, cb = PRM[:, k8:k8 + 1], PRM[:, 8 + k8:9 + k8], PRM[:, 16 + k8:17 + k8], PRM[:, 24 + k8:25 + k8]
                    self.act(AC[:], RAW[:], AF.Identity, ['RAW', 'PRM'], ['AC'], bias=cb, scale=w1)
                    for (a0, a1) in [(0, 256), (256, NT)]:
                        self.stt(AC[:, a0 + 1:a1], RAW[:, a0:a1 - 1], w0, AC[:, a0 + 1:a1], ALU.mult, ALU.add, ['RAW', 'AC', 'PRM'], ['AC'])
                        self.stt(AC[:, a0:a1 - 1], RAW[:, a0 + 1:a1], w2, AC[:, a0:a1 - 1], ALU.mult, ALU.add, ['RAW', 'AC', 'PRM'], ['AC'])
                    dstfm = XFM[:] if k8 < 4 else BCT[:, k8 - 4, :]
                    dkey = ['XFM'] if k8 < 4 else [('BCT', k8 - 4)]
                    self.act(dstfm, AC[:], AF.Silu, ['AC'], dkey)
                    if k8 < 6:
                        for c4 in range(0, NCH, 4):
                            nn = min(4, NCH - c4)
                            bank = (c4 // 4) % 2
                            pv = PS[bank][:].bitcast(BF16)
                            for i in range(nn):
                                ch = c4 + i
                                self.tr(pv[:, i * 128:(i + 1) * 128], dstfm[:, ch * 128:(ch + 1) * 128], self.IDB[:], dkey + ['IDB'], [('ps', bank)])
                            self.act(XTOK[:, c4:c4 + nn, k8 * 128:(k8 + 1) * 128], pv[:, 0:nn * 128].rearrange("p (a t) -> p a t", t=128), AF.Copy,
                                     [('ps', bank)], [('XTOK', c4 + i) for i in range(nn)])
                p.barrier()
            if self.stop == 'ssdprep':
                self.tap("XTOK", XTOK[:], [128, NCH, 768], BF16, []); self.tap("BCT", BCT[:], [128, 4, NT], BF16, [])
                self.tap("DTs", DT[:], [128, NCH, 16], F32, [])
                return
            STALL = P("STALL", [128, NCH, 2, 128], BF16)
            HS = [P("HSs%d" % d, [128, 8, 64]) for d in range(2)]; HSB = [P("HSB%d" % d, [128, 8, 64], BF16) for d in range(2)]
            LFB = [P("LFBs%d" % i, [128, 128]) for i in range(8)]; DTm = [P("DTm%d" % i, [128, 128], BF16) for i in range(8)]
            PT = [P("PTs%d" % i, [128, 128], BF16) for i in range(8)]
            XW = [P("XWs%d" % d, [128, 8, 64], BF16) for d in range(2)]; TBs = [P("TBs%d" % d, [128, 8, 64]) for d in range(2)]
            T3 = [P("T3s%d" % d, [128, 8, 64]) for d in range(2)]
            for ch in range(NCH):
                bank = ch % 2
                tsl = slice(ch * 128, (ch + 1) * 128)
                for g in range(2):
                    self.mm(PS[bank][:, g * 128:(g + 1) * 128], BCT[:, g, tsl], BCT[:, 2 + g, tsl], True, True, [('BCT', g), ('BCT', 2 + g)], [('ps', bank)])
                self.act(STALL[:, ch, :, :], PS[bank][:, 0:256].rearrange("p (g t) -> p g t", t=128), AF.Copy, [('ps', bank)], [('STALL', ch)])
            self.memset(YS[:], 0.0, [('YS', c) for c in range(NCH)])
            for d in range(2):
                self.memset(HS[d][:], 0.0, [('HS', d)])
                self.memset(HSB[d][:], 0.0, [('HSB', d)], eng='pool')
            for step in range(NCH):
                chs = [order[d][step] for d in range(2)]
                for rnd in range(2):
                    for d in range(2):
                        ch = chs[d]
                        if ch < 2:
                            continue
                        ya = 2 + d * 3
                        db = d
                        hs_ = list(range(4 * rnd, 4 * rnd + 4))
                        sl = lambda h: d * 4 + (h % 4)
                        for h in hs_:
                            self.act(LFB[sl(h)][:], self.ONESF[:], AF.Identity, [('DA', ch), 'ONESF'], [('LFB', sl(h))], scale=DA[:, ch, d * 8 + h:d * 8 + h + 1])
                        for h in hs_:
                            c0 = (h % 4) * 128
                            self.mm(PS[db][:, c0:c0 + 128], LFB[sl(h)][:], self.TRI[:, 3 * d + 0, :], True, False, [('LFB', sl(h)), 'TRI'], [('ps', db)], skip_group_check=True)
                            self.mm(PS[db][:, c0:c0 + 128], self.IDF[:], self.TRI[:, 3 * d + 2, :], False, True, ['IDF', 'TRI'], [('ps', db)], skip_group_check=True)
                        for h in hs_:
                            c0 = (h % 4) * 128
                            self.act(DTm[sl(h)][:], PS[db][:, c0:c0 + 128], AF.Exp, [('ps', db), ('BIASD', ch, d)], [('DTm', sl(h))], bias=BIASD[:, ch, d, h:h + 1], scale=1.0)
                        for h in hs_:
                            self.tt(PT[sl(h)][:], STALL[:, ch, h // 4, :], DTm[sl(h)][:], ALU.mult, [('STALL', ch), ('DTm', sl(h))], [('PT', sl(h))], eng='pool')
                        for h in hs_:
                            self.mm(PS[ya][:, h * 64:(h + 1) * 64], PT[sl(h)][:], XTOK[:, ch, h * 64:(h + 1) * 64], h == 0, h == 7, [('PT', sl(h)), ('XTOK', ch)], [('ps', ya)],
                                    skip_group_check=True)
                for d in range(2):
                    ch = chs[d]
                    tsl = slice(ch * 128, (ch + 1) * 128)
                    ya, yb, ub = 2 + d * 3, 3 + d * 3, 4 + d * 3
                    if ch >= 2:
                        for g in range(2):
                            self.mm(PS[yb][:, g * 256:(g + 1) * 256], BCT[:, 2 + g, tsl], HSB[d][:, 4 * g:4 * g + 4, :].rearrange("p a c -> p (a c)"), True, True,
                                    [('BCT', 2 + g), ('HSB', d)], [('ps', yb)])
                    self.tt(XW[d][:], XTOK[:, ch, 0:512].rearrange("p (h c) -> p h c", c=64), WE[:, ch, d, :].unsqueeze(2).to_broadcast([128, 8, 64]), ALU.mult,
                            [('XTOK', ch), ('WE', ch, d)], [('XW', d)])
                    for g in range(2):
                        self.mm(PS[ub][:, g * 256:(g + 1) * 256], XTOK[:, ch, 512 + g * 128:512 + (g + 1) * 128], XW[d][:, 4 * g:4 * g + 4, :].rearrange("p a c -> p (a c)"),
                                True, True, [('XTOK', ch), ('XW', d)], [('ps', ub)])
                    self.tt(T3[d][:], HS[d][:], DEC[:, ch, d, :].unsqueeze(2).to_broadcast([128, 8, 64]), ALU.mult, [('HS', d), ('DEC', ch, d)], [('T3', d)])
                    self.tt(HS[d][:], PS[ub][:, :].rearrange("p (h c) -> p h c", c=64), T3[d][:], ALU.add, [('ps', ub), ('T3', d)], [('HS', d)])
                    self.act(HSB[d][:], HS[d][:], AF.Copy, [('HS', d)], [('HSB', d)])
                    if ch >= 2:
                        self.tt(TBs[d][:], PS[yb][:, :].rearrange("p (h c) -> p h c", c=64), GJ[:, ch, d, :].unsqueeze(2).to_broadcast([128, 8, 64]), ALU.mult,
                                [('ps', yb), ('GJ', ch, d)], [('TBs', d)])
                        self.tt(TBs[d][:], PS[ya][:, :].rearrange("p (h c) -> p h c", c=64), TBs[d][:], ALU.add, [('ps', ya), ('TBs', d)], [('TBs', d)])
                        ysv = YS[:, ch, :].rearrange("p (h c) -> p h c", c=64)
                        self.tt(ysv, ysv, TBs[d][:], ALU.add, [('YS', ch), ('TBs', d)], [('YS', ch)], eng='pool')
            p.barrier()
            if self.stop == 'ssdraw':
                self.tap("YS", YS[:], [128, NCH, 512], BF16, [])
                return
            self.alo = markA
            WZ = P("WZ", [128, 8, 512], BF16)
            YF = [P("YF%d" % i, [128, 512]) for i in range(2)]; SZ = [P("SZ%d" % i, [128, 512]) for i in range(2)]
            JKs = P("JKs", [128, 512]); SSQ = P("SSQs", [128, 2]); OB = [P("OBs%d" % i, [128, 512], BF16) for i in range(2)]
            self.dmas('pool', [(WZ[:, i * 4:(i + 1) * 4, :], w_in[:, i * 4:(i + 1) * 4, 512:1024]) for i in range(2)], writes=['WZ'])
            for ch in range(2, NCH):
                s2 = ch % 2
                zb = ch % 2
                tb = 2 + ch % 2
                hk = tk('HT', ch * 128, 128)
                for k in range(8):
                    self.mm(PS[zb][:, :], self.HT[:, k, ch * 128:(ch + 1) * 128], WZ[:, k, :], k == 0, k == 7, hk + ['WZ'], [('ps', zb)])
                self.act(SZ[s2][:], PS[zb][:, :], AF.Silu, [('ps', zb)], [('SZ', s2)])
                yf = YF[s2]
                self.tt(yf[:].rearrange("p (h c) -> p h c", c=64), XTOK[:, ch, 0:512].rearrange("p (h c) -> p h c", c=64),
                        DSK[:].unsqueeze(2).to_broadcast([128, 8, 64]), ALU.mult, [('XTOK', ch), 'DSK'], [('YF', s2)])
                self.tt(yf[:], yf[:], YS[:, ch, :], ALU.add, [('YF', s2), ('YS', ch)], [('YF', s2)])
                self.tt(yf[:], yf[:], SZ[s2][:], ALU.mult, [('YF', s2), ('SZ', s2)], [('YF', s2)])
                self.act(JKs[:], yf[:], AF.Square, [('YF', s2)], ['JKs'])
                self.p.op('dve', lambda e: e.reduce_sum(out=SSQ[:, 0:1], in_=JKs[:], axis=AX.X), ['JKs'], [('SSQ', 0)])
                self.act(SSQ[:, 0:1], SSQ[:, 0:1], AF.Sqrt, [('SSQ', 0)], [('SSQ', 0)], bias=self.EPSC[:, 0:1], scale=1.0 / 512)
                self.recip(SSQ[:, 1:2], SSQ[:, 0:1], [('SSQ', 0)], [('SSQ', 1)])
                self.stt(OB[s2][:], yf[:], SSQ[:, 1:2], GN[:], ALU.mult, ALU.mult, [('YF', s2), ('SSQ', 1), 'GN'], [('OB', s2)])
                pv = PS[tb][:].bitcast(BF16)
                for j in range(4):
                    self.tr(pv[:, j * 128:(j + 1) * 128], OB[s2][:, j * 128:(j + 1) * 128], self.IDB[:], [('OB', s2), 'IDB'], [('ps', tb)])
                self.act(self.MIXT[:, 4:8, ch * 128:(ch + 1) * 128], pv[:, 0:512].rearrange("p (a t) -> p a t", t=128), AF.Copy, [('ps', tb)], [('MIXT', ch)])
            p.barrier()

def make_consts():
    tri = np.zeros((6, 128, 128), np.float32)
    i = np.arange(128)
    tri[0] = (i[:, None] <= i[None, :])
    tri[1] = (i[:, None] > i[None, :])
    tri[2] = np.where(i[None, :] >= i[:, None], 0.0, -30000.0)
    tri[3] = (i[:, None] >= i[None, :])
    tri[4] = (i[:, None] < i[None, :])
    tri[5] = np.where(i[None, :] <= i[:, None], 0.0, -30000.0)
    t = np.arange(2048)
    row = (t // 64).astype(np.float32)
    col = (t % 64).astype(np.float32)
    inv = (1.0 / (np.float32(10000.0) ** (np.arange(16, dtype=np.float32) / np.float32(16)))).astype(np.float32)
    ar = row[:, None] * inv
    ac = col[:, None] * inv
    cosr, sinr, cosc, sinc = np.cos(ar), np.sin(ar), np.cos(ac), np.sin(ac)
    rope = np.zeros((2, 2048, 64), np.float32)
    rope[0] = np.concatenate([cosr, cosr, cosc, cosc], -1)
    rope[1] = np.concatenate([-sinr, sinr, -sinc, sinc], -1)
    sel = np.zeros((16, 16, 128), np.float32)
    for e in range(16):
        sel[e, e, :] = 1.0
    mask8 = (np.arange(128)[:, None] // 16 == np.arange(8)[None, :]).astype(np.float32)
    pidx = np.arange(128, dtype=np.float32)
    posc = np.stack([pidx + 1, 128 - pidx, -(pidx + 1), -(128 - pidx)], 1).astype(np.float32)
    posr = np.zeros((128, 2, 128), np.float32)
    posr[:, 0, :] = (pidx + 1)[None, :]
    posr[:, 1, :] = (128 - pidx)[None, :]
    return {"k_ident": np.eye(128, dtype=np.float32), "k_tri": tri, "k_rope": rope, "k_sel": sel,
            "k_mask8": mask8, "k_posc": posc, "k_posr": posr}


def make_in_maps(inputs, nb=NB, ncores=NCORES):
    consts = make_consts()
    maps = []
    for ci in range(ncores):
        m = {}
        for k, v in inputs.items():
            v = np.asarray(v)
            if k in ("x", "c", "ctx"):
                m[k] = np.ascontiguousarray(v[ci * nb:(ci + 1) * nb])
            else:
                m[k] = np.ascontiguousarray(v)
        m.update(consts)
        maps.append(m)
    return maps


def kernel(**inputs):
    bld = Builder()
    nc = bld.build()
    maps = make_in_maps(inputs)
    res = run_bass_kernel_spmd(nc, maps, core_ids=list(range(NCORES)))
    return np.concatenate([r["out"] for r in res.results], axis=0).astype(np.float32)
```

```python
import math
import os
import numpy as np
import concourse.bass as bass
import concourse.mybir as mybir
from concourse.bass_utils import run_bass_kernel_spmd
from contextlib import ExitStack

F32 = mybir.dt.float32
BF16 = mybir.dt.bfloat16
I32 = mybir.dt.int32
AF = mybir.ActivationFunctionType
ALU = mybir.AluOpType
AX = mybir.AxisListType

ENGS = ['pe', 'act', 'dve', 'pool', 'sp']
NDMASEM = 40
NCORES = 8
NB = 2
NT = 2304
NCH = 18
EPS = 1e-6
TILES = [(0, 256), (256, 512), (768, 512), (1280, 512), (1792, 512)]


class _Op:
    __slots__ = ('eng', 'fn', 'waits', 'dwaits', 'idx', 'signal', 'isdma', 'dsem', 'dval', 'know')


class Prog:
    def __init__(self, nc):
        self.nc = nc
        self.ops = {e: [] for e in ENGS}
        self.lastw = {}
        self.readers = {}
        self.know = {e: {f: -1 for f in ENGS} for e in ENGS}
        self.dknow = {e: {} for e in ENGS}
        self.dsem_last = [None] * NDMASEM
        self.dsem_val = [0] * NDMASEM
        self.dsem_rr = 0
        self.dsem_rr_sw = 0

    def _dep_tokens(self, reads, writes):
        toks = []
        for r in reads:
            t = self.lastw.get(r)
            if t is not None:
                toks.append(t)
        for w in writes:
            t = self.lastw.get(w)
            if t is not None:
                toks.append(t)
            toks.extend(self.readers.get(w, ()))
        return toks

    def _record(self, op, reads, writes):
        for r in reads:
            self.readers.setdefault(r, []).append(op)
        for w in writes:
            self.lastw[w] = op
            self.readers[w] = []

    def _resolve(self, eng, toks, op, same_ok=False):
        need = {}
        dneed = {}
        for t in toks:
            if t.isdma:
                if self.dknow[eng].get(t.dsem, 0) >= t.dval:
                    continue
                dneed[t.dsem] = max(dneed.get(t.dsem, 0), t.dval)
            else:
                if t.eng == eng and same_ok:
                    continue
                if self.know[eng][t.eng] >= t.idx:
                    continue
                need[t.eng] = max(need.get(t.eng, -1), t.idx)
        for f, j in need.items():
            src = self.ops[f][j]
            src.signal = True
            kn = src.know
            for g in ENGS:
                if kn[g] > self.know[eng][g]:
                    self.know[eng][g] = kn[g]
            if j > self.know[eng][f]:
                self.know[eng][f] = j
        for s, v in dneed.items():
            self.dknow[eng][s] = v
        op.waits = list(need.items())
        op.dwaits = list(dneed.items())

    def op(self, eng, fn, reads=(), writes=(), same_ok=False):
        if eng != 'pe':
            psr = [r for r in reads if isinstance(r, tuple) and r[0] == 'ps']
            if psr:
                writes = list(writes) + psr
        o = _Op()
        o.eng = eng
        o.fn = fn
        o.isdma = False
        o.signal = False
        o.idx = len(self.ops[eng])
        toks = self._dep_tokens(reads, writes)
        self._resolve(eng, toks, o, same_ok=same_ok)
        kn = dict(self.know[eng])
        kn[eng] = o.idx
        o.know = kn
        self.ops[eng].append(o)
        self._record(o, reads, writes)
        return o

    def dma(self, q, fns, reads=(), writes=()):
        if not isinstance(fns, (list, tuple)):
            fns = [fns]
        o = _Op()
        o.eng = q
        o.fn = list(fns)
        o.isdma = True
        o.signal = False
        o.idx = len(self.ops[q])
        half = NDMASEM // 2
        if q == 'pool':
            s = half + self.dsem_rr_sw
            self.dsem_rr_sw = (self.dsem_rr_sw + 1) % (NDMASEM - half)
        else:
            s = self.dsem_rr
            self.dsem_rr = (self.dsem_rr + 1) % half
        toks = self._dep_tokens(reads, writes)
        if self.dsem_last[s] is not None:
            toks.append(self.dsem_last[s])
        self._resolve(q, toks, o)
        o.dsem = s
        self.dsem_val[s] += 16 * len(fns)
        o.dval = self.dsem_val[s]
        self.dsem_last[s] = o
        o.know = dict(self.know[q])
        self.ops[q].append(o)
        self._record(o, reads, writes)
        return o

    def _waitop(self, eng, toks):
        o = _Op()
        o.eng = eng
        o.fn = None
        o.isdma = False
        o.signal = False
        o.idx = len(self.ops[eng])
        self._resolve(eng, toks, o)
        kn = dict(self.know[eng])
        kn[eng] = o.idx
        o.know = kn
        self.ops[eng].append(o)
        return o

    def barrier(self):
        last = {e: (self.ops[e][-1] if self.ops[e] else None) for e in ENGS}
        dtoks = [t for t in self.dsem_last if t is not None]
        for e in ENGS:
            toks = list(dtoks)
            for f in ENGS:
                t = last[f]
                if t is None:
                    continue
                if t.isdma or t.fn is None:
                    j = len(self.ops[f]) - 1
                    while j >= 0 and (self.ops[f][j].isdma or self.ops[f][j].fn is None):
                        j -= 1
                    if j < 0:
                        continue
                    t = self.ops[f][j]
                toks.append(t)
            self._waitop(e, toks)
        self.lastw = {}
        self.readers = {}

    def wait_all_dma(self, eng='sp'):
        toks = [t for t in self.dsem_last if t is not None]
        return self._waitop(eng, toks)

    def emit(self, stack):
        nc = self.nc
        esem = {e: stack.enter_context(nc.semaphore("s_" + e)) for e in ENGS}
        dsem = [stack.enter_context(nc.semaphore("d%d" % i)) for i in range(NDMASEM)]
        semval = {}
        for e in ENGS:
            c = 0
            vals = []
            for o in self.ops[e]:
                if o.signal and not o.isdma and o.fn is not None:
                    c += 1
                vals.append(c)
            semval[e] = vals
        engobj = {'pe': 'tensor', 'act': 'scalar', 'dve': 'vector', 'pool': 'gpsimd', 'sp': 'sync'}
        self.stats = {e: (len(self.ops[e]), sum(len(o.waits) + len(o.dwaits) for o in self.ops[e]),
                          semval[e][-1] if semval[e] else 0) for e in ENGS}
        block = stack.enter_context(nc.Block())

        def body_for(e):
            def body(engine):
                for o in self.ops[e]:
                    for f, j in o.waits:
                        engine.wait_ge(esem[f], semval[f][j])
                    for s, v in o.dwaits:
                        engine.wait_ge(dsem[s], v)
                    if o.fn is None:
                        continue
                    if o.isdma:
                        for fn in o.fn:
                            fn(engine).then_inc(dsem[o.dsem], 16)
                    else:
                        ins = o.fn(engine)
                        if o.signal:
                            ins.then_inc(esem[e], 1)
            return body

        for e in ENGS:
            if self.ops[e]:
                getattr(block, engobj[e])(body_for(e))


class ArenaScope:
    def __init__(self, bld):
        self.bld = bld

    def __enter__(self):
        self.mark = self.bld.alo
        return self

    def __exit__(self, *a):
        self.bld.alo = self.mark
        return False


def tk(name, t0, n):
    return [(name, c) for c in range(t0 // 128, (t0 + n + 127) // 128)]


class Builder:
    def __init__(self, taps=(), nb=NB, stop=None):
        self.taps = set(taps)
        self.nb = nb
        self.stop = stop
        self.tap_specs = {}
        self.nc = bass.Bass("TRN2", target_bir_lowering=False)
        self.p = Prog(self.nc)
        self.dram = {}

    def din(self, name, shape, dt=F32):
        t = self.nc.dram_tensor(name, list(shape), dt, kind="ExternalInput").ap()
        self.dram[name] = t
        return t

    def dout(self, name, shape, dt=F32):
        t = self.nc.dram_tensor(name, list(shape), dt, kind="ExternalOutput").ap()
        self.dram[name] = t
        return t

    def tap(self, name, src_ap, shape, dt, reads):
        if name not in self.taps:
            return
        d = self.dout("tap_" + name, shape, dt)
        self.tap_specs[name] = (shape, dt)
        self.p.dma('sp', lambda e: e.dma_start(out=d, in_=src_ap), reads=reads)

    def dmas(self, q, pairs, reads=(), writes=(), slow=False):
        kw = {'allow_slow_non_contiguous': True} if slow else {}
        fns = [(lambda e, o=o, i=i: e.dma_start(out=o, in_=i, **kw)) for (o, i) in pairs]
        self.p.dma(q, fns, reads=reads, writes=writes)

    def mm(self, out, lhsT, rhs, start, stop, reads, writes, **kw):
        self.p.op('pe', lambda e: e.matmul(out, lhsT=lhsT, rhs=rhs, start=start, stop=stop, **kw),
                  reads, writes, same_ok=True)

    def tr(self, out, in_, ident, reads, writes):
        self.p.op('pe', lambda e: e.transpose(out, in_, ident), reads, writes, same_ok=True)

    def act(self, out, in_, func, reads, writes, bias=None, scale=None, accum_out=None, eng='act'):
        kw = {}
        if bias is not None:
            kw['bias'] = bias
        if scale is not None:
            kw['scale'] = scale
        if accum_out is not None:
            kw['accum_out'] = accum_out
        self.p.op('act', lambda e: e.activation(out=out, in_=in_, func=func, **kw), reads, writes)

    def tt(self, out, in0, in1, op, reads, writes, eng='dve'):
        self.p.op(eng, lambda e: e.tensor_tensor(out=out, in0=in0, in1=in1, op=op), reads, writes)

    def ts(self, out, in0, s1, s2, op0, op1, reads, writes, eng='dve'):
        if op1 is None:
            self.p.op(eng, lambda e: e.tensor_scalar(out=out, in0=in0, scalar1=s1, scalar2=None, op0=op0), reads, writes)
        else:
            self.p.op(eng, lambda e: e.tensor_scalar(out=out, in0=in0, scalar1=s1, scalar2=s2, op0=op0, op1=op1), reads, writes)

    def stt(self, out, in0, scalar, in1, op0, op1, reads, writes):
        self.p.op('dve', lambda e: e.scalar_tensor_tensor(out=out, in0=in0, scalar=scalar, in1=in1, op0=op0, op1=op1),
                  reads, writes)

    def cp(self, out, in_, reads, writes, eng='dve'):
        self.p.op(eng, lambda e: e.tensor_copy(out=out, in_=in_), reads, writes)

    def recip(self, out, in_, reads, writes):
        self.p.op('dve', lambda e: e.reciprocal(out=out, in_=in_), reads, writes)

    def memset(self, ap, val, writes, eng='dve'):
        self.p.op(eng, lambda e: e.memset(ap, val), (), writes)

    def sb(self, st, name, shape, dt):
        if isinstance(st, ArenaScope):
            return self.carve(shape, dt)
        self._uid = getattr(self, '_uid', 0) + 1
        return st.enter_context(self.nc.sbuf_tensor("%s_%d" % (name, self._uid), list(shape), dt))

    def carve(self, shape, dt, high=False):
        nelem = 1
        for d in shape[1:]:
            nelem *= d
        n32 = nelem if dt != BF16 else (nelem + 1) // 2
        n32 = (n32 + 7) // 8 * 8
        if high:
            self.ahi -= n32
            off = self.ahi
        else:
            off = self.alo
            self.alo += n32
        assert self.alo <= self.ahi, "arena overflow: lo=%d hi=%d" % (self.alo, self.ahi)
        ap = self.ARENA[0:shape[0], off:off + n32]
        if dt == BF16:
            ap = ap.bitcast(BF16)
        ap = ap[:, 0:nelem]
        if len(shape) == 3:
            ap = ap.rearrange("p (a b) -> p a b", b=shape[2])
        elif len(shape) == 4:
            ap = ap.rearrange("p (a b c) -> p a b c", b=shape[2], c=shape[3])
        elif len(shape) == 5:
            ap = ap.rearrange("p (a b c d) -> p a b c d", b=shape[2], c=shape[3], d=shape[4])
        return ap

    def scope(self):
        return ArenaScope(self)

    def build(self):
        nc, p = self.nc, self.p
        D = self.din
        nb = self.nb
        x = D("x", [nb, 2048, 1024]); c = D("c", [nb, 1024]); ctx = D("ctx", [nb, 256, 1024]); c_ctx = D("c_ctx", [1024])
        ada_w = D("ada_w", [2, 1024, 6144]); ada_b = D("ada_b", [2, 6144])
        norm_mix = D("norm_mix", [2, 1024]); norm_ffn = D("norm_ffn", [2, 1024])
        self.ev_w_in = D("ev_w_in", [1, 1024, 2832]); self.ev_w_out = D("ev_w_out", [1, 1024, 1024])
        self.ml_gate_b = D("ml_gate_b", [1, 16]); self.ml_norm = D("ml_norm", [1, 512])
        self.at_q_norm = D("at_q_norm", [1, 64]); self.at_k_norm = D("at_k_norm", [1, 64])
        self.od_w_in = D("od_w_in", [1, 1024, 2064]); self.od_w_out = D("od_w_out", [1, 1024, 1024])
        for nm, shp in [("s5_a_re", [1, 2, 32, 64]), ("s5_a_im", [1, 2, 32, 64]), ("s5_log_dt", [1, 2, 32]),
                        ("s5_b_re", [1, 2, 32, 64, 16]), ("s5_b_im", [1, 2, 32, 64, 16]),
                        ("s5_c_re", [1, 2, 32, 16, 64]), ("s5_c_im", [1, 2, 32, 16, 64]),
                        ("s5_d", [1, 512]), ("s5_glu_w", [1, 512, 512]), ("s5_glu_b", [1, 512]),
                        ("ssd_conv_w", [1, 3, 1024]), ("ssd_conv_b", [1, 1024]), ("ssd_dt_bias", [1, 2, 8]),
                        ("ssd_a_log", [1, 2, 8]), ("ssd_d", [1, 8]), ("ssd_norm", [1, 512]),
                        ("moe_gr_w", [2, 1024, 4]), ("moe_gr_b", [2, 4]), ("moe_er_w", [2, 1024, 16]),
                        ("moe_er_b", [2, 16]), ("moe_w_gate", [2, 16, 1024, 512]), ("moe_w_up", [2, 16, 1024, 512]),
                        ("moe_w_down", [2, 16, 512, 1024])]:
            setattr(self, nm, D(nm, shp))
        k_ident = D("k_ident", [128, 128]); k_tri = D("k_tri", [6, 128, 128])
        k_rope = D("k_rope", [2, 2048, 64]); k_sel = D("k_sel", [16, 16, 128])
        self.k_tri = k_tri
        self.k_mask8 = D("k_mask8", [128, 8]); self.k_posc = D("k_posc", [128, 4]); self.k_posr = D("k_posr", [128, 2, 128])
        out = self.dout("out", [nb, 2048, 1024])

        with ExitStack() as st:
            S = lambda name, shape, dt=F32: self.sb(st, name, shape, dt)
            self.IDF = S("IDF", [128, 128]); self.IDB = S("IDB", [128, 128], BF16)
            self.ONESB = S("ONESB", [128, 128], BF16); self.ONESF = S("ONESF", [128, 128])
            self.TRI = S("TRI", [128, 6, 128])
            self.MOD = S("MOD", [128, 2, 48, 3])
            self.GSC = S("GSC", [128, 2, 2, 8, 3])
            self.EPSC = S("EPSC", [128, 1])
            self.memset(self.EPSC[:], EPS, ['EPSC'])
            ARENA_WORDS = 46080
            self.ARENA = S("ARENA", [128, ARENA_WORDS])
            self.alo, self.ahi = 0, ARENA_WORDS
            self.XS = [nc.dram_tensor("xscr%d" % i, [128, 8, NT], F32, kind="Internal").ap() for i in range(nb)]
            self.HT = self.carve([128, 8, NT], BF16)
            self.PS = [st.enter_context(nc.psum_tensor("ps%d" % i, [128, 512], F32)) for i in range(8)]
            PS = self.PS
            p.dma('sp', lambda e: e.dma_start(out=self.IDF[:], in_=k_ident), writes=['IDF'])
            p.dma('pool', lambda e: e.dma_start(out=self.IDB[:], in_=k_ident), writes=['IDB'])
            p.dma('sp', lambda e: e.dma_start(out=self.TRI[:], in_=k_tri.rearrange("a p n -> p a n")), writes=['TRI'])
            self.memset(self.ONESB[:], 1.0, ['ONESB'])
            self.memset(self.ONESF[:], 1.0, ['ONESF'])
            self.k_rope = k_rope; self.k_sel = k_sel

            with self.scope() as ph:
                P = lambda name, shape, dt=F32: self.sb(ph, name, shape, dt)
                STG = P("STG", [128, 128]); FM = P("FM", [128, 128])
                AW = [P("AW%d" % i, [128, 8, 512]) for i in range(2)]
                SC = P("SC", [128, 8, 3])
                self.memset(STG[:], 0.0, ['STG'])
                r_c = 32
                r_cc = 32 + 8 * nb
                p.dma('sp', [lambda e: e.dma_start(out=STG[0:16, :], in_=norm_mix.rearrange("l (k p) -> (l k) p", p=128)),
                             lambda e: e.dma_start(out=STG[16:32, :], in_=norm_ffn.rearrange("l (k p) -> (l k) p", p=128)),
                             lambda e: e.dma_start(out=STG[r_c:r_c + 8 * nb, :], in_=c.rearrange("b (k p) -> (b k) p", p=128)),
                             lambda e: e.dma_start(out=STG[r_cc:r_cc + 8, :], in_=c_ctx.rearrange("(k p) -> k p", p=128))],
                      reads=['STG'], writes=['STG'])
                self.tr(PS[0][:, 0:128], STG[:], self.IDF[:], ['STG', 'IDF'], [('ps', 0)])
                self.cp(FM[:], PS[0][:, 0:128], [('ps', 0)], ['FM'])
                for b in range(nb):
                    self.act(SC[:, :, b], FM[:, r_c + 8 * b:r_c + 8 * b + 8], AF.Silu, ['FM'], [('SC', b)])
                self.act(SC[:, :, 2], FM[:, r_cc:r_cc + 8], AF.Silu, ['FM'], [('SC', 2)])
                if nb == 1:
                    self.cp(SC[:, :, 1], SC[:, :, 0], [('SC', 0)], [('SC', 1)])
                STG2 = P("STG2", [128, 128]); ADAB = P("ADAB", [128, 96])
                self.memset(STG2[:], 0.0, ['STG2'])
                p.dma('sp', lambda e: e.dma_start(out=STG2[0:96, :], in_=ada_b.rearrange("l (j p) -> (l j) p", p=128)),
                      reads=['STG2'], writes=['STG2'])
                self.tr(PS[1][:, 0:128], STG2[:], self.IDF[:], ['STG2', 'IDF'], [('ps', 1)])
                self.cp(ADAB[:], PS[1][:, 0:96], [('ps', 1)], ['ADAB'])
                screads = [('SC', 0), ('SC', 1), ('SC', 2)]
                for l in range(2):
                    for pc in range(12):
                        slot = pc % 2
                        aw = AW[slot]
                        src = ada_w[l].rearrange("(k p) n -> p k n", p=128)
                        p.dma('sp' if slot == 0 else 'act',
                              [lambda e, src=src, aw=aw, pc=pc, h=h: e.dma_start(out=aw[:, h * 4:(h + 1) * 4, :], in_=src[:, h * 4:(h + 1) * 4, pc * 512:(pc + 1) * 512])
                               for h in range(2)], writes=[('AW', slot)])
                        for j in range(4):
                            col = (pc * 4 + j) * 3
                            for k in range(8):
                                self.mm(PS[2 + l][:, col:col + 3], aw[:, k, j * 128:(j + 1) * 128], SC[:, k, :],
                                        k == 0, k == 7, [('AW', slot)] + screads, [('ps', 2 + l)])
                    self.tt(self.MOD[:, l], PS[2 + l][:, 0:144].rearrange("p (j t) -> p j t", t=3),
                            ADAB[:, l * 48:(l + 1) * 48].unsqueeze(2).to_broadcast([128, 48, 3]), ALU.add,
                            [('ps', 2 + l), 'ADAB'], [('MOD', l)])
                    for w in range(2):
                        mi = 1 if w == 0 else 4
                        g_ap = FM[:, 16 * w + 8 * l:16 * w + 8 * l + 8]
                        gb = g_ap.unsqueeze(2).to_broadcast([128, 8, 3])
                        self.tt(self.GSC[:, l, w], self.MOD[:, l, mi * 8:(mi + 1) * 8, :], gb, ALU.mult,
                                [('MOD', l), 'FM'], [('GSC', l, w)])
                        self.tt(self.GSC[:, l, w], self.GSC[:, l, w], gb, ALU.add,
                                [('GSC', l, w), 'FM'], [('GSC', l, w)])
                self.tap("mod", self.MOD[:], [128, 2, 48, 3], F32, [('MOD', 0), ('MOD', 1)])
                p.barrier()
            if self.stop == 'mod':
                return self.finish(st)

            for b in range(nb):
                self.run_batch(st, b, x, ctx, out)
                if self.stop is not None:
                    break
            return self.finish(st)

    def finish(self, st):
        self.p.wait_all_dma('sp')
        self.p.emit(st)
        return self.nc

    def load_x(self, b, x, ctx):
        p, PS = self.p, self.PS
        with self.scope() as ph:
            XT = [self.sb(ph, "XT%d" % i, [128, 1024], F32) for i in range(3)]
            for ch in range(NCH):
                s = ch % 3
                src = ctx[b, ch * 128:(ch + 1) * 128, :] if ch < 2 else x[b, (ch - 2) * 128:(ch - 1) * 128, :]
                p.dma('sp' if ch % 2 == 0 else 'act', lambda e, s=s, src=src: e.dma_start(out=XT[s][:], in_=src), writes=[('XT', s)])
                for hf in range(2):
                    bank = (ch * 2 + hf) % 4
                    for kk in range(4):
                        k = hf * 4 + kk
                        self.tr(PS[bank][:, kk * 128:(kk + 1) * 128], XT[s][:, k * 128:(k + 1) * 128], self.IDF[:],
                                [('XT', s), 'IDF'], [('ps', bank)])
                    dst = self.X[:, hf * 4:(hf + 1) * 4, ch * 128:(ch + 1) * 128]
                    srcp = PS[bank][:].rearrange("p (k t) -> p k t", t=128)
                    if hf == 0:
                        self.act(dst, srcp, AF.Copy, [('ps', bank)], [('X', ch)])
                    else:
                        self.cp(dst, srcp, [('ps', bank)], [('X', ch)])
            p.barrier()

    def norm(self, b, l, w, f32_cb=None, skip_ctx=False):
        p, PS = self.p, self.PS
        mi = 0 if w == 0 else 3
        with self.scope() as ph:
            SQ = [self.sb(ph, "SQ%d" % i, [128, 512], BF16) for i in range(3)]
            SD = self.sb(ph, "SD", [128, 512], F32)
            RS = self.sb(ph, "RS", [128, 512], F32)
            TM = [self.sb(ph, "TM%d" % i, [128, 512], F32) for i in range(2)]
            H32 = self.sb(ph, "H32", [128, 8, 512], F32) if f32_cb is not None else None
            cnt = 0
            for ti, (t0, n) in enumerate(TILES):
                if skip_ctx and ti == 0:
                    continue
                j = 2 if ti == 0 else b
                xk = tk('X', t0, n)
                bank = ti % 2
                for k in range(8):
                    s = cnt % 3
                    cnt += 1
                    self.act(SQ[s][:, :n], self.X[:, k, t0:t0 + n], AF.Square, xk, [('SQ', s)])
                    self.mm(PS[bank][:, :n], self.ONESB[:], SQ[s][:, :n], k == 0, k == 7, [('SQ', s), 'ONESB'], [('ps', bank)])
                self.act(SD[:, :n], PS[bank][:, :n], AF.Sqrt, [('ps', bank)], ['SD'], bias=self.EPSC[:, 0:1], scale=1.0 / 1024)
                self.recip(RS[:, :n], SD[:, :n], ['SD'], ['RS'])
                for k in range(8):
                    s = k % 2
                    self.stt(TM[s][:, :n], self.X[:, k, t0:t0 + n], self.GSC[:, l, w, k, j:j + 1], RS[:, :n], ALU.mult, ALU.mult,
                             xk + ['RS', ('GSC', l, w)], [('TM', s)])
                    if f32_cb is not None:
                        self.act(H32[:, k, :n], TM[s][:, :n], AF.Identity, [('TM', s)], [('H32', k)],
                                 bias=self.MOD[:, l, mi * 8 + k, j:j + 1], scale=1.0)
                        self.cp(self.HT[:, k, t0:t0 + n], H32[:, k, :n], [('H32', k)], tk('HT', t0, n), eng='pool')
                    else:
                        self.act(self.HT[:, k, t0:t0 + n], TM[s][:, :n], AF.Identity, [('TM', s)], tk('HT', t0, n),
                                 bias=self.MOD[:, l, mi * 8 + k, j:j + 1], scale=1.0)
                if f32_cb is not None:
                    f32_cb(ti, t0, n, H32)
            p.barrier()

    def x_alloc(self):
        self._ahi_mark = self.ahi
        self.X = self.carve([128, 8, NT], F32, high=True)

    def x_free(self):
        self.ahi = self._ahi_mark

    def x_spill(self, b):
        p = self.p
        p.dma('sp', [lambda e, k=k: e.dma_start(out=self.XS[b][:, 2 * k:2 * k + 2, :], in_=self.X[:, 2 * k:2 * k + 2, :]) for k in range(4)],
              reads=tk('X', 0, NT), writes=['XS'])
        p.barrier()

    def x_reload(self, b):
        p = self.p
        p.dma('sp', [lambda e, k=k: e.dma_start(out=self.X[:, 2 * k:2 * k + 2, :], in_=self.XS[b][:, 2 * k:2 * k + 2, :]) for k in range(4)],
              reads=['XS'], writes=tk('X', 0, NT))

    def run_batch(self, st, b, x, ctx, out):
        p = self.p
        self.x_alloc()
        self.load_x(b, x, ctx)
        if self.stop == 'loadx':
            self.tap("X", self.X[:], [128, 8, NT], F32, tk('X', 0, NT))
            return
        skipl0 = bool(os.environ.get('SKIPL0'))
        if not skipl0:
            self.norm(b, 0, 0)
        if self.stop == 'h0':
            self.tap("h0", self.HT[:], [128, 8, NT], BF16, tk('HT', 0, NT))
            return
        if not skipl0:
            self.layer0(b)
            if self.stop is not None and self.stop in ('mlstm', 'attn', 'xmid0', 'xout0', 'mlgate', 'mlproj', 'mlloop'):
                return
        self.layer1(b, out)

    def layer0(self, b):
        p = self.p
        self.x_spill(b)
        self.x_free()
        with self.scope() as lay:
            self.MIXT = self.carve([128, 8, NT], BF16)
            if 'ml' not in os.environ.get('SKIPMIX', ''):
                self.mlstm(b)
            if self.stop == 'mlstm':
                self.tap("mix0", self.MIXT[:], [128, 8, NT], BF16, [])
                return
            self.attention(b)
            if self.stop == 'attn':
                self.tap("mix0", self.MIXT[:], [128, 8, NT], BF16, [])
                return
            self.tap("mix0", self.MIXT[:], [128, 8, NT], BF16, [])
            self.x_alloc()
            self.x_reload(b)
            self.outproj(b, 0, self.ev_w_out[0])
            self.tap("xmid0", self.X[:], [128, 8, NT], F32, [])
        if self.stop == 'xmid0':
            self.tap("X", self.X[:], [128, 8, NT], F32, tk('X', 0, NT))
            return
        if 'moe0' not in os.environ.get('SKIPMIX', ''):
            self.moe(b, 0)
        if self.stop == 'xout0':
            self.tap("X", self.X[:], [128, 8, NT], F32, tk('X', 0, NT))
            return

    def layer1(self, b, out):
        p = self.p
        self.norm(b, 1, 0)
        self.tap("h1", self.HT[:], [128, 8, NT], BF16, [])
        self.x_spill(b)
        self.x_free()
        with self.scope() as lay:
            self.MIXT = self.carve([128, 8, NT], BF16)
            if 's5' not in os.environ.get('SKIPMIX', ''):
                self.s5(b)
            if self.stop in ('s5', 's5tab'):
                if self.stop == 's5':
                    self.tap("s5y", self.S5_YT[:], [128, 4, NT], BF16, [])
                return
            self.ssd(b)
            if self.stop in ('ssdprep', 'ssdraw'):
                return
            self.tap("mix1", self.MIXT[:], [128, 8, NT], BF16, [])
            if self.stop == 'mix1':
                return
            self.x_alloc()
            self.x_reload(b)
            self.outproj(b, 1, self.od_w_out[0])
        self.tap("xmid1", self.X[:], [128, 8, NT], F32, [])
        if self.stop == 'xmid1':
            return
        self.moe(b, 1)
        self.tap("xout1", self.X[:], [128, 8, NT], F32, [])
        self.write_out(b, out)
        self.x_free()

    def mlstm(self, b):
        p, PS, nc = self.p, self.PS, self.nc
        w_in = self.ev_w_in[0].rearrange("(k p) n -> p k n", p=128)
        order = [list(range(NCH)), [1, 0] + list(range(NCH - 1, 1, -1))]
        with self.scope() as ph:
            P = lambda name, shape, dt=F32: self.sb(ph, name, shape, dt)
            GT = P("GT", [128, NCH, 16]); LF = P("LF", [128, NCH, 8]); GB = P("GB", [128, 16])
            GJ = P("GJ", [128, NCH, 2, 4]); BIAS = P("BIAS", [128, NCH, 2, 4]); WE = P("WE", [128, NCH, 2, 4]); DEC = P("DEC", [128, NCH, 2, 4])
            MLN = P("MLN", [128, 512]); WG = P("WG", [128, 8, 16], BF16); ONEC = P("ONEC", [128, 1])
            ET = P("ET", [128, 8]); T12 = P("T12", [128, 2, 4])
            self.memset(ONEC[:], 1.0, ['ONEC'])
            p.dma('sp', [lambda e: e.dma_start(out=GB[:], in_=self.ml_gate_b[0].partition_broadcast(128)),
                         lambda e: e.dma_start(out=MLN[:], in_=self.ml_norm[0].partition_broadcast(128))], writes=['GB', 'MLN'])
            p.dma('pool', lambda e: e.dma_start(out=WG[:], in_=w_in[:, :, 2048:2064]), writes=['WG'])
            for ch in range(NCH):
                bank = ch % 2
                hk = tk('HT', ch * 128, 128)
                for k in range(8):
                    self.mm(PS[bank][:, 0:16], self.HT[:, k, ch * 128:(ch + 1) * 128], WG[:, k, :], k == 0, k == 7,
                            hk + ['WG'], [('ps', bank)])
                self.tt(GT[:, ch, :], PS[bank][:, 0:16], GB[:], ALU.add, [('ps', bank), 'GB'], [('GT', ch)])
                gv = GT[:, ch, :].rearrange("p (d g h) -> p d g h", d=2, g=2)
                lfv = LF[:, ch, :].rearrange("p (d h) -> p d h", d=2)
                etv = ET[:].rearrange("p (d h) -> p d h", d=2)
                self.act(etv, gv[:, :, 1, :], AF.Exp, [('GT', ch)], ['ET'], scale=-1.0)
                self.act(etv, etv, AF.Ln, ['ET'], ['ET'], bias=ONEC[:, 0:1], scale=1.0)
                self.ts(lfv, etv, -1.0, None, ALU.mult, None, ['ET'], [('LF', ch)])
                for d in range(2):
                    bk = 2 + d
                    rhs = LF[:, ch, d * 4:(d + 1) * 4]
                    self.mm(PS[bk][:, 0:4], self.TRI[:, 3 * d + 0, :], rhs, True, True, [('LF', ch), 'TRI'], [('ps', bk)])
                    self.mm(PS[bk][:, 4:8], self.TRI[:, 3 * d + 1, :], rhs, True, True, [('LF', ch), 'TRI'], [('ps', bk)])
                    self.mm(PS[bk][:, 8:12], self.ONESF[:], rhs, True, True, [('LF', ch), 'ONESF'], [('ps', bk)])
                    li = gv[:, d, 0, :]
                    self.act(GJ[:, ch, d, :], PS[bk][:, 0:4], AF.Exp, [('ps', bk)], [('GJ', ch, d)])
                    self.tt(BIAS[:, ch, d, :], li, PS[bk][:, 0:4], ALU.subtract, [('ps', bk), ('GT', ch)], [('BIAS', ch, d)])
                    self.tt(T12[:, d, :], li, PS[bk][:, 4:8], ALU.add, [('ps', bk), ('GT', ch)], [('T12', d)])
                    self.act(WE[:, ch, d, :], T12[:, d, :], AF.Exp, [('T12', d)], [('WE', ch, d)])
                    self.act(DEC[:, ch, d, :], PS[bk][:, 8:12], AF.Exp, [('ps', bk)], [('DEC', ch, d)])
            p.barrier()
            if self.stop == 'mlgate':
                for nm, t in [('GJ', GJ), ('BIAS', BIAS), ('WE', WE), ('DEC', DEC)]:
                    self.tap(nm, t[:], [128, NCH, 2, 4], F32, [])
                return
            for hg in range(2):
                heads = [2 * hg, 2 * hg + 1]
                with self.scope() as hs:
                    H = lambda name, shape, dt=F32: self.sb(hs, name, shape, dt)
                    B_ = {}
                    for hd in heads:
                        B_[hd] = dict(
                            W4=H("W4", [128, 8, 4, 128], BF16), QT=H("QT", [128, NT], BF16), KT=H("KT", [128, NT], BF16),
                            KTOK=H("KTOK", [128, NCH, 128], BF16), VP=H("VP", [128, NCH, 129], BF16), SO=H("SO", [128, NCH, 128], BF16),
                            HS=H("HS", [128, NCH, 128], BF16), SALL=H("SALL", [128, NCH, 128], BF16))
                    U_ = {}
                    for hd in heads:
                        for d in range(2):
                            U_[(hd, d)] = dict(CS=H("CS", [128, 129]), CSB=H("CSB", [128, 129], BF16), LFB=H("LFB", [128, 128]),
                                               DT=H("DT", [128, 128], BF16), PT=[H("PT", [128, 128], BF16) for _ in range(2)], TB=H("TB", [128, 129]),
                                               DEN=H("DEN", [128, 2]), VW=[H("VW", [128, 129], BF16) for _ in range(2)])
                            U_[(hd, d)]['NUM'] = U_[(hd, d)]['TB']
                    for hd in heads:
                        bb = B_[hd]
                        W4 = bb['W4']
                        self.dmas('pool', [(W4[:, :, i, :], w_in[:, :, i * 512 + hd * 128:i * 512 + (hd + 1) * 128]) for i in range(4)], writes=[('W4', hd)])
                        self.memset(bb['HS'][:], 0.0, [('HS', hd, c) for c in range(NCH)])
                        self.memset(bb['VP'][:, :, 128:129], 1.0, [('VP1', hd)], eng='pool')
                        for d in range(2):
                            self.memset(U_[(hd, d)]['CS'][:], 0.0, [('CS', hd, d)])
                            self.memset(U_[(hd, d)]['CSB'][:], 0.0, [('CSB', hd, d)], eng='pool')
                    cnt = 0
                    for hd in heads:
                        bb = B_[hd]
                        for ti, (t0, n) in enumerate(TILES):
                            hk = tk('HT', t0, n)
                            for i, nm in enumerate(['QT', 'KT']):
                                bank = cnt % 4
                                cnt += 1
                                for k in range(8):
                                    self.mm(PS[bank][:, :n], bb['W4'][:, k, i, :], self.HT[:, k, t0:t0 + n], k == 0, k == 7, hk + [('W4', hd)], [('ps', bank)])
                                self.act(bb[nm][:, t0:t0 + n], PS[bank][:, :n], AF.Copy, [('ps', bank)], [(nm, hd, c) for c in range(t0 // 128, (t0 + n) // 128)],
                                         scale=1.0 if i == 0 else 128.0 ** -0.5)
                    for hd in heads:
                        bb = B_[hd]
                        for ch in range(NCH):
                            bank = 4 + ch % 2
                            sbk = 6 + ch % 2
                            tsl = slice(ch * 128, (ch + 1) * 128)
                            hk = tk('HT', ch * 128, 128)
                            for k in range(8):
                                self.mm(PS[bank][:, 0:384], self.HT[:, k, tsl], bb['W4'][:, k, 1:4, :].rearrange("p a n -> p (a n)"),
                                        k == 0, k == 7, hk + [('W4', hd)], [('ps', bank)])
                            self.act(bb['KTOK'][:, ch, :], PS[bank][:, 0:128], AF.Copy, [('ps', bank)], [('KTOK', hd, ch)], scale=128.0 ** -0.5)
                            self.cp(bb['VP'][:, ch, 0:128], PS[bank][:, 128:256], [('ps', bank)], [('VP', hd, ch)])
                            self.act(bb['SO'][:, ch, :], PS[bank][:, 256:384], AF.Sigmoid, [('ps', bank)], [('SO', hd, ch)])
                            self.mm(PS[sbk][:, 0:128], bb['KT'][:, tsl], bb['QT'][:, tsl], True, True, [('KT', hd, ch), ('QT', hd, ch)], [('ps', sbk)])
                            self.cp(bb['SALL'][:, ch, :], PS[sbk][:, 0:128], [('ps', sbk)], [('SALL', hd, ch)])
                    units = [(hd, d) for hd in heads for d in range(2)]

                    def mk_ctxs(step):
                        ctxs = []
                        for ui, (hd, d) in enumerate(units):
                            ch = order[d][step]
                            ctxs.append((ui, hd, d, ch, slice(ch * 128, (ch + 1) * 128), B_[hd], U_[(hd, d)], 2 * ui, 2 * ui + 1))
                        return ctxs

                    def front(step):
                        ctxs = mk_ctxs(step)
                        sp_ = step % 2
                        for (ui, hd, d, ch, tsl, bb, uu, bx, by) in ctxs:
                            self.act(uu['LFB'][:], self.ONESF[:], AF.Identity, [('LF', ch), 'ONESF'], [('LFB', hd, d)], scale=LF[:, ch, d * 4 + hd:d * 4 + hd + 1])
                        for (ui, hd, d, ch, tsl, bb, uu, bx, by) in ctxs:
                            self.mm(PS[bx][:, 0:128], uu['LFB'][:], self.TRI[:, 3 * d + 0, :], True, False, [('LFB', hd, d), 'TRI'], [('ps', bx)])
                            self.mm(PS[bx][:, 0:128], self.IDF[:], self.TRI[:, 3 * d + 2, :], False, True, ['IDF', 'TRI'], [('ps', bx)])
                        for (ui, hd, d, ch, tsl, bb, uu, bx, by) in ctxs:
                            self.act(uu['DT'][:], PS[bx][:, 0:128], AF.Exp, [('ps', bx), ('BIAS', ch, d)], [('DT', hd, d)], bias=BIAS[:, ch, d, hd:hd + 1], scale=1.0)
                        for (ui, hd, d, ch, tsl, bb, uu, bx, by) in ctxs:
                            self.tt(uu['PT'][sp_][:], bb['SALL'][:, ch, :], uu['DT'][:], ALU.mult, [('SALL', hd, ch), ('DT', hd, d)], [('PT', hd, d, sp_)], eng='pool')
                            self.ts(uu['VW'][sp_][:], bb['VP'][:, ch, :], WE[:, ch, d, hd:hd + 1], None, ALU.mult, None,
                                    [('VP', hd, ch), ('VP1', hd), ('WE', ch, d)], [('VW', hd, d, sp_)])

                    def back(step):
                        ctxs = mk_ctxs(step)
                        sp_ = step % 2
                        for (ui, hd, d, ch, tsl, bb, uu, bx, by) in ctxs:
                            self.mm(PS[by][:, 0:129], uu['PT'][sp_][:], bb['VP'][:, ch, :], True, True, [('PT', hd, d, sp_), ('VP', hd, ch), ('VP1', hd)], [('ps', by)])
                            self.mm(PS[by][:, 129:258], bb['QT'][:, tsl], uu['CSB'][:], True, True, [('QT', hd, ch), ('CSB', hd, d)], [('ps', by)])
                            self.mm(PS[by][:, 258:387], bb['KTOK'][:, ch, :], uu['VW'][sp_][:], True, True, [('KTOK', hd, ch), ('VW', hd, d, sp_)], [('ps', by)])
                        for (ui, hd, d, ch, tsl, bb, uu, bx, by) in ctxs:
                            self.act(uu['TB'][:], PS[by][:, 129:258], AF.Identity, [('ps', by), ('GJ', ch, d)], [('TB', hd, d)], scale=GJ[:, ch, d, hd:hd + 1])
                        for (ui, hd, d, ch, tsl, bb, uu, bx, by) in ctxs:
                            self.stt(uu['CS'][:], uu['CS'][:], DEC[:, ch, d, hd:hd + 1], PS[by][:, 258:387], ALU.mult, ALU.add,
                                     [('CS', hd, d), ('DEC', ch, d), ('ps', by)], [('CS', hd, d)])
                            self.tt(uu['NUM'][:], PS[by][:, 0:129], uu['TB'][:], ALU.add, [('ps', by), ('TB', hd, d)], [('NUM', hd, d), ('TB', hd, d)])
                        for (ui, hd, d, ch, tsl, bb, uu, bx, by) in ctxs:
                            self.act(uu['CSB'][:], uu['CS'][:], AF.Copy, [('CS', hd, d)], [('CSB', hd, d)])
                        for (ui, hd, d, ch, tsl, bb, uu, bx, by) in ctxs:
                            DEN = uu['DEN']
                            self.stt(DEN[:, 0:1], uu['NUM'][:, 128:129], -1.0, uu['NUM'][:, 128:129], ALU.mult, ALU.max, [('NUM', hd, d), ('TB', hd, d)], [('DEN', hd, d, 0)])
                        for (ui, hd, d, ch, tsl, bb, uu, bx, by) in ctxs:
                            DEN = uu['DEN']
                            self.ts(DEN[:, 0:1], DEN[:, 0:1], 1.0, None, ALU.max, None, [('DEN', hd, d, 0)], [('DEN', hd, d, 0)])
                        for (ui, hd, d, ch, tsl, bb, uu, bx, by) in ctxs:
                            DEN = uu['DEN']
                            self.recip(DEN[:, 1:2], DEN[:, 0:1], [('DEN', hd, d, 0)], [('DEN', hd, d, 1)])
                        for (ui, hd, d, ch, tsl, bb, uu, bx, by) in ctxs:
                            DEN = uu['DEN']
                            self.stt(bb['HS'][:, ch, :], uu['NUM'][:, 0:128], DEN[:, 1:2], bb['HS'][:, ch, :], ALU.mult, ALU.add,
                                     [('NUM', hd, d), ('TB', hd, d), ('DEN', hd, d, 1), ('HS', hd, ch)], [('HS', hd, ch)])

                    front(0)
                    for step in range(NCH):
                        if step + 1 < NCH:
                            front(step + 1)
                        back(step)
                    SSQ = H("SSQ", [128, 4]); JK = [H("JK0", [128, 128])] * 2; T1 = [H("T10", [128, 128])] * 2
                    MT = [H("MT%d" % i, [128, 128], BF16) for i in range(2)]
                    for hi_, hd in enumerate(heads):
                        bb = B_[hd]
                        for ch in range(NCH):
                            bank = (hi_ * NCH + ch) % 4
                            s2 = ch % 2
                            o0 = 2 * s2
                            self.act(JK[s2][:], bb['HS'][:, ch, :], AF.Square, [('HS', hd, ch)], [('JK', 0)])
                            self.p.op('dve', lambda e, s2=s2, o0=o0, JK=JK, SSQ=SSQ: e.reduce_sum(out=SSQ[:, o0:o0 + 1], in_=JK[s2][:], axis=AX.X), [('JK', 0)], [('SSQ', o0)])
                            self.act(SSQ[:, o0:o0 + 1], SSQ[:, o0:o0 + 1], AF.Sqrt, [('SSQ', o0)], [('SSQ', o0)], bias=self.EPSC[:, 0:1], scale=1.0 / 128)
                            self.recip(SSQ[:, o0 + 1:o0 + 2], SSQ[:, o0:o0 + 1], [('SSQ', o0)], [('SSQ', o0 + 1)])
                            self.stt(T1[s2][:], bb['HS'][:, ch, :], SSQ[:, o0 + 1:o0 + 2], MLN[:, hd * 128:(hd + 1) * 128], ALU.mult, ALU.mult,
                                     [('HS', hd, ch), ('SSQ', o0 + 1), 'MLN'], [('T1', 0)])
                            self.tt(MT[s2][:], T1[s2][:], bb['SO'][:, ch, :], ALU.mult, [('T1', 0), ('SO', hd, ch)], [('MT', s2)])
                            pv = PS[bank][:].bitcast(BF16)
                            self.tr(pv[:, 0:128], MT[s2][:], self.IDB[:], [('MT', s2), 'IDB'], [('ps', bank)])
                            self.act(self.MIXT[:, hd, ch * 128:(ch + 1) * 128], pv[:, 0:128], AF.Copy, [('ps', bank)], [('MIXT', ch)])
                    p.barrier()

    def attention(self, b):
        p, PS, nc = self.p, self.PS, self.nc
        w_in = self.ev_w_in[0].rearrange("(k p) n -> p k n", p=128)
        with self.scope() as ph:
            P = lambda name, shape, dt=F32: self.sb(ph, name, shape, dt)
            WA = P("WA", [128, 8, 768], BF16)
            GQ = P("GQ", [128, 640]); RC = P("RC", [128, 16, 64]); RSN = P("RSN", [128, 16, 64])
            QT4 = P("QT4", [128, 4, NT], BF16); KT = P("KTa", [128, NT], BF16); VP = P("VPa", [128, NCH, 2, 65], BF16)
            ATT = P("ATT", [128, NCH, 512], BF16)
            QS = [P("QS%d" % i, [128, 768]) for i in range(2)]
            SQ = P("SQa", [128, 640]); SSQ = P("SSQa", [128, 10]); RSTD = P("RSTDa", [128, 10])
            QN1 = P("QN1", [128, 640]); T1 = P("T1a", [128, 640]); T2 = P("T2a", [128, 640])
            QRq = P("QRq", [128, 4, 2, 64], BF16); QRk = P("QRk", [128, 128], BF16)
            ET = [P("ET%d" % i, [128, 512], BF16) for i in range(3)]
            RD = [P("RD%d" % i, [128, 4]) for i in range(2)]
            p.dma('pool', [lambda e, i=i: e.dma_start(out=WA[:, i * 4:(i + 1) * 4, :], in_=w_in[:, i * 4:(i + 1) * 4, 2064:2832]) for i in range(2)], writes=['WA'])
            p.dma('sp', [lambda e, i=i: e.dma_start(out=GQ[:, i * 64:(i + 1) * 64], in_=self.at_q_norm[0].partition_broadcast(128)) for i in range(8)] +
                        [lambda e, i=i: e.dma_start(out=GQ[:, 512 + i * 64:512 + (i + 1) * 64], in_=self.at_k_norm[0].partition_broadcast(128)) for i in range(2)],
                  writes=['GQ'])
            p.dma('act', [lambda e: e.dma_start(out=RC[:], in_=self.k_rope[0].rearrange("(c p) d -> p c d", p=128)),
                          lambda e: e.dma_start(out=RSN[:], in_=self.k_rope[1].rearrange("(c p) d -> p c d", p=128))], writes=['ROPE'])
            self.ts(GQ[:, 0:512], GQ[:, 0:512], 0.125, None, ALU.mult, None, ['GQ'], ['GQ'])
            self.memset(VP[:, :, :, 64:65], 1.0, [('VP1',)], eng='pool')
            for ch in range(NCH):
                hk = tk('HT', ch * 128, 128)
                qs = QS[ch % 2]
                bA, bB, bT = (ch % 2) * 3, (ch % 2) * 3 + 1, (ch % 2) * 3 + 2
                for k in range(8):
                    self.mm(PS[bA][:, 0:512], self.HT[:, k, ch * 128:(ch + 1) * 128], WA[:, k, 0:512], k == 0, k == 7, hk + ['WA'], [('ps', bA)])
                for k in range(8):
                    self.mm(PS[bB][:, 0:256], self.HT[:, k, ch * 128:(ch + 1) * 128], WA[:, k, 512:768], k == 0, k == 7, hk + ['WA'], [('ps', bB)])
                self.act(qs[:, 0:512], PS[bA][:, 0:512], AF.Copy, [('ps', bA)], [('QS', ch % 2)])
                self.act(qs[:, 512:768], PS[bB][:, 0:256], AF.Copy, [('ps', bB)], [('QS', ch % 2)])
                self.act(SQ[:], qs[:, 0:640], AF.Square, [('QS', ch % 2)], ['SQ'])
                self.p.op('dve', lambda e: e.tensor_reduce(out=SSQ[:], in_=SQ[:].rearrange("p (h d) -> p h d", d=64), axis=AX.X, op=ALU.add), ['SQ'], ['SSQ'])
                self.act(SSQ[:], SSQ[:], AF.Sqrt, ['SSQ'], ['SSQ'], bias=self.EPSC[:, 0:1], scale=1.0 / 64)
                self.recip(RSTD[:], SSQ[:], ['SSQ'], ['RSTD'])
                self.tt(QN1[:].rearrange("p (h d) -> p h d", d=64), qs[:, 0:640].rearrange("p (h d) -> p h d", d=64),
                        RSTD[:].unsqueeze(2).to_broadcast([128, 10, 64]), ALU.mult, [('QS', ch % 2), 'RSTD'], ['QN1'])
                self.tt(QN1[:], QN1[:], GQ[:], ALU.mult, ['QN1', 'GQ'], ['QN1'])
                qdst = QRq[:].rearrange("p pr hl d -> p hl pr d")
                if ch >= 2:
                    lc = ch - 2
                    self.tt(T1[:].rearrange("p (h d) -> p h d", d=64), QN1[:].rearrange("p (h d) -> p h d", d=64),
                            RC[:, lc, :].unsqueeze(1).to_broadcast([128, 10, 64]), ALU.mult, ['QN1', 'ROPE'], ['T1'], eng='pool')
                    q3 = QN1[:].rearrange("p (h d) -> p h d", d=64)
                    t3 = T2[:].rearrange("p (h d) -> p h d", d=64)
                    for rc in range(2):
                        for f in range(2):
                            o0 = rc * 32 + f * 16
                            i0 = rc * 32 + (1 - f) * 16
                            self.tt(t3[:, :, o0:o0 + 16], q3[:, :, i0:i0 + 16], RSN[:, lc, o0:o0 + 16].unsqueeze(1).to_broadcast([128, 10, 16]),
                                    ALU.mult, ['QN1', 'ROPE'], ['T2'])
                    self.tt(qdst, T1[:, 0:512].rearrange("p (hl pr d) -> p hl pr d", hl=2, pr=4), T2[:, 0:512].rearrange("p (hl pr d) -> p hl pr d", hl=2, pr=4),
                            ALU.add, ['T1', 'T2'], ['QRq'])
                    self.tt(QRk[:], T1[:, 512:640], T2[:, 512:640], ALU.add, ['T1', 'T2'], ['QRk'])
                else:
                    self.cp(qdst, QN1[:, 0:512].rearrange("p (hl pr d) -> p hl pr d", hl=2, pr=4), ['QN1'], ['QRq'])
                    self.cp(QRk[:], QN1[:, 512:640], ['QN1'], ['QRk'])
                self.cp(VP[:, ch, :, 0:64], qs[:, 640:768].rearrange("p (k d) -> p k d", d=64), [('QS', ch % 2)], [('VP', ch)], eng='pool')
                pv = PS[bT][:].bitcast(BF16)
                for pr in range(4):
                    self.tr(pv[:, pr * 128:(pr + 1) * 128], QRq[:, pr, :, :].rearrange("p a d -> p (a d)"), self.IDB[:], ['QRq', 'IDB'], [('ps', bT)])
                self.tr(pv[:, 512:640], QRk[:], self.IDB[:], ['QRk', 'IDB'], [('ps', bT)])
                self.act(QT4[:, :, ch * 128:(ch + 1) * 128], pv[:, 0:512].rearrange("p (a t) -> p a t", t=128), AF.Copy, [('ps', bT)], [('QT4', ch)])
                self.cp(KT[:, ch * 128:(ch + 1) * 128], pv[:, 512:640], [('ps', bT)], [('KTa', ch)])
            p.barrier()
            jobs = []
            for h in range(8):
                jobs.append((h, 0, 256, [0, 1]))
                for i in range(4):
                    jobs.append((h, 256 + 512 * i, 512, list(range(NCH))))
            sct = 0
            for ji, (h, q0, n, kcs) in enumerate(jobs):
                hl, pr = h // 4, h % 4
                ob = 4 + ji % 4
                nsub = n // 128
                qk = [('QT4', c) for c in range(q0 // 128, (q0 + n) // 128)]

                def issue_s(ki, sct):
                    kc = kcs[ki]
                    sb_ = sct % 4
                    self.mm(PS[sb_][:, :n], KT[hl * 64:(hl + 1) * 64, kc * 128:(kc + 1) * 128], QT4[hl * 64:(hl + 1) * 64, pr, q0:q0 + n],
                            True, True, [('KTa', kc)] + qk, [('ps', sb_)])
                issue_s(0, sct)
                for ki, kc in enumerate(kcs):
                    if ki + 1 < len(kcs):
                        issue_s(ki + 1, sct + 1)
                    sb_ = sct % 4
                    et = ET[sct % 3]
                    self.act(et[:, :n], PS[sb_][:, :n], AF.Exp, [('ps', sb_)], [('ET', sct % 3)])
                    for j in range(nsub):
                        self.mm(PS[ob][:, j * 65:(j + 1) * 65], et[:, j * 128:(j + 1) * 128], VP[:, kc, hl, :], ki == 0 and j == 0, ki == len(kcs) - 1,
                                [('ET', sct % 3), ('VP', kc), ('VP1',)], [('ps', ob)], skip_group_check=True)
                    sct += 1
                rd = RD[ji % 2]
                ov = PS[ob][:, 0:nsub * 65].rearrange("p (j d) -> p j d", d=65)
                self.recip(rd[:, 0:nsub], ov[:, :, 64], [('ps', ob)], [('RD', ji % 2)])
                c0 = q0 // 128
                self.tt(ATT[:, c0:c0 + nsub, h * 64:(h + 1) * 64], ov[:, :, 0:64], rd[:, 0:nsub].unsqueeze(2).to_broadcast([128, nsub, 64]), ALU.mult,
                        [('ps', ob), ('RD', ji % 2)], [('ATT', c) for c in range(c0, c0 + nsub)])
            for ch in range(NCH):
                bank = ch % 4
                pv = PS[bank][:].bitcast(BF16)
                for j in range(4):
                    self.tr(pv[:, j * 128:(j + 1) * 128], ATT[:, ch, j * 128:(j + 1) * 128], self.IDB[:], [('ATT', ch), 'IDB'], [('ps', bank)])
                self.act(self.MIXT[:, 4:8, ch * 128:(ch + 1) * 128], pv[:, 0:512].rearrange("p (a t) -> p a t", t=128), AF.Copy, [('ps', bank)], [('MIXT', ch)])
            p.barrier()

    def outproj(self, b, l, w_out):
        p, PS = self.p, self.PS
        with self.scope() as ph:
            WO = self.sb(ph, "WO", [128, 8, 1024], BF16)
            src = w_out.rearrange("(k p) n -> p k n", p=128)
            p.dma('pool', [lambda e, i=i: e.dma_start(out=WO[:, i * 4:(i + 1) * 4, :], in_=src[:, i * 4:(i + 1) * 4, :]) for i in range(2)], writes=['WO'])
            cnt = 0
            for ti, (t0, n) in enumerate(TILES):
                if l == 1 and ti == 0:
                    continue
                j = 2 if ti == 0 else b
                mk = tk('MIXT', t0, n)
                xk = tk('X', t0, n)
                for c in range(8):
                    bank = cnt % 4
                    cnt += 1
                    for k in range(8):
                        self.mm(PS[bank][:, :n], WO[:, k, c * 128:(c + 1) * 128], self.MIXT[:, k, t0:t0 + n], k == 0, k == 7, mk + ['WO'], [('ps', bank)])
                    self.stt(self.X[:, c, t0:t0 + n], PS[bank][:, :n], self.MOD[:, l, 16 + c, j:j + 1], self.X[:, c, t0:t0 + n], ALU.mult, ALU.add,
                             [('ps', bank)] + xk, xk)
            p.barrier()

    def moe(self, b, l):
        p, PS = self.p, self.PS
        tiles = TILES[1:] if l == 1 else TILES
        with self.scope() as ph:
            P = lambda name, shape, dt=F32: self.sb(ph, name, shape, dt)
            COMBT = P("COMBT", [16, NT], BF16)
            SEL = P("SEL", [16, 16, 128], BF16)
            WGU = [None, None]
            WD = [None, None]
            p.dma('pool', lambda e: e.dma_start(out=SEL[:], in_=self.k_sel.rearrange("e r n -> r e n")), writes=['SEL'])

            def load_expert(e):
                slot = e % 2
                g = self.moe_w_gate[l, e].rearrange("(k p) n -> p k n", p=128)
                u = self.moe_w_up[l, e].rearrange("(k p) n -> p k n", p=128)
                dn = self.moe_w_down[l, e].rearrange("(k p) n -> p k n", p=128)
                p.dma('pool', [lambda en, i=i: en.dma_start(out=WGU[slot][:, i * 4:(i + 1) * 4, 0:512], in_=g[:, i * 4:(i + 1) * 4, :]) for i in range(2)] +
                              [lambda en, i=i: en.dma_start(out=WGU[slot][:, i * 4:(i + 1) * 4, 512:1024], in_=u[:, i * 4:(i + 1) * 4, :]) for i in range(2)],
                      writes=[('WGU', slot)])
                p.dma('pool', [lambda en, i=i: en.dma_start(out=WD[slot][:, i * 2:(i + 1) * 2, :], in_=dn[:, i * 2:(i + 1) * 2, :]) for i in range(2)],
                      writes=[('WD', slot)])
            with self.scope() as rs:
                R = lambda name, shape, dt=F32: self.sb(rs, name, shape, dt)
                WR = R("WR", [128, 8, 20]); RB = R("RB", [128, 20]); L = R("L", [128, 20])
                SM = R("SM", [128, 16]); GM = R("GM", [128, 4]); GE = R("GE", [128, 4]); PEN = R("PEN", [128, 4])
                EM = R("EM", [128, 16]); EM2 = R("EM2", [128, 16]); M1 = R("M1", [128, 16]); M2 = R("M2", [128, 16]); COMB = R("COMB", [128, 16])
                p.dma('sp', [lambda e: e.dma_start(out=WR[:, :, 0:4], in_=self.moe_gr_w[l].rearrange("(k p) n -> p k n", p=128)),
                             lambda e: e.dma_start(out=WR[:, :, 4:20], in_=self.moe_er_w[l].rearrange("(k p) n -> p k n", p=128)),
                             lambda e: e.dma_start(out=RB[:, 0:4], in_=self.moe_gr_b[l].partition_broadcast(128)),
                             lambda e: e.dma_start(out=RB[:, 4:20], in_=self.moe_er_b[l].partition_broadcast(128))], writes=['WR', 'RB'])

                def router(ti, t0, n, H32):
                    for sub in range(n // 128):
                        rb = 2 + sub % 2
                        tb = 4 + sub % 2
                        for k in range(8):
                            self.mm(PS[rb][:, 0:20], H32[:, k, sub * 128:(sub + 1) * 128], WR[:, k, :], k == 0, k == 7, [('H32', k), 'WR'], [('ps', rb)])
                        self.tt(L[:], PS[rb][:, 0:20], RB[:], ALU.add, [('ps', rb), 'RB'], ['L'])
                        sm = lambda i: SM[:, i:i + 1]
                        self.p.op('dve', lambda e: e.reduce_max(out=SM[:, 0:1], in_=L[:, 0:4], axis=AX.X), ['L'], [('SM', 0)])
                        self.ts(GM[:], L[:, 0:4], sm(0), None, ALU.is_equal, None, ['L', ('SM', 0)], ['GM'])
                        self.ts(sm(1), sm(0), -1.0, None, ALU.mult, None, [('SM', 0)], [('SM', 1)])
                        self.act(GE[:], L[:, 0:4], AF.Exp, ['L', ('SM', 1)], ['GE'], bias=sm(1), scale=1.0)
                        self.p.op('dve', lambda e: e.reduce_sum(out=SM[:, 2:3], in_=GE[:], axis=AX.X), ['GE'], [('SM', 2)])
                        self.recip(sm(3), sm(2), [('SM', 2)], [('SM', 3)])
                        self.ts(PEN[:], GM[:], 1e30, -1e30, ALU.mult, ALU.add, ['GM'], ['PEN'])
                        self.tt(EM[:].rearrange("p (g e) -> p g e", e=4), L[:, 4:20].rearrange("p (g e) -> p g e", e=4),
                                PEN[:].unsqueeze(2).to_broadcast([128, 4, 4]), ALU.add, ['L', 'PEN'], ['EM'])
                        self.p.op('dve', lambda e: e.reduce_max(out=SM[:, 4:5], in_=EM[:], axis=AX.X), ['EM'], [('SM', 4)])
                        self.ts(M1[:], EM[:], sm(4), None, ALU.is_equal, None, ['EM', ('SM', 4)], ['M1'])
                        self.stt(EM2[:], M1[:], -1e30, EM[:], ALU.mult, ALU.add, ['M1', 'EM'], ['EM2'])
                        self.p.op('dve', lambda e: e.reduce_max(out=SM[:, 5:6], in_=EM2[:], axis=AX.X), ['EM2'], [('SM', 5)])
                        self.ts(M2[:], EM2[:], sm(5), None, ALU.is_equal, None, ['EM2', ('SM', 5)], ['M2'])
                        self.tt(sm(6), sm(5), sm(4), ALU.subtract, [('SM', 5), ('SM', 4)], [('SM', 6)])
                        self.act(sm(7), sm(6), AF.Exp, [('SM', 6)], [('SM', 7)])
                        self.ts(sm(8), sm(7), 1.0, None, ALU.add, None, [('SM', 7)], [('SM', 8)])
                        self.recip(sm(9), sm(8), [('SM', 8)], [('SM', 9)])
                        self.tt(sm(10), sm(9), sm(3), ALU.mult, [('SM', 9), ('SM', 3)], [('SM', 10)])
                        self.tt(sm(11), sm(10), sm(7), ALU.mult, [('SM', 10), ('SM', 7)], [('SM', 11)])
                        self.ts(COMB[:], M1[:], sm(10), None, ALU.mult, None, ['M1', ('SM', 10)], ['COMB'])
                        self.stt(COMB[:], M2[:], sm(11), COMB[:], ALU.mult, ALU.add, ['M2', ('SM', 11), 'COMB'], ['COMB'])
                        self.tr(PS[tb][0:16, 0:128], COMB[:], self.IDF[:], ['COMB', 'IDF'], [('ps', tb)])
                        tt0 = t0 + sub * 128
                        self.act(COMBT[:, tt0:tt0 + 128], PS[tb][0:16, 0:128], AF.Copy, [('ps', tb)], [('COMBT', tt0 // 128)])
                self.norm(b, l, 1, f32_cb=router, skip_ctx=(l == 1))
            for i in range(2):
                WGU[i] = P("WGU%d" % i, [128, 8, 1024], BF16)
                WD[i] = P("WD%d" % i, [128, 4, 1024], BF16)
            load_expert(0)
            load_expert(1)
            CB = [P("CB%d" % i, [128, 512], BF16) for i in range(2)]
            SG = [P("SG%d" % i, [128, 512], BF16) for i in range(2)]
            ACTT = [P("ACTT%d" % i, [128, 4, 512], BF16) for i in range(2)]
            items = [(e, ti) for e in range(16) for ti in range(len(tiles))]

            def stage_a(ii):
                e, ti = items[ii]
                t0, n = tiles[ti]
                slot = e % 2
                hk = tk('HT', t0, n)
                cb = CB[ii % 2]
                self.mm(PS[0][:, :n], SEL[:, e, :], COMBT[:, t0:t0 + n], True, True, ['SEL'] + tk('COMBT', t0, n), [('ps', 0)])
                self.act(cb[:, :n], PS[0][:, :n], AF.Copy, [('ps', 0)], [('CB', ii % 2)])
                for j in range(4):
                    gb = 1 + j % 2
                    ub = 3 + j % 2
                    sg = SG[j % 2]
                    for k in range(8):
                        self.mm(PS[gb][:, :n], WGU[slot][:, k, j * 128:(j + 1) * 128], self.HT[:, k, t0:t0 + n], k == 0, k == 7,
                                hk + [('WGU', slot)], [('ps', gb)])
                    for k in range(8):
                        self.mm(PS[ub][:, :n], WGU[slot][:, k, 512 + j * 128:512 + (j + 1) * 128], self.HT[:, k, t0:t0 + n], k == 0, k == 7,
                                hk + [('WGU', slot)], [('ps', ub)])
                    self.act(sg[:, :n], PS[gb][:, :n], AF.Silu, [('ps', gb)], [('SG', j % 2)])
                    self.tt(sg[:, :n], sg[:, :n], cb[:, :n], ALU.mult, [('SG', j % 2), ('CB', ii % 2)], [('SG', j % 2)])
                    self.tt(ACTT[ii % 2][:, j, :n], sg[:, :n], PS[ub][:, :n], ALU.mult, [('SG', j % 2), ('ps', ub)], [('ACTT', ii % 2, j)])

            def stage_b(ii):
                e, ti = items[ii]
                t0, n = tiles[ti]
                slot = e % 2
                j_ = 2 if (l == 0 and ti == 0) else b
                xk = tk('X', t0, n)
                for c in range(8):
                    db = 5 + c % 3
                    for j in range(4):
                        self.mm(PS[db][:, :n], WD[slot][:, j, c * 128:(c + 1) * 128], ACTT[ii % 2][:, j, :n], j == 0, j == 3,
                                [('WD', slot), ('ACTT', ii % 2, j)], [('ps', db)])
                    self.stt(self.X[:, c, t0:t0 + n], PS[db][:, :n], self.MOD[:, l, 40 + c, j_:j_ + 1], self.X[:, c, t0:t0 + n], ALU.mult, ALU.add,
                             [('ps', db)] + xk, xk)
            stage_a(0)
            for ii in range(len(items)):
                if ii + 1 < len(items):
                    stage_a(ii + 1)
                stage_b(ii)
                e_, ti_ = items[ii]
                if ti_ == len(tiles) - 1 and e_ + 2 < 16:
                    load_expert(e_ + 2)
            p.barrier()

    def write_out(self, b, out):
        p, PS = self.p, self.PS
        with self.scope() as ph:
            OT = [self.sb(ph, "OT%d" % i, [128, 1024], F32) for i in range(2)]
            for ch in range(2, NCH):
                s = ch % 2
                for hf in range(2):
                    bank = (ch * 2 + hf) % 4
                    for kk in range(4):
                        k = hf * 4 + kk
                        self.tr(PS[bank][:, kk * 128:(kk + 1) * 128], self.X[:, k, ch * 128:(ch + 1) * 128], self.IDF[:], [('X', ch), 'IDF'], [('ps', bank)])
                    if hf == 0:
                        self.act(OT[s][:, 0:512], PS[bank][:], AF.Copy, [('ps', bank)], [('OT', s, 0)])
                    else:
                        self.cp(OT[s][:, 512:1024], PS[bank][:], [('ps', bank)], [('OT', s, 1)])
                p.dma('sp' if ch % 2 == 0 else 'act', lambda e, s=s, ch=ch: e.dma_start(out=out[b, (ch - 2) * 128:(ch - 1) * 128, :], in_=OT[s][:]),
                      reads=[('OT', s, 0), ('OT', s, 1)])
            p.barrier()

    def cis(self, R, MAG, ORE, OIM, I, F, G, S, key, neg_im=False):
        TWO_PI = 6.2831845
        MAGIC = 12582912.0
        k = lambda n: (key, n)
        self.ts(I, R, MAGIC, None, ALU.add, None, [k('R')], [k('I')])
        self.ts(I, I, MAGIC, None, ALU.subtract, None, [k('I')], [k('I')])
        self.tt(F, R, I, ALU.subtract, [k('R'), k('I')], [k('F')])
        self.act(S, F, AF.Sin, [k('F')], [k('S')], scale=TWO_PI)
        self.act(G, MAG, AF.Exp, [k('MAG'), k('G')], [k('G')])
        if neg_im:
            self.stt(OIM, G, -1.0, S, ALU.mult, ALU.mult, [k('G'), k('S')], [k('OIM')])
        else:
            self.tt(OIM, G, S, ALU.mult, [k('G'), k('S')], [k('OIM')])
        self.ts(F, F, 0.25, None, ALU.add, None, [k('F')], [k('F')])
        self.ts(I, F, MAGIC, None, ALU.add, None, [k('F')], [k('I')])
        self.ts(I, I, MAGIC, None, ALU.subtract, None, [k('I')], [k('I')])
        self.tt(F, F, I, ALU.subtract, [k('F'), k('I')], [k('F')])
        self.act(S, F, AF.Sin, [k('F')], [k('S')], scale=TWO_PI)
        self.tt(ORE, G, S, ALU.mult, [k('G'), k('S')], [k('ORE')])

    def s5(self, b):
        p, PS, nc = self.p, self.PS, self.nc
        w_in = self.od_w_in[0].rearrange("(k p) n -> p k n", p=128)
        order = [list(range(NCH)), [1, 0] + list(range(NCH - 1, 1, -1))]
        INV2PI = 1.0 / (2.0 * math.pi)
        with self.scope() as ph:
            P = lambda name, shape, dt=F32: self.sb(ph, name, shape, dt)
            UT = P("UT", [128, 4, NT], BF16); YT = self.MIXT[:, 0:4, :]
            self.S5_UT, self.S5_YT = UT, YT
            TRIB = P("TRIB", [128, 6, 128], BF16); MASK8 = P("MASK8", [128, 8]); POSC = P("POSC", [128, 4]); POSR = P("POSR", [128, 2, 128])
            self.dmas('pool', [(TRIB[:], self.k_tri.rearrange("a p n -> p a n"))], writes=['TRIB'])
            self.dmas('sp', [(MASK8[:], self.k_mask8), (POSC[:], self.k_posc),
                         (POSR[:], self.k_posr)], writes=['MASK8', 'POSC', 'POSR'])
            with self.scope() as s1:
                WU = self.sb(s1, "WU", [128, 8, 512], BF16)
                self.dmas('pool', [(WU[:, i * 4:(i + 1) * 4, :], w_in[:, i * 4:(i + 1) * 4, 0:512]) for i in range(2)], writes=['WU'])
                cnt = 0
                for ti, (t0, n) in enumerate(TILES):
                    hk = tk('HT', t0, n)
                    for c in range(4):
                        bank = cnt % 4
                        cnt += 1
                        for k in range(8):
                            self.mm(PS[bank][:, :n], WU[:, k, c * 128:(c + 1) * 128], self.HT[:, k, t0:t0 + n], k == 0, k == 7, hk + ['WU'], [('ps', bank)])
                        self.act(UT[:, c, t0:t0 + n], PS[bank][:, :n], AF.Copy, [('ps', bank)], tk('UT', t0, n))
                p.barrier()
            for d in range(2):
                with self.scope() as sd:
                    D = lambda name, shape, dt=F32: self.sb(sd, name, shape, dt)
                    KR = D("KR", [128, 2048], BF16); KI = D("KI", [128, 2048], BF16)
                    QR = D("QR", [128, 16, 128], BF16); QI = D("QI", [128, 16, 128], BF16)
                    BDR = D("BDR", [128, 4, 512], BF16); BDI = D("BDI", [128, 4, 512], BF16)
                    CPR = D("CPR", [128, 16, 128], BF16); CPI = D("CPI", [128, 16, 128], BF16)
                    with self.scope() as st_:
                        T = lambda name, shape, dt=F32: self.sb(st_, name, shape, dt)
                        AB = T("AB", [128, 512]); OM = T("OM", [128, 512]); LDT = T("LDT", [128, 32])
                        RR = T("RR", [128, 512]); MG = T("MG", [128, 512]); II = T("II", [128, 512])
                        FF = T("FF", [128, 512]); GG = T("GG", [128, 512]); SS = T("SS", [128, 512])
                        self.dmas('sp', [(LDT[:], self.s5_log_dt[0, d].partition_broadcast(128))], writes=['LDT'])
                        self.act(LDT[:], LDT[:], AF.Exp, ['LDT'], ['LDT'])
                        for q in range(4):
                            are_q = self.s5_a_re[0, d].rearrange("g n -> (g n)")[q * 512:(q + 1) * 512]
                            aim_q = self.s5_a_im[0, d].rearrange("g n -> (g n)")[q * 512:(q + 1) * 512]
                            self.dmas('sp', [(AB[:], are_q.partition_broadcast(128)),
                                         (OM[:], aim_q.partition_broadcast(128))],
                                  reads=[('kt', 'ORE'), ('kt', 'OIM')], writes=[('kt', 'ORE'), ('kt', 'OIM')])
                            dtb = LDT[:, q * 8:(q + 1) * 8].unsqueeze(2).to_broadcast([128, 8, 64])
                            v3 = lambda t: t[:].rearrange("p (g n) -> p g n", n=64)
                            self.tt(v3(AB), v3(AB), dtb, ALU.mult, [('kt', 'ORE'), 'LDT'], [('kt', 'ORE')])
                            self.tt(v3(OM), v3(OM), dtb, ALU.mult, [('kt', 'OIM'), 'LDT'], [('kt', 'OIM')])
                            self.ts(RR[:], OM[:], POSC[:, d:d + 1], INV2PI, ALU.mult, ALU.mult, [('kt', 'OIM'), 'POSC'], [('kt', 'R')])
                            self.ts(MG[:], AB[:], POSC[:, 2 + d:3 + d], None, ALU.mult, None, [('kt', 'ORE'), 'POSC'], [('kt', 'MAG')])
                            self.cis(RR[:], MG[:], AB[:], OM[:], II[:], FF[:], GG[:], SS[:], 'kt', neg_im=True)
                            self.cp(KR[:, q * 512:(q + 1) * 512], AB[:], [('kt', 'ORE')], ['KR'])
                            self.cp(KI[:, q * 512:(q + 1) * 512], OM[:], [('kt', 'OIM')], ['KI'])
                        p.barrier()
                    with self.scope() as st_:
                        T = lambda name, shape, dt=F32: self.sb(st_, name, shape, dt)
                        STG = T("STG", [128, 128]); PRM = T("PRM", [128, 32]); LD2 = T("LD2", [128, 16])
                        RHO = T("RHO", [128, 16]); OMT = T("OMT", [128, 16])
                        RR = T("RR", [128, 4, 128]); MG = T("MG", [128, 4, 128]); II = T("II", [128, 4, 128])
                        FF = T("FF", [128, 4, 128]); GG = T("GG", [128, 4, 128]); SS = T("SS", [128, 4, 128])
                        self.memset(STG[:], 0.0, ['STG'])
                        self.dmas('sp', [(STG[0:16, :], self.s5_a_re[0, d].rearrange("(pr g2) n -> pr (g2 n)", g2=2)),
                                     (STG[16:32, :], self.s5_a_im[0, d].rearrange("(pr g2) n -> pr (g2 n)", g2=2))],
                              reads=['STG'], writes=['STG'])
                        ld = self.s5_log_dt[0, d].rearrange("(pr g2) -> g2 pr", g2=2)
                        self.dmas('sp', [(LD2[g2 * 64:(g2 + 1) * 64, :], ld[g2].partition_broadcast(64))
                                     for g2 in range(2)], writes=['LD2'], slow=True)
                        self.tr(PS[0][:, 0:128], STG[:], self.IDF[:], ['STG', 'IDF'], [('ps', 0)])
                        self.cp(PRM[:], PS[0][:, 0:32], [('ps', 0)], ['PRM'])
                        self.act(LD2[:], LD2[:], AF.Exp, ['LD2'], ['LD2'])
                        self.tt(RHO[:], PRM[:, 0:16], LD2[:], ALU.mult, ['PRM', 'LD2'], ['RHO'])
                        self.stt(OMT[:], PRM[:, 16:32], INV2PI, LD2[:], ALU.mult, ALU.mult, ['PRM', 'LD2'], ['OMT'])
                        posb = POSR[:, d, :].unsqueeze(1).to_broadcast([128, 4, 128])
                        f2 = lambda t: t.rearrange("p a b -> p (a b)")
                        for q in range(4):
                            self.tt(RR[:], OMT[:, 4 * q:4 * q + 4].unsqueeze(2).to_broadcast([128, 4, 128]), posb, ALU.mult, ['OMT', 'POSR'], [('qt', 'R')])
                            self.tt(MG[:], RHO[:, 4 * q:4 * q + 4].unsqueeze(2).to_broadcast([128, 4, 128]), posb, ALU.mult, ['RHO', 'POSR'], [('qt', 'MAG')])
                            self.cis(f2(RR[:]), f2(MG[:]), f2(QR[:, 4 * q:4 * q + 4, :]), f2(QI[:, 4 * q:4 * q + 4, :]), f2(II[:]), f2(FF[:]), f2(GG[:]), f2(SS[:]), 'qt')
                        p.barrier()
                    with self.scope() as st_:
                        T = lambda name, shape, dt=F32: self.sb(st_, name, shape, dt)
                        STG = T("STG", [128, 64]); AT = T("AT", [64, 64]); DTB = T("DTB", [64, 32])
                        RHO = T("RHO", [64, 32]); OMT = T("OMT", [64, 32]); ABR = T("ABR", [64, 32]); ABI = T("ABI", [64, 32])
                        II = T("II", [64, 32]); FF = T("FF", [64, 32]); GG = T("GG", [64, 32]); SS = T("SS", [64, 32])
                        DEN = T("DEN", [64, 32]); ZR = T("ZR", [64, 32]); ZI = T("ZI", [64, 32]); TT1 = T("TT1", [64, 32]); TT2 = T("TT2", [64, 32])
                        BRE = T("BRE", [64, 32, 16]); BIM = T("BIM", [64, 32, 16]); BBR = T("BBR", [64, 32, 16]); BBI = T("BBI", [64, 32, 16]); TB3 = T("TB3", [64, 32, 16])
                        TRS = T("TRS", [128, 64]); CC = T("CC", [16, 2, 32, 64], BF16)
                        self.memset(STG[:], 0.0, ['STG'])
                        self.dmas('sp', [(STG[0:32, :], self.s5_a_re[0, d]),
                                     (STG[32:64, :], self.s5_a_im[0, d])], reads=['STG'], writes=['STG'])
                        self.dmas('sp', [(DTB[:], self.s5_log_dt[0, d].partition_broadcast(64)),
                                     (BRE[:], self.s5_b_re[0, d].rearrange("g n c -> n g c")),
                                     (BIM[:], self.s5_b_im[0, d].rearrange("g n c -> n g c"))],
                              writes=['DTB', 'BRE', 'BIM'])
                        self.dmas('pool', [(CC[:, 0], self.s5_c_re[0, d].rearrange("g c n -> c g n")),
                                       (CC[:, 1], self.s5_c_im[0, d].rearrange("g c n -> c g n"))], writes=['CC'])
                        self.tr(PS[0][0:64, 0:128], STG[:], self.IDF[:], ['STG', 'IDF'], [('ps', 0)])
                        self.cp(AT[:], PS[0][0:64, 0:64], [('ps', 0)], ['AT'])
                        self.act(DTB[:], DTB[:], AF.Exp, ['DTB'], ['DTB'])
                        self.tt(RHO[:], AT[:, 0:32], DTB[:], ALU.mult, ['AT', 'DTB'], [('zt', 'MAG')])
                        self.stt(OMT[:], AT[:, 32:64], INV2PI, DTB[:], ALU.mult, ALU.mult, ['AT', 'DTB'], [('zt', 'R')])
                        self.cis(OMT[:], RHO[:], ABR[:], ABI[:], II[:], FF[:], GG[:], SS[:], 'zt')
                        are, aim = AT[:, 0:32], AT[:, 32:64]
                        self.tt(DEN[:], are, are, ALU.mult, ['AT'], ['DEN'])
                        self.tt(TT1[:], aim, aim, ALU.mult, ['AT'], ['TT1'])
                        self.tt(DEN[:], DEN[:], TT1[:], ALU.add, ['DEN', 'TT1'], ['DEN'])
                        self.recip(DEN[:], DEN[:], ['DEN'], ['DEN'])
                        self.ts(ABR[:], ABR[:], -1.0, None, ALU.add, None, [('zt', 'ORE')], [('zt', 'ORE')])
                        self.tt(TT1[:], ABR[:], are, ALU.mult, [('zt', 'ORE'), 'AT'], ['TT1'])
                        self.tt(TT2[:], ABI[:], aim, ALU.mult, [('zt', 'OIM'), 'AT'], ['TT2'])
                        self.tt(TT1[:], TT1[:], TT2[:], ALU.add, ['TT1', 'TT2'], ['TT1'])
                        self.tt(ZR[:], TT1[:], DEN[:], ALU.mult, ['TT1', 'DEN'], ['ZR'])
                        self.tt(TT1[:], ABI[:], are, ALU.mult, [('zt', 'OIM'), 'AT'], ['TT1'])
                        self.tt(TT2[:], ABR[:], aim, ALU.mult, [('zt', 'ORE'), 'AT'], ['TT2'])
                        self.tt(TT1[:], TT1[:], TT2[:], ALU.subtract, ['TT1', 'TT2'], ['TT1'])
                        self.tt(ZI[:], TT1[:], DEN[:], ALU.mult, ['TT1', 'DEN'], ['ZI'])
                        zrb = ZR[:].unsqueeze(2).to_broadcast([64, 32, 16])
                        zib = ZI[:].unsqueeze(2).to_broadcast([64, 32, 16])
                        self.tt(BBR[:], BRE[:], zrb, ALU.mult, ['BRE', 'ZR'], ['BBR'])
                        self.tt(TB3[:], BIM[:], zib, ALU.mult, ['BIM', 'ZI'], ['TB3'])
                        self.tt(BBR[:], BBR[:], TB3[:], ALU.subtract, ['BBR', 'TB3'], ['BBR'])
                        self.tt(BBI[:], BIM[:], zrb, ALU.mult, ['BIM', 'ZR'], ['BBI'])
                        self.tt(TB3[:], BRE[:], zib, ALU.mult, ['BRE', 'ZI'], ['TB3'])
                        self.tt(BBI[:], BBI[:], TB3[:], ALU.add, ['BBI', 'TB3'], ['BBI'])
                        mk = MASK8[:].unsqueeze(2).to_broadcast([128, 8, 64])
                        for ri, (bb, bd) in enumerate([(BBR, BDR), (BBI, BDI)]):
                            for q in range(4):
                                bank = (ri * 4 + q) % 2
                                self.tr(PS[bank][:, 0:64], bb[:, q * 8:(q + 1) * 8, :].rearrange("p g c -> p (g c)"), self.IDF[0:64, 0:64],
                                        ['BBR', 'BBI', 'IDF'], [('ps', bank)])
                                self.cp(TRS[:], PS[bank][:, 0:64], [('ps', bank)], ['TRS'])
                                self.tt(bd[:, q, :].rearrange("p (g n) -> p g n", n=64), TRS[:].unsqueeze(1).to_broadcast([128, 8, 64]), mk, ALU.mult,
                                        ['TRS', 'MASK8'], [('BD', ri)])
                        self.memset(CPR[:], 0.0, [('CP', 0)], eng='pool')
                        self.memset(CPI[:], 0.0, [('CP', 1)], eng='pool')
                        for ri, cp_ in enumerate([CPR, CPI]):
                            for g2 in range(2):
                                bank = 2 + (ri * 2 + g2) % 2
                                for pr in range(16):
                                    g = 2 * pr + g2
                                    self.tr(PS[bank][:].bitcast(BF16)[g2 * 64:(g2 + 1) * 64, pr * 16:(pr + 1) * 16], CC[:, ri, g, :], self.IDB[0:16, 0:16], ['CC', 'IDB'], [('ps', bank)])
                                for pq in range(4):
                                    src = PS[bank][:].bitcast(BF16)[g2 * 64:(g2 + 1) * 64, 0:256].rearrange("p (q r c) -> p q r c", q=4, r=4)[:, :, pq, :]
                                    c0 = (2 * pq + g2) * 16
                                    dst = cp_[g2 * 64:(g2 + 1) * 64, :, c0:c0 + 16].rearrange("p (q r) c -> p q r c", r=4)[:, :, pq, :]
                                    self.act(dst, src, AF.Copy, [('ps', bank)], [('CP', ri)], scale=(1.0 if ri == 0 else -1.0))
                        p.barrier()
                    if self.stop == 's5tab' and d == 0:
                        self.tap("KR", KR[:], [128, 2048], BF16, []); self.tap("KI", KI[:], [128, 2048], BF16, [])
                        self.tap("QR", QR[:], [128, 16, 128], F32, []); self.tap("QI", QI[:], [128, 16, 128], F32, [])
                        self.tap("BDR", BDR[:], [128, 4, 512], BF16, []); self.tap("BDI", BDI[:], [128, 4, 512], BF16, [])
                        self.tap("CPR", CPR[:], [128, 16, 128], BF16, []); self.tap("CPI", CPI[:], [128, 16, 128], BF16, [])
                        return
                    BUR = [D("BUR%d" % i, [128, 512], BF16) for i in range(3)]; BUI = [D("BUI%d" % i, [128, 512], BF16) for i in range(3)]
                    P1 = [D("P1%d" % i, [128, 512], BF16) for i in range(4)]
                    XR = [D("XR%d" % i, [128, 512], BF16) for i in range(3)]; XI = [D("XI%d" % i, [128, 512], BF16) for i in range(3)]
                    HR = [D("HR%d" % i, [128, 4, 128], BF16) for i in range(2)]; HI = [D("HI%d" % i, [128, 4, 128], BF16) for i in range(2)]
                    P2 = [D("P2%d" % i, [128, 4, 128], BF16) for i in range(4)]
                    GFB = [D("GF%d" % i, [128, 2, 4, 128], BF16) for i in range(2)]
                    H0S = [[D("H0S%d%d" % (i, q), [128, 2, 4]) for q in range(4)] for i in range(2)]
                    GB = GFB
                    ZC = D("ZC", [128, 1])
                    self.memset(ZC[:], 0.0, ['ZC'])
                    tinc = 0 if d == 0 else 3
                    lastcol = 127 if d == 0 else 0
                    units = [(step, q) for step in range(NCH) for q in range(4)]

                    def stage_a(ui):
                        step, q = units[ui]
                        ch = order[d][step]
                        tsl = slice(ch * 128, (ch + 1) * 128)
                        s2 = ui % 3
                        ba, bb_ = (0, 1) if ui % 2 == 0 else (6, 7)
                        self.mm(PS[ba][:, :], UT[:, q, tsl], BDR[:, q, :], True, True, [('UT', ch), ('BD', 0)], [('ps', ba)])
                        self.mm(PS[bb_][:, :], UT[:, q, tsl], BDI[:, q, :], True, True, [('UT', ch), ('BD', 1)], [('ps', bb_)])
                        self.act(BUR[s2][:], PS[ba][:, :], AF.Copy, [('ps', ba)], [('BUR', s2)])
                        self.act(BUI[s2][:], PS[bb_][:, :], AF.Copy, [('ps', bb_)], [('BUI', s2)])
                        kr = KR[:, q * 512:(q + 1) * 512]
                        ki = KI[:, q * 512:(q + 1) * 512]
                        self.tt(P1[0][:], kr, BUR[s2][:], ALU.mult, ['KR', ('BUR', s2)], [('P1', 0)])
                        self.tt(P1[2][:], kr, BUI[s2][:], ALU.mult, ['KR', ('BUI', s2)], [('P1', 2)], eng='pool')
                        self.tt(P1[1][:], ki, BUI[s2][:], ALU.mult, ['KI', ('BUI', s2)], [('P1', 1)])
                        self.tt(XR[s2][:], P1[0][:], P1[1][:], ALU.subtract, [('P1', 0), ('P1', 1)], [('XR', s2)])
                        self.tt(P1[3][:], ki, BUR[s2][:], ALU.mult, ['KI', ('BUR', s2)], [('P1', 3)])
                        self.tt(XI[s2][:], P1[2][:], P1[3][:], ALU.add, [('P1', 2), ('P1', 3)], [('XI', s2)], eng='pool')

                    def stage_b(ui):
                        step, q = units[ui]
                        ch = order[d][step]
                        tsl = slice(ch * 128, (ch + 1) * 128)
                        s2 = ui % 2
                        s3 = ui % 3
                        par = step % 2
                        gf = GFB[s2]
                        h0p = H0S[1 - par][q]
                        for pq in range(4):
                            self.mm(PS[2][:, pq * 128:(pq + 1) * 128], XR[s3][:, pq * 128:(pq + 1) * 128], TRIB[:, tinc, :], True, True,
                                    [('XR', s3), 'TRIB'], [('ps', 2)])
                        for pq in range(4):
                            self.mm(PS[3][:, pq * 128:(pq + 1) * 128], XI[s3][:, pq * 128:(pq + 1) * 128], TRIB[:, tinc, :], True, True,
                                    [('XI', s3), 'TRIB'], [('ps', 3)])
                        for pq in range(4):
                            if step == 0:
                                br, bi = ZC[:, 0:1], ZC[:, 0:1]
                                rk = ['ZC']
                            else:
                                br, bi = h0p[:, 0, pq:pq + 1], h0p[:, 1, pq:pq + 1]
                                rk = [('H0S', 1 - par, q)]
                            self.act(HR[s2][:, pq, :], PS[2][:, pq * 128:(pq + 1) * 128], AF.Identity, [('ps', 2)] + rk, [('HR', s2)], bias=br, scale=1.0)
                            self.act(HI[s2][:, pq, :], PS[3][:, pq * 128:(pq + 1) * 128], AF.Identity, [('ps', 3)] + rk, [('HI', s2)], bias=bi, scale=1.0)
                        qr = QR[:, 4 * q:4 * q + 4, :]
                        qi = QI[:, 4 * q:4 * q + 4, :]
                        self.tt(P2[0][:], qr, HR[s2][:], ALU.mult, ['Q', ('HR', s2)], [('P2', 0)])
                        self.tt(P2[2][:], qr, HI[s2][:], ALU.mult, ['Q', ('HI', s2)], [('P2', 2)], eng='pool')
                        self.tt(P2[1][:], qi, HI[s2][:], ALU.mult, ['Q', ('HI', s2)], [('P2', 1)])
                        self.tt(P2[3][:], qi, HR[s2][:], ALU.mult, ['Q', ('HR', s2)], [('P2', 3)], eng='pool')
                        self.tt(gf[:, 0], P2[0][:], P2[1][:], ALU.subtract, [('P2', 0), ('P2', 1)], [('GF', s2, 0)])
                        self.tt(gf[:, 1], P2[2][:], P2[3][:], ALU.add, [('P2', 2), ('P2', 3)], [('GF', s2, 1)], eng='pool')
                        self.act(H0S[par][q][:], gf[:, :, :, lastcol], AF.Copy, [('GF', s2, 0), ('GF', s2, 1)], [('H0S', par, q)])

                    def stage_c(ui):
                        step, q = units[ui]
                        ch = order[d][step]
                        tsl = slice(ch * 128, (ch + 1) * 128)
                        s2 = ui % 2
                        if ch >= 2:
                            yb_ = 4 + step % 2
                            for pq in range(4):
                                pr = 4 * q + pq
                                self.mm(PS[yb_][:, q * 128:(q + 1) * 128], CPR[:, pr, :], GB[s2][:, 0, pq, :], q == 0 and pq == 0, False,
                                        [('CP', 0), ('GF', s2, 0)], [('ps', yb_)], skip_group_check=True)
                                self.mm(PS[yb_][:, q * 128:(q + 1) * 128], CPI[:, pr, :], GB[s2][:, 1, pq, :], False, q == 3 and pq == 3,
                                        [('CP', 1), ('GF', s2, 1)], [('ps', yb_)], skip_group_check=True)
                            if q == 3:
                                yv = YT[:, :, tsl]
                                pv = PS[yb_][:, :].rearrange("p (q t) -> p q t", t=128)
                                if d == 0:
                                    self.cp(yv, pv, [('ps', yb_)], [('YT', ch)])
                                else:
                                    self.tt(yv, pv, yv, ALU.add, [('ps', yb_), ('YT', ch)], [('YT', ch)])

                    stage_a(0)
                    stage_a(1)
                    for ui in range(len(units)):
                        if ui + 2 < len(units):
                            stage_a(ui + 2)
                        stage_b(ui)
                        if ui >= 1:
                            stage_c(ui - 1)
                    stage_c(len(units) - 1)
                    p.barrier()
            if self.stop == 's5':
                return
            with self.scope() as so:
                O = lambda name, shape, dt=F32: self.sb(so, name, shape, dt)
                GLW = O("GLW", [128, 4, 512], BF16); STG = O("STG", [128, 128]); PR1 = O("PR1", [128, 8])
                TT_ = [O("TTo%d" % i, [128, 512]) for i in range(2)]; T2_ = [O("T2o%d" % i, [128, 512]) for i in range(2)]
                GTt = O("GTt", [128, 4, 512], BF16); SGo = [O("SGo%d" % i, [128, 512], BF16) for i in range(2)]
                self.dmas('pool', [(GLW[:], self.s5_glu_w[0].rearrange("(k p) n -> p k n", p=128))], writes=['GLW'])
                self.memset(STG[:], 0.0, ['STG'])
                self.dmas('sp', [(STG[0:4, :], self.s5_d[0].rearrange("(k p) -> k p", p=128)),
                                 (STG[4:8, :], self.s5_glu_b[0].rearrange("(k p) -> k p", p=128))], reads=['STG'], writes=['STG'])
                self.tr(PS[0][:, 0:128], STG[:], self.IDF[:], ['STG', 'IDF'], [('ps', 0)])
                self.cp(PR1[:], PS[0][:, 0:8], [('ps', 0)], ['PR1'])
                for ti, (t0, n) in enumerate(TILES[1:]):
                    yk = tk('YT', t0, n)
                    for c in range(4):
                        s2 = c % 2
                        xg = TT_[s2]
                        self.stt(xg[:], UT[:, c, t0:t0 + n], PR1[:, c:c + 1], YT[:, c, t0:t0 + n], ALU.mult, ALU.add, tk('UT', t0, n) + yk + ['PR1'], [('TTo', s2)])
                        self.tt(T2_[s2][:], xg[:], xg[:], ALU.mult, [('TTo', s2)], [('T2o', s2)], eng='pool')
                        self.ts(T2_[s2][:], T2_[s2][:], 0.044715, 1.0, ALU.mult, ALU.add, [('T2o', s2)], [('T2o', s2)])
                        self.tt(T2_[s2][:], T2_[s2][:], xg[:], ALU.mult, [('T2o', s2), ('TTo', s2)], [('T2o', s2)], eng='pool')
                        self.act(T2_[s2][:], T2_[s2][:], AF.Sigmoid, [('T2o', s2)], [('T2o', s2)], scale=1.5957691)
                        self.tt(GTt[:, c, :], xg[:], T2_[s2][:], ALU.mult, [('TTo', s2), ('T2o', s2)], [('GTt', c)])
                    for c in range(4):
                        bank = 1 + c % 2
                        for k in range(4):
                            self.mm(PS[bank][:, :n], GLW[:, k, c * 128:(c + 1) * 128], GTt[:, k, :], k == 0, k == 3, ['GLW', ('GTt', k)], [('ps', bank)])
                        self.act(SGo[c % 2][:], PS[bank][:, :n], AF.Sigmoid, [('ps', bank), 'PR1'], [('SGo', c % 2)], bias=PR1[:, 4 + c:5 + c], scale=1.0)
                        self.tt(self.MIXT[:, c, t0:t0 + n], GTt[:, c, :], SGo[c % 2][:], ALU.mult, [('GTt', c), ('SGo', c % 2)], yk + tk('MIXT', t0, n))
                p.barrier()

    def ssd(self, b):
        p, PS, nc = self.p, self.PS, self.nc
        w_in = self.od_w_in[0].rearrange("(k p) n -> p k n", p=128)
        order = [list(range(NCH)), [1, 0] + list(range(NCH - 1, 1, -1))]
        with self.scope() as ph:
            P = lambda name, shape, dt=F32: self.sb(ph, name, shape, dt)
            XTOK = P("XTOK", [128, NCH, 768], BF16); YS = P("YS", [128, NCH, 512], BF16)
            DT = P("DTs", [128, NCH, 16]); DA = P("DAs", [128, NCH, 16]); LNDT = P("LNDT", [128, NCH, 16])
            GJ = P("GJs", [128, NCH, 2, 8]); BIASD = P("BIASD", [128, NCH, 2, 8]); WE = P("WEs", [128, NCH, 2, 8]); DEC = P("DECs", [128, NCH, 2, 8])
            PRM = P("PRMs", [128, 48]); DTB = P("DTBs", [128, 16]); AN = P("ANs", [128, 16]); DSK = P("DSK", [128, 8]); GN = P("GNs", [128, 512])
            ONEC = P("ONECs", [128, 1]); WDT = P("WDT", [128, 8, 16], BF16); TRIB = P("TRIBs", [128, 6, 128], BF16)
            markA = self.alo
            BCT = P("BCT", [128, 4, NT], BF16)
            self.memset(ONEC[:], 1.0, ['ONEC'])
            self.dmas('pool', [(WDT[:], w_in[:, :, 2048:2064]), (TRIB[:], self.k_tri.rearrange("a p n -> p a n"))], writes=['WDT', 'TRIB'])
            self.dmas('sp', [(DTB[:], self.ssd_dt_bias[0].rearrange("d h -> (d h)").partition_broadcast(128)),
                             (AN[:], self.ssd_a_log[0].rearrange("d h -> (d h)").partition_broadcast(128)),
                             (DSK[:], self.ssd_d[0].partition_broadcast(128)),
                             (GN[:], self.ssd_norm[0].partition_broadcast(128))], writes=['DTB', 'AN', 'DSK', 'GN'])
            self.act(AN[:], AN[:], AF.Exp, ['AN'], ['AN'])
            self.ts(AN[:], AN[:], -1.0, None, ALU.mult, None, ['AN'], ['AN'])
            with self.scope() as cs:
                C_ = lambda name, shape, dt=F32: self.sb(cs, name, shape, dt)
                STG = C_("STGc", [128, 128]); RAW = C_("RAW", [128, NT]); AC = C_("AC", [128, NT]); XFM = C_("XFM", [128, NT], BF16)
                WX = [C_("WX%d" % i, [128, 8, 128], BF16) for i in range(2)]
                self.memset(STG[:], 0.0, ['STG'])
                self.dmas('sp', [(STG[0:24, :], self.ssd_conv_w[0].rearrange("j (k p) -> (j k) p", p=128)),
                                 (STG[24:32, :], self.ssd_conv_b[0].rearrange("(k p) -> k p", p=128))], reads=['STG'], writes=['STG'])
                self.tr(PS[0][:, 0:128], STG[:], self.IDF[:], ['STG', 'IDF'], [('ps', 0)])
                self.cp(PRM[:, 0:32], PS[0][:, 0:32], [('ps', 0)], ['PRM'])
                for ch in range(NCH):
                    bank = 1 + ch % 2
                    hk = tk('HT', ch * 128, 128)
                    for k in range(8):
                        self.mm(PS[bank][:, 0:16], self.HT[:, k, ch * 128:(ch + 1) * 128], WDT[:, k, :], k == 0, k == 7, hk + ['WDT'], [('ps', bank)])
                    self.tt(DT[:, ch, :], PS[bank][:, 0:16], DTB[:], ALU.add, [('ps', bank), 'DTB'], [('DT', ch)])
                    self.act(DT[:, ch, :], DT[:, ch, :], AF.Exp, [('DT', ch)], [('DT', ch)])
                    self.act(DT[:, ch, :], DT[:, ch, :], AF.Ln, [('DT', ch)], [('DT', ch)], bias=ONEC[:, 0:1], scale=1.0)
                    self.act(LNDT[:, ch, :], DT[:, ch, :], AF.Ln, [('DT', ch)], [('LNDT', ch)])
                    self.tt(DA[:, ch, :], DT[:, ch, :], AN[:], ALU.mult, [('DT', ch), 'AN'], [('DA', ch)])
                    for d in range(2):
                        bk = 3 + d
                        rhs = DA[:, ch, d * 8:(d + 1) * 8]
                        self.mm(PS[bk][:, 0:8], self.TRI[:, 3 * d + 0, :], rhs, True, True, [('DA', ch), 'TRI'], [('ps', bk)])
                        self.mm(PS[bk][:, 8:16], self.TRI[:, 3 * d + 1, :], rhs, True, True, [('DA', ch), 'TRI'], [('ps', bk)])
                        self.mm(PS[bk][:, 16:24], self.ONESF[:], rhs, True, True, [('DA', ch), 'ONESF'], [('ps', bk)])
                        li = LNDT[:, ch, d * 8:(d + 1) * 8]
                        self.act(GJ[:, ch, d, :], PS[bk][:, 0:8], AF.Exp, [('ps', bk)], [('GJ', ch, d)])
                        self.tt(BIASD[:, ch, d, :], li, PS[bk][:, 0:8], ALU.subtract, [('ps', bk), ('LNDT', ch)], [('BIASD', ch, d)])
                        self.tt(WE[:, ch, d, :], li, PS[bk][:, 8:16], ALU.add, [('ps', bk), ('LNDT', ch)], [('WE', ch, d)])
                        self.act(WE[:, ch, d, :], WE[:, ch, d, :], AF.Exp, [('WE', ch, d)], [('WE', ch, d)])
                        self.act(DEC[:, ch, d, :], PS[bk][:, 16:24], AF.Exp, [('ps', bk)], [('DEC', ch, d)])
                for k8 in range(8):
                    wx = WX[k8 % 2]
                    self.dmas('pool', [(wx[:], w_in[:, :, 1024 + k8 * 128:1024 + (k8 + 1) * 128])], writes=[('WX', k8 % 2)])
                    for ti, (t0, n) in enumerate(TILES):
                        bank = 5 + ti % 3
                        hk = tk('HT', t0, n)
                        for k in range(8):
                            self.mm(PS[bank][:, :n], wx[:, k, :], self.HT[:, k, t0:t0 + n], k == 0, k == 7, hk + [('WX', k8 % 2)], [('ps', bank)])
                        self.act(RAW[:, t0:t0 + n], PS[bank][:, :n], AF.Copy, [('ps', bank)], ['RAW'])
                    w0, w1, w2, cb = PRM[:, k8:k8 + 1], PRM[:, 8 + k8:9 + k8], PRM[:, 16 + k8:17 + k8], PRM[:, 24 + k8:25 + k8]
                    self.act(AC[:], RAW[:], AF.Identity, ['RAW', 'PRM'], ['AC'], bias=cb, scale=w1)
                    for (a0, a1) in [(0, 256), (256, NT)]:
                        self.stt(AC[:, a0 + 1:a1], RAW[:, a0:a1 - 1], w0, AC[:, a0 + 1:a1], ALU.mult, ALU.add, ['RAW', 'AC', 'PRM'], ['AC'])
                        self.stt(AC[:, a0:a1 - 1], RAW[:, a0 + 1:a1], w2, AC[:, a0:a1 - 1], ALU.mult, ALU.add, ['RAW', 'AC', 'PRM'], ['AC'])
                    dstfm = XFM[:] if k8 < 4 else BCT[:, k8 - 4, :]
                    dkey = ['XFM'] if k8 < 4 else [('BCT', k8 - 4)]
                    self.act(dstfm, AC[:], AF.Silu, ['AC'], dkey)
                    if k8 < 6:
                        for c4 in range(0, NCH, 4):
                            nn = min(4, NCH - c4)
                            bank = (c4 // 4) % 2
                            pv = PS[bank][:].bitcast(BF16)
                            for i in range(nn):
                                ch = c4 + i
                                self.tr(pv[:, i * 128:(i + 1) * 128], dstfm[:, ch * 128:(ch + 1) * 128], self.IDB[:], dkey + ['IDB'], [('ps', bank)])
                            self.act(XTOK[:, c4:c4 + nn, k8 * 128:(k8 + 1) * 128], pv[:, 0:nn * 128].rearrange("p (a t) -> p a t", t=128), AF.Copy,
                                     [('ps', bank)], [('XTOK', c4 + i) for i in range(nn)])
                p.barrier()
            if self.stop == 'ssdprep':
                self.tap("XTOK", XTOK[:], [128, NCH, 768], BF16, []); self.tap("BCT", BCT[:], [128, 4, NT], BF16, [])
                self.tap("DTs", DT[:], [128, NCH, 16], F32, [])
                return
            STALL = P("STALL", [128, NCH, 2, 128], BF16)
            HS = [P("HSs%d" % d, [128, 8, 64]) for d in range(2)]; HSB = [P("HSB%d" % d, [128, 8, 64], BF16) for d in range(2)]
            LFB = [P("LFBs%d" % i, [128, 128]) for i in range(4)] * 2; DTm = [P("DTm%d" % i, [128, 128], BF16) for i in range(8)]
            PT = [P("PTs%d" % i, [128, 128], BF16) for i in range(32)]
            XW = [P("XWs0", [128, 8, 64], BF16)] * 2; TBs = [P("TBs0", [128, 8, 64])] * 2
            for ch in range(NCH):
                bank = ch % 2
                tsl = slice(ch * 128, (ch + 1) * 128)
                for g in range(2):
                    self.mm(PS[bank][:, g * 128:(g + 1) * 128], BCT[:, g, tsl], BCT[:, 2 + g, tsl], True, True, [('BCT', g), ('BCT', 2 + g)], [('ps', bank)])
                self.act(STALL[:, ch, :, :], PS[bank][:, 0:256].rearrange("p (g t) -> p g t", t=128), AF.Copy, [('ps', bank)], [('STALL', ch)])
            self.memset(YS[:], 0.0, [('YS', c) for c in range(NCH)])
            for d in range(2):
                self.memset(HS[d][:], 0.0, [('HS', d)])
                self.memset(HSB[d][:], 0.0, [('HSB', d)], eng='pool')
            def ssd_front(step):
                chs = [order[d][step] for d in range(2)]
                for rnd in range(2):
                    for d in range(2):
                        ch = chs[d]
                        if ch < 2:
                            continue
                        ya = 2 + d * 3
                        db = d
                        hs_ = list(range(4 * rnd, 4 * rnd + 4))
                        sl = lambda h: d * 4 + (h % 4)
                        for h in hs_:
                            self.act(LFB[sl(h)][:], self.ONESF[:], AF.Identity, [('DA', ch), 'ONESF'], [('LFB', sl(h) % 4)], scale=DA[:, ch, d * 8 + h:d * 8 + h + 1])
                        for h in hs_:
                            c0 = (h % 4) * 128
                            self.mm(PS[db][:, c0:c0 + 128], LFB[sl(h)][:], self.TRI[:, 3 * d + 0, :], True, False, [('LFB', sl(h) % 4), 'TRI'], [('ps', db)], skip_group_check=True)
                            self.mm(PS[db][:, c0:c0 + 128], self.IDF[:], self.TRI[:, 3 * d + 2, :], False, True, ['IDF', 'TRI'], [('ps', db)], skip_group_check=True)
                        for h in hs_:
                            c0 = (h % 4) * 128
                            self.act(DTm[sl(h)][:], PS[db][:, c0:c0 + 128], AF.Exp, [('ps', db), ('BIASD', ch, d)], [('DTm', sl(h))], bias=BIASD[:, ch, d, h:h + 1], scale=1.0)
                        for h in hs_:
                            pi = (step % 2) * 16 + d * 8 + h
                            self.tt(PT[pi][:], STALL[:, ch, h // 4, :], DTm[sl(h)][:], ALU.mult, [('STALL', ch), ('DTm', sl(h))], [('PT', d, pi)], eng='pool')

            def ssd_back(step):
                chs = [order[d][step] for d in range(2)]
                for d in range(2):
                    ch = chs[d]
                    tsl = slice(ch * 128, (ch + 1) * 128)
                    ya, yb, ub = 2 + d * 3, 3 + d * 3, 4 + d * 3
                    if ch >= 2:
                        for h in range(8):
                            pi = (step % 2) * 16 + d * 8 + h
                            self.mm(PS[ya][:, h * 64:(h + 1) * 64], PT[pi][:], XTOK[:, ch, h * 64:(h + 1) * 64], h == 0, h == 7, [('PT', d, pi), ('XTOK', ch)], [('ps', ya)],
                                    skip_group_check=True)
                        for g in range(2):
                            self.mm(PS[yb][:, g * 256:(g + 1) * 256], BCT[:, 2 + g, tsl], HSB[d][:, 4 * g:4 * g + 4, :].rearrange("p a c -> p (a c)"), True, True,
                                    [('BCT', 2 + g), ('HSB', d)], [('ps', yb)])
                    self.tt(XW[d][:], XTOK[:, ch, 0:512].rearrange("p (h c) -> p h c", c=64), WE[:, ch, d, :].unsqueeze(2).to_broadcast([128, 8, 64]), ALU.mult,
                            [('XTOK', ch), ('WE', ch, d)], [('XW', 0)])
                    for g in range(2):
                        self.mm(PS[ub][:, g * 256:(g + 1) * 256], XTOK[:, ch, 512 + g * 128:512 + (g + 1) * 128], XW[d][:, 4 * g:4 * g + 4, :].rearrange("p a c -> p (a c)"),
                                True, True, [('XTOK', ch), ('XW', 0)], [('ps', ub)])
                    self.tt(HS[d][:], HS[d][:], DEC[:, ch, d, :].unsqueeze(2).to_broadcast([128, 8, 64]), ALU.mult, [('HS', d), ('DEC', ch, d)], [('HS', d)])
                    self.tt(HS[d][:], PS[ub][:, :].rearrange("p (h c) -> p h c", c=64), HS[d][:], ALU.add, [('ps', ub), ('HS', d)], [('HS', d)])
                    self.act(HSB[d][:], HS[d][:], AF.Copy, [('HS', d)], [('HSB', d)])
                    if ch >= 2:
                        self.tt(TBs[d][:], PS[yb][:, :].rearrange("p (h c) -> p h c", c=64), GJ[:, ch, d, :].unsqueeze(2).to_broadcast([128, 8, 64]), ALU.mult,
                                [('ps', yb), ('GJ', ch, d)], [('TBs', 0)])
                        self.tt(TBs[d][:], PS[ya][:, :].rearrange("p (h c) -> p h c", c=64), TBs[d][:], ALU.add, [('ps', ya), ('TBs', 0)], [('TBs', 0)])
                        ysv = YS[:, ch, :].rearrange("p (h c) -> p h c", c=64)
                        self.tt(ysv, ysv, TBs[d][:], ALU.add, [('YS', ch), ('TBs', 0)], [('YS', ch)], eng='pool')

            ssd_front(0)
            for step in range(NCH):
                if step + 1 < NCH:
                    ssd_front(step + 1)
                ssd_back(step)
            p.barrier()
            if self.stop == 'ssdraw':
                self.tap("YS", YS[:], [128, NCH, 512], BF16, [])
                return
            self.alo = markA
            WZ = P("WZ", [128, 8, 512], BF16)
            YF = [P("YF%d" % i, [128, 512]) for i in range(2)]; SZ = [P("SZ%d" % i, [128, 512]) for i in range(2)]
            JKs = P("JKs", [128, 512]); SSQ = P("SSQs", [128, 2]); OB = [P("OBs%d" % i, [128, 512], BF16) for i in range(2)]
            self.dmas('pool', [(WZ[:, i * 4:(i + 1) * 4, :], w_in[:, i * 4:(i + 1) * 4, 512:1024]) for i in range(2)], writes=['WZ'])
            for ch in range(2, NCH):
                s2 = ch % 2
                zb = ch % 2
                tb = 2 + ch % 2
                hk = tk('HT', ch * 128, 128)
                for k in range(8):
                    self.mm(PS[zb][:, :], self.HT[:, k, ch * 128:(ch + 1) * 128], WZ[:, k, :], k == 0, k == 7, hk + ['WZ'], [('ps', zb)])
                self.act(SZ[s2][:], PS[zb][:, :], AF.Silu, [('ps', zb)], [('SZ', s2)])
                yf = YF[s2]
                self.tt(yf[:].rearrange("p (h c) -> p h c", c=64), XTOK[:, ch, 0:512].rearrange("p (h c) -> p h c", c=64),
                        DSK[:].unsqueeze(2).to_broadcast([128, 8, 64]), ALU.mult, [('XTOK', ch), 'DSK'], [('YF', s2)])
                self.tt(yf[:], yf[:], YS[:, ch, :], ALU.add, [('YF', s2), ('YS', ch)], [('YF', s2)])
                self.tt(yf[:], yf[:], SZ[s2][:], ALU.mult, [('YF', s2), ('SZ', s2)], [('YF', s2)])
                self.act(JKs[:], yf[:], AF.Square, [('YF', s2)], ['JKs'])
                self.p.op('dve', lambda e: e.reduce_sum(out=SSQ[:, 0:1], in_=JKs[:], axis=AX.X), ['JKs'], [('SSQ', 0)])
                self.act(SSQ[:, 0:1], SSQ[:, 0:1], AF.Sqrt, [('SSQ', 0)], [('SSQ', 0)], bias=self.EPSC[:, 0:1], scale=1.0 / 512)
                self.recip(SSQ[:, 1:2], SSQ[:, 0:1], [('SSQ', 0)], [('SSQ', 1)])
                self.stt(OB[s2][:], yf[:], SSQ[:, 1:2], GN[:], ALU.mult, ALU.mult, [('YF', s2), ('SSQ', 1), 'GN'], [('OB', s2)])
                pv = PS[tb][:].bitcast(BF16)
                for j in range(4):
                    self.tr(pv[:, j * 128:(j + 1) * 128], OB[s2][:, j * 128:(j + 1) * 128], self.IDB[:], [('OB', s2), 'IDB'], [('ps', tb)])
                self.act(self.MIXT[:, 4:8, ch * 128:(ch + 1) * 128], pv[:, 0:512].rearrange("p (a t) -> p a t", t=128), AF.Copy, [('ps', tb)], [('MIXT', ch)])
            p.barrier()

def make_consts():
    tri = np.zeros((6, 128, 128), np.float32)
    i = np.arange(128)
    tri[0] = (i[:, None] <= i[None, :])
    tri[1] = (i[:, None] > i[None, :])
    tri[2] = np.where(i[None, :] >= i[:, None], 0.0, -30000.0)
    tri[3] = (i[:, None] >= i[None, :])
    tri[4] = (i[:, None] < i[None, :])
    tri[5] = np.where(i[None, :] <= i[:, None], 0.0, -30000.0)
    t = np.arange(2048)
    row = (t // 64).astype(np.float32)
    col = (t % 64).astype(np.float32)
    inv = (1.0 / (np.float32(10000.0) ** (np.arange(16, dtype=np.float32) / np.float32(16)))).astype(np.float32)
    ar = row[:, None] * inv
    ac = col[:, None] * inv
    cosr, sinr, cosc, sinc = np.cos(ar), np.sin(ar), np.cos(ac), np.sin(ac)
    rope = np.zeros((2, 2048, 64), np.float32)
    rope[0] = np.concatenate([cosr, cosr, cosc, cosc], -1)
    rope[1] = np.concatenate([-sinr, sinr, -sinc, sinc], -1)
    sel = np.zeros((16, 16, 128), np.float32)
    for e in range(16):
        sel[e, e, :] = 1.0
    mask8 = (np.arange(128)[:, None] // 16 == np.arange(8)[None, :]).astype(np.float32)
    pidx = np.arange(128, dtype=np.float32)
    posc = np.stack([pidx + 1, 128 - pidx, -(pidx + 1), -(128 - pidx)], 1).astype(np.float32)
    posr = np.zeros((128, 2, 128), np.float32)
    posr[:, 0, :] = (pidx + 1)[None, :]
    posr[:, 1, :] = (128 - pidx)[None, :]
    return {"k_ident": np.eye(128, dtype=np.float32), "k_tri": tri, "k_rope": rope, "k_sel": sel,
            "k_mask8": mask8, "k_posc": posc, "k_posr": posr}


def make_in_maps(inputs, nb=NB, ncores=NCORES):
    consts = make_consts()
    maps = []
    for ci in range(ncores):
        m = {}
        for k, v in inputs.items():
            v = np.asarray(v)
            if k in ("x", "c", "ctx"):
                m[k] = np.ascontiguousarray(v[ci * nb:(ci + 1) * nb])
            else:
                m[k] = np.ascontiguousarray(v)
        m.update(consts)
        maps.append(m)
    return maps


def kernel(**inputs):
    bld = Builder()
    nc = bld.build()
    maps = make_in_maps(inputs)
    res = run_bass_kernel_spmd(nc, maps, core_ids=list(range(NCORES)))
    return np.concatenate([r["out"] for r in res.results], axis=0).astype(np.float32)
```

```python
import math
import os
import numpy as np
import concourse.bass as bass
import concourse.mybir as mybir
from concourse.bass_utils import run_bass_kernel_spmd
from contextlib import ExitStack

F32 = mybir.dt.float32
BF16 = mybir.dt.bfloat16
I32 = mybir.dt.int32
AF = mybir.ActivationFunctionType
ALU = mybir.AluOpType
AX = mybir.AxisListType

ENGS = ['pe', 'act', 'dve', 'pool', 'sp']
NDMASEM = 40
NCORES = 8
NB = 2
NT = 2304
NCH = 18
EPS = 1e-6
TILES = [(0, 256), (256, 512), (768, 512), (1280, 512), (1792, 512)]


class _Op:
    __slots__ = ('eng', 'fn', 'waits', 'dwaits', 'idx', 'signal', 'isdma', 'dsem', 'dval', 'know')


class Prog:
    def __init__(self, nc):
        self.nc = nc
        self.ops = {e: [] for e in ENGS}
        self.lastw = {}
        self.readers = {}
        self.know = {e: {f: -1 for f in ENGS} for e in ENGS}
        self.dknow = {e: {} for e in ENGS}
        self.dsem_last = [None] * NDMASEM
        self.dsem_val = [0] * NDMASEM
        self.dsem_rr = 0
        self.dsem_rr_sw = 0

    def _dep_tokens(self, reads, writes):
        toks = []
        for r in reads:
            t = self.lastw.get(r)
            if t is not None:
                toks.append(t)
        for w in writes:
            t = self.lastw.get(w)
            if t is not None:
                toks.append(t)
            toks.extend(self.readers.get(w, ()))
        return toks

    def _record(self, op, reads, writes):
        for r in reads:
            self.readers.setdefault(r, []).append(op)
        for w in writes:
            self.lastw[w] = op
            self.readers[w] = []

    def _resolve(self, eng, toks, op, same_ok=False):
        need = {}
        dneed = {}
        for t in toks:
            if t.isdma:
                if self.dknow[eng].get(t.dsem, 0) >= t.dval:
                    continue
                dneed[t.dsem] = max(dneed.get(t.dsem, 0), t.dval)
            else:
                if t.eng == eng and same_ok:
                    continue
                if self.know[eng][t.eng] >= t.idx:
                    continue
                need[t.eng] = max(need.get(t.eng, -1), t.idx)
        for f, j in need.items():
            src = self.ops[f][j]
            src.signal = True
            kn = src.know
            for g in ENGS:
                if kn[g] > self.know[eng][g]:
                    self.know[eng][g] = kn[g]
            if j > self.know[eng][f]:
                self.know[eng][f] = j
        for s, v in dneed.items():
            self.dknow[eng][s] = v
        op.waits = list(need.items())
        op.dwaits = list(dneed.items())

    def op(self, eng, fn, reads=(), writes=(), same_ok=False):
        if eng != 'pe':
            psr = [r for r in reads if isinstance(r, tuple) and r[0] == 'ps']
            if psr:
                writes = list(writes) + psr
        o = _Op()
        o.eng = eng
        o.fn = fn
        o.isdma = False
        o.signal = False
        o.idx = len(self.ops[eng])
        toks = self._dep_tokens(reads, writes)
        self._resolve(eng, toks, o, same_ok=same_ok)
        kn = dict(self.know[eng])
        kn[eng] = o.idx
        o.know = kn
        self.ops[eng].append(o)
        self._record(o, reads, writes)
        return o

    def dma(self, q, fns, reads=(), writes=()):
        if not isinstance(fns, (list, tuple)):
            fns = [fns]
        o = _Op()
        o.eng = q
        o.fn = list(fns)
        o.isdma = True
        o.signal = False
        o.idx = len(self.ops[q])
        half = NDMASEM // 2
        if q == 'pool':
            s = half + self.dsem_rr_sw
            self.dsem_rr_sw = (self.dsem_rr_sw + 1) % (NDMASEM - half)
        else:
            s = self.dsem_rr
            self.dsem_rr = (self.dsem_rr + 1) % half
        toks = self._dep_tokens(reads, writes)
        if self.dsem_last[s] is not None:
            toks.append(self.dsem_last[s])
        self._resolve(q, toks, o)
        o.dsem = s
        self.dsem_val[s] += 16 * len(fns)
        o.dval = self.dsem_val[s]
        self.dsem_last[s] = o
        o.know = dict(self.know[q])
        self.ops[q].append(o)
        self._record(o, reads, writes)
        return o

    def _waitop(self, eng, toks):
        o = _Op()
        o.eng = eng
        o.fn = None
        o.isdma = False
        o.signal = False
        o.idx = len(self.ops[eng])
        self._resolve(eng, toks, o)
        kn = dict(self.know[eng])
        kn[eng] = o.idx
        o.know = kn
        self.ops[eng].append(o)
        return o

    def barrier(self):
        last = {e: (self.ops[e][-1] if self.ops[e] else None) for e in ENGS}
        dtoks = [t for t in self.dsem_last if t is not None]
        for e in ENGS:
            toks = list(dtoks)
            for f in ENGS:
                t = last[f]
                if t is None:
                    continue
                if t.isdma or t.fn is None:
                    j = len(self.ops[f]) - 1
                    while j >= 0 and (self.ops[f][j].isdma or self.ops[f][j].fn is None):
                        j -= 1
                    if j < 0:
                        continue
                    t = self.ops[f][j]
                toks.append(t)
            self._waitop(e, toks)
        self.lastw = {}
        self.readers = {}

    def wait_all_dma(self, eng='sp'):
        toks = [t for t in self.dsem_last if t is not None]
        return self._waitop(eng, toks)

    def emit(self, stack):
        nc = self.nc
        esem = {e: stack.enter_context(nc.semaphore("s_" + e)) for e in ENGS}
        dsem = [stack.enter_context(nc.semaphore("d%d" % i)) for i in range(NDMASEM)]
        semval = {}
        for e in ENGS:
            c = 0
            vals = []
            for o in self.ops[e]:
                if o.signal and not o.isdma and o.fn is not None:
                    c += 1
                vals.append(c)
            semval[e] = vals
        engobj = {'pe': 'tensor', 'act': 'scalar', 'dve': 'vector', 'pool': 'gpsimd', 'sp': 'sync'}
        self.stats = {e: (len(self.ops[e]), sum(len(o.waits) + len(o.dwaits) for o in self.ops[e]),
                          semval[e][-1] if semval[e] else 0) for e in ENGS}
        block = stack.enter_context(nc.Block())

        def body_for(e):
            def body(engine):
                for o in self.ops[e]:
                    for f, j in o.waits:
                        engine.wait_ge(esem[f], semval[f][j])
                    for s, v in o.dwaits:
                        engine.wait_ge(dsem[s], v)
                    if o.fn is None:
                        continue
                    if o.isdma:
                        for fn in o.fn:
                            fn(engine).then_inc(dsem[o.dsem], 16)
                    else:
                        ins = o.fn(engine)
                        if o.signal:
                            ins.then_inc(esem[e], 1)
            return body

        for e in ENGS:
            if self.ops[e]:
                getattr(block, engobj[e])(body_for(e))


class ArenaScope:
    def __init__(self, bld):
        self.bld = bld

    def __enter__(self):
        self.mark = self.bld.alo
        return self

    def __exit__(self, *a):
        self.bld.alo = self.mark
        return False


def tk(name, t0, n):
    return [(name, c) for c in range(t0 // 128, (t0 + n + 127) // 128)]


class Builder:
    def __init__(self, taps=(), nb=NB, stop=None):
        self.taps = set(taps)
        self.nb = nb
        self.stop = stop
        self.tap_specs = {}
        self.nc = bass.Bass("TRN2", target_bir_lowering=False)
        self.p = Prog(self.nc)
        self.dram = {}

    def din(self, name, shape, dt=F32):
        t = self.nc.dram_tensor(name, list(shape), dt, kind="ExternalInput").ap()
        self.dram[name] = t
        return t

    def dout(self, name, shape, dt=F32):
        t = self.nc.dram_tensor(name, list(shape), dt, kind="ExternalOutput").ap()
        self.dram[name] = t
        return t

    def tap(self, name, src_ap, shape, dt, reads):
        if name not in self.taps:
            return
        d = self.dout("tap_" + name, shape, dt)
        self.tap_specs[name] = (shape, dt)
        self.p.dma('sp', lambda e: e.dma_start(out=d, in_=src_ap), reads=reads)

    def dmas(self, q, pairs, reads=(), writes=(), slow=False):
        kw = {'allow_slow_non_contiguous': True} if slow else {}
        fns = [(lambda e, o=o, i=i: e.dma_start(out=o, in_=i, **kw)) for (o, i) in pairs]
        self.p.dma(q, fns, reads=reads, writes=writes)

    def mm(self, out, lhsT, rhs, start, stop, reads, writes, **kw):
        self.p.op('pe', lambda e: e.matmul(out, lhsT=lhsT, rhs=rhs, start=start, stop=stop, **kw),
                  reads, writes, same_ok=True)

    def tr(self, out, in_, ident, reads, writes):
        self.p.op('pe', lambda e: e.transpose(out, in_, ident), reads, writes, same_ok=True)

    def act(self, out, in_, func, reads, writes, bias=None, scale=None, accum_out=None, eng='act'):
        kw = {}
        if bias is not None:
            kw['bias'] = bias
        if scale is not None:
            kw['scale'] = scale
        if accum_out is not None:
            kw['accum_out'] = accum_out
        self.p.op('act', lambda e: e.activation(out=out, in_=in_, func=func, **kw), reads, writes)

    def tt(self, out, in0, in1, op, reads, writes, eng='dve'):
        self.p.op(eng, lambda e: e.tensor_tensor(out=out, in0=in0, in1=in1, op=op), reads, writes)

    def ts(self, out, in0, s1, s2, op0, op1, reads, writes, eng='dve'):
        if op1 is None:
            self.p.op(eng, lambda e: e.tensor_scalar(out=out, in0=in0, scalar1=s1, scalar2=None, op0=op0), reads, writes)
        else:
            self.p.op(eng, lambda e: e.tensor_scalar(out=out, in0=in0, scalar1=s1, scalar2=s2, op0=op0, op1=op1), reads, writes)

    def stt(self, out, in0, scalar, in1, op0, op1, reads, writes):
        self.p.op('dve', lambda e: e.scalar_tensor_tensor(out=out, in0=in0, scalar=scalar, in1=in1, op0=op0, op1=op1),
                  reads, writes)

    def cp(self, out, in_, reads, writes, eng='dve'):
        self.p.op(eng, lambda e: e.tensor_copy(out=out, in_=in_), reads, writes)

    def recip(self, out, in_, reads, writes):
        self.p.op('dve', lambda e: e.reciprocal(out=out, in_=in_), reads, writes)

    def memset(self, ap, val, writes, eng='dve'):
        self.p.op(eng, lambda e: e.memset(ap, val), (), writes)

    def sb(self, st, name, shape, dt):
        if isinstance(st, ArenaScope):
            return self.carve(shape, dt)
        self._uid = getattr(self, '_uid', 0) + 1
        return st.enter_context(self.nc.sbuf_tensor("%s_%d" % (name, self._uid), list(shape), dt))

    def carve(self, shape, dt, high=False):
        nelem = 1
        for d in shape[1:]:
            nelem *= d
        n32 = nelem if dt != BF16 else (nelem + 1) // 2
        n32 = (n32 + 7) // 8 * 8
        if high:
            self.ahi -= n32
            off = self.ahi
        else:
            off = self.alo
            self.alo += n32
        assert self.alo <= self.ahi, "arena overflow: lo=%d hi=%d" % (self.alo, self.ahi)
        ap = self.ARENA[0:shape[0], off:off + n32]
        if dt == BF16:
            ap = ap.bitcast(BF16)
        ap = ap[:, 0:nelem]
        if len(shape) == 3:
            ap = ap.rearrange("p (a b) -> p a b", b=shape[2])
        elif len(shape) == 4:
            ap = ap.rearrange("p (a b c) -> p a b c", b=shape[2], c=shape[3])
        elif len(shape) == 5:
            ap = ap.rearrange("p (a b c d) -> p a b c d", b=shape[2], c=shape[3], d=shape[4])
        return ap

    def scope(self):
        return ArenaScope(self)

    def build(self):
        nc, p = self.nc, self.p
        D = self.din
        nb = self.nb
        x = D("x", [nb, 2048, 1024]); c = D("c", [nb, 1024]); ctx = D("ctx", [nb, 256, 1024]); c_ctx = D("c_ctx", [1024])
        ada_w = D("ada_w", [2, 1024, 6144]); ada_b = D("ada_b", [2, 6144])
        norm_mix = D("norm_mix", [2, 1024]); norm_ffn = D("norm_ffn", [2, 1024])
        self.ev_w_in = D("ev_w_in", [1, 1024, 2832]); self.ev_w_out = D("ev_w_out", [1, 1024, 1024])
        self.ml_gate_b = D("ml_gate_b", [1, 16]); self.ml_norm = D("ml_norm", [1, 512])
        self.at_q_norm = D("at_q_norm", [1, 64]); self.at_k_norm = D("at_k_norm", [1, 64])
        self.od_w_in = D("od_w_in", [1, 1024, 2064]); self.od_w_out = D("od_w_out", [1, 1024, 1024])
        for nm, shp in [("s5_a_re", [1, 2, 32, 64]), ("s5_a_im", [1, 2, 32, 64]), ("s5_log_dt", [1, 2, 32]),
                        ("s5_b_re", [1, 2, 32, 64, 16]), ("s5_b_im", [1, 2, 32, 64, 16]),
                        ("s5_c_re", [1, 2, 32, 16, 64]), ("s5_c_im", [1, 2, 32, 16, 64]),
                        ("s5_d", [1, 512]), ("s5_glu_w", [1, 512, 512]), ("s5_glu_b", [1, 512]),
                        ("ssd_conv_w", [1, 3, 1024]), ("ssd_conv_b", [1, 1024]), ("ssd_dt_bias", [1, 2, 8]),
                        ("ssd_a_log", [1, 2, 8]), ("ssd_d", [1, 8]), ("ssd_norm", [1, 512]),
                        ("moe_gr_w", [2, 1024, 4]), ("moe_gr_b", [2, 4]), ("moe_er_w", [2, 1024, 16]),
                        ("moe_er_b", [2, 16]), ("moe_w_gate", [2, 16, 1024, 512]), ("moe_w_up", [2, 16, 1024, 512]),
                        ("moe_w_down", [2, 16, 512, 1024])]:
            setattr(self, nm, D(nm, shp))
        k_ident = D("k_ident", [128, 128]); k_tri = D("k_tri", [6, 128, 128])
        k_rope = D("k_rope", [2, 2048, 64]); k_sel = D("k_sel", [16, 16, 128])
        self.k_tri = k_tri
        self.k_mask8 = D("k_mask8", [128, 8]); self.k_posc = D("k_posc", [128, 4]); self.k_posr = D("k_posr", [128, 2, 128])
        out = self.dout("out", [nb, 2048, 1024])

        with ExitStack() as st:
            S = lambda name, shape, dt=F32: self.sb(st, name, shape, dt)
            self.IDF = S("IDF", [128, 128]); self.IDB = S("IDB", [128, 128], BF16)
            self.ONESB = S("ONESB", [128, 128], BF16); self.ONESF = S("ONESF", [128, 128])
            self.TRI = S("TRI", [128, 6, 128])
            self.MOD = S("MOD", [128, 2, 48, 3])
            self.GSC = S("GSC", [128, 2, 2, 8, 3])
            self.EPSC = S("EPSC", [128, 1])
            self.memset(self.EPSC[:], EPS, ['EPSC'])
            ARENA_WORDS = 46080
            self.ARENA = S("ARENA", [128, ARENA_WORDS])
            self.alo, self.ahi = 0, ARENA_WORDS
            self.XS = [nc.dram_tensor("xscr%d" % i, [128, 8, NT], F32, kind="Internal").ap() for i in range(nb)]
            self.HT = self.carve([128, 8, NT], BF16)
            self.PS = [st.enter_context(nc.psum_tensor("ps%d" % i, [128, 512], F32)) for i in range(8)]
            PS = self.PS
            p.dma('sp', lambda e: e.dma_start(out=self.IDF[:], in_=k_ident), writes=['IDF'])
            p.dma('pool', lambda e: e.dma_start(out=self.IDB[:], in_=k_ident), writes=['IDB'])
            p.dma('sp', lambda e: e.dma_start(out=self.TRI[:], in_=k_tri.rearrange("a p n -> p a n")), writes=['TRI'])
            self.memset(self.ONESB[:], 1.0, ['ONESB'])
            self.memset(self.ONESF[:], 1.0, ['ONESF'])
            self.k_rope = k_rope; self.k_sel = k_sel

            with self.scope() as ph:
                P = lambda name, shape, dt=F32: self.sb(ph, name, shape, dt)
                STG = P("STG", [128, 128]); FM = P("FM", [128, 128])
                AW = [P("AW%d" % i, [128, 8, 512]) for i in range(2)]
                SC = P("SC", [128, 8, 3])
                self.memset(STG[:], 0.0, ['STG'])
                r_c = 32
                r_cc = 32 + 8 * nb
                p.dma('sp', [lambda e: e.dma_start(out=STG[0:16, :], in_=norm_mix.rearrange("l (k p) -> (l k) p", p=128)),
                             lambda e: e.dma_start(out=STG[16:32, :], in_=norm_ffn.rearrange("l (k p) -> (l k) p", p=128)),
                             lambda e: e.dma_start(out=STG[r_c:r_c + 8 * nb, :], in_=c.rearrange("b (k p) -> (b k) p", p=128)),
                             lambda e: e.dma_start(out=STG[r_cc:r_cc + 8, :], in_=c_ctx.rearrange("(k p) -> k p", p=128))],
                      reads=['STG'], writes=['STG'])
                self.tr(PS[0][:, 0:128], STG[:], self.IDF[:], ['STG', 'IDF'], [('ps', 0)])
                self.cp(FM[:], PS[0][:, 0:128], [('ps', 0)], ['FM'])
                for b in range(nb):
                    self.act(SC[:, :, b], FM[:, r_c + 8 * b:r_c + 8 * b + 8], AF.Silu, ['FM'], [('SC', b)])
                self.act(SC[:, :, 2], FM[:, r_cc:r_cc + 8], AF.Silu, ['FM'], [('SC', 2)])
                if nb == 1:
                    self.cp(SC[:, :, 1], SC[:, :, 0], [('SC', 0)], [('SC', 1)])
                STG2 = P("STG2", [128, 128]); ADAB = P("ADAB", [128, 96])
                self.memset(STG2[:], 0.0, ['STG2'])
                p.dma('sp', lambda e: e.dma_start(out=STG2[0:96, :], in_=ada_b.rearrange("l (j p) -> (l j) p", p=128)),
                      reads=['STG2'], writes=['STG2'])
                self.tr(PS[1][:, 0:128], STG2[:], self.IDF[:], ['STG2', 'IDF'], [('ps', 1)])
                self.cp(ADAB[:], PS[1][:, 0:96], [('ps', 1)], ['ADAB'])
                screads = [('SC', 0), ('SC', 1), ('SC', 2)]
                for l in range(2):
                    for pc in range(12):
                        slot = pc % 2
                        aw = AW[slot]
                        src = ada_w[l].rearrange("(k p) n -> p k n", p=128)
                        p.dma('sp' if slot == 0 else 'act',
                              [lambda e, src=src, aw=aw, pc=pc, h=h: e.dma_start(out=aw[:, h * 4:(h + 1) * 4, :], in_=src[:, h * 4:(h + 1) * 4, pc * 512:(pc + 1) * 512])
                               for h in range(2)], writes=[('AW', slot)])
                        for j in range(4):
                            col = (pc * 4 + j) * 3
                            for k in range(8):
                                self.mm(PS[2 + l][:, col:col + 3], aw[:, k, j * 128:(j + 1) * 128], SC[:, k, :],
                                        k == 0, k == 7, [('AW', slot)] + screads, [('ps', 2 + l)])
                    self.tt(self.MOD[:, l], PS[2 + l][:, 0:144].rearrange("p (j t) -> p j t", t=3),
                            ADAB[:, l * 48:(l + 1) * 48].unsqueeze(2).to_broadcast([128, 48, 3]), ALU.add,
                            [('ps', 2 + l), 'ADAB'], [('MOD', l)])
                    for w in range(2):
                        mi = 1 if w == 0 else 4
                        g_ap = FM[:, 16 * w + 8 * l:16 * w + 8 * l + 8]
                        gb = g_ap.unsqueeze(2).to_broadcast([128, 8, 3])
                        self.tt(self.GSC[:, l, w], self.MOD[:, l, mi * 8:(mi + 1) * 8, :], gb, ALU.mult,
                                [('MOD', l), 'FM'], [('GSC', l, w)])
                        self.tt(self.GSC[:, l, w], self.GSC[:, l, w], gb, ALU.add,
                                [('GSC', l, w), 'FM'], [('GSC', l, w)])
                self.tap("mod", self.MOD[:], [128, 2, 48, 3], F32, [('MOD', 0), ('MOD', 1)])
                p.barrier()
            if self.stop == 'mod':
                return self.finish(st)

            for b in range(nb):
                self.run_batch(st, b, x, ctx, out)
                if self.stop is not None:
                    break
            return self.finish(st)

    def finish(self, st):
        self.p.wait_all_dma('sp')
        self.p.emit(st)
        return self.nc

    def load_x(self, b, x, ctx):
        p, PS = self.p, self.PS
        with self.scope() as ph:
            XT = [self.sb(ph, "XT%d" % i, [128, 1024], F32) for i in range(3)]
            for ch in range(NCH):
                s = ch % 3
                src = ctx[b, ch * 128:(ch + 1) * 128, :] if ch < 2 else x[b, (ch - 2) * 128:(ch - 1) * 128, :]
                p.dma('sp' if ch % 2 == 0 else 'act', lambda e, s=s, src=src: e.dma_start(out=XT[s][:], in_=src), writes=[('XT', s)])
                for hf in range(2):
                    bank = (ch * 2 + hf) % 4
                    for kk in range(4):
                        k = hf * 4 + kk
                        self.tr(PS[bank][:, kk * 128:(kk + 1) * 128], XT[s][:, k * 128:(k + 1) * 128], self.IDF[:],
                                [('XT', s), 'IDF'], [('ps', bank)])
                    dst = self.X[:, hf * 4:(hf + 1) * 4, ch * 128:(ch + 1) * 128]
                    srcp = PS[bank][:].rearrange("p (k t) -> p k t", t=128)
                    if hf == 0:
                        self.act(dst, srcp, AF.Copy, [('ps', bank)], [('X', ch)])
                    else:
                        self.cp(dst, srcp, [('ps', bank)], [('X', ch)])
            p.barrier()

    def norm(self, b, l, w, f32_cb=None, skip_ctx=False):
        p, PS = self.p, self.PS
        mi = 0 if w == 0 else 3
        with self.scope() as ph:
            SQ = [self.sb(ph, "SQ%d" % i, [128, 512], BF16) for i in range(3)]
            SD = self.sb(ph, "SD", [128, 512], F32)
            RS = self.sb(ph, "RS", [128, 512], F32)
            TM = [self.sb(ph, "TM%d" % i, [128, 512], F32) for i in range(2)]
            H32 = self.sb(ph, "H32", [128, 8, 512], F32) if f32_cb is not None else None
            cnt = 0
            for ti, (t0, n) in enumerate(TILES):
                if skip_ctx and ti == 0:
                    continue
                j = 2 if ti == 0 else b
                xk = tk('X', t0, n)
                bank = ti % 2
                for k in range(8):
                    s = cnt % 3
                    cnt += 1
                    self.act(SQ[s][:, :n], self.X[:, k, t0:t0 + n], AF.Square, xk, [('SQ', s)])
                    self.mm(PS[bank][:, :n], self.ONESB[:], SQ[s][:, :n], k == 0, k == 7, [('SQ', s), 'ONESB'], [('ps', bank)])
                self.act(SD[:, :n], PS[bank][:, :n], AF.Sqrt, [('ps', bank)], ['SD'], bias=self.EPSC[:, 0:1], scale=1.0 / 1024)
                self.recip(RS[:, :n], SD[:, :n], ['SD'], ['RS'])
                for k in range(8):
                    s = k % 2
                    self.stt(TM[s][:, :n], self.X[:, k, t0:t0 + n], self.GSC[:, l, w, k, j:j + 1], RS[:, :n], ALU.mult, ALU.mult,
                             xk + ['RS', ('GSC', l, w)], [('TM', s)])
                    if f32_cb is not None:
                        self.act(H32[:, k, :n], TM[s][:, :n], AF.Identity, [('TM', s)], [('H32', k)],
                                 bias=self.MOD[:, l, mi * 8 + k, j:j + 1], scale=1.0)
                        self.cp(self.HT[:, k, t0:t0 + n], H32[:, k, :n], [('H32', k)], tk('HT', t0, n), eng='pool')
                    else:
                        self.act(self.HT[:, k, t0:t0 + n], TM[s][:, :n], AF.Identity, [('TM', s)], tk('HT', t0, n),
                                 bias=self.MOD[:, l, mi * 8 + k, j:j + 1], scale=1.0)
                if f32_cb is not None:
                    f32_cb(ti, t0, n, H32)
            p.barrier()

    def x_alloc(self):
        self._ahi_mark = self.ahi
        self.X = self.carve([128, 8, NT], F32, high=True)

    def x_free(self):
        self.ahi = self._ahi_mark

    def x_spill(self, b):
        p = self.p
        p.dma('sp', [lambda e, k=k: e.dma_start(out=self.XS[b][:, 2 * k:2 * k + 2, :], in_=self.X[:, 2 * k:2 * k + 2, :]) for k in range(4)],
              reads=tk('X', 0, NT), writes=['XS'])
        p.barrier()

    def x_reload(self, b):
        p = self.p
        p.dma('sp', [lambda e, k=k: e.dma_start(out=self.X[:, 2 * k:2 * k + 2, :], in_=self.XS[b][:, 2 * k:2 * k + 2, :]) for k in range(4)],
              reads=['XS'], writes=tk('X', 0, NT))

    def run_batch(self, st, b, x, ctx, out):
        p = self.p
        self.x_alloc()
        self.load_x(b, x, ctx)
        if self.stop == 'loadx':
            self.tap("X", self.X[:], [128, 8, NT], F32, tk('X', 0, NT))
            return
        skipl0 = bool(os.environ.get('SKIPL0'))
        if not skipl0:
            self.norm(b, 0, 0)
        if self.stop == 'h0':
            self.tap("h0", self.HT[:], [128, 8, NT], BF16, tk('HT', 0, NT))
            return
        if not skipl0:
            self.layer0(b)
            if self.stop is not None and self.stop in ('mlstm', 'attn', 'xmid0', 'xout0', 'mlgate', 'mlproj', 'mlloop'):
                return
        self.layer1(b, out)

    def layer0(self, b):
        p = self.p
        self.x_spill(b)
        self.x_free()
        with self.scope() as lay:
            self.MIXT = self.carve([128, 8, NT], BF16)
            if 'ml' not in os.environ.get('SKIPMIX', ''):
                self.mlstm(b)
            if self.stop == 'mlstm':
                self.tap("mix0", self.MIXT[:], [128, 8, NT], BF16, [])
                return
            self.attention(b)
            if self.stop == 'attn':
                self.tap("mix0", self.MIXT[:], [128, 8, NT], BF16, [])
                return
            self.tap("mix0", self.MIXT[:], [128, 8, NT], BF16, [])
            self.x_alloc()
            self.x_reload(b)
            self.outproj(b, 0, self.ev_w_out[0])
            self.tap("xmid0", self.X[:], [128, 8, NT], F32, [])
        if self.stop == 'xmid0':
            self.tap("X", self.X[:], [128, 8, NT], F32, tk('X', 0, NT))
            return
        if 'moe0' not in os.environ.get('SKIPMIX', ''):
            self.moe(b, 0)
        if self.stop == 'xout0':
            self.tap("X", self.X[:], [128, 8, NT], F32, tk('X', 0, NT))
            return

    def layer1(self, b, out):
        p = self.p
        self.norm(b, 1, 0)
        self.tap("h1", self.HT[:], [128, 8, NT], BF16, [])
        self.x_spill(b)
        self.x_free()
        with self.scope() as lay:
            self.MIXT = self.carve([128, 8, NT], BF16)
            if 's5' not in os.environ.get('SKIPMIX', ''):
                self.s5(b)
            if self.stop in ('s5', 's5tab'):
                if self.stop == 's5':
                    self.tap("s5y", self.S5_YT[:], [128, 4, NT], BF16, [])
                return
            self.ssd(b)
            if self.stop in ('ssdprep', 'ssdraw'):
                return
            self.tap("mix1", self.MIXT[:], [128, 8, NT], BF16, [])
            if self.stop == 'mix1':
                return
            self.x_alloc()
            self.x_reload(b)
            self.outproj(b, 1, self.od_w_out[0])
        self.tap("xmid1", self.X[:], [128, 8, NT], F32, [])
        if self.stop == 'xmid1':
            return
        self.moe(b, 1)
        self.tap("xout1", self.X[:], [128, 8, NT], F32, [])
        self.write_out(b, out)
        self.x_free()

    def mlstm(self, b):
        p, PS, nc = self.p, self.PS, self.nc
        w_in = self.ev_w_in[0].rearrange("(k p) n -> p k n", p=128)
        order = [list(range(NCH)), [1, 0] + list(range(NCH - 1, 1, -1))]
        with self.scope() as ph:
            P = lambda name, shape, dt=F32: self.sb(ph, name, shape, dt)
            GT = P("GT", [128, NCH, 16]); LF = P("LF", [128, NCH, 8]); GB = P("GB", [128, 16])
            GJ = P("GJ", [128, NCH, 2, 4]); BIAS = P("BIAS", [128, NCH, 2, 4]); WE = P("WE", [128, NCH, 2, 4]); DEC = P("DEC", [128, NCH, 2, 4])
            MLN = P("MLN", [128, 512]); WG = P("WG", [128, 8, 16], BF16); ONEC = P("ONEC", [128, 1])
            ET = P("ET", [128, 8]); T12 = P("T12", [128, 2, 4])
            self.memset(ONEC[:], 1.0, ['ONEC'])
            p.dma('sp', [lambda e: e.dma_start(out=GB[:], in_=self.ml_gate_b[0].partition_broadcast(128)),
                         lambda e: e.dma_start(out=MLN[:], in_=self.ml_norm[0].partition_broadcast(128))], writes=['GB', 'MLN'])
            p.dma('pool', lambda e: e.dma_start(out=WG[:], in_=w_in[:, :, 2048:2064]), writes=['WG'])
            for ch in range(NCH):
                bank = ch % 2
                hk = tk('HT', ch * 128, 128)
                for k in range(8):
                    self.mm(PS[bank][:, 0:16], self.HT[:, k, ch * 128:(ch + 1) * 128], WG[:, k, :], k == 0, k == 7,
                            hk + ['WG'], [('ps', bank)])
                self.tt(GT[:, ch, :], PS[bank][:, 0:16], GB[:], ALU.add, [('ps', bank), 'GB'], [('GT', ch)])
                gv = GT[:, ch, :].rearrange("p (d g h) -> p d g h", d=2, g=2)
                lfv = LF[:, ch, :].rearrange("p (d h) -> p d h", d=2)
                etv = ET[:].rearrange("p (d h) -> p d h", d=2)
                self.act(etv, gv[:, :, 1, :], AF.Exp, [('GT', ch)], ['ET'], scale=-1.0)
                self.act(etv, etv, AF.Ln, ['ET'], ['ET'], bias=ONEC[:, 0:1], scale=1.0)
                self.ts(lfv, etv, -1.0, None, ALU.mult, None, ['ET'], [('LF', ch)])
                for d in range(2):
                    bk = 2 + d
                    rhs = LF[:, ch, d * 4:(d + 1) * 4]
                    self.mm(PS[bk][:, 0:4], self.TRI[:, 3 * d + 0, :], rhs, True, True, [('LF', ch), 'TRI'], [('ps', bk)])
                    self.mm(PS[bk][:, 4:8], self.TRI[:, 3 * d + 1, :], rhs, True, True, [('LF', ch), 'TRI'], [('ps', bk)])
                    self.mm(PS[bk][:, 8:12], self.ONESF[:], rhs, True, True, [('LF', ch), 'ONESF'], [('ps', bk)])
                    li = gv[:, d, 0, :]
                    self.act(GJ[:, ch, d, :], PS[bk][:, 0:4], AF.Exp, [('ps', bk)], [('GJ', ch, d)])
                    self.tt(BIAS[:, ch, d, :], li, PS[bk][:, 0:4], ALU.subtract, [('ps', bk), ('GT', ch)], [('BIAS', ch, d)])
                    self.tt(T12[:, d, :], li, PS[bk][:, 4:8], ALU.add, [('ps', bk), ('GT', ch)], [('T12', d)])
                    self.act(WE[:, ch, d, :], T12[:, d, :], AF.Exp, [('T12', d)], [('WE', ch, d)])
                    self.act(DEC[:, ch, d, :], PS[bk][:, 8:12], AF.Exp, [('ps', bk)], [('DEC', ch, d)])
            p.barrier()
            if self.stop == 'mlgate':
                for nm, t in [('GJ', GJ), ('BIAS', BIAS), ('WE', WE), ('DEC', DEC)]:
                    self.tap(nm, t[:], [128, NCH, 2, 4], F32, [])
                return
            for hg in range(2):
                heads = [2 * hg, 2 * hg + 1]
                with self.scope() as hs:
                    H = lambda name, shape, dt=F32: self.sb(hs, name, shape, dt)
                    B_ = {}
                    for hd in heads:
                        B_[hd] = dict(
                            W4=H("W4", [128, 8, 4, 128], BF16), QT=H("QT", [128, NT], BF16), KT=H("KT", [128, NT], BF16),
                            KTOK=H("KTOK", [128, NCH, 128], BF16), VP=H("VP", [128, NCH, 129], BF16), SO=H("SO", [128, NCH, 128], BF16),
                            HS=H("HS", [128, NCH, 128], BF16), SALL=H("SALL", [128, NCH, 128], BF16))
                    U_ = {}
                    for hd in heads:
                        for d in range(2):
                            U_[(hd, d)] = dict(CS=H("CS", [128, 129]), CSB=H("CSB", [128, 129], BF16), LFB=H("LFB", [128, 128]),
                                               DT=H("DT", [128, 128], BF16), PT=[H("PT", [128, 128], BF16) for _ in range(2)], TB=H("TB", [128, 129]),
                                               DEN=H("DEN", [128, 2]), VW=[H("VW", [128, 129], BF16) for _ in range(2)])
                            U_[(hd, d)]['NUM'] = U_[(hd, d)]['TB']
                    for hd in heads:
                        bb = B_[hd]
                        W4 = bb['W4']
                        self.dmas('pool', [(W4[:, :, i, :], w_in[:, :, i * 512 + hd * 128:i * 512 + (hd + 1) * 128]) for i in range(4)], writes=[('W4', hd)])
                        self.memset(bb['HS'][:], 0.0, [('HS', hd, c) for c in range(NCH)])
                        self.memset(bb['VP'][:, :, 128:129], 1.0, [('VP1', hd)], eng='pool')
                        for d in range(2):
                            self.memset(U_[(hd, d)]['CS'][:], 0.0, [('CS', hd, d)])
                            self.memset(U_[(hd, d)]['CSB'][:], 0.0, [('CSB', hd, d)], eng='pool')
                    cnt = 0
                    for hd in heads:
                        bb = B_[hd]
                        for ti, (t0, n) in enumerate(TILES):
                            hk = tk('HT', t0, n)
                            for i, nm in enumerate(['QT', 'KT']):
                                bank = cnt % 4
                                cnt += 1
                                for k in range(8):
                                    self.mm(PS[bank][:, :n], bb['W4'][:, k, i, :], self.HT[:, k, t0:t0 + n], k == 0, k == 7, hk + [('W4', hd)], [('ps', bank)])
                                self.act(bb[nm][:, t0:t0 + n], PS[bank][:, :n], AF.Copy, [('ps', bank)], [(nm, hd, c) for c in range(t0 // 128, (t0 + n) // 128)],
                                         scale=1.0 if i == 0 else 128.0 ** -0.5)
                    for hd in heads:
                        bb = B_[hd]
                        for ch in range(NCH):
                            bank = 4 + ch % 2
                            sbk = 6 + ch % 2
                            tsl = slice(ch * 128, (ch + 1) * 128)
                            hk = tk('HT', ch * 128, 128)
                            for k in range(8):
                                self.mm(PS[bank][:, 0:384], self.HT[:, k, tsl], bb['W4'][:, k, 1:4, :].rearrange("p a n -> p (a n)"),
                                        k == 0, k == 7, hk + [('W4', hd)], [('ps', bank)])
                            self.act(bb['KTOK'][:, ch, :], PS[bank][:, 0:128], AF.Copy, [('ps', bank)], [('KTOK', hd, ch)], scale=128.0 ** -0.5)
                            self.cp(bb['VP'][:, ch, 0:128], PS[bank][:, 128:256], [('ps', bank)], [('VP', hd, ch)])
                            self.act(bb['SO'][:, ch, :], PS[bank][:, 256:384], AF.Sigmoid, [('ps', bank)], [('SO', hd, ch)])
                            self.mm(PS[sbk][:, 0:128], bb['KT'][:, tsl], bb['QT'][:, tsl], True, True, [('KT', hd, ch), ('QT', hd, ch)], [('ps', sbk)])
                            self.cp(bb['SALL'][:, ch, :], PS[sbk][:, 0:128], [('ps', sbk)], [('SALL', hd, ch)])
                    units = [(hd, d) for hd in heads for d in range(2)]

                    def mk_ctxs(step):
                        ctxs = []
                        for ui, (hd, d) in enumerate(units):
                            ch = order[d][step]
                            ctxs.append((ui, hd, d, ch, slice(ch * 128, (ch + 1) * 128), B_[hd], U_[(hd, d)], 2 * ui, 2 * ui + 1))
                        return ctxs

                    def front(step):
                        ctxs = mk_ctxs(step)
                        sp_ = step % 2
                        for (ui, hd, d, ch, tsl, bb, uu, bx, by) in ctxs:
                            self.act(uu['LFB'][:], self.ONESF[:], AF.Identity, [('LF', ch), 'ONESF'], [('LFB', hd, d)], scale=LF[:, ch, d * 4 + hd:d * 4 + hd + 1])
                        for (ui, hd, d, ch, tsl, bb, uu, bx, by) in ctxs:
                            self.mm(PS[bx][:, 0:128], uu['LFB'][:], self.TRI[:, 3 * d + 0, :], True, False, [('LFB', hd, d), 'TRI'], [('ps', bx)])
                            self.mm(PS[bx][:, 0:128], self.IDF[:], self.TRI[:, 3 * d + 2, :], False, True, ['IDF', 'TRI'], [('ps', bx)])
                        for (ui, hd, d, ch, tsl, bb, uu, bx, by) in ctxs:
                            self.act(uu['DT'][:], PS[bx][:, 0:128], AF.Exp, [('ps', bx), ('BIAS', ch, d)], [('DT', hd, d)], bias=BIAS[:, ch, d, hd:hd + 1], scale=1.0)
                        for (ui, hd, d, ch, tsl, bb, uu, bx, by) in ctxs:
                            self.tt(uu['PT'][sp_][:], bb['SALL'][:, ch, :], uu['DT'][:], ALU.mult, [('SALL', hd, ch), ('DT', hd, d)], [('PT', hd, d, sp_)], eng='pool')
                            self.ts(uu['VW'][sp_][:], bb['VP'][:, ch, :], WE[:, ch, d, hd:hd + 1], None, ALU.mult, None,
                                    [('VP', hd, ch), ('VP1', hd), ('WE', ch, d)], [('VW', hd, d, sp_)])

                    def back(step):
                        ctxs = mk_ctxs(step)
                        sp_ = step % 2
                        for (ui, hd, d, ch, tsl, bb, uu, bx, by) in ctxs:
                            self.mm(PS[by][:, 0:129], uu['PT'][sp_][:], bb['VP'][:, ch, :], True, True, [('PT', hd, d, sp_), ('VP', hd, ch), ('VP1', hd)], [('ps', by)])
                            self.mm(PS[by][:, 129:258], bb['QT'][:, tsl], uu['CSB'][:], True, True, [('QT', hd, ch), ('CSB', hd, d)], [('ps', by)])
                            self.mm(PS[by][:, 258:387], bb['KTOK'][:, ch, :], uu['VW'][sp_][:], True, True, [('KTOK', hd, ch), ('VW', hd, d, sp_)], [('ps', by)])
                        for (ui, hd, d, ch, tsl, bb, uu, bx, by) in ctxs:
                            self.act(uu['TB'][:], PS[by][:, 129:258], AF.Identity, [('ps', by), ('GJ', ch, d)], [('TB', hd, d)], scale=GJ[:, ch, d, hd:hd + 1])
                        for (ui, hd, d, ch, tsl, bb, uu, bx, by) in ctxs:
                            self.stt(uu['CS'][:], uu['CS'][:], DEC[:, ch, d, hd:hd + 1], PS[by][:, 258:387], ALU.mult, ALU.add,
                                     [('CS', hd, d), ('DEC', ch, d), ('ps', by)], [('CS', hd, d)])
                            self.tt(uu['NUM'][:], PS[by][:, 0:129], uu['TB'][:], ALU.add, [('ps', by), ('TB', hd, d)], [('NUM', hd, d), ('TB', hd, d)])
                        for (ui, hd, d, ch, tsl, bb, uu, bx, by) in ctxs:
                            self.act(uu['CSB'][:], uu['CS'][:], AF.Copy, [('CS', hd, d)], [('CSB', hd, d)])
                        for (ui, hd, d, ch, tsl, bb, uu, bx, by) in ctxs:
                            DEN = uu['DEN']
                            self.stt(DEN[:, 0:1], uu['NUM'][:, 128:129], -1.0, uu['NUM'][:, 128:129], ALU.mult, ALU.max, [('NUM', hd, d), ('TB', hd, d)], [('DEN', hd, d, 0)])
                        for (ui, hd, d, ch, tsl, bb, uu, bx, by) in ctxs:
                            DEN = uu['DEN']
                            self.ts(DEN[:, 0:1], DEN[:, 0:1], 1.0, None, ALU.max, None, [('DEN', hd, d, 0)], [('DEN', hd, d, 0)])
                        for (ui, hd, d, ch, tsl, bb, uu, bx, by) in ctxs:
                            DEN = uu['DEN']
                            self.recip(DEN[:, 1:2], DEN[:, 0:1], [('DEN', hd, d, 0)], [('DEN', hd, d, 1)])
                        for (ui, hd, d, ch, tsl, bb, uu, bx, by) in ctxs:
                            DEN = uu['DEN']
                            self.stt(bb['HS'][:, ch, :], uu['NUM'][:, 0:128], DEN[:, 1:2], bb['HS'][:, ch, :], ALU.mult, ALU.add,
                                     [('NUM', hd, d), ('TB', hd, d), ('DEN', hd, d, 1), ('HS', hd, ch)], [('HS', hd, ch)])

                    front(0)
                    for step in range(NCH):
                        if step + 1 < NCH:
                            front(step + 1)
                        back(step)
                    SSQ = H("SSQ", [128, 4]); JK = [H("JK0", [128, 128])] * 2; T1 = [H("T10", [128, 128])] * 2
                    MT = [H("MT%d" % i, [128, 128], BF16) for i in range(2)]
                    for hi_, hd in enumerate(heads):
                        bb = B_[hd]
                        for ch in range(NCH):
                            bank = (hi_ * NCH + ch) % 4
                            s2 = ch % 2
                            o0 = 2 * s2
                            self.act(JK[s2][:], bb['HS'][:, ch, :], AF.Square, [('HS', hd, ch)], [('JK', 0)])
                            self.p.op('dve', lambda e, s2=s2, o0=o0, JK=JK, SSQ=SSQ: e.reduce_sum(out=SSQ[:, o0:o0 + 1], in_=JK[s2][:], axis=AX.X), [('JK', 0)], [('SSQ', o0)])
                            self.act(SSQ[:, o0:o0 + 1], SSQ[:, o0:o0 + 1], AF.Sqrt, [('SSQ', o0)], [('SSQ', o0)], bias=self.EPSC[:, 0:1], scale=1.0 / 128)
                            self.recip(SSQ[:, o0 + 1:o0 + 2], SSQ[:, o0:o0 + 1], [('SSQ', o0)], [('SSQ', o0 + 1)])
                            self.stt(T1[s2][:], bb['HS'][:, ch, :], SSQ[:, o0 + 1:o0 + 2], MLN[:, hd * 128:(hd + 1) * 128], ALU.mult, ALU.mult,
                                     [('HS', hd, ch), ('SSQ', o0 + 1), 'MLN'], [('T1', 0)])
                            self.tt(MT[s2][:], T1[s2][:], bb['SO'][:, ch, :], ALU.mult, [('T1', 0), ('SO', hd, ch)], [('MT', s2)])
                            pv = PS[bank][:].bitcast(BF16)
                            self.tr(pv[:, 0:128], MT[s2][:], self.IDB[:], [('MT', s2), 'IDB'], [('ps', bank)])
                            self.act(self.MIXT[:, hd, ch * 128:(ch + 1) * 128], pv[:, 0:128], AF.Copy, [('ps', bank)], [('MIXT', ch)])
                    p.barrier()

    def attention(self, b):
        p, PS, nc = self.p, self.PS, self.nc
        w_in = self.ev_w_in[0].rearrange("(k p) n -> p k n", p=128)
        with self.scope() as ph:
            P = lambda name, shape, dt=F32: self.sb(ph, name, shape, dt)
            WA = P("WA", [128, 8, 768], BF16)
            GQ = P("GQ", [128, 640]); RC = P("RC", [128, 16, 64]); RSN = P("RSN", [128, 16, 64])
            QT4 = P("QT4", [128, 4, NT], BF16); KT = P("KTa", [128, NT], BF16); VP = P("VPa", [128, NCH, 2, 65], BF16)
            ATT = P("ATT", [128, NCH, 512], BF16)
            QS = [P("QS%d" % i, [128, 768]) for i in range(2)]
            SQ = P("SQa", [128, 640]); SSQ = P("SSQa", [128, 10]); RSTD = P("RSTDa", [128, 10])
            QN1 = P("QN1", [128, 640]); T1 = P("T1a", [128, 640]); T2 = P("T2a", [128, 640])
            QRq = P("QRq", [128, 4, 2, 64], BF16); QRk = P("QRk", [128, 128], BF16)
            ET = [P("ET%d" % i, [128, 512], BF16) for i in range(3)]
            RD = [P("RD%d" % i, [128, 4]) for i in range(2)]
            p.dma('pool', [lambda e, i=i: e.dma_start(out=WA[:, i * 4:(i + 1) * 4, :], in_=w_in[:, i * 4:(i + 1) * 4, 2064:2832]) for i in range(2)], writes=['WA'])
            p.dma('sp', [lambda e, i=i: e.dma_start(out=GQ[:, i * 64:(i + 1) * 64], in_=self.at_q_norm[0].partition_broadcast(128)) for i in range(8)] +
                        [lambda e, i=i: e.dma_start(out=GQ[:, 512 + i * 64:512 + (i + 1) * 64], in_=self.at_k_norm[0].partition_broadcast(128)) for i in range(2)],
                  writes=['GQ'])
            p.dma('act', [lambda e: e.dma_start(out=RC[:], in_=self.k_rope[0].rearrange("(c p) d -> p c d", p=128)),
                          lambda e: e.dma_start(out=RSN[:], in_=self.k_rope[1].rearrange("(c p) d -> p c d", p=128))], writes=['ROPE'])
            self.ts(GQ[:, 0:512], GQ[:, 0:512], 0.125, None, ALU.mult, None, ['GQ'], ['GQ'])
            self.memset(VP[:, :, :, 64:65], 1.0, [('VP1',)], eng='pool')
            for ch in range(NCH):
                hk = tk('HT', ch * 128, 128)
                qs = QS[ch % 2]
                bA, bB, bT = (ch % 2) * 3, (ch % 2) * 3 + 1, (ch % 2) * 3 + 2
                for k in range(8):
                    self.mm(PS[bA][:, 0:512], self.HT[:, k, ch * 128:(ch + 1) * 128], WA[:, k, 0:512], k == 0, k == 7, hk + ['WA'], [('ps', bA)])
                for k in range(8):
                    self.mm(PS[bB][:, 0:256], self.HT[:, k, ch * 128:(ch + 1) * 128], WA[:, k, 512:768], k == 0, k == 7, hk + ['WA'], [('ps', bB)])
                self.act(qs[:, 0:512], PS[bA][:, 0:512], AF.Copy, [('ps', bA)], [('QS', ch % 2)])
                self.act(qs[:, 512:768], PS[bB][:, 0:256], AF.Copy, [('ps', bB)], [('QS', ch % 2)])
                self.act(SQ[:], qs[:, 0:640], AF.Square, [('QS', ch % 2)], ['SQ'])
                self.p.op('dve', lambda e: e.tensor_reduce(out=SSQ[:], in_=SQ[:].rearrange("p (h d) -> p h d", d=64), axis=AX.X, op=ALU.add), ['SQ'], ['SSQ'])
                self.act(SSQ[:], SSQ[:], AF.Sqrt, ['SSQ'], ['SSQ'], bias=self.EPSC[:, 0:1], scale=1.0 / 64)
                self.recip(RSTD[:], SSQ[:], ['SSQ'], ['RSTD'])
                self.tt(QN1[:].rearrange("p (h d) -> p h d", d=64), qs[:, 0:640].rearrange("p (h d) -> p h d", d=64),
                        RSTD[:].unsqueeze(2).to_broadcast([128, 10, 64]), ALU.mult, [('QS', ch % 2), 'RSTD'], ['QN1'])
                self.tt(QN1[:], QN1[:], GQ[:], ALU.mult, ['QN1', 'GQ'], ['QN1'])
                qdst = QRq[:].rearrange("p pr hl d -> p hl pr d")
                if ch >= 2:
                    lc = ch - 2
                    self.tt(T1[:].rearrange("p (h d) -> p h d", d=64), QN1[:].rearrange("p (h d) -> p h d", d=64),
                            RC[:, lc, :].unsqueeze(1).to_broadcast([128, 10, 64]), ALU.mult, ['QN1', 'ROPE'], ['T1'], eng='pool')
                    q3 = QN1[:].rearrange("p (h d) -> p h d", d=64)
                    t3 = T2[:].rearrange("p (h d) -> p h d", d=64)
                    for rc in range(2):
                        for f in range(2):
                            o0 = rc * 32 + f * 16
                            i0 = rc * 32 + (1 - f) * 16
                            self.tt(t3[:, :, o0:o0 + 16], q3[:, :, i0:i0 + 16], RSN[:, lc, o0:o0 + 16].unsqueeze(1).to_broadcast([128, 10, 16]),
                                    ALU.mult, ['QN1', 'ROPE'], ['T2'])
                    self.tt(qdst, T1[:, 0:512].rearrange("p (hl pr d) -> p hl pr d", hl=2, pr=4), T2[:, 0:512].rearrange("p (hl pr d) -> p hl pr d", hl=2, pr=4),
                            ALU.add, ['T1', 'T2'], ['QRq'])
                    self.tt(QRk[:], T1[:, 512:640], T2[:, 512:640], ALU.add, ['T1', 'T2'], ['QRk'])
                else:
                    self.cp(qdst, QN1[:, 0:512].rearrange("p (hl pr d) -> p hl pr d", hl=2, pr=4), ['QN1'], ['QRq'])
                    self.cp(QRk[:], QN1[:, 512:640], ['QN1'], ['QRk'])
                self.cp(VP[:, ch, :, 0:64], qs[:, 640:768].rearrange("p (k d) -> p k d", d=64), [('QS', ch % 2)], [('VP', ch)], eng='pool')
                pv = PS[bT][:].bitcast(BF16)
                for pr in range(4):
                    self.tr(pv[:, pr * 128:(pr + 1) * 128], QRq[:, pr, :, :].rearrange("p a d -> p (a d)"), self.IDB[:], ['QRq', 'IDB'], [('ps', bT)])
                self.tr(pv[:, 512:640], QRk[:], self.IDB[:], ['QRk', 'IDB'], [('ps', bT)])
                self.act(QT4[:, :, ch * 128:(ch + 1) * 128], pv[:, 0:512].rearrange("p (a t) -> p a t", t=128), AF.Copy, [('ps', bT)], [('QT4', ch)])
                self.cp(KT[:, ch * 128:(ch + 1) * 128], pv[:, 512:640], [('ps', bT)], [('KTa', ch)])
            p.barrier()
            jobs = []
            for h in range(8):
                jobs.append((h, 0, 256, [0, 1]))
                for i in range(4):
                    jobs.append((h, 256 + 512 * i, 512, list(range(NCH))))
            sct = 0
            for ji, (h, q0, n, kcs) in enumerate(jobs):
                hl, pr = h // 4, h % 4
                ob = 4 + ji % 4
                nsub = n // 128
                qk = [('QT4', c) for c in range(q0 // 128, (q0 + n) // 128)]

                def issue_s(ki, sct):
                    kc = kcs[ki]
                    sb_ = sct % 4
                    self.mm(PS[sb_][:, :n], KT[hl * 64:(hl + 1) * 64, kc * 128:(kc + 1) * 128], QT4[hl * 64:(hl + 1) * 64, pr, q0:q0 + n],
                            True, True, [('KTa', kc)] + qk, [('ps', sb_)])
                issue_s(0, sct)
                for ki, kc in enumerate(kcs):
                    if ki + 1 < len(kcs):
                        issue_s(ki + 1, sct + 1)
                    sb_ = sct % 4
                    et = ET[sct % 3]
                    self.act(et[:, :n], PS[sb_][:, :n], AF.Exp, [('ps', sb_)], [('ET', sct % 3)])
                    for j in range(nsub):
                        self.mm(PS[ob][:, j * 65:(j + 1) * 65], et[:, j * 128:(j + 1) * 128], VP[:, kc, hl, :], ki == 0 and j == 0, ki == len(kcs) - 1,
                                [('ET', sct % 3), ('VP', kc), ('VP1',)], [('ps', ob)], skip_group_check=True)
                    sct += 1
                rd = RD[ji % 2]
                ov = PS[ob][:, 0:nsub * 65].rearrange("p (j d) -> p j d", d=65)
                self.recip(rd[:, 0:nsub], ov[:, :, 64], [('ps', ob)], [('RD', ji % 2)])
                c0 = q0 // 128
                self.tt(ATT[:, c0:c0 + nsub, h * 64:(h + 1) * 64], ov[:, :, 0:64], rd[:, 0:nsub].unsqueeze(2).to_broadcast([128, nsub, 64]), ALU.mult,
                        [('ps', ob), ('RD', ji % 2)], [('ATT', c) for c in range(c0, c0 + nsub)])
            for ch in range(NCH):
                bank = ch % 4
                pv = PS[bank][:].bitcast(BF16)
                for j in range(4):
                    self.tr(pv[:, j * 128:(j + 1) * 128], ATT[:, ch, j * 128:(j + 1) * 128], self.IDB[:], [('ATT', ch), 'IDB'], [('ps', bank)])
                self.act(self.MIXT[:, 4:8, ch * 128:(ch + 1) * 128], pv[:, 0:512].rearrange("p (a t) -> p a t", t=128), AF.Copy, [('ps', bank)], [('MIXT', ch)])
            p.barrier()

    def outproj(self, b, l, w_out):
        p, PS = self.p, self.PS
        with self.scope() as ph:
            WO = self.sb(ph, "WO", [128, 8, 1024], BF16)
            src = w_out.rearrange("(k p) n -> p k n", p=128)
            p.dma('pool', [lambda e, i=i: e.dma_start(out=WO[:, i * 4:(i + 1) * 4, :], in_=src[:, i * 4:(i + 1) * 4, :]) for i in range(2)], writes=['WO'])
            cnt = 0
            for ti, (t0, n) in enumerate(TILES):
                if l == 1 and ti == 0:
                    continue
                j = 2 if ti == 0 else b
                mk = tk('MIXT', t0, n)
                xk = tk('X', t0, n)
                for c in range(8):
                    bank = cnt % 4
                    cnt += 1
                    for k in range(8):
                        self.mm(PS[bank][:, :n], WO[:, k, c * 128:(c + 1) * 128], self.MIXT[:, k, t0:t0 + n], k == 0, k == 7, mk + ['WO'], [('ps', bank)])
                    self.stt(self.X[:, c, t0:t0 + n], PS[bank][:, :n], self.MOD[:, l, 16 + c, j:j + 1], self.X[:, c, t0:t0 + n], ALU.mult, ALU.add,
                             [('ps', bank)] + xk, xk)
            p.barrier()

    def moe(self, b, l):
        p, PS = self.p, self.PS
        tiles = TILES[1:] if l == 1 else TILES
        with self.scope() as ph:
            P = lambda name, shape, dt=F32: self.sb(ph, name, shape, dt)
            COMBT = P("COMBT", [16, NT], BF16)
            SEL = P("SEL", [16, 16, 128], BF16)
            WGU = [None, None]
            WD = [None, None]
            p.dma('pool', lambda e: e.dma_start(out=SEL[:], in_=self.k_sel.rearrange("e r n -> r e n")), writes=['SEL'])

            def load_expert(e):
                slot = e % 2
                g = self.moe_w_gate[l, e].rearrange("(k p) n -> p k n", p=128)
                u = self.moe_w_up[l, e].rearrange("(k p) n -> p k n", p=128)
                dn = self.moe_w_down[l, e].rearrange("(k p) n -> p k n", p=128)
                p.dma('pool', [lambda en, i=i: en.dma_start(out=WGU[slot][:, i * 4:(i + 1) * 4, 0:512], in_=g[:, i * 4:(i + 1) * 4, :]) for i in range(2)] +
                              [lambda en, i=i: en.dma_start(out=WGU[slot][:, i * 4:(i + 1) * 4, 512:1024], in_=u[:, i * 4:(i + 1) * 4, :]) for i in range(2)],
                      writes=[('WGU', slot)])
                p.dma('pool', [lambda en, i=i: en.dma_start(out=WD[slot][:, i * 2:(i + 1) * 2, :], in_=dn[:, i * 2:(i + 1) * 2, :]) for i in range(2)],
                      writes=[('WD', slot)])
            with self.scope() as rs:
                R = lambda name, shape, dt=F32: self.sb(rs, name, shape, dt)
                WR = R("WR", [128, 8, 20]); RB = R("RB", [128, 20]); L = R("L", [128, 20])
                SM = R("SM", [128, 16]); GM = R("GM", [128, 4]); GE = R("GE", [128, 4]); PEN = R("PEN", [128, 4])
                EM = R("EM", [128, 16]); EM2 = R("EM2", [128, 16]); M1 = R("M1", [128, 16]); M2 = R("M2", [128, 16]); COMB = R("COMB", [128, 16])
                p.dma('sp', [lambda e: e.dma_start(out=WR[:, :, 0:4], in_=self.moe_gr_w[l].rearrange("(k p) n -> p k n", p=128)),
                             lambda e: e.dma_start(out=WR[:, :, 4:20], in_=self.moe_er_w[l].rearrange("(k p) n -> p k n", p=128)),
                             lambda e: e.dma_start(out=RB[:, 0:4], in_=self.moe_gr_b[l].partition_broadcast(128)),
                             lambda e: e.dma_start(out=RB[:, 4:20], in_=self.moe_er_b[l].partition_broadcast(128))], writes=['WR', 'RB'])

                def router(ti, t0, n, H32):
                    for sub in range(n // 128):
                        rb = 2 + sub % 2
                        tb = 4 + sub % 2
                        for k in range(8):
                            self.mm(PS[rb][:, 0:20], H32[:, k, sub * 128:(sub + 1) * 128], WR[:, k, :], k == 0, k == 7, [('H32', k), 'WR'], [('ps', rb)])
                        self.tt(L[:], PS[rb][:, 0:20], RB[:], ALU.add, [('ps', rb), 'RB'], ['L'])
                        sm = lambda i: SM[:, i:i + 1]
                        self.p.op('dve', lambda e: e.reduce_max(out=SM[:, 0:1], in_=L[:, 0:4], axis=AX.X), ['L'], [('SM', 0)])
                        self.ts(GM[:], L[:, 0:4], sm(0), None, ALU.is_equal, None, ['L', ('SM', 0)], ['GM'])
                        self.ts(sm(1), sm(0), -1.0, None, ALU.mult, None, [('SM', 0)], [('SM', 1)])
                        self.act(GE[:], L[:, 0:4], AF.Exp, ['L', ('SM', 1)], ['GE'], bias=sm(1), scale=1.0)
                        self.p.op('dve', lambda e: e.reduce_sum(out=SM[:, 2:3], in_=GE[:], axis=AX.X), ['GE'], [('SM', 2)])
                        self.recip(sm(3), sm(2), [('SM', 2)], [('SM', 3)])
                        self.ts(PEN[:], GM[:], 1e30, -1e30, ALU.mult, ALU.add, ['GM'], ['PEN'])
                        self.tt(EM[:].rearrange("p (g e) -> p g e", e=4), L[:, 4:20].rearrange("p (g e) -> p g e", e=4),
                                PEN[:].unsqueeze(2).to_broadcast([128, 4, 4]), ALU.add, ['L', 'PEN'], ['EM'])
                        self.p.op('dve', lambda e: e.reduce_max(out=SM[:, 4:5], in_=EM[:], axis=AX.X), ['EM'], [('SM', 4)])
                        self.ts(M1[:], EM[:], sm(4), None, ALU.is_equal, None, ['EM', ('SM', 4)], ['M1'])
                        self.stt(EM2[:], M1[:], -1e30, EM[:], ALU.mult, ALU.add, ['M1', 'EM'], ['EM2'])
                        self.p.op('dve', lambda e: e.reduce_max(out=SM[:, 5:6], in_=EM2[:], axis=AX.X), ['EM2'], [('SM', 5)])
                        self.ts(M2[:], EM2[:], sm(5), None, ALU.is_equal, None, ['EM2', ('SM', 5)], ['M2'])
                        self.tt(sm(6), sm(5), sm(4), ALU.subtract, [('SM', 5), ('SM', 4)], [('SM', 6)])
                        self.act(sm(7), sm(6), AF.Exp, [('SM', 6)], [('SM', 7)])
                        self.ts(sm(8), sm(7), 1.0, None, ALU.add, None, [('SM', 7)], [('SM', 8)])
                        self.recip(sm(9), sm(8), [('SM', 8)], [('SM', 9)])
                        self.tt(sm(10), sm(9), sm(3), ALU.mult, [('SM', 9), ('SM', 3)], [('SM', 10)])
                        self.tt(sm(11), sm(10), sm(7), ALU.mult, [('SM', 10), ('SM', 7)], [('SM', 11)])
                        self.ts(COMB[:], M1[:], sm(10), None, ALU.mult, None, ['M1', ('SM', 10)], ['COMB'])
                        self.stt(COMB[:], M2[:], sm(11), COMB[:], ALU.mult, ALU.add, ['M2', ('SM', 11), 'COMB'], ['COMB'])
                        self.tr(PS[tb][0:16, 0:128], COMB[:], self.IDF[:], ['COMB', 'IDF'], [('ps', tb)])
                        tt0 = t0 + sub * 128
                        self.act(COMBT[:, tt0:tt0 + 128], PS[tb][0:16, 0:128], AF.Copy, [('ps', tb)], [('COMBT', tt0 // 128)])
                self.norm(b, l, 1, f32_cb=router, skip_ctx=(l == 1))
            for i in range(2):
                WGU[i] = P("WGU%d" % i, [128, 8, 1024], BF16)
                WD[i] = P("WD%d" % i, [128, 4, 1024], BF16)
            load_expert(0)
            load_expert(1)
            CB = [P("CB%d" % i, [128, 512], BF16) for i in range(2)]
            SG = [P("SG%d" % i, [128, 512], BF16) for i in range(2)]
            ACTT = [P("ACTT%d" % i, [128, 4, 512], BF16) for i in range(2)]
            items = [(e, ti) for e in range(16) for ti in range(len(tiles))]

            def stage_a(ii):
                e, ti = items[ii]
                t0, n = tiles[ti]
                slot = e % 2
                hk = tk('HT', t0, n)
                cb = CB[ii % 2]
                self.mm(PS[0][:, :n], SEL[:, e, :], COMBT[:, t0:t0 + n], True, True, ['SEL'] + tk('COMBT', t0, n), [('ps', 0)])
                self.act(cb[:, :n], PS[0][:, :n], AF.Copy, [('ps', 0)], [('CB', ii % 2)])
                for j in range(4):
                    gb = 1 + j % 2
                    ub = 3 + j % 2
                    sg = SG[j % 2]
                    for k in range(8):
                        self.mm(PS[gb][:, :n], WGU[slot][:, k, j * 128:(j + 1) * 128], self.HT[:, k, t0:t0 + n], k == 0, k == 7,
                                hk + [('WGU', slot)], [('ps', gb)])
                    for k in range(8):
                        self.mm(PS[ub][:, :n], WGU[slot][:, k, 512 + j * 128:512 + (j + 1) * 128], self.HT[:, k, t0:t0 + n], k == 0, k == 7,
                                hk + [('WGU', slot)], [('ps', ub)])
                    self.act(sg[:, :n], PS[gb][:, :n], AF.Silu, [('ps', gb)], [('SG', j % 2)])
                    self.tt(sg[:, :n], sg[:, :n], cb[:, :n], ALU.mult, [('SG', j % 2), ('CB', ii % 2)], [('SG', j % 2)])
                    self.tt(ACTT[ii % 2][:, j, :n], sg[:, :n], PS[ub][:, :n], ALU.mult, [('SG', j % 2), ('ps', ub)], [('ACTT', ii % 2, j)])

            def stage_b(ii):
                e, ti = items[ii]
                t0, n = tiles[ti]
                slot = e % 2
                j_ = 2 if (l == 0 and ti == 0) else b
                xk = tk('X', t0, n)
                for c in range(8):
                    db = 5 + c % 3
                    for j in range(4):
                        self.mm(PS[db][:, :n], WD[slot][:, j, c * 128:(c + 1) * 128], ACTT[ii % 2][:, j, :n], j == 0, j == 3,
                                [('WD', slot), ('ACTT', ii % 2, j)], [('ps', db)])
                    self.stt(self.X[:, c, t0:t0 + n], PS[db][:, :n], self.MOD[:, l, 40 + c, j_:j_ + 1], self.X[:, c, t0:t0 + n], ALU.mult, ALU.add,
                             [('ps', db)] + xk, xk)
            stage_a(0)
            for ii in range(len(items)):
                if ii + 1 < len(items):
                    stage_a(ii + 1)
                stage_b(ii)
                e_, ti_ = items[ii]
                if ti_ == len(tiles) - 1 and e_ + 2 < 16:
                    load_expert(e_ + 2)
            p.barrier()

    def write_out(self, b, out):
        p, PS = self.p, self.PS
        with self.scope() as ph:
            OT = [self.sb(ph, "OT%d" % i, [128, 1024], F32) for i in range(2)]
            for ch in range(2, NCH):
                s = ch % 2
                for hf in range(2):
                    bank = (ch * 2 + hf) % 4
                    for kk in range(4):
                        k = hf * 4 + kk
                        self.tr(PS[bank][:, kk * 128:(kk + 1) * 128], self.X[:, k, ch * 128:(ch + 1) * 128], self.IDF[:], [('X', ch), 'IDF'], [('ps', bank)])
                    if hf == 0:
                        self.act(OT[s][:, 0:512], PS[bank][:], AF.Copy, [('ps', bank)], [('OT', s, 0)])
                    else:
                        self.cp(OT[s][:, 512:1024], PS[bank][:], [('ps', bank)], [('OT', s, 1)])
                p.dma('sp' if ch % 2 == 0 else 'act', lambda e, s=s, ch=ch: e.dma_start(out=out[b, (ch - 2) * 128:(ch - 1) * 128, :], in_=OT[s][:]),
                      reads=[('OT', s, 0), ('OT', s, 1)])
            p.barrier()

    def cis(self, R, MAG, ORE, OIM, I, F, G, S, key, neg_im=False):
        TWO_PI = 6.2831845
        MAGIC = 12582912.0
        k = lambda n: (key, n)
        self.ts(I, R, MAGIC, None, ALU.add, None, [k('R')], [k('I')])
        self.ts(I, I, MAGIC, None, ALU.subtract, None, [k('I')], [k('I')])
        self.tt(F, R, I, ALU.subtract, [k('R'), k('I')], [k('F')])
        self.act(S, F, AF.Sin, [k('F')], [k('S')], scale=TWO_PI)
        self.act(G, MAG, AF.Exp, [k('MAG'), k('G')], [k('G')])
        if neg_im:
            self.stt(OIM, G, -1.0, S, ALU.mult, ALU.mult, [k('G'), k('S')], [k('OIM')])
        else:
            self.tt(OIM, G, S, ALU.mult, [k('G'), k('S')], [k('OIM')])
        self.ts(F, F, 0.25, None, ALU.add, None, [k('F')], [k('F')])
        self.ts(I, F, MAGIC, None, ALU.add, None, [k('F')], [k('I')])
        self.ts(I, I, MAGIC, None, ALU.subtract, None, [k('I')], [k('I')])
        self.tt(F, F, I, ALU.subtract, [k('F'), k('I')], [k('F')])
        self.act(S, F, AF.Sin, [k('F')], [k('S')], scale=TWO_PI)
        self.tt(ORE, G, S, ALU.mult, [k('G'), k('S')], [k('ORE')])

    def s5(self, b):
        p, PS, nc = self.p, self.PS, self.nc
        w_in = self.od_w_in[0].rearrange("(k p) n -> p k n", p=128)
        order = [list(range(NCH)), [1, 0] + list(range(NCH - 1, 1, -1))]
        INV2PI = 1.0 / (2.0 * math.pi)
        with self.scope() as ph:
            P = lambda name, shape, dt=F32: self.sb(ph, name, shape, dt)
            UT = P("UT", [128, 4, NT], BF16); YT = self.MIXT[:, 0:4, :]
            self.S5_UT, self.S5_YT = UT, YT
            TRIB = P("TRIB", [128, 6, 128], BF16); MASK8 = P("MASK8", [128, 8]); POSC = P("POSC", [128, 4]); POSR = P("POSR", [128, 2, 128])
            TRIBN = P("TRIBN", [128, 2, 128], BF16)
            self.dmas('pool', [(TRIB[:], self.k_tri.rearrange("a p n -> p a n"))], writes=['TRIB'])
            self.ts(TRIBN[:, 0, :], TRIB[:, 0, :], -1.0, None, ALU.mult, None, ['TRIB'], ['TRIBN'])
            self.ts(TRIBN[:, 1, :], TRIB[:, 3, :], -1.0, None, ALU.mult, None, ['TRIB'], ['TRIBN'])
            self.dmas('sp', [(MASK8[:], self.k_mask8), (POSC[:], self.k_posc),
                         (POSR[:], self.k_posr)], writes=['MASK8', 'POSC', 'POSR'])
            with self.scope() as s1:
                WU = self.sb(s1, "WU", [128, 8, 512], BF16)
                self.dmas('pool', [(WU[:, i * 4:(i + 1) * 4, :], w_in[:, i * 4:(i + 1) * 4, 0:512]) for i in range(2)], writes=['WU'])
                cnt = 0
                for ti, (t0, n) in enumerate(TILES):
                    hk = tk('HT', t0, n)
                    for c in range(4):
                        bank = cnt % 4
                        cnt += 1
                        for k in range(8):
                            self.mm(PS[bank][:, :n], WU[:, k, c * 128:(c + 1) * 128], self.HT[:, k, t0:t0 + n], k == 0, k == 7, hk + ['WU'], [('ps', bank)])
                        self.act(UT[:, c, t0:t0 + n], PS[bank][:, :n], AF.Copy, [('ps', bank)], tk('UT', t0, n))
                p.barrier()
            for d in range(2):
                with self.scope() as sd:
                    D = lambda name, shape, dt=F32: self.sb(sd, name, shape, dt)
                    KR = D("KR", [128, 2048], BF16); KI = D("KI", [128, 2048], BF16)
                    QR = D("QR", [128, 16, 128], BF16); QI = D("QI", [128, 16, 128], BF16)
                    BDR = D("BDR", [128, 4, 512], BF16); BDI = D("BDI", [128, 4, 512], BF16)
                    CPR = D("CPR", [128, 16, 128], BF16); CPI = D("CPI", [128, 16, 128], BF16); CPRN = D("CPRN", [128, 16, 128], BF16)
                    with self.scope() as st_:
                        T = lambda name, shape, dt=F32: self.sb(st_, name, shape, dt)
                        AB = T("AB", [128, 512]); OM = T("OM", [128, 512]); LDT = T("LDT", [128, 32])
                        RR = T("RR", [128, 512]); MG = T("MG", [128, 512]); II = T("II", [128, 512])
                        FF = T("FF", [128, 512]); GG = T("GG", [128, 512]); SS = T("SS", [128, 512])
                        self.dmas('sp', [(LDT[:], self.s5_log_dt[0, d].partition_broadcast(128))], writes=['LDT'])
                        self.act(LDT[:], LDT[:], AF.Exp, ['LDT'], ['LDT'])
                        for q in range(4):
                            are_q = self.s5_a_re[0, d].rearrange("g n -> (g n)")[q * 512:(q + 1) * 512]
                            aim_q = self.s5_a_im[0, d].rearrange("g n -> (g n)")[q * 512:(q + 1) * 512]
                            self.dmas('sp', [(AB[:], are_q.partition_broadcast(128)),
                                         (OM[:], aim_q.partition_broadcast(128))],
                                  reads=[('kt', 'ORE'), ('kt', 'OIM')], writes=[('kt', 'ORE'), ('kt', 'OIM')])
                            dtb = LDT[:, q * 8:(q + 1) * 8].unsqueeze(2).to_broadcast([128, 8, 64])
                            v3 = lambda t: t[:].rearrange("p (g n) -> p g n", n=64)
                            self.tt(v3(AB), v3(AB), dtb, ALU.mult, [('kt', 'ORE'), 'LDT'], [('kt', 'ORE')])
                            self.tt(v3(OM), v3(OM), dtb, ALU.mult, [('kt', 'OIM'), 'LDT'], [('kt', 'OIM')])
                            self.ts(RR[:], OM[:], POSC[:, d:d + 1], INV2PI, ALU.mult, ALU.mult, [('kt', 'OIM'), 'POSC'], [('kt', 'R')])
                            self.ts(MG[:], AB[:], POSC[:, 2 + d:3 + d], None, ALU.mult, None, [('kt', 'ORE'), 'POSC'], [('kt', 'MAG')])
                            self.cis(RR[:], MG[:], AB[:], OM[:], II[:], FF[:], GG[:], SS[:], 'kt', neg_im=True)
                            self.cp(KR[:, q * 512:(q + 1) * 512], AB[:], [('kt', 'ORE')], ['KR'])
                            self.cp(KI[:, q * 512:(q + 1) * 512], OM[:], [('kt', 'OIM')], ['KI'])
                        p.barrier()
                    with self.scope() as st_:
                        T = lambda name, shape, dt=F32: self.sb(st_, name, shape, dt)
                        STG = T("STG", [128, 128]); PRM = T("PRM", [128, 32]); LD2 = T("LD2", [128, 16])
                        RHO = T("RHO", [128, 16]); OMT = T("OMT", [128, 16])
                        RR = T("RR", [128, 4, 128]); MG = T("MG", [128, 4, 128]); II = T("II", [128, 4, 128])
                        FF = T("FF", [128, 4, 128]); GG = T("GG", [128, 4, 128]); SS = T("SS", [128, 4, 128])
                        self.memset(STG[:], 0.0, ['STG'])
                        self.dmas('sp', [(STG[0:16, :], self.s5_a_re[0, d].rearrange("(pr g2) n -> pr (g2 n)", g2=2)),
                                     (STG[16:32, :], self.s5_a_im[0, d].rearrange("(pr g2) n -> pr (g2 n)", g2=2))],
                              reads=['STG'], writes=['STG'])
                        ld = self.s5_log_dt[0, d].rearrange("(pr g2) -> g2 pr", g2=2)
                        self.dmas('sp', [(LD2[g2 * 64:(g2 + 1) * 64, :], ld[g2].partition_broadcast(64))
                                     for g2 in range(2)], writes=['LD2'], slow=True)
                        self.tr(PS[0][:, 0:128], STG[:], self.IDF[:], ['STG', 'IDF'], [('ps', 0)])
                        self.cp(PRM[:], PS[0][:, 0:32], [('ps', 0)], ['PRM'])
                        self.act(LD2[:], LD2[:], AF.Exp, ['LD2'], ['LD2'])
                        self.tt(RHO[:], PRM[:, 0:16], LD2[:], ALU.mult, ['PRM', 'LD2'], ['RHO'])
                        self.stt(OMT[:], PRM[:, 16:32], INV2PI, LD2[:], ALU.mult, ALU.mult, ['PRM', 'LD2'], ['OMT'])
                        posb = POSR[:, d, :].unsqueeze(1).to_broadcast([128, 4, 128])
                        f2 = lambda t: t.rearrange("p a b -> p (a b)")
                        for q in range(4):
                            self.tt(RR[:], OMT[:, 4 * q:4 * q + 4].unsqueeze(2).to_broadcast([128, 4, 128]), posb, ALU.mult, ['OMT', 'POSR'], [('qt', 'R')])
                            self.tt(MG[:], RHO[:, 4 * q:4 * q + 4].unsqueeze(2).to_broadcast([128, 4, 128]), posb, ALU.mult, ['RHO', 'POSR'], [('qt', 'MAG')])
                            self.cis(f2(RR[:]), f2(MG[:]), f2(QR[:, 4 * q:4 * q + 4, :]), f2(QI[:, 4 * q:4 * q + 4, :]), f2(II[:]), f2(FF[:]), f2(GG[:]), f2(SS[:]), 'qt')
                        p.barrier()
                    with self.scope() as st_:
                        T = lambda name, shape, dt=F32: self.sb(st_, name, shape, dt)
                        STG = T("STG", [128, 64]); AT = T("AT", [64, 64]); DTB = T("DTB", [64, 32])
                        RHO = T("RHO", [64, 32]); OMT = T("OMT", [64, 32]); ABR = T("ABR", [64, 32]); ABI = T("ABI", [64, 32])
                        II = T("II", [64, 32]); FF = T("FF", [64, 32]); GG = T("GG", [64, 32]); SS = T("SS", [64, 32])
                        DEN = T("DEN", [64, 32]); ZR = T("ZR", [64, 32]); ZI = T("ZI", [64, 32]); TT1 = T("TT1", [64, 32]); TT2 = T("TT2", [64, 32])
                        BRE = T("BRE", [64, 32, 16]); BIM = T("BIM", [64, 32, 16]); BBR = T("BBR", [64, 32, 16]); BBI = T("BBI", [64, 32, 16]); TB3 = T("TB3", [64, 32, 16])
                        TRS = T("TRS", [128, 64]); CC = T("CC", [16, 2, 32, 64], BF16)
                        self.memset(STG[:], 0.0, ['STG'])
                        self.dmas('sp', [(STG[0:32, :], self.s5_a_re[0, d]),
                                     (STG[32:64, :], self.s5_a_im[0, d])], reads=['STG'], writes=['STG'])
                        self.dmas('sp', [(DTB[:], self.s5_log_dt[0, d].partition_broadcast(64)),
                                     (BRE[:], self.s5_b_re[0, d].rearrange("g n c -> n g c")),
                                     (BIM[:], self.s5_b_im[0, d].rearrange("g n c -> n g c"))],
                              writes=['DTB', 'BRE', 'BIM'])
                        self.dmas('pool', [(CC[:, 0], self.s5_c_re[0, d].rearrange("g c n -> c g n")),
                                       (CC[:, 1], self.s5_c_im[0, d].rearrange("g c n -> c g n"))], writes=['CC'])
                        self.tr(PS[0][0:64, 0:128], STG[:], self.IDF[:], ['STG', 'IDF'], [('ps', 0)])
                        self.cp(AT[:], PS[0][0:64, 0:64], [('ps', 0)], ['AT'])
                        self.act(DTB[:], DTB[:], AF.Exp, ['DTB'], ['DTB'])
                        self.tt(RHO[:], AT[:, 0:32], DTB[:], ALU.mult, ['AT', 'DTB'], [('zt', 'MAG')])
                        self.stt(OMT[:], AT[:, 32:64], INV2PI, DTB[:], ALU.mult, ALU.mult, ['AT', 'DTB'], [('zt', 'R')])
                        self.cis(OMT[:], RHO[:], ABR[:], ABI[:], II[:], FF[:], GG[:], SS[:], 'zt')
                        are, aim = AT[:, 0:32], AT[:, 32:64]
                        self.tt(DEN[:], are, are, ALU.mult, ['AT'], ['DEN'])
                        self.tt(TT1[:], aim, aim, ALU.mult, ['AT'], ['TT1'])
                        self.tt(DEN[:], DEN[:], TT1[:], ALU.add, ['DEN', 'TT1'], ['DEN'])
                        self.recip(DEN[:], DEN[:], ['DEN'], ['DEN'])
                        self.ts(ABR[:], ABR[:], -1.0, None, ALU.add, None, [('zt', 'ORE')], [('zt', 'ORE')])
                        self.tt(TT1[:], ABR[:], are, ALU.mult, [('zt', 'ORE'), 'AT'], ['TT1'])
                        self.tt(TT2[:], ABI[:], aim, ALU.mult, [('zt', 'OIM'), 'AT'], ['TT2'])
                        self.tt(TT1[:], TT1[:], TT2[:], ALU.add, ['TT1', 'TT2'], ['TT1'])
                        self.tt(ZR[:], TT1[:], DEN[:], ALU.mult, ['TT1', 'DEN'], ['ZR'])
                        self.tt(TT1[:], ABI[:], are, ALU.mult, [('zt', 'OIM'), 'AT'], ['TT1'])
                        self.tt(TT2[:], ABR[:], aim, ALU.mult, [('zt', 'ORE'), 'AT'], ['TT2'])
                        self.tt(TT1[:], TT1[:], TT2[:], ALU.subtract, ['TT1', 'TT2'], ['TT1'])
                        self.tt(ZI[:], TT1[:], DEN[:], ALU.mult, ['TT1', 'DEN'], ['ZI'])
                        zrb = ZR[:].unsqueeze(2).to_broadcast([64, 32, 16])
                        zib = ZI[:].unsqueeze(2).to_broadcast([64, 32, 16])
                        self.tt(BBR[:], BRE[:], zrb, ALU.mult, ['BRE', 'ZR'], ['BBR'])
                        self.tt(TB3[:], BIM[:], zib, ALU.mult, ['BIM', 'ZI'], ['TB3'])
                        self.tt(BBR[:], BBR[:], TB3[:], ALU.subtract, ['BBR', 'TB3'], ['BBR'])
                        self.tt(BBI[:], BIM[:], zrb, ALU.mult, ['BIM', 'ZR'], ['BBI'])
                        self.tt(TB3[:], BRE[:], zib, ALU.mult, ['BRE', 'ZI'], ['TB3'])
                        self.tt(BBI[:], BBI[:], TB3[:], ALU.add, ['BBI', 'TB3'], ['BBI'])
                        mk = MASK8[:].unsqueeze(2).to_broadcast([128, 8, 64])
                        for ri, (bb, bd) in enumerate([(BBR, BDR), (BBI, BDI)]):
                            for q in range(4):
                                bank = (ri * 4 + q) % 2
                                self.tr(PS[bank][:, 0:64], bb[:, q * 8:(q + 1) * 8, :].rearrange("p g c -> p (g c)"), self.IDF[0:64, 0:64],
                                        ['BBR', 'BBI', 'IDF'], [('ps', bank)])
                                self.cp(TRS[:], PS[bank][:, 0:64], [('ps', bank)], ['TRS'])
                                self.tt(bd[:, q, :].rearrange("p (g n) -> p g n", n=64), TRS[:].unsqueeze(1).to_broadcast([128, 8, 64]), mk, ALU.mult,
                                        ['TRS', 'MASK8'], [('BD', ri)])
                        self.memset(CPR[:], 0.0, [('CP', 0)], eng='pool')
                        self.memset(CPI[:], 0.0, [('CP', 1)], eng='pool')
                        self.memset(CPRN[:], 0.0, [('CP', 2)], eng='pool')
                        for ri, cp_ in enumerate([CPR, CPI]):
                            for g2 in range(2):
                                bank = 2 + (ri * 2 + g2) % 2
                                for pr in range(16):
                                    g = 2 * pr + g2
                                    self.tr(PS[bank][:].bitcast(BF16)[g2 * 64:(g2 + 1) * 64, pr * 16:(pr + 1) * 16], CC[:, ri, g, :], self.IDB[0:16, 0:16], ['CC', 'IDB'], [('ps', bank)])
                                for pq in range(4):
                                    src = PS[bank][:].bitcast(BF16)[g2 * 64:(g2 + 1) * 64, 0:256].rearrange("p (q r c) -> p q r c", q=4, r=4)[:, :, pq, :]
                                    c0 = (2 * pq + g2) * 16
                                    dst = cp_[g2 * 64:(g2 + 1) * 64, :, c0:c0 + 16].rearrange("p (q r) c -> p q r c", r=4)[:, :, pq, :]
                                    self.act(dst, src, AF.Copy, [('ps', bank)], [('CP', ri)], scale=(1.0 if ri == 0 else -1.0))
                                    if ri == 0:
                                        dstn = CPRN[g2 * 64:(g2 + 1) * 64, :, c0:c0 + 16].rearrange("p (q r) c -> p q r c", r=4)[:, :, pq, :]
                                        self.act(dstn, src, AF.Copy, [('ps', bank)], [('CP', 2)], scale=-1.0)
                        p.barrier()
                    if self.stop == 's5tab' and d == 0:
                        self.tap("KR", KR[:], [128, 2048], BF16, []); self.tap("KI", KI[:], [128, 2048], BF16, [])
                        self.tap("QR", QR[:], [128, 16, 128], F32, []); self.tap("QI", QI[:], [128, 16, 128], F32, [])
                        self.tap("BDR", BDR[:], [128, 4, 512], BF16, []); self.tap("BDI", BDI[:], [128, 4, 512], BF16, [])
                        self.tap("CPR", CPR[:], [128, 16, 128], BF16, []); self.tap("CPI", CPI[:], [128, 16, 128], BF16, [])
                        return
                    BUR = [D("BUR%d" % i, [128, 512], BF16) for i in range(3)]; BUI = [D("BUI%d" % i, [128, 512], BF16) for i in range(3)]
                    P1S = [[D("P1%d%d" % (j, i), [128, 512], BF16) for i in range(4)] for j in range(3)]
                    HR = [D("HR%d" % i, [128, 4, 128], BF16) for i in range(2)]; HI = [D("HI%d" % i, [128, 4, 128], BF16) for i in range(2)]
                    P2S = [[D("P2%d%d" % (j, i), [128, 4, 128], BF16) for i in range(4)] for j in range(2)]
                    H0S = [[D("H0S%d%d" % (i, q), [128, 2, 4]) for q in range(4)] for i in range(2)]
                    ZC = D("ZC", [128, 1])
                    self.memset(ZC[:], 0.0, ['ZC'])
                    tinc = 0 if d == 0 else 3
                    lastcol = 127 if d == 0 else 0
                    units = [(step, q) for step in range(NCH) for q in range(4)]

                    def stage_a(ui):
                        step, q = units[ui]
                        ch = order[d][step]
                        tsl = slice(ch * 128, (ch + 1) * 128)
                        s2 = ui % 3
                        ba, bb_ = (0, 1) if ui % 2 == 0 else (6, 7)
                        self.mm(PS[ba][:, :], UT[:, q, tsl], BDR[:, q, :], True, True, [('UT', ch), ('BD', 0)], [('ps', ba)])
                        self.mm(PS[bb_][:, :], UT[:, q, tsl], BDI[:, q, :], True, True, [('UT', ch), ('BD', 1)], [('ps', bb_)])
                        self.act(BUR[s2][:], PS[ba][:, :], AF.Copy, [('ps', ba)], [('BUR', s2)])
                        self.act(BUI[s2][:], PS[bb_][:, :], AF.Copy, [('ps', bb_)], [('BUI', s2)])
                        kr = KR[:, q * 512:(q + 1) * 512]
                        ki = KI[:, q * 512:(q + 1) * 512]
                        P1 = P1S[s2]
                        self.tt(P1[0][:], kr, BUR[s2][:], ALU.mult, ['KR', ('BUR', s2)], [('P1', s2, 0)])
                        self.tt(P1[2][:], kr, BUI[s2][:], ALU.mult, ['KR', ('BUI', s2)], [('P1', s2, 2)], eng='pool')
                        self.tt(P1[1][:], ki, BUI[s2][:], ALU.mult, ['KI', ('BUI', s2)], [('P1', s2, 1)])
                        self.tt(P1[3][:], ki, BUR[s2][:], ALU.mult, ['KI', ('BUR', s2)], [('P1', s2, 3)])

                    def stage_b(ui):
                        step, q = units[ui]
                        ch = order[d][step]
                        tsl = slice(ch * 128, (ch + 1) * 128)
                        s2 = ui % 2
                        s3 = ui % 3
                        par = step % 2
                        P2 = P2S[s2]
                        h0p = H0S[1 - par][q]
                        P1 = P1S[s3]
                        tneg = TRIBN[:, 0 if d == 0 else 1, :]
                        for pq in range(4):
                            cs_ = slice(pq * 128, (pq + 1) * 128)
                            self.mm(PS[2][:, cs_], P1[0][:, cs_], TRIB[:, tinc, :], pq == 0, False, [('P1', s3, 0), 'TRIB'], [('ps', 2)], skip_group_check=True)
                            self.mm(PS[2][:, cs_], P1[1][:, cs_], tneg, False, pq == 3, [('P1', s3, 1), 'TRIBN'], [('ps', 2)], skip_group_check=True)
                        for pq in range(4):
                            cs_ = slice(pq * 128, (pq + 1) * 128)
                            self.mm(PS[3][:, cs_], P1[2][:, cs_], TRIB[:, tinc, :], pq == 0, False, [('P1', s3, 2), 'TRIB'], [('ps', 3)], skip_group_check=True)
                            self.mm(PS[3][:, cs_], P1[3][:, cs_], TRIB[:, tinc, :], False, pq == 3, [('P1', s3, 3), 'TRIB'], [('ps', 3)], skip_group_check=True)
                        for pq in range(4):
                            if step == 0:
                                br, bi = ZC[:, 0:1], ZC[:, 0:1]
                                rk = ['ZC']
                            else:
                                br, bi = h0p[:, 0, pq:pq + 1], h0p[:, 1, pq:pq + 1]
                                rk = [('H0S', 1 - par, q)]
                            self.act(HR[s2][:, pq, :], PS[2][:, pq * 128:(pq + 1) * 128], AF.Identity, [('ps', 2)] + rk, [('HR', s2)], bias=br, scale=1.0)
                            self.act(HI[s2][:, pq, :], PS[3][:, pq * 128:(pq + 1) * 128], AF.Identity, [('ps', 3)] + rk, [('HI', s2)], bias=bi, scale=1.0)
                        qr = QR[:, 4 * q:4 * q + 4, :]
                        qi = QI[:, 4 * q:4 * q + 4, :]
                        self.tt(P2[0][:], qr, HR[s2][:], ALU.mult, ['Q', ('HR', s2)], [('P2', s2, 0)])
                        self.tt(P2[2][:], qr, HI[s2][:], ALU.mult, ['Q', ('HI', s2)], [('P2', s2, 2)], eng='pool')
                        self.tt(P2[1][:], qi, HI[s2][:], ALU.mult, ['Q', ('HI', s2)], [('P2', s2, 1)])
                        self.tt(P2[3][:], qi, HR[s2][:], ALU.mult, ['Q', ('HR', s2)], [('P2', s2, 3)], eng='pool')
                        self.tt(H0S[par][q][:, 0, :], P2[0][:, :, lastcol], P2[1][:, :, lastcol], ALU.subtract, [('P2', s2, 0), ('P2', s2, 1)], [('H0S', par, q)])
                        self.tt(H0S[par][q][:, 1, :], P2[2][:, :, lastcol], P2[3][:, :, lastcol], ALU.add, [('P2', s2, 2), ('P2', s2, 3)], [('H0S', par, q)])

                    def stage_c(ui):
                        step, q = units[ui]
                        ch = order[d][step]
                        tsl = slice(ch * 128, (ch + 1) * 128)
                        s2 = ui % 2
                        if ch >= 2:
                            yb_ = 4 + step % 2
                            for pq in range(4):
                                pr = 4 * q + pq
                                P2 = P2S[s2]
                                ys_ = PS[yb_][:, q * 128:(q + 1) * 128]
                                self.mm(ys_, CPR[:, pr, :], P2[0][:, pq, :], q == 0 and pq == 0, False, [('CP', 0), ('P2', s2, 0)], [('ps', yb_)], skip_group_check=True)
                                self.mm(ys_, CPRN[:, pr, :], P2[1][:, pq, :], False, False, [('CP', 2), ('P2', s2, 1)], [('ps', yb_)], skip_group_check=True)
                                self.mm(ys_, CPI[:, pr, :], P2[2][:, pq, :], False, False, [('CP', 1), ('P2', s2, 2)], [('ps', yb_)], skip_group_check=True)
                                self.mm(ys_, CPI[:, pr, :], P2[3][:, pq, :], False, q == 3 and pq == 3, [('CP', 1), ('P2', s2, 3)], [('ps', yb_)], skip_group_check=True)
                            if q == 3:
                                yv = YT[:, :, tsl]
                                pv = PS[yb_][:, :].rearrange("p (q t) -> p q t", t=128)
                                if d == 0:
                                    self.cp(yv, pv, [('ps', yb_)], [('YT', ch)])
                                else:
                                    self.tt(yv, pv, yv, ALU.add, [('ps', yb_), ('YT', ch)], [('YT', ch)])

                    stage_a(0)
                    stage_a(1)
                    for ui in range(len(units)):
                        if ui + 2 < len(units):
                            stage_a(ui + 2)
                        stage_b(ui)
                        if ui >= 1:
                            stage_c(ui - 1)
                    stage_c(len(units) - 1)
                    p.barrier()
            if self.stop == 's5':
                return
            with self.scope() as so:
                O = lambda name, shape, dt=F32: self.sb(so, name, shape, dt)
                GLW = O("GLW", [128, 4, 512], BF16); STG = O("STG", [128, 128]); PR1 = O("PR1", [128, 8])
                TT_ = [O("TTo%d" % i, [128, 512]) for i in range(2)]; T2_ = [O("T2o%d" % i, [128, 512]) for i in range(2)]
                GTt = O("GTt", [128, 4, 512], BF16); SGo = [O("SGo%d" % i, [128, 512], BF16) for i in range(2)]
                self.dmas('pool', [(GLW[:], self.s5_glu_w[0].rearrange("(k p) n -> p k n", p=128))], writes=['GLW'])
                self.memset(STG[:], 0.0, ['STG'])
                self.dmas('sp', [(STG[0:4, :], self.s5_d[0].rearrange("(k p) -> k p", p=128)),
                                 (STG[4:8, :], self.s5_glu_b[0].rearrange("(k p) -> k p", p=128))], reads=['STG'], writes=['STG'])
                self.tr(PS[0][:, 0:128], STG[:], self.IDF[:], ['STG', 'IDF'], [('ps', 0)])
                self.cp(PR1[:], PS[0][:, 0:8], [('ps', 0)], ['PR1'])
                for ti, (t0, n) in enumerate(TILES[1:]):
                    yk = tk('YT', t0, n)
                    for c in range(4):
                        s2 = c % 2
                        xg = TT_[s2]
                        self.stt(xg[:], UT[:, c, t0:t0 + n], PR1[:, c:c + 1], YT[:, c, t0:t0 + n], ALU.mult, ALU.add, tk('UT', t0, n) + yk + ['PR1'], [('TTo', s2)])
                        self.tt(T2_[s2][:], xg[:], xg[:], ALU.mult, [('TTo', s2)], [('T2o', s2)], eng='pool')
                        self.ts(T2_[s2][:], T2_[s2][:], 0.044715, 1.0, ALU.mult, ALU.add, [('T2o', s2)], [('T2o', s2)])
                        self.tt(T2_[s2][:], T2_[s2][:], xg[:], ALU.mult, [('T2o', s2), ('TTo', s2)], [('T2o', s2)], eng='pool')
                        self.act(T2_[s2][:], T2_[s2][:], AF.Sigmoid, [('T2o', s2)], [('T2o', s2)], scale=1.5957691)
                        self.tt(GTt[:, c, :], xg[:], T2_[s2][:], ALU.mult, [('TTo', s2), ('T2o', s2)], [('GTt', c)])
                    for c in range(4):
                        bank = 1 + c % 2
                        for k in range(4):
                            self.mm(PS[bank][:, :n], GLW[:, k, c * 128:(c + 1) * 128], GTt[:, k, :], k == 0, k == 3, ['GLW', ('GTt', k)], [('ps', bank)])
                        self.act(SGo[c % 2][:], PS[bank][:, :n], AF.Sigmoid, [('ps', bank), 'PR1'], [('SGo', c % 2)], bias=PR1[:, 4 + c:5 + c], scale=1.0)
                        self.tt(self.MIXT[:, c, t0:t0 + n], GTt[:, c, :], SGo[c % 2][:], ALU.mult, [('GTt', c), ('SGo', c % 2)], yk + tk('MIXT', t0, n))
                p.barrier()

    def ssd(self, b):
        p, PS, nc = self.p, self.PS, self.nc
        w_in = self.od_w_in[0].rearrange("(k p) n -> p k n", p=128)
        order = [list(range(NCH)), [1, 0] + list(range(NCH - 1, 1, -1))]
        with self.scope() as ph:
            P = lambda name, shape, dt=F32: self.sb(ph, name, shape, dt)
            XTOK = P("XTOK", [128, NCH, 768], BF16); YS = P("YS", [128, NCH, 512], BF16)
            DT = P("DTs", [128, NCH, 16]); DA = P("DAs", [128, NCH, 16]); LNDT = P("LNDT", [128, NCH, 16])
            GJ = P("GJs", [128, NCH, 2, 8]); BIASD = P("BIASD", [128, NCH, 2, 8]); WE = P("WEs", [128, NCH, 2, 8]); DEC = P("DECs", [128, NCH, 2, 8])
            PRM = P("PRMs", [128, 48]); DTB = P("DTBs", [128, 16]); AN = P("ANs", [128, 16]); DSK = P("DSK", [128, 8]); GN = P("GNs", [128, 512])
            ONEC = P("ONECs", [128, 1]); WDT = P("WDT", [128, 8, 16], BF16); TRIB = P("TRIBs", [128, 6, 128], BF16)
            markA = self.alo
            BCT = P("BCT", [128, 4, NT], BF16)
            self.memset(ONEC[:], 1.0, ['ONEC'])
            self.dmas('pool', [(WDT[:], w_in[:, :, 2048:2064]), (TRIB[:], self.k_tri.rearrange("a p n -> p a n"))], writes=['WDT', 'TRIB'])
            self.dmas('sp', [(DTB[:], self.ssd_dt_bias[0].rearrange("d h -> (d h)").partition_broadcast(128)),
                             (AN[:], self.ssd_a_log[0].rearrange("d h -> (d h)").partition_broadcast(128)),
                             (DSK[:], self.ssd_d[0].partition_broadcast(128)),
                             (GN[:], self.ssd_norm[0].partition_broadcast(128))], writes=['DTB', 'AN', 'DSK', 'GN'])
            self.act(AN[:], AN[:], AF.Exp, ['AN'], ['AN'])
            self.ts(AN[:], AN[:], -1.0, None, ALU.mult, None, ['AN'], ['AN'])
            with self.scope() as cs:
                C_ = lambda name, shape, dt=F32: self.sb(cs, name, shape, dt)
                STG = C_("STGc", [128, 128]); RAW = C_("RAW", [128, NT]); AC = C_("AC", [128, NT]); XFM = C_("XFM", [128, NT], BF16)
                WX = [C_("WX%d" % i, [128, 8, 128], BF16) for i in range(2)]
                self.memset(STG[:], 0.0, ['STG'])
                self.dmas('sp', [(STG[0:24, :], self.ssd_conv_w[0].rearrange("j (k p) -> (j k) p", p=128)),
                                 (STG[24:32, :], self.ssd_conv_b[0].rearrange("(k p) -> k p", p=128))], reads=['STG'], writes=['STG'])
                self.tr(PS[0][:, 0:128], STG[:], self.IDF[:], ['STG', 'IDF'], [('ps', 0)])
                self.cp(PRM[:, 0:32], PS[0][:, 0:32], [('ps', 0)], ['PRM'])
                for ch in range(NCH):
                    bank = 1 + ch % 2
                    hk = tk('HT', ch * 128, 128)
                    for k in range(8):
                        self.mm(PS[bank][:, 0:16], self.HT[:, k, ch * 128:(ch + 1) * 128], WDT[:, k, :], k == 0, k == 7, hk + ['WDT'], [('ps', bank)])
                    self.tt(DT[:, ch, :], PS[bank][:, 0:16], DTB[:], ALU.add, [('ps', bank), 'DTB'], [('DT', ch)])
                    self.act(DT[:, ch, :], DT[:, ch, :], AF.Exp, [('DT', ch)], [('DT', ch)])
                    self.act(DT[:, ch, :], DT[:, ch, :], AF.Ln, [('DT', ch)], [('DT', ch)], bias=ONEC[:, 0:1], scale=1.0)
                    self.act(LNDT[:, ch, :], DT[:, ch, :], AF.Ln, [('DT', ch)], [('LNDT', ch)])
                    self.tt(DA[:, ch, :], DT[:, ch, :], AN[:], ALU.mult, [('DT', ch), 'AN'], [('DA', ch)])
                    for d in range(2):
                        bk = 3 + d
                        rhs = DA[:, ch, d * 8:(d + 1) * 8]
                        self.mm(PS[bk][:, 0:8], self.TRI[:, 3 * d + 0, :], rhs, True, True, [('DA', ch), 'TRI'], [('ps', bk)])
                        self.mm(PS[bk][:, 8:16], self.TRI[:, 3 * d + 1, :], rhs, True, True, [('DA', ch), 'TRI'], [('ps', bk)])
                        self.mm(PS[bk][:, 16:24], self.ONESF[:], rhs, True, True, [('DA', ch), 'ONESF'], [('ps', bk)])
                        li = LNDT[:, ch, d * 8:(d + 1) * 8]
                        self.act(GJ[:, ch, d, :], PS[bk][:, 0:8], AF.Exp, [('ps', bk)], [('GJ', ch, d)])
                        self.tt(BIASD[:, ch, d, :], li, PS[bk][:, 0:8], ALU.subtract, [('ps', bk), ('LNDT', ch)], [('BIASD', ch, d)])
                        self.tt(WE[:, ch, d, :], li, PS[bk][:, 8:16], ALU.add, [('ps', bk), ('LNDT', ch)], [('WE', ch, d)])
                        self.act(WE[:, ch, d, :], WE[:, ch, d, :], AF.Exp, [('WE', ch, d)], [('WE', ch, d)])
                        self.act(DEC[:, ch, d, :], PS[bk][:, 16:24], AF.Exp, [('ps', bk)], [('DEC', ch, d)])
                for k8 in range(8):
                    wx = WX[k8 % 2]
                    self.dmas('pool', [(wx[:], w_in[:, :, 1024 + k8 * 128:1024 + (k8 + 1) * 128])], writes=[('WX', k8 % 2)])
                    for ti, (t0, n) in enumerate(TILES):
                        bank = 5 + ti % 3
                        hk = tk('HT', t0, n)
                        for k in range(8):
                            self.mm(PS[bank][:, :n], wx[:, k, :], self.HT[:, k, t0:t0 + n], k == 0, k == 7, hk + [('WX', k8 % 2)], [('ps', bank)])
                        self.act(RAW[:, t0:t0 + n], PS[bank][:, :n], AF.Copy, [('ps', bank)], ['RAW'])
                    w0, w1, w2, cb = PRM[:, k8:k8 + 1], PRM[:, 8 + k8:9 + k8], PRM[:, 16 + k8:17 + k8], PRM[:, 24 + k8:25 + k8]
                    self.act(AC[:], RAW[:], AF.Identity, ['RAW', 'PRM'], ['AC'], bias=cb, scale=w1)
                    for (a0, a1) in [(0, 256), (256, NT)]:
                        self.stt(AC[:, a0 + 1:a1], RAW[:, a0:a1 - 1], w0, AC[:, a0 + 1:a1], ALU.mult, ALU.add, ['RAW', 'AC', 'PRM'], ['AC'])
                        self.stt(AC[:, a0:a1 - 1], RAW[:, a0 + 1:a1], w2, AC[:, a0:a1 - 1], ALU.mult, ALU.add, ['RAW', 'AC', 'PRM'], ['AC'])
                    dstfm = XFM[:] if k8 < 4 else BCT[:, k8 - 4, :]
                    dkey = ['XFM'] if k8 < 4 else [('BCT', k8 - 4)]
                    self.act(dstfm, AC[:], AF.Silu, ['AC'], dkey)
                    if k8 < 6:
                        for c4 in range(0, NCH, 4):
                            nn = min(4, NCH - c4)
                            bank = (c4 // 4) % 2
                            pv = PS[bank][:].bitcast(BF16)
                            for i in range(nn):
                                ch = c4 + i
                                self.tr(pv[:, i * 128:(i + 1) * 128], dstfm[:, ch * 128:(ch + 1) * 128], self.IDB[:], dkey + ['IDB'], [('ps', bank)])
                            self.act(XTOK[:, c4:c4 + nn, k8 * 128:(k8 + 1) * 128], pv[:, 0:nn * 128].rearrange("p (a t) -> p a t", t=128), AF.Copy,
                                     [('ps', bank)], [('XTOK', c4 + i) for i in range(nn)])
                p.barrier()
            if self.stop == 'ssdprep':
                self.tap("XTOK", XTOK[:], [128, NCH, 768], BF16, []); self.tap("BCT", BCT[:], [128, 4, NT], BF16, [])
                self.tap("DTs", DT[:], [128, NCH, 16], F32, [])
                return
            STALL = P("STALL", [128, NCH, 2, 128], BF16)
            HS = [P("HSs%d" % d, [128, 8, 64]) for d in range(2)]; HSB = [P("HSB%d" % d, [128, 8, 64], BF16) for d in range(2)]
            LFB = [P("LFBs%d" % i, [128, 128]) for i in range(4)] * 2; DTm = [P("DTm%d" % i, [128, 128], BF16) for i in range(8)]
            PT = [P("PTs%d" % i, [128, 128], BF16) for i in range(32)]
            XW = [P("XWs0", [128, 8, 64], BF16)] * 2; TBs = [P("TBs0", [128, 8, 64])] * 2
            for ch in range(NCH):
                bank = ch % 2
                tsl = slice(ch * 128, (ch + 1) * 128)
                for g in range(2):
                    self.mm(PS[bank][:, g * 128:(g + 1) * 128], BCT[:, g, tsl], BCT[:, 2 + g, tsl], True, True, [('BCT', g), ('BCT', 2 + g)], [('ps', bank)])
                self.act(STALL[:, ch, :, :], PS[bank][:, 0:256].rearrange("p (g t) -> p g t", t=128), AF.Copy, [('ps', bank)], [('STALL', ch)])
            self.memset(YS[:], 0.0, [('YS', c) for c in range(NCH)])
            for d in range(2):
                self.memset(HS[d][:], 0.0, [('HS', d)])
                self.memset(HSB[d][:], 0.0, [('HSB', d)], eng='pool')
            def ssd_front(step):
                chs = [order[d][step] for d in range(2)]
                for rnd in range(2):
                    for d in range(2):
                        ch = chs[d]
                        if ch < 2:
                            continue
                        ya = 2 + d * 3
                        db = d
                        hs_ = list(range(4 * rnd, 4 * rnd + 4))
                        sl = lambda h: d * 4 + (h % 4)
                        for h in hs_:
                            self.act(LFB[sl(h)][:], self.ONESF[:], AF.Identity, [('DA', ch), 'ONESF'], [('LFB', sl(h) % 4)], scale=DA[:, ch, d * 8 + h:d * 8 + h + 1])
                        for h in hs_:
                            c0 = (h % 4) * 128
                            self.mm(PS[db][:, c0:c0 + 128], LFB[sl(h)][:], self.TRI[:, 3 * d + 0, :], True, False, [('LFB', sl(h) % 4), 'TRI'], [('ps', db)], skip_group_check=True)
                            self.mm(PS[db][:, c0:c0 + 128], self.IDF[:], self.TRI[:, 3 * d + 2, :], False, True, ['IDF', 'TRI'], [('ps', db)], skip_group_check=True)
                        for h in hs_:
                            c0 = (h % 4) * 128
                            self.act(DTm[sl(h)][:], PS[db][:, c0:c0 + 128], AF.Exp, [('ps', db), ('BIASD', ch, d)], [('DTm', sl(h))], bias=BIASD[:, ch, d, h:h + 1], scale=1.0)
                        for h in hs_:
                            pi = (step % 2) * 16 + d * 8 + h
                            self.tt(PT[pi][:], STALL[:, ch, h // 4, :], DTm[sl(h)][:], ALU.mult, [('STALL', ch), ('DTm', sl(h))], [('PT', d, pi)], eng='pool')

            def ssd_back(step):
                chs = [order[d][step] for d in range(2)]
                for d in range(2):
                    ch = chs[d]
                    tsl = slice(ch * 128, (ch + 1) * 128)
                    ya, yb, ub = 2 + d * 3, 3 + d * 3, 4 + d * 3
                    if ch >= 2:
                        for h in range(8):
                            pi = (step % 2) * 16 + d * 8 + h
                            self.mm(PS[ya][:, h * 64:(h + 1) * 64], PT[pi][:], XTOK[:, ch, h * 64:(h + 1) * 64], h == 0, h == 7, [('PT', d, pi), ('XTOK', ch)], [('ps', ya)],
                                    skip_group_check=True)
                        for g in range(2):
                            self.mm(PS[yb][:, g * 256:(g + 1) * 256], BCT[:, 2 + g, tsl], HSB[d][:, 4 * g:4 * g + 4, :].rearrange("p a c -> p (a c)"), True, True,
                                    [('BCT', 2 + g), ('HSB', d)], [('ps', yb)])
                    self.tt(XW[d][:], XTOK[:, ch, 0:512].rearrange("p (h c) -> p h c", c=64), WE[:, ch, d, :].unsqueeze(2).to_broadcast([128, 8, 64]), ALU.mult,
                            [('XTOK', ch), ('WE', ch, d)], [('XW', 0)])
                    for g in range(2):
                        self.mm(PS[ub][:, g * 256:(g + 1) * 256], XTOK[:, ch, 512 + g * 128:512 + (g + 1) * 128], XW[d][:, 4 * g:4 * g + 4, :].rearrange("p a c -> p (a c)"),
                                True, True, [('XTOK', ch), ('XW', 0)], [('ps', ub)])
                    self.tt(HS[d][:], HS[d][:], DEC[:, ch, d, :].unsqueeze(2).to_broadcast([128, 8, 64]), ALU.mult, [('HS', d), ('DEC', ch, d)], [('HS', d)])
                    self.tt(HS[d][:], PS[ub][:, :].rearrange("p (h c) -> p h c", c=64), HS[d][:], ALU.add, [('ps', ub), ('HS', d)], [('HS', d)])
                    self.act(HSB[d][:], HS[d][:], AF.Copy, [('HS', d)], [('HSB', d)])
                    if ch >= 2:
                        self.tt(TBs[d][:], PS[yb][:, :].rearrange("p (h c) -> p h c", c=64), GJ[:, ch, d, :].unsqueeze(2).to_broadcast([128, 8, 64]), ALU.mult,
                                [('ps', yb), ('GJ', ch, d)], [('TBs', 0)])
                        self.tt(TBs[d][:], PS[ya][:, :].rearrange("p (h c) -> p h c", c=64), TBs[d][:], ALU.add, [('ps', ya), ('TBs', 0)], [('TBs', 0)])
                        ysv = YS[:, ch, :].rearrange("p (h c) -> p h c", c=64)
                        self.tt(ysv, ysv, TBs[d][:], ALU.add, [('YS', ch), ('TBs', 0)], [('YS', ch)], eng='pool')

            ssd_front(0)
            for step in range(NCH):
                if step + 1 < NCH:
                    ssd_front(step + 1)
                ssd_back(step)
            p.barrier()
            if self.stop == 'ssdraw':
                self.tap("YS", YS[:], [128, NCH, 512], BF16, [])
                return
            self.alo = markA
            WZ = P("WZ", [128, 8, 512], BF16)
            YF = [P("YF%d" % i, [128, 512]) for i in range(2)]; SZ = [P("SZ%d" % i, [128, 512]) for i in range(2)]
            JKs = P("JKs", [128, 512]); SSQ = P("SSQs", [128, 2]); OB = [P("OBs%d" % i, [128, 512], BF16) for i in range(2)]
            self.dmas('pool', [(WZ[:, i * 4:(i + 1) * 4, :], w_in[:, i * 4:(i + 1) * 4, 512:1024]) for i in range(2)], writes=['WZ'])
            for ch in range(2, NCH):
                s2 = ch % 2
                zb = ch % 2
                tb = 2 + ch % 2
                hk = tk('HT', ch * 128, 128)
                for k in range(8):
                    self.mm(PS[zb][:, :], self.HT[:, k, ch * 128:(ch + 1) * 128], WZ[:, k, :], k == 0, k == 7, hk + ['WZ'], [('ps', zb)])
                self.act(SZ[s2][:], PS[zb][:, :], AF.Silu, [('ps', zb)], [('SZ', s2)])
                yf = YF[s2]
                self.tt(yf[:].rearrange("p (h c) -> p h c", c=64), XTOK[:, ch, 0:512].rearrange("p (h c) -> p h c", c=64),
                        DSK[:].unsqueeze(2).to_broadcast([128, 8, 64]), ALU.mult, [('XTOK', ch), 'DSK'], [('YF', s2)])
                self.tt(yf[:], yf[:], YS[:, ch, :], ALU.add, [('YF', s2), ('YS', ch)], [('YF', s2)])
                self.tt(yf[:], yf[:], SZ[s2][:], ALU.mult, [('YF', s2), ('SZ', s2)], [('YF', s2)])
                self.act(JKs[:], yf[:], AF.Square, [('YF', s2)], ['JKs'])
                self.p.op('dve', lambda e: e.reduce_sum(out=SSQ[:, 0:1], in_=JKs[:], axis=AX.X), ['JKs'], [('SSQ', 0)])
                self.act(SSQ[:, 0:1], SSQ[:, 0:1], AF.Sqrt, [('SSQ', 0)], [('SSQ', 0)], bias=self.EPSC[:, 0:1], scale=1.0 / 512)
                self.recip(SSQ[:, 1:2], SSQ[:, 0:1], [('SSQ', 0)], [('SSQ', 1)])
                self.stt(OB[s2][:], yf[:], SSQ[:, 1:2], GN[:], ALU.mult, ALU.mult, [('YF', s2), ('SSQ', 1), 'GN'], [('OB', s2)])
                pv = PS[tb][:].bitcast(BF16)
                for j in range(4):
                    self.tr(pv[:, j * 128:(j + 1) * 128], OB[s2][:, j * 128:(j + 1) * 128], self.IDB[:], [('OB', s2), 'IDB'], [('ps', tb)])
                self.act(self.MIXT[:, 4:8, ch * 128:(ch + 1) * 128], pv[:, 0:512].rearrange("p (a t) -> p a t", t=128), AF.Copy, [('ps', tb)], [('MIXT', ch)])
            p.barrier()

def make_consts():
    tri = np.zeros((6, 128, 128), np.float32)
    i = np.arange(128)
    tri[0] = (i[:, None] <= i[None, :])
    tri[1] = (i[:, None] > i[None, :])
    tri[2] = np.where(i[None, :] >= i[:, None], 0.0, -30000.0)
    tri[3] = (i[:, None] >= i[None, :])
    tri[4] = (i[:, None] < i[None, :])
    tri[5] = np.where(i[None, :] <= i[:, None], 0.0, -30000.0)
    t = np.arange(2048)
    row = (t // 64).astype(np.float32)
    col = (t % 64).astype(np.float32)
    inv = (1.0 / (np.float32(10000.0) ** (np.arange(16, dtype=np.float32) / np.float32(16)))).astype(np.float32)
    ar = row[:, None] * inv
    ac = col[:, None] * inv
    cosr, sinr, cosc, sinc = np.cos(ar), np.sin(ar), np.cos(ac), np.sin(ac)
    rope = np.zeros((2, 2048, 64), np.float32)
    rope[0] = np.concatenate([cosr, cosr, cosc, cosc], -1)
    rope[1] = np.concatenate([-sinr, sinr, -sinc, sinc], -1)
    sel = np.zeros((16, 16, 128), np.float32)
    for e in range(16):
        sel[e, e, :] = 1.0
    mask8 = (np.arange(128)[:, None] // 16 == np.arange(8)[None, :]).astype(np.float32)
    pidx = np.arange(128, dtype=np.float32)
    posc = np.stack([pidx + 1, 128 - pidx, -(pidx + 1), -(128 - pidx)], 1).astype(np.float32)
    posr = np.zeros((128, 2, 128), np.float32)
    posr[:, 0, :] = (pidx + 1)[None, :]
    posr[:, 1, :] = (128 - pidx)[None, :]
    return {"k_ident": np.eye(128, dtype=np.float32), "k_tri": tri, "k_rope": rope, "k_sel": sel,
            "k_mask8": mask8, "k_posc": posc, "k_posr": posr}


def make_in_maps(inputs, nb=NB, ncores=NCORES):
    consts = make_consts()
    maps = []
    for ci in range(ncores):
        m = {}
        for k, v in inputs.items():
            v = np.asarray(v)
            if k in ("x", "c", "ctx"):
                m[k] = np.ascontiguousarray(v[ci * nb:(ci + 1) * nb])
            else:
                m[k] = np.ascontiguousarray(v)
        m.update(consts)
        maps.append(m)
    return maps


def kernel(**inputs):
    bld = Builder()
    nc = bld.build()
    maps = make_in_maps(inputs)
    res = run_bass_kernel_spmd(nc, maps, core_ids=list(range(NCORES)))
    return np.concatenate([r["out"] for r in res.results], axis=0).astype(np.float32)
```

```python
import math
import os
import numpy as np
import concourse.bass as bass
import concourse.mybir as mybir
from concourse.bass_utils import run_bass_kernel_spmd
from contextlib import ExitStack

F32 = mybir.dt.float32
BF16 = mybir.dt.bfloat16
I32 = mybir.dt.int32
AF = mybir.ActivationFunctionType
ALU = mybir.AluOpType
AX = mybir.AxisListType

ENGS = ['pe', 'act', 'dve', 'pool', 'sp']
NDMASEM = 40
NCORES = 8
NB = 2
NT = 2304
NCH = 18
EPS = 1e-6
TILES = [(0, 256), (256, 512), (768, 512), (1280, 512), (1792, 512)]


class _Op:
    __slots__ = ('eng', 'fn', 'waits', 'dwaits', 'idx', 'signal', 'isdma', 'dsem', 'dval', 'know')


class Prog:
    def __init__(self, nc):
        self.nc = nc
        self.ops = {e: [] for e in ENGS}
        self.lastw = {}
        self.readers = {}
        self.know = {e: {f: -1 for f in ENGS} for e in ENGS}
        self.dknow = {e: {} for e in ENGS}
        self.dsem_last = [None] * NDMASEM
        self.dsem_val = [0] * NDMASEM
        self.dsem_rr = 0
        self.dsem_rr_sw = 0

    def _dep_tokens(self, reads, writes):
        toks = []
        for r in reads:
            t = self.lastw.get(r)
            if t is not None:
                toks.append(t)
        for w in writes:
            t = self.lastw.get(w)
            if t is not None:
                toks.append(t)
            toks.extend(self.readers.get(w, ()))
        return toks

    def _record(self, op, reads, writes):
        for r in reads:
            self.readers.setdefault(r, []).append(op)
        for w in writes:
            self.lastw[w] = op
            self.readers[w] = []

    def _resolve(self, eng, toks, op, same_ok=False):
        need = {}
        dneed = {}
        for t in toks:
            if t.isdma:
                if self.dknow[eng].get(t.dsem, 0) >= t.dval:
                    continue
                dneed[t.dsem] = max(dneed.get(t.dsem, 0), t.dval)
            else:
                if t.eng == eng and same_ok:
                    continue
                if self.know[eng][t.eng] >= t.idx:
                    continue
                need[t.eng] = max(need.get(t.eng, -1), t.idx)
        for f, j in need.items():
            src = self.ops[f][j]
            src.signal = True
            kn = src.know
            for g in ENGS:
                if kn[g] > self.know[eng][g]:
                    self.know[eng][g] = kn[g]
            if j > self.know[eng][f]:
                self.know[eng][f] = j
        for s, v in dneed.items():
            self.dknow[eng][s] = v
        op.waits = list(need.items())
        op.dwaits = list(dneed.items())

    def op(self, eng, fn, reads=(), writes=(), same_ok=False):
        if eng != 'pe':
            psr = [r for r in reads if isinstance(r, tuple) and r[0] == 'ps']
            if psr:
                writes = list(writes) + psr
        o = _Op()
        o.eng = eng
        o.fn = fn
        o.isdma = False
        o.signal = False
        o.idx = len(self.ops[eng])
        toks = self._dep_tokens(reads, writes)
        self._resolve(eng, toks, o, same_ok=same_ok)
        kn = dict(self.know[eng])
        kn[eng] = o.idx
        o.know = kn
        self.ops[eng].append(o)
        self._record(o, reads, writes)
        return o

    def dma(self, q, fns, reads=(), writes=()):
        if not isinstance(fns, (list, tuple)):
            fns = [fns]
        o = _Op()
        o.eng = q
        o.fn = list(fns)
        o.isdma = True
        o.signal = False
        o.idx = len(self.ops[q])
        half = NDMASEM // 2
        if q == 'pool':
            s = half + self.dsem_rr_sw
            self.dsem_rr_sw = (self.dsem_rr_sw + 1) % (NDMASEM - half)
        else:
            s = self.dsem_rr
            self.dsem_rr = (self.dsem_rr + 1) % half
        toks = self._dep_tokens(reads, writes)
        if self.dsem_last[s] is not None:
            toks.append(self.dsem_last[s])
        self._resolve(q, toks, o)
        o.dsem = s
        self.dsem_val[s] += 16 * len(fns)
        o.dval = self.dsem_val[s]
        self.dsem_last[s] = o
        o.know = dict(self.know[q])
        self.ops[q].append(o)
        self._record(o, reads, writes)
        return o

    def _waitop(self, eng, toks):
        o = _Op()
        o.eng = eng
        o.fn = None
        o.isdma = False
        o.signal = False
        o.idx = len(self.ops[eng])
        self._resolve(eng, toks, o)
        kn = dict(self.know[eng])
        kn[eng] = o.idx
        o.know = kn
        self.ops[eng].append(o)
        return o

    def barrier(self):
        last = {e: (self.ops[e][-1] if self.ops[e] else None) for e in ENGS}
        dtoks = [t for t in self.dsem_last if t is not None]
        for e in ENGS:
            toks = list(dtoks)
            for f in ENGS:
                t = last[f]
                if t is None:
                    continue
                if t.isdma or t.fn is None:
                    j = len(self.ops[f]) - 1
                    while j >= 0 and (self.ops[f][j].isdma or self.ops[f][j].fn is None):
                        j -= 1
                    if j < 0:
                        continue
                    t = self.ops[f][j]
                toks.append(t)
            self._waitop(e, toks)
        self.lastw = {}
        self.readers = {}

    def wait_all_dma(self, eng='sp'):
        toks = [t for t in self.dsem_last if t is not None]
        return self._waitop(eng, toks)

    def emit(self, stack):
        nc = self.nc
        esem = {e: stack.enter_context(nc.semaphore("s_" + e)) for e in ENGS}
        dsem = [stack.enter_context(nc.semaphore("d%d" % i)) for i in range(NDMASEM)]
        semval = {}
        for e in ENGS:
            c = 0
            vals = []
            for o in self.ops[e]:
                if o.signal and not o.isdma and o.fn is not None:
                    c += 1
                vals.append(c)
            semval[e] = vals
        engobj = {'pe': 'tensor', 'act': 'scalar', 'dve': 'vector', 'pool': 'gpsimd', 'sp': 'sync'}
        self.stats = {e: (len(self.ops[e]), sum(len(o.waits) + len(o.dwaits) for o in self.ops[e]),
                          semval[e][-1] if semval[e] else 0) for e in ENGS}
        block = stack.enter_context(nc.Block())

        def body_for(e):
            def body(engine):
                for o in self.ops[e]:
                    for f, j in o.waits:
                        engine.wait_ge(esem[f], semval[f][j])
                    for s, v in o.dwaits:
                        engine.wait_ge(dsem[s], v)
                    if o.fn is None:
                        continue
                    if o.isdma:
                        for fn in o.fn:
                            fn(engine).then_inc(dsem[o.dsem], 16)
                    else:
                        ins = o.fn(engine)
                        if o.signal:
                            ins.then_inc(esem[e], 1)
            return body

        for e in ENGS:
            if self.ops[e]:
                getattr(block, engobj[e])(body_for(e))


class ArenaScope:
    def __init__(self, bld):
        self.bld = bld

    def __enter__(self):
        self.mark = self.bld.alo
        return self

    def __exit__(self, *a):
        self.bld.alo = self.mark
        return False


def tk(name, t0, n):
    return [(name, c) for c in range(t0 // 128, (t0 + n + 127) // 128)]


class Builder:
    def __init__(self, taps=(), nb=NB, stop=None):
        self.taps = set(taps)
        self.nb = nb
        self.stop = stop
        self.tap_specs = {}
        self.nc = bass.Bass("TRN2", target_bir_lowering=False)
        self.p = Prog(self.nc)
        self.dram = {}

    def din(self, name, shape, dt=F32):
        t = self.nc.dram_tensor(name, list(shape), dt, kind="ExternalInput").ap()
        self.dram[name] = t
        return t

    def dout(self, name, shape, dt=F32):
        t = self.nc.dram_tensor(name, list(shape), dt, kind="ExternalOutput").ap()
        self.dram[name] = t
        return t

    def tap(self, name, src_ap, shape, dt, reads):
        if name not in self.taps:
            return
        d = self.dout("tap_" + name, shape, dt)
        self.tap_specs[name] = (shape, dt)
        self.p.dma('sp', lambda e: e.dma_start(out=d, in_=src_ap), reads=reads)

    def dmas(self, q, pairs, reads=(), writes=(), slow=False):
        kw = {'allow_slow_non_contiguous': True} if slow else {}
        fns = [(lambda e, o=o, i=i: e.dma_start(out=o, in_=i, **kw)) for (o, i) in pairs]
        self.p.dma(q, fns, reads=reads, writes=writes)

    def mm(self, out, lhsT, rhs, start, stop, reads, writes, **kw):
        self.p.op('pe', lambda e: e.matmul(out, lhsT=lhsT, rhs=rhs, start=start, stop=stop, **kw),
                  reads, writes, same_ok=True)

    def tr(self, out, in_, ident, reads, writes):
        self.p.op('pe', lambda e: e.transpose(out, in_, ident), reads, writes, same_ok=True)

    def act(self, out, in_, func, reads, writes, bias=None, scale=None, accum_out=None, eng='act'):
        kw = {}
        if bias is not None:
            kw['bias'] = bias
        if scale is not None:
            kw['scale'] = scale
        if accum_out is not None:
            kw['accum_out'] = accum_out
        self.p.op('act', lambda e: e.activation(out=out, in_=in_, func=func, **kw), reads, writes)

    def tt(self, out, in0, in1, op, reads, writes, eng='dve'):
        self.p.op(eng, lambda e: e.tensor_tensor(out=out, in0=in0, in1=in1, op=op), reads, writes)

    def ts(self, out, in0, s1, s2, op0, op1, reads, writes, eng='dve'):
        if op1 is None:
            self.p.op(eng, lambda e: e.tensor_scalar(out=out, in0=in0, scalar1=s1, scalar2=None, op0=op0), reads, writes)
        else:
            self.p.op(eng, lambda e: e.tensor_scalar(out=out, in0=in0, scalar1=s1, scalar2=s2, op0=op0, op1=op1), reads, writes)

    def stt(self, out, in0, scalar, in1, op0, op1, reads, writes):
        self.p.op('dve', lambda e: e.scalar_tensor_tensor(out=out, in0=in0, scalar=scalar, in1=in1, op0=op0, op1=op1),
                  reads, writes)

    def cp(self, out, in_, reads, writes, eng='dve'):
        self.p.op(eng, lambda e: e.tensor_copy(out=out, in_=in_), reads, writes)

    def recip(self, out, in_, reads, writes):
        self.p.op('dve', lambda e: e.reciprocal(out=out, in_=in_), reads, writes)

    def memset(self, ap, val, writes, eng='dve'):
        self.p.op(eng, lambda e: e.memset(ap, val), (), writes)

    def sb(self, st, name, shape, dt):
        if isinstance(st, ArenaScope):
            return self.carve(shape, dt)
        self._uid = getattr(self, '_uid', 0) + 1
        return st.enter_context(self.nc.sbuf_tensor("%s_%d" % (name, self._uid), list(shape), dt))

    def carve(self, shape, dt, high=False):
        nelem = 1
        for d in shape[1:]:
            nelem *= d
        n32 = nelem if dt != BF16 else (nelem + 1) // 2
        n32 = (n32 + 7) // 8 * 8
        if high:
            self.ahi -= n32
            off = self.ahi
        else:
            off = self.alo
            self.alo += n32
        assert self.alo <= self.ahi, "arena overflow: lo=%d hi=%d" % (self.alo, self.ahi)
        ap = self.ARENA[0:shape[0], off:off + n32]
        if dt == BF16:
            ap = ap.bitcast(BF16)
        ap = ap[:, 0:nelem]
        if len(shape) == 3:
            ap = ap.rearrange("p (a b) -> p a b", b=shape[2])
        elif len(shape) == 4:
            ap = ap.rearrange("p (a b c) -> p a b c", b=shape[2], c=shape[3])
        elif len(shape) == 5:
            ap = ap.rearrange("p (a b c d) -> p a b c d", b=shape[2], c=shape[3], d=shape[4])
        return ap

    def scope(self):
        return ArenaScope(self)

    def build(self):
        nc, p = self.nc, self.p
        D = self.din
        nb = self.nb
        x = D("x", [nb, 2048, 1024]); c = D("c", [nb, 1024]); ctx = D("ctx", [nb, 256, 1024]); c_ctx = D("c_ctx", [1024])
        ada_w = D("ada_w", [2, 1024, 6144]); ada_b = D("ada_b", [2, 6144])
        norm_mix = D("norm_mix", [2, 1024]); norm_ffn = D("norm_ffn", [2, 1024])
        self.ev_w_in = D("ev_w_in", [1, 1024, 2832]); self.ev_w_out = D("ev_w_out", [1, 1024, 1024])
        self.ml_gate_b = D("ml_gate_b", [1, 16]); self.ml_norm = D("ml_norm", [1, 512])
        self.at_q_norm = D("at_q_norm", [1, 64]); self.at_k_norm = D("at_k_norm", [1, 64])
        self.od_w_in = D("od_w_in", [1, 1024, 2064]); self.od_w_out = D("od_w_out", [1, 1024, 1024])
        for nm, shp in [("s5_a_re", [1, 2, 32, 64]), ("s5_a_im", [1, 2, 32, 64]), ("s5_log_dt", [1, 2, 32]),
                        ("s5_b_re", [1, 2, 32, 64, 16]), ("s5_b_im", [1, 2, 32, 64, 16]),
                        ("s5_c_re", [1, 2, 32, 16, 64]), ("s5_c_im", [1, 2, 32, 16, 64]),
                        ("s5_d", [1, 512]), ("s5_glu_w", [1, 512, 512]), ("s5_glu_b", [1, 512]),
                        ("ssd_conv_w", [1, 3, 1024]), ("ssd_conv_b", [1, 1024]), ("ssd_dt_bias", [1, 2, 8]),
                        ("ssd_a_log", [1, 2, 8]), ("ssd_d", [1, 8]), ("ssd_norm", [1, 512]),
                        ("moe_gr_w", [2, 1024, 4]), ("moe_gr_b", [2, 4]), ("moe_er_w", [2, 1024, 16]),
                        ("moe_er_b", [2, 16]), ("moe_w_gate", [2, 16, 1024, 512]), ("moe_w_up", [2, 16, 1024, 512]),
                        ("moe_w_down", [2, 16, 512, 1024])]:
            setattr(self, nm, D(nm, shp))
        k_ident = D("k_ident", [128, 128]); k_tri = D("k_tri", [6, 128, 128])
        k_rope = D("k_rope", [2, 2048, 64]); k_sel = D("k_sel", [16, 16, 128])
        self.k_tri = k_tri
        self.k_mask8 = D("k_mask8", [128, 8]); self.k_posc = D("k_posc", [128, 4]); self.k_posr = D("k_posr", [128, 2, 128])
        out = self.dout("out", [nb, 2048, 1024])

        with ExitStack() as st:
            S = lambda name, shape, dt=F32: self.sb(st, name, shape, dt)
            self.IDF = S("IDF", [128, 128]); self.IDB = S("IDB", [128, 128], BF16)
            self.ONESB = S("ONESB", [128, 128], BF16); self.ONESF = S("ONESF", [128, 128])
            self.TRI = S("TRI", [128, 6, 128])
            self.MOD = S("MOD", [128, 2, 48, 3])
            self.GSC = S("GSC", [128, 2, 2, 8, 3])
            self.EPSC = S("EPSC", [128, 1])
            self.memset(self.EPSC[:], EPS, ['EPSC'])
            ARENA_WORDS = 46080
            self.ARENA = S("ARENA", [128, ARENA_WORDS])
            self.alo, self.ahi = 0, ARENA_WORDS
            self.XS = [nc.dram_tensor("xscr%d" % i, [128, 8, NT], F32, kind="Internal").ap() for i in range(nb)]
            self.HT = self.carve([128, 8, NT], BF16)
            self.PS = [st.enter_context(nc.psum_tensor("ps%d" % i, [128, 512], F32)) for i in range(8)]
            PS = self.PS
            p.dma('sp', lambda e: e.dma_start(out=self.IDF[:], in_=k_ident), writes=['IDF'])
            p.dma('pool', lambda e: e.dma_start(out=self.IDB[:], in_=k_ident), writes=['IDB'])
            p.dma('sp', lambda e: e.dma_start(out=self.TRI[:], in_=k_tri.rearrange("a p n -> p a n")), writes=['TRI'])
            self.memset(self.ONESB[:], 1.0, ['ONESB'])
            self.memset(self.ONESF[:], 1.0, ['ONESF'])
            self.k_rope = k_rope; self.k_sel = k_sel

            with self.scope() as ph:
                P = lambda name, shape, dt=F32: self.sb(ph, name, shape, dt)
                STG = P("STG", [128, 128]); FM = P("FM", [128, 128])
                AW = [P("AW%d" % i, [128, 8, 512]) for i in range(2)]
                SC = P("SC", [128, 8, 3])
                self.memset(STG[:], 0.0, ['STG'])
                r_c = 32
                r_cc = 32 + 8 * nb
                p.dma('sp', [lambda e: e.dma_start(out=STG[0:16, :], in_=norm_mix.rearrange("l (k p) -> (l k) p", p=128)),
                             lambda e: e.dma_start(out=STG[16:32, :], in_=norm_ffn.rearrange("l (k p) -> (l k) p", p=128)),
                             lambda e: e.dma_start(out=STG[r_c:r_c + 8 * nb, :], in_=c.rearrange("b (k p) -> (b k) p", p=128)),
                             lambda e: e.dma_start(out=STG[r_cc:r_cc + 8, :], in_=c_ctx.rearrange("(k p) -> k p", p=128))],
                      reads=['STG'], writes=['STG'])
                self.tr(PS[0][:, 0:128], STG[:], self.IDF[:], ['STG', 'IDF'], [('ps', 0)])
                self.cp(FM[:], PS[0][:, 0:128], [('ps', 0)], ['FM'])
                for b in range(nb):
                    self.act(SC[:, :, b], FM[:, r_c + 8 * b:r_c + 8 * b + 8], AF.Silu, ['FM'], [('SC', b)])
                self.act(SC[:, :, 2], FM[:, r_cc:r_cc + 8], AF.Silu, ['FM'], [('SC', 2)])
                if nb == 1:
                    self.cp(SC[:, :, 1], SC[:, :, 0], [('SC', 0)], [('SC', 1)])
                STG2 = P("STG2", [128, 128]); ADAB = P("ADAB", [128, 96])
                self.memset(STG2[:], 0.0, ['STG2'])
                p.dma('sp', lambda e: e.dma_start(out=STG2[0:96, :], in_=ada_b.rearrange("l (j p) -> (l j) p", p=128)),
                      reads=['STG2'], writes=['STG2'])
                self.tr(PS[1][:, 0:128], STG2[:], self.IDF[:], ['STG2', 'IDF'], [('ps', 1)])
                self.cp(ADAB[:], PS[1][:, 0:96], [('ps', 1)], ['ADAB'])
                screads = [('SC', 0), ('SC', 1), ('SC', 2)]
                for l in range(2):
                    for pc in range(12):
                        slot = pc % 2
                        aw = AW[slot]
                        src = ada_w[l].rearrange("(k p) n -> p k n", p=128)
                        p.dma('sp' if slot == 0 else 'act',
                              [lambda e, src=src, aw=aw, pc=pc, h=h: e.dma_start(out=aw[:, h * 4:(h + 1) * 4, :], in_=src[:, h * 4:(h + 1) * 4, pc * 512:(pc + 1) * 512])
                               for h in range(2)], writes=[('AW', slot)])
                        for j in range(4):
                            col = (pc * 4 + j) * 3
                            for k in range(8):
                                self.mm(PS[2 + l][:, col:col + 3], aw[:, k, j * 128:(j + 1) * 128], SC[:, k, :],
                                        k == 0, k == 7, [('AW', slot)] + screads, [('ps', 2 + l)])
                    self.tt(self.MOD[:, l], PS[2 + l][:, 0:144].rearrange("p (j t) -> p j t", t=3),
                            ADAB[:, l * 48:(l + 1) * 48].unsqueeze(2).to_broadcast([128, 48, 3]), ALU.add,
                            [('ps', 2 + l), 'ADAB'], [('MOD', l)])
                    for w in range(2):
                        mi = 1 if w == 0 else 4
                        g_ap = FM[:, 16 * w + 8 * l:16 * w + 8 * l + 8]
                        gb = g_ap.unsqueeze(2).to_broadcast([128, 8, 3])
                        self.tt(self.GSC[:, l, w], self.MOD[:, l, mi * 8:(mi + 1) * 8, :], gb, ALU.mult,
                                [('MOD', l), 'FM'], [('GSC', l, w)])
                        self.tt(self.GSC[:, l, w], self.GSC[:, l, w], gb, ALU.add,
                                [('GSC', l, w), 'FM'], [('GSC', l, w)])
                self.tap("mod", self.MOD[:], [128, 2, 48, 3], F32, [('MOD', 0), ('MOD', 1)])
                p.barrier()
            if self.stop == 'mod':
                return self.finish(st)

            for b in range(nb):
                self.run_batch(st, b, x, ctx, out)
                if self.stop is not None:
                    break
            return self.finish(st)

    def finish(self, st):
        self.p.wait_all_dma('sp')
        self.p.emit(st)
        return self.nc

    def load_x(self, b, x, ctx):
        p, PS = self.p, self.PS
        with self.scope() as ph:
            XT = [self.sb(ph, "XT%d" % i, [128, 1024], F32) for i in range(3)]
            for ch in range(NCH):
                s = ch % 3
                src = ctx[b, ch * 128:(ch + 1) * 128, :] if ch < 2 else x[b, (ch - 2) * 128:(ch - 1) * 128, :]
                p.dma('sp' if ch % 2 == 0 else 'act', lambda e, s=s, src=src: e.dma_start(out=XT[s][:], in_=src), writes=[('XT', s)])
                for hf in range(2):
                    bank = (ch * 2 + hf) % 4
                    for kk in range(4):
                        k = hf * 4 + kk
                        self.tr(PS[bank][:, kk * 128:(kk + 1) * 128], XT[s][:, k * 128:(k + 1) * 128], self.IDF[:],
                                [('XT', s), 'IDF'], [('ps', bank)])
                    dst = self.X[:, hf * 4:(hf + 1) * 4, ch * 128:(ch + 1) * 128]
                    srcp = PS[bank][:].rearrange("p (k t) -> p k t", t=128)
                    if hf == 0:
                        self.act(dst, srcp, AF.Copy, [('ps', bank)], [('X', ch)])
                    else:
                        self.cp(dst, srcp, [('ps', bank)], [('X', ch)])
            p.barrier()

    def norm(self, b, l, w, f32_cb=None, skip_ctx=False):
        p, PS = self.p, self.PS
        mi = 0 if w == 0 else 3
        with self.scope() as ph:
            SQ = [self.sb(ph, "SQ%d" % i, [128, 512], BF16) for i in range(3)]
            SD = self.sb(ph, "SD", [128, 512], F32)
            RS = self.sb(ph, "RS", [128, 512], F32)
            TM = [self.sb(ph, "TM%d" % i, [128, 512], F32) for i in range(2)]
            H32 = self.sb(ph, "H32", [128, 8, 512], F32) if f32_cb is not None else None
            cnt = 0
            for ti, (t0, n) in enumerate(TILES):
                if skip_ctx and ti == 0:
                    continue
                j = 2 if ti == 0 else b
                xk = tk('X', t0, n)
                bank = ti % 2
                for k in range(8):
                    s = cnt % 3
                    cnt += 1
                    self.act(SQ[s][:, :n], self.X[:, k, t0:t0 + n], AF.Square, xk, [('SQ', s)])
                    self.mm(PS[bank][:, :n], self.ONESB[:], SQ[s][:, :n], k == 0, k == 7, [('SQ', s), 'ONESB'], [('ps', bank)])
                self.act(SD[:, :n], PS[bank][:, :n], AF.Sqrt, [('ps', bank)], ['SD'], bias=self.EPSC[:, 0:1], scale=1.0 / 1024)
                self.recip(RS[:, :n], SD[:, :n], ['SD'], ['RS'])
                for k in range(8):
                    s = k % 2
                    self.stt(TM[s][:, :n], self.X[:, k, t0:t0 + n], self.GSC[:, l, w, k, j:j + 1], RS[:, :n], ALU.mult, ALU.mult,
                             xk + ['RS', ('GSC', l, w)], [('TM', s)])
                    if f32_cb is not None:
                        self.act(H32[:, k, :n], TM[s][:, :n], AF.Identity, [('TM', s)], [('H32', k)],
                                 bias=self.MOD[:, l, mi * 8 + k, j:j + 1], scale=1.0)
                        self.cp(self.HT[:, k, t0:t0 + n], H32[:, k, :n], [('H32', k)], tk('HT', t0, n), eng='pool')
                    else:
                        self.act(self.HT[:, k, t0:t0 + n], TM[s][:, :n], AF.Identity, [('TM', s)], tk('HT', t0, n),
                                 bias=self.MOD[:, l, mi * 8 + k, j:j + 1], scale=1.0)
                if f32_cb is not None:
                    f32_cb(ti, t0, n, H32)
            p.barrier()

    def x_alloc(self):
        self._ahi_mark = self.ahi
        self.X = self.carve([128, 8, NT], F32, high=True)

    def x_free(self):
        self.ahi = self._ahi_mark

    def x_spill(self, b):
        p = self.p
        p.dma('sp', [lambda e, k=k: e.dma_start(out=self.XS[b][:, 2 * k:2 * k + 2, :], in_=self.X[:, 2 * k:2 * k + 2, :]) for k in range(4)],
              reads=tk('X', 0, NT), writes=['XS'])
        p.barrier()

    def x_reload(self, b):
        p = self.p
        p.dma('sp', [lambda e, k=k: e.dma_start(out=self.X[:, 2 * k:2 * k + 2, :], in_=self.XS[b][:, 2 * k:2 * k + 2, :]) for k in range(4)],
              reads=['XS'], writes=tk('X', 0, NT))

    def run_batch(self, st, b, x, ctx, out):
        p = self.p
        self.x_alloc()
        self.load_x(b, x, ctx)
        if self.stop == 'loadx':
            self.tap("X", self.X[:], [128, 8, NT], F32, tk('X', 0, NT))
            return
        skipl0 = bool(os.environ.get('SKIPL0'))
        if not skipl0:
            self.norm(b, 0, 0)
        if self.stop == 'h0':
            self.tap("h0", self.HT[:], [128, 8, NT], BF16, tk('HT', 0, NT))
            return
        if not skipl0:
            self.layer0(b)
            if self.stop is not None and self.stop in ('mlstm', 'attn', 'xmid0', 'xout0', 'mlgate', 'mlproj', 'mlloop'):
                return
        self.layer1(b, out)

    def layer0(self, b):
        p = self.p
        self.x_spill(b)
        self.x_free()
        with self.scope() as lay:
            self.MIXT = self.carve([128, 8, NT], BF16)
            if 'ml' not in os.environ.get('SKIPMIX', ''):
                self.mlstm(b)
            if self.stop == 'mlstm':
                self.tap("mix0", self.MIXT[:], [128, 8, NT], BF16, [])
                return
            self.attention(b)
            if self.stop == 'attn':
                self.tap("mix0", self.MIXT[:], [128, 8, NT], BF16, [])
                return
            self.tap("mix0", self.MIXT[:], [128, 8, NT], BF16, [])
            self.x_alloc()
            self.x_reload(b)
            self.outproj(b, 0, self.ev_w_out[0])
            self.tap("xmid0", self.X[:], [128, 8, NT], F32, [])
        if self.stop == 'xmid0':
            self.tap("X", self.X[:], [128, 8, NT], F32, tk('X', 0, NT))
            return
        if 'moe0' not in os.environ.get('SKIPMIX', ''):
            self.moe(b, 0)
        if self.stop == 'xout0':
            self.tap("X", self.X[:], [128, 8, NT], F32, tk('X', 0, NT))
            return

    def layer1(self, b, out):
        p = self.p
        self.norm(b, 1, 0)
        self.tap("h1", self.HT[:], [128, 8, NT], BF16, [])
        self.x_spill(b)
        self.x_free()
        with self.scope() as lay:
            self.MIXT = self.carve([128, 8, NT], BF16)
            if 's5' not in os.environ.get('SKIPMIX', ''):
                self.s5(b)
            if self.stop in ('s5', 's5tab'):
                if self.stop == 's5':
                    self.tap("s5y", self.S5_YT[:], [128, 4, NT], BF16, [])
                return
            self.ssd(b)
            if self.stop in ('ssdprep', 'ssdraw'):
                return
            self.tap("mix1", self.MIXT[:], [128, 8, NT], BF16, [])
            if self.stop == 'mix1':
                return
            self.x_alloc()
            self.x_reload(b)
            self.outproj(b, 1, self.od_w_out[0])
        self.tap("xmid1", self.X[:], [128, 8, NT], F32, [])
        if self.stop == 'xmid1':
            return
        self.moe(b, 1)
        self.tap("xout1", self.X[:], [128, 8, NT], F32, [])
        self.write_out(b, out)
        self.x_free()

    def mlstm(self, b):
        p, PS, nc = self.p, self.PS, self.nc
        w_in = self.ev_w_in[0].rearrange("(k p) n -> p k n", p=128)
        order = [list(range(NCH)), [1, 0] + list(range(NCH - 1, 1, -1))]
        with self.scope() as ph:
            P = lambda name, shape, dt=F32: self.sb(ph, name, shape, dt)
            GT = P("GT", [128, NCH, 16]); LF = P("LF", [128, NCH, 8]); GB = P("GB", [128, 16])
            GJ = P("GJ", [128, NCH, 2, 4]); BIAS = P("BIAS", [128, NCH, 2, 4]); WE = P("WE", [128, NCH, 2, 4]); DEC = P("DEC", [128, NCH, 2, 4])
            MLN = P("MLN", [128, 512]); WG = P("WG", [128, 8, 16], BF16); ONEC = P("ONEC", [128, 1])
            ET = P("ET", [128, 8]); T12 = P("T12", [128, 2, 4])
            self.memset(ONEC[:], 1.0, ['ONEC'])
            p.dma('sp', [lambda e: e.dma_start(out=GB[:], in_=self.ml_gate_b[0].partition_broadcast(128)),
                         lambda e: e.dma_start(out=MLN[:], in_=self.ml_norm[0].partition_broadcast(128))], writes=['GB', 'MLN'])
            p.dma('pool', lambda e: e.dma_start(out=WG[:], in_=w_in[:, :, 2048:2064]), writes=['WG'])
            for ch in range(NCH):
                bank = ch % 2
                hk = tk('HT', ch * 128, 128)
                for k in range(8):
                    self.mm(PS[bank][:, 0:16], self.HT[:, k, ch * 128:(ch + 1) * 128], WG[:, k, :], k == 0, k == 7,
                            hk + ['WG'], [('ps', bank)])
                self.tt(GT[:, ch, :], PS[bank][:, 0:16], GB[:], ALU.add, [('ps', bank), 'GB'], [('GT', ch)])
                gv = GT[:, ch, :].rearrange("p (d g h) -> p d g h", d=2, g=2)
                lfv = LF[:, ch, :].rearrange("p (d h) -> p d h", d=2)
                etv = ET[:].rearrange("p (d h) -> p d h", d=2)
                self.act(etv, gv[:, :, 1, :], AF.Exp, [('GT', ch)], ['ET'], scale=-1.0)
                self.act(etv, etv, AF.Ln, ['ET'], ['ET'], bias=ONEC[:, 0:1], scale=1.0)
                self.ts(lfv, etv, -1.0, None, ALU.mult, None, ['ET'], [('LF', ch)])
                for d in range(2):
                    bk = 2 + d
                    rhs = LF[:, ch, d * 4:(d + 1) * 4]
                    self.mm(PS[bk][:, 0:4], self.TRI[:, 3 * d + 0, :], rhs, True, True, [('LF', ch), 'TRI'], [('ps', bk)])
                    self.mm(PS[bk][:, 4:8], self.TRI[:, 3 * d + 1, :], rhs, True, True, [('LF', ch), 'TRI'], [('ps', bk)])
                    self.mm(PS[bk][:, 8:12], self.ONESF[:], rhs, True, True, [('LF', ch), 'ONESF'], [('ps', bk)])
                    li = gv[:, d, 0, :]
                    self.act(GJ[:, ch, d, :], PS[bk][:, 0:4], AF.Exp, [('ps', bk)], [('GJ', ch, d)])
                    self.tt(BIAS[:, ch, d, :], li, PS[bk][:, 0:4], ALU.subtract, [('ps', bk), ('GT', ch)], [('BIAS', ch, d)])
                    self.tt(T12[:, d, :], li, PS[bk][:, 4:8], ALU.add, [('ps', bk), ('GT', ch)], [('T12', d)])
                    self.act(WE[:, ch, d, :], T12[:, d, :], AF.Exp, [('T12', d)], [('WE', ch, d)])
                    self.act(DEC[:, ch, d, :], PS[bk][:, 8:12], AF.Exp, [('ps', bk)], [('DEC', ch, d)])
            p.barrier()
            if self.stop == 'mlgate':
                for nm, t in [('GJ', GJ), ('BIAS', BIAS), ('WE', WE), ('DEC', DEC)]:
                    self.tap(nm, t[:], [128, NCH, 2, 4], F32, [])
                return
            for hg in range(2):
                heads = [2 * hg, 2 * hg + 1]
                with self.scope() as hs:
                    H = lambda name, shape, dt=F32: self.sb(hs, name, shape, dt)
                    B_ = {}
                    for hd in heads:
                        B_[hd] = dict(
                            W4=H("W4", [128, 8, 4, 128], BF16), QT=H("QT", [128, NT], BF16), KT=H("KT", [128, NT], BF16),
                            KTOK=H("KTOK", [128, NCH, 128], BF16), VP=H("VP", [128, NCH, 129], BF16), SO=H("SO", [128, NCH, 128], BF16),
                            HS=H("HS", [128, NCH, 128], BF16), SALL=H("SALL", [128, NCH, 128], BF16))
                    U_ = {}
                    for hd in heads:
                        for d in range(2):
                            U_[(hd, d)] = dict(CS=H("CS", [128, 129]), CSB=H("CSB", [128, 129], BF16), LFB=H("LFB", [128, 128]),
                                               DT=H("DT", [128, 128], BF16), PT=[H("PT", [128, 128], BF16) for _ in range(2)], TB=H("TB", [128, 129]),
                                               DEN=H("DEN", [128, 2]), VW=[H("VW", [128, 129], BF16) for _ in range(2)])
                            U_[(hd, d)]['NUM'] = U_[(hd, d)]['TB']
                    for hd in heads:
                        bb = B_[hd]
                        W4 = bb['W4']
                        self.dmas('pool', [(W4[:, :, i, :], w_in[:, :, i * 512 + hd * 128:i * 512 + (hd + 1) * 128]) for i in range(4)], writes=[('W4', hd)])
                        self.memset(bb['HS'][:], 0.0, [('HS', hd, c) for c in range(NCH)])
                        self.memset(bb['VP'][:, :, 128:129], 1.0, [('VP1', hd)], eng='pool')
                        for d in range(2):
                            self.memset(U_[(hd, d)]['CS'][:], 0.0, [('CS', hd, d)])
                            self.memset(U_[(hd, d)]['CSB'][:], 0.0, [('CSB', hd, d)], eng='pool')
                    cnt = 0
                    for hd in heads:
                        bb = B_[hd]
                        for ti, (t0, n) in enumerate(TILES):
                            hk = tk('HT', t0, n)
                            for i, nm in enumerate(['QT', 'KT']):
                                bank = cnt % 4
                                cnt += 1
                                for k in range(8):
                                    self.mm(PS[bank][:, :n], bb['W4'][:, k, i, :], self.HT[:, k, t0:t0 + n], k == 0, k == 7, hk + [('W4', hd)], [('ps', bank)])
                                self.act(bb[nm][:, t0:t0 + n], PS[bank][:, :n], AF.Copy, [('ps', bank)], [(nm, hd, c) for c in range(t0 // 128, (t0 + n) // 128)],
                                         scale=1.0 if i == 0 else 128.0 ** -0.5)
                    for hd in heads:
                        bb = B_[hd]
                        for ch in range(NCH):
                            bank = 4 + ch % 2
                            sbk = 6 + ch % 2
                            tsl = slice(ch * 128, (ch + 1) * 128)
                            hk = tk('HT', ch * 128, 128)
                            for k in range(8):
                                self.mm(PS[bank][:, 0:384], self.HT[:, k, tsl], bb['W4'][:, k, 1:4, :].rearrange("p a n -> p (a n)"),
                                        k == 0, k == 7, hk + [('W4', hd)], [('ps', bank)])
                            self.act(bb['KTOK'][:, ch, :], PS[bank][:, 0:128], AF.Copy, [('ps', bank)], [('KTOK', hd, ch)], scale=128.0 ** -0.5)
                            self.cp(bb['VP'][:, ch, 0:128], PS[bank][:, 128:256], [('ps', bank)], [('VP', hd, ch)])
                            self.act(bb['SO'][:, ch, :], PS[bank][:, 256:384], AF.Sigmoid, [('ps', bank)], [('SO', hd, ch)])
                            self.mm(PS[sbk][:, 0:128], bb['KT'][:, tsl], bb['QT'][:, tsl], True, True, [('KT', hd, ch), ('QT', hd, ch)], [('ps', sbk)])
                            self.cp(bb['SALL'][:, ch, :], PS[sbk][:, 0:128], [('ps', sbk)], [('SALL', hd, ch)])
                    units = [(hd, d) for hd in heads for d in range(2)]

                    def mk_ctxs(step):
                        ctxs = []
                        for ui, (hd, d) in enumerate(units):
                            ch = order[d][step]
                            ctxs.append((ui, hd, d, ch, slice(ch * 128, (ch + 1) * 128), B_[hd], U_[(hd, d)], 2 * ui, 2 * ui + 1))
                        return ctxs

                    def front(step):
                        ctxs = mk_ctxs(step)
                        sp_ = step % 2
                        for (ui, hd, d, ch, tsl, bb, uu, bx, by) in ctxs:
                            self.act(uu['LFB'][:], self.ONESF[:], AF.Identity, [('LF', ch), 'ONESF'], [('LFB', hd, d)], scale=LF[:, ch, d * 4 + hd:d * 4 + hd + 1])
                        for (ui, hd, d, ch, tsl, bb, uu, bx, by) in ctxs:
                            self.mm(PS[bx][:, 0:128], uu['LFB'][:], self.TRI[:, 3 * d + 0, :], True, False, [('LFB', hd, d), 'TRI'], [('ps', bx)])
                            self.mm(PS[bx][:, 0:128], self.IDF[:], self.TRI[:, 3 * d + 2, :], False, True, ['IDF', 'TRI'], [('ps', bx)])
                        for (ui, hd, d, ch, tsl, bb, uu, bx, by) in ctxs:
                            self.act(uu['DT'][:], PS[bx][:, 0:128], AF.Exp, [('ps', bx), ('BIAS', ch, d)], [('DT', hd, d)], bias=BIAS[:, ch, d, hd:hd + 1], scale=1.0)
                        for (ui, hd, d, ch, tsl, bb, uu, bx, by) in ctxs:
                            self.tt(uu['PT'][sp_][:], bb['SALL'][:, ch, :], uu['DT'][:], ALU.mult, [('SALL', hd, ch), ('DT', hd, d)], [('PT', hd, d, sp_)], eng='pool')
                            self.ts(uu['VW'][sp_][:], bb['VP'][:, ch, :], WE[:, ch, d, hd:hd + 1], None, ALU.mult, None,
                                    [('VP', hd, ch), ('VP1', hd), ('WE', ch, d)], [('VW', hd, d, sp_)])

                    def back(step):
                        ctxs = mk_ctxs(step)
                        sp_ = step % 2
                        for (ui, hd, d, ch, tsl, bb, uu, bx, by) in ctxs:
                            self.mm(PS[by][:, 0:129], uu['PT'][sp_][:], bb['VP'][:, ch, :], True, True, [('PT', hd, d, sp_), ('VP', hd, ch), ('VP1', hd)], [('ps', by)])
                            self.mm(PS[by][:, 129:258], bb['QT'][:, tsl], uu['CSB'][:], True, True, [('QT', hd, ch), ('CSB', hd, d)], [('ps', by)])
                            self.mm(PS[by][:, 258:387], bb['KTOK'][:, ch, :], uu['VW'][sp_][:], True, True, [('KTOK', hd, ch), ('VW', hd, d, sp_)], [('ps', by)])
                        for (ui, hd, d, ch, tsl, bb, uu, bx, by) in ctxs:
                            self.act(uu['TB'][:], PS[by][:, 129:258], AF.Identity, [('ps', by), ('GJ', ch, d)], [('TB', hd, d)], scale=GJ[:, ch, d, hd:hd + 1])
                        for (ui, hd, d, ch, tsl, bb, uu, bx, by) in ctxs:
                            self.stt(uu['CS'][:], uu['CS'][:], DEC[:, ch, d, hd:hd + 1], PS[by][:, 258:387], ALU.mult, ALU.add,
                                     [('CS', hd, d), ('DEC', ch, d), ('ps', by)], [('CS', hd, d)])
                            self.tt(uu['NUM'][:], PS[by][:, 0:129], uu['TB'][:], ALU.add, [('ps', by), ('TB', hd, d)], [('NUM', hd, d), ('TB', hd, d)])
                        for (ui, hd, d, ch, tsl, bb, uu, bx, by) in ctxs:
                            self.act(uu['CSB'][:], uu['CS'][:], AF.Copy, [('CS', hd, d)], [('CSB', hd, d)])
                        for (ui, hd, d, ch, tsl, bb, uu, bx, by) in ctxs:
                            DEN = uu['DEN']
                            self.stt(DEN[:, 0:1], uu['NUM'][:, 128:129], -1.0, uu['NUM'][:, 128:129], ALU.mult, ALU.max, [('NUM', hd, d), ('TB', hd, d)], [('DEN', hd, d, 0)])
                        for (ui, hd, d, ch, tsl, bb, uu, bx, by) in ctxs:
                            DEN = uu['DEN']
                            self.ts(DEN[:, 0:1], DEN[:, 0:1], 1.0, None, ALU.max, None, [('DEN', hd, d, 0)], [('DEN', hd, d, 0)])
                        for (ui, hd, d, ch, tsl, bb, uu, bx, by) in ctxs:
                            DEN = uu['DEN']
                            self.recip(DEN[:, 1:2], DEN[:, 0:1], [('DEN', hd, d, 0)], [('DEN', hd, d, 1)])
                        for (ui, hd, d, ch, tsl, bb, uu, bx, by) in ctxs:
                            DEN = uu['DEN']
                            self.stt(bb['HS'][:, ch, :], uu['NUM'][:, 0:128], DEN[:, 1:2], bb['HS'][:, ch, :], ALU.mult, ALU.add,
                                     [('NUM', hd, d), ('TB', hd, d), ('DEN', hd, d, 1), ('HS', hd, ch)], [('HS', hd, ch)])

                    front(0)
                    for step in range(NCH):
                        if step + 1 < NCH:
                            front(step + 1)
                        back(step)
                    SSQ = H("SSQ", [128, 4]); JK = [H("JK0", [128, 128])] * 2; T1 = [H("T10", [128, 128])] * 2
                    MT = [H("MT%d" % i, [128, 128], BF16) for i in range(2)]
                    for hi_, hd in enumerate(heads):
                        bb = B_[hd]
                        for ch in range(NCH):
                            bank = (hi_ * NCH + ch) % 4
                            s2 = ch % 2
                            o0 = 2 * s2
                            self.act(JK[s2][:], bb['HS'][:, ch, :], AF.Square, [('HS', hd, ch)], [('JK', 0)])
                            self.p.op('dve', lambda e, s2=s2, o0=o0, JK=JK, SSQ=SSQ: e.reduce_sum(out=SSQ[:, o0:o0 + 1], in_=JK[s2][:], axis=AX.X), [('JK', 0)], [('SSQ', o0)])
                            self.act(SSQ[:, o0:o0 + 1], SSQ[:, o0:o0 + 1], AF.Sqrt, [('SSQ', o0)], [('SSQ', o0)], bias=self.EPSC[:, 0:1], scale=1.0 / 128)
                            self.recip(SSQ[:, o0 + 1:o0 + 2], SSQ[:, o0:o0 + 1], [('SSQ', o0)], [('SSQ', o0 + 1)])
                            self.stt(T1[s2][:], bb['HS'][:, ch, :], SSQ[:, o0 + 1:o0 + 2], MLN[:, hd * 128:(hd + 1) * 128], ALU.mult, ALU.mult,
                                     [('HS', hd, ch), ('SSQ', o0 + 1), 'MLN'], [('T1', 0)])
                            self.tt(MT[s2][:], T1[s2][:], bb['SO'][:, ch, :], ALU.mult, [('T1', 0), ('SO', hd, ch)], [('MT', s2)])
                            pv = PS[bank][:].bitcast(BF16)
                            self.tr(pv[:, 0:128], MT[s2][:], self.IDB[:], [('MT', s2), 'IDB'], [('ps', bank)])
                            self.act(self.MIXT[:, hd, ch * 128:(ch + 1) * 128], pv[:, 0:128], AF.Copy, [('ps', bank)], [('MIXT', ch)])
                    p.barrier()

    def attention(self, b):
        p, PS, nc = self.p, self.PS, self.nc
        w_in = self.ev_w_in[0].rearrange("(k p) n -> p k n", p=128)
        with self.scope() as ph:
            P = lambda name, shape, dt=F32: self.sb(ph, name, shape, dt)
            WA = P("WA", [128, 8, 768], BF16)
            GQ = P("GQ", [128, 640]); RC = P("RC", [128, 16, 64]); RSN = P("RSN", [128, 16, 64])
            QT4 = P("QT4", [128, 4, NT], BF16); KT = P("KTa", [128, NT], BF16); VP = P("VPa", [128, NCH, 2, 65], BF16)
            ATT = P("ATT", [128, NCH, 512], BF16)
            QS = [P("QS%d" % i, [128, 768]) for i in range(2)]
            SQ = P("SQa", [128, 640]); SSQ = P("SSQa", [128, 10]); RSTD = P("RSTDa", [128, 10])
            QN1 = P("QN1", [128, 640]); T1 = P("T1a", [128, 640]); T2 = P("T2a", [128, 640])
            QRq = P("QRq", [128, 4, 2, 64], BF16); QRk = P("QRk", [128, 128], BF16)
            ET = [P("ET%d" % i, [128, 512], BF16) for i in range(3)]
            RD = [P("RD%d" % i, [128, 4]) for i in range(2)]
            p.dma('pool', [lambda e, i=i: e.dma_start(out=WA[:, i * 4:(i + 1) * 4, :], in_=w_in[:, i * 4:(i + 1) * 4, 2064:2832]) for i in range(2)], writes=['WA'])
            p.dma('sp', [lambda e, i=i: e.dma_start(out=GQ[:, i * 64:(i + 1) * 64], in_=self.at_q_norm[0].partition_broadcast(128)) for i in range(8)] +
                        [lambda e, i=i: e.dma_start(out=GQ[:, 512 + i * 64:512 + (i + 1) * 64], in_=self.at_k_norm[0].partition_broadcast(128)) for i in range(2)],
                  writes=['GQ'])
            p.dma('act', [lambda e: e.dma_start(out=RC[:], in_=self.k_rope[0].rearrange("(c p) d -> p c d", p=128)),
                          lambda e: e.dma_start(out=RSN[:], in_=self.k_rope[1].rearrange("(c p) d -> p c d", p=128))], writes=['ROPE'])
            self.ts(GQ[:, 0:512], GQ[:, 0:512], 0.125, None, ALU.mult, None, ['GQ'], ['GQ'])
            self.memset(VP[:, :, :, 64:65], 1.0, [('VP1',)], eng='pool')
            for ch in range(NCH):
                hk = tk('HT', ch * 128, 128)
                qs = QS[ch % 2]
                bA, bB, bT = (ch % 2) * 3, (ch % 2) * 3 + 1, (ch % 2) * 3 + 2
                for k in range(8):
                    self.mm(PS[bA][:, 0:512], self.HT[:, k, ch * 128:(ch + 1) * 128], WA[:, k, 0:512], k == 0, k == 7, hk + ['WA'], [('ps', bA)])
                for k in range(8):
                    self.mm(PS[bB][:, 0:256], self.HT[:, k, ch * 128:(ch + 1) * 128], WA[:, k, 512:768], k == 0, k == 7, hk + ['WA'], [('ps', bB)])
                self.act(qs[:, 0:512], PS[bA][:, 0:512], AF.Copy, [('ps', bA)], [('QS', ch % 2)])
                self.act(qs[:, 512:768], PS[bB][:, 0:256], AF.Copy, [('ps', bB)], [('QS', ch % 2)])
                self.act(SQ[:], qs[:, 0:640], AF.Square, [('QS', ch % 2)], ['SQ'])
                self.p.op('dve', lambda e: e.tensor_reduce(out=SSQ[:], in_=SQ[:].rearrange("p (h d) -> p h d", d=64), axis=AX.X, op=ALU.add), ['SQ'], ['SSQ'])
                self.act(SSQ[:], SSQ[:], AF.Sqrt, ['SSQ'], ['SSQ'], bias=self.EPSC[:, 0:1], scale=1.0 / 64)
                self.recip(RSTD[:], SSQ[:], ['SSQ'], ['RSTD'])
                self.tt(QN1[:].rearrange("p (h d) -> p h d", d=64), qs[:, 0:640].rearrange("p (h d) -> p h d", d=64),
                        RSTD[:].unsqueeze(2).to_broadcast([128, 10, 64]), ALU.mult, [('QS', ch % 2), 'RSTD'], ['QN1'])
                self.tt(QN1[:], QN1[:], GQ[:], ALU.mult, ['QN1', 'GQ'], ['QN1'])
                qdst = QRq[:].rearrange("p pr hl d -> p hl pr d")
                if ch >= 2:
                    lc = ch - 2
                    self.tt(T1[:].rearrange("p (h d) -> p h d", d=64), QN1[:].rearrange("p (h d) -> p h d", d=64),
                            RC[:, lc, :].unsqueeze(1).to_broadcast([128, 10, 64]), ALU.mult, ['QN1', 'ROPE'], ['T1'], eng='pool')
                    q3 = QN1[:].rearrange("p (h d) -> p h d", d=64)
                    t3 = T2[:].rearrange("p (h d) -> p h d", d=64)
                    for rc in range(2):
                        for f in range(2):
                            o0 = rc * 32 + f * 16
                            i0 = rc * 32 + (1 - f) * 16
                            self.tt(t3[:, :, o0:o0 + 16], q3[:, :, i0:i0 + 16], RSN[:, lc, o0:o0 + 16].unsqueeze(1).to_broadcast([128, 10, 16]),
                                    ALU.mult, ['QN1', 'ROPE'], ['T2'])
                    self.tt(qdst, T1[:, 0:512].rearrange("p (hl pr d) -> p hl pr d", hl=2, pr=4), T2[:, 0:512].rearrange("p (hl pr d) -> p hl pr d", hl=2, pr=4),
                            ALU.add, ['T1', 'T2'], ['QRq'])
                    self.tt(QRk[:], T1[:, 512:640], T2[:, 512:640], ALU.add, ['T1', 'T2'], ['QRk'])
                else:
                    self.cp(qdst, QN1[:, 0:512].rearrange("p (hl pr d) -> p hl pr d", hl=2, pr=4), ['QN1'], ['QRq'])
                    self.cp(QRk[:], QN1[:, 512:640], ['QN1'], ['QRk'])
                self.cp(VP[:, ch, :, 0:64], qs[:, 640:768].rearrange("p (k d) -> p k d", d=64), [('QS', ch % 2)], [('VP', ch)], eng='pool')
                pv = PS[bT][:].bitcast(BF16)
                for pr in range(4):
                    self.tr(pv[:, pr * 128:(pr + 1) * 128], QRq[:, pr, :, :].rearrange("p a d -> p (a d)"), self.IDB[:], ['QRq', 'IDB'], [('ps', bT)])
                self.tr(pv[:, 512:640], QRk[:], self.IDB[:], ['QRk', 'IDB'], [('ps', bT)])
                self.act(QT4[:, :, ch * 128:(ch + 1) * 128], pv[:, 0:512].rearrange("p (a t) -> p a t", t=128), AF.Copy, [('ps', bT)], [('QT4', ch)])
                self.cp(KT[:, ch * 128:(ch + 1) * 128], pv[:, 512:640], [('ps', bT)], [('KTa', ch)])
            p.barrier()
            jobs = []
            for h in range(8):
                jobs.append((h, 0, 256, [0, 1]))
                for i in range(4):
                    jobs.append((h, 256 + 512 * i, 512, list(range(NCH))))
            sct = 0
            for ji, (h, q0, n, kcs) in enumerate(jobs):
                hl, pr = h // 4, h % 4
                ob = 4 + ji % 4
                nsub = n // 128
                qk = [('QT4', c) for c in range(q0 // 128, (q0 + n) // 128)]

                def issue_s(ki, sct):
                    kc = kcs[ki]
                    sb_ = sct % 4
                    self.mm(PS[sb_][:, :n], KT[hl * 64:(hl + 1) * 64, kc * 128:(kc + 1) * 128], QT4[hl * 64:(hl + 1) * 64, pr, q0:q0 + n],
                            True, True, [('KTa', kc)] + qk, [('ps', sb_)])
                issue_s(0, sct)
                for ki, kc in enumerate(kcs):
                    if ki + 1 < len(kcs):
                        issue_s(ki + 1, sct + 1)
                    sb_ = sct % 4
                    et = ET[sct % 3]
                    self.act(et[:, :n], PS[sb_][:, :n], AF.Exp, [('ps', sb_)], [('ET', sct % 3)])
                    for j in range(nsub):
                        self.mm(PS[ob][:, j * 65:(j + 1) * 65], et[:, j * 128:(j + 1) * 128], VP[:, kc, hl, :], ki == 0 and j == 0, ki == len(kcs) - 1,
                                [('ET', sct % 3), ('VP', kc), ('VP1',)], [('ps', ob)], skip_group_check=True)
                    sct += 1
                rd = RD[ji % 2]
                ov = PS[ob][:, 0:nsub * 65].rearrange("p (j d) -> p j d", d=65)
                self.recip(rd[:, 0:nsub], ov[:, :, 64], [('ps', ob)], [('RD', ji % 2)])
                c0 = q0 // 128
                self.tt(ATT[:, c0:c0 + nsub, h * 64:(h + 1) * 64], ov[:, :, 0:64], rd[:, 0:nsub].unsqueeze(2).to_broadcast([128, nsub, 64]), ALU.mult,
                        [('ps', ob), ('RD', ji % 2)], [('ATT', c) for c in range(c0, c0 + nsub)])
            for ch in range(NCH):
                bank = ch % 4
                pv = PS[bank][:].bitcast(BF16)
                for j in range(4):
                    self.tr(pv[:, j * 128:(j + 1) * 128], ATT[:, ch, j * 128:(j + 1) * 128], self.IDB[:], [('ATT', ch), 'IDB'], [('ps', bank)])
                self.act(self.MIXT[:, 4:8, ch * 128:(ch + 1) * 128], pv[:, 0:512].rearrange("p (a t) -> p a t", t=128), AF.Copy, [('ps', bank)], [('MIXT', ch)])
            p.barrier()

    def outproj(self, b, l, w_out):
        p, PS = self.p, self.PS
        with self.scope() as ph:
            WO = self.sb(ph, "WO", [128, 8, 1024], BF16)
            src = w_out.rearrange("(k p) n -> p k n", p=128)
            p.dma('pool', [lambda e, i=i: e.dma_start(out=WO[:, i * 4:(i + 1) * 4, :], in_=src[:, i * 4:(i + 1) * 4, :]) for i in range(2)], writes=['WO'])
            cnt = 0
            for ti, (t0, n) in enumerate(TILES):
                if l == 1 and ti == 0:
                    continue
                j = 2 if ti == 0 else b
                mk = tk('MIXT', t0, n)
                xk = tk('X', t0, n)
                for c in range(8):
                    bank = cnt % 4
                    cnt += 1
                    for k in range(8):
                        self.mm(PS[bank][:, :n], WO[:, k, c * 128:(c + 1) * 128], self.MIXT[:, k, t0:t0 + n], k == 0, k == 7, mk + ['WO'], [('ps', bank)])
                    self.stt(self.X[:, c, t0:t0 + n], PS[bank][:, :n], self.MOD[:, l, 16 + c, j:j + 1], self.X[:, c, t0:t0 + n], ALU.mult, ALU.add,
                             [('ps', bank)] + xk, xk)
            p.barrier()

    def moe(self, b, l):
        p, PS = self.p, self.PS
        tiles = TILES[1:] if l == 1 else TILES
        with self.scope() as ph:
            P = lambda name, shape, dt=F32: self.sb(ph, name, shape, dt)
            COMBT = P("COMBT", [16, NT], BF16)
            SEL = P("SEL", [16, 16, 128], BF16)
            WGU = [None, None]
            WD = [None, None]
            p.dma('pool', lambda e: e.dma_start(out=SEL[:], in_=self.k_sel.rearrange("e r n -> r e n")), writes=['SEL'])

            def load_expert(e):
                slot = e % 2
                g = self.moe_w_gate[l, e].rearrange("(k p) n -> p k n", p=128)
                u = self.moe_w_up[l, e].rearrange("(k p) n -> p k n", p=128)
                dn = self.moe_w_down[l, e].rearrange("(k p) n -> p k n", p=128)
                p.dma('pool', [lambda en, i=i: en.dma_start(out=WGU[slot][:, i * 4:(i + 1) * 4, 0:512], in_=g[:, i * 4:(i + 1) * 4, :]) for i in range(2)] +
                              [lambda en, i=i: en.dma_start(out=WGU[slot][:, i * 4:(i + 1) * 4, 512:1024], in_=u[:, i * 4:(i + 1) * 4, :]) for i in range(2)],
                      writes=[('WGU', slot)])
                p.dma('pool', [lambda en, i=i: en.dma_start(out=WD[slot][:, i * 2:(i + 1) * 2, :], in_=dn[:, i * 2:(i + 1) * 2, :]) for i in range(2)],
                      writes=[('WD', slot)])
            with self.scope() as rs:
                R = lambda name, shape, dt=F32: self.sb(rs, name, shape, dt)
                WR = R("WR", [128, 8, 20]); RB = R("RB", [128, 20]); L = R("L", [128, 4, 20])
                SM = R("SM", [128, 12, 4]); GM = R("GM", [128, 4, 4]); GE = R("GE", [128, 4, 4]); PEN = R("PEN", [128, 4, 4])
                EM = R("EM", [128, 4, 16]); EM2 = R("EM2", [128, 4, 16]); M1 = R("M1", [128, 4, 16]); M2 = R("M2", [128, 4, 16]); COMB = R("COMB", [128, 4, 16])
                p.dma('sp', [lambda e: e.dma_start(out=WR[:, :, 0:4], in_=self.moe_gr_w[l].rearrange("(k p) n -> p k n", p=128)),
                             lambda e: e.dma_start(out=WR[:, :, 4:20], in_=self.moe_er_w[l].rearrange("(k p) n -> p k n", p=128)),
                             lambda e: e.dma_start(out=RB[:, 0:4], in_=self.moe_gr_b[l].partition_broadcast(128)),
                             lambda e: e.dma_start(out=RB[:, 4:20], in_=self.moe_er_b[l].partition_broadcast(128))], writes=['WR', 'RB'])

                def router(ti, t0, n, H32):
                    ns = n // 128
                    rb, tb = 2, 4
                    for sub in range(ns):
                        for k in range(8):
                            self.mm(PS[rb][:, sub * 20:(sub + 1) * 20], H32[:, k, sub * 128:(sub + 1) * 128], WR[:, k, :], k == 0, k == 7,
                                    [('H32', k), 'WR'], [('ps', rb)], skip_group_check=True)
                    lv = L[:, 0:ns, :]
                    self.tt(lv, PS[rb][:, 0:ns * 20].rearrange("p (s c) -> p s c", c=20), RB[:].unsqueeze(1).to_broadcast([128, ns, 20]), ALU.add,
                            [('ps', rb), 'RB'], ['L'])
                    sm = lambda i: SM[:, i, 0:ns]
                    bc4 = lambda a: a.unsqueeze(2).to_broadcast([128, ns, 4])
                    bc16 = lambda a: a.unsqueeze(2).to_broadcast([128, ns, 16])
                    lg = L[:, 0:ns, 0:4]
                    self.p.op('dve', lambda e, ns=ns: e.tensor_reduce(out=SM[:, 0, 0:ns], in_=L[:, 0:ns, 0:4], axis=AX.X, op=ALU.max), ['L'], [('SM', 0)])
                    self.tt(GM[:, 0:ns, :], lg, bc4(sm(0)), ALU.is_equal, ['L', ('SM', 0)], ['GM'])
                    self.tt(GE[:, 0:ns, :], lg, bc4(sm(0)), ALU.subtract, ['L', ('SM', 0)], ['GE'])
                    self.act(GE[:, 0:ns, :], GE[:, 0:ns, :], AF.Exp, ['GE'], ['GE'])
                    self.p.op('dve', lambda e, ns=ns: e.tensor_reduce(out=SM[:, 2, 0:ns], in_=GE[:, 0:ns, :], axis=AX.X, op=ALU.add), ['GE'], [('SM', 2)])
                    self.recip(sm(3), sm(2), [('SM', 2)], [('SM', 3)])
                    self.ts(PEN[:, 0:ns, :], GM[:, 0:ns, :], 1e30, -1e30, ALU.mult, ALU.add, ['GM'], ['PEN'])
                    self.tt(EM[:, 0:ns, :].rearrange("p s (g e) -> p s g e", e=4), L[:, 0:ns, 4:20].rearrange("p s (g e) -> p s g e", e=4),
                            PEN[:, 0:ns, :].unsqueeze(3).to_broadcast([128, ns, 4, 4]), ALU.add, ['L', 'PEN'], ['EM'])
                    self.p.op('dve', lambda e, ns=ns: e.tensor_reduce(out=SM[:, 4, 0:ns], in_=EM[:, 0:ns, :], axis=AX.X, op=ALU.max), ['EM'], [('SM', 4)])
                    self.tt(M1[:, 0:ns, :], EM[:, 0:ns, :], bc16(sm(4)), ALU.is_equal, ['EM', ('SM', 4)], ['M1'])
                    self.stt(EM2[:, 0:ns, :], M1[:, 0:ns, :], -1e30, EM[:, 0:ns, :], ALU.mult, ALU.add, ['M1', 'EM'], ['EM2'])
                    self.p.op('dve', lambda e, ns=ns: e.tensor_reduce(out=SM[:, 5, 0:ns], in_=EM2[:, 0:ns, :], axis=AX.X, op=ALU.max), ['EM2'], [('SM', 5)])
                    self.tt(M2[:, 0:ns, :], EM2[:, 0:ns, :], bc16(sm(5)), ALU.is_equal, ['EM2', ('SM', 5)], ['M2'])
                    self.tt(sm(6), sm(5), sm(4), ALU.subtract, [('SM', 5), ('SM', 4)], [('SM', 6)])
                    self.act(sm(7), sm(6), AF.Exp, [('SM', 6)], [('SM', 7)])
                    self.ts(sm(8), sm(7), 1.0, None, ALU.add, None, [('SM', 7)], [('SM', 8)])
                    self.recip(sm(9), sm(8), [('SM', 8)], [('SM', 9)])
                    self.tt(sm(10), sm(9), sm(3), ALU.mult, [('SM', 9), ('SM', 3)], [('SM', 10)])
                    self.tt(sm(11), sm(10), sm(7), ALU.mult, [('SM', 10), ('SM', 7)], [('SM', 11)])
                    self.tt(COMB[:, 0:ns, :], M1[:, 0:ns, :], bc16(sm(10)), ALU.mult, ['M1', ('SM', 10)], ['COMB'])
                    self.tt(M2[:, 0:ns, :], M2[:, 0:ns, :], bc16(sm(11)), ALU.mult, ['M2', ('SM', 11)], ['M2'])
                    self.tt(COMB[:, 0:ns, :], COMB[:, 0:ns, :], M2[:, 0:ns, :], ALU.add, ['COMB', 'M2'], ['COMB'])
                    for sub in range(ns):
                        self.tr(PS[tb][0:16, sub * 128:(sub + 1) * 128], COMB[:, sub, :], self.IDF[:], ['COMB', 'IDF'], [('ps', tb)])
                    self.act(COMBT[:, t0:t0 + n], PS[tb][0:16, 0:n], AF.Copy, [('ps', tb)], tk('COMBT', t0, n))
                self.norm(b, l, 1, f32_cb=router, skip_ctx=(l == 1))
            for i in range(2):
                WGU[i] = P("WGU%d" % i, [128, 8, 1024], BF16)
                WD[i] = P("WD%d" % i, [128, 4, 1024], BF16)
            load_expert(0)
            load_expert(1)
            CB = [P("CB%d" % i, [128, 512], BF16) for i in range(2)]
            SG = [P("SG%d" % i, [128, 512], BF16) for i in range(2)]
            ACTT = [P("ACTT%d" % i, [128, 4, 512], BF16) for i in range(2)]
            items = [(e, ti) for e in range(16) for ti in range(len(tiles))]

            def stage_a(ii):
                e, ti = items[ii]
                t0, n = tiles[ti]
                slot = e % 2
                hk = tk('HT', t0, n)
                cb = CB[ii % 2]
                self.mm(PS[0][:, :n], SEL[:, e, :], COMBT[:, t0:t0 + n], True, True, ['SEL'] + tk('COMBT', t0, n), [('ps', 0)])
                self.act(cb[:, :n], PS[0][:, :n], AF.Copy, [('ps', 0)], [('CB', ii % 2)])
                for j in range(4):
                    gb = 1 + j % 2
                    ub = 3 + j % 2
                    sg = SG[j % 2]
                    for k in range(8):
                        self.mm(PS[gb][:, :n], WGU[slot][:, k, j * 128:(j + 1) * 128], self.HT[:, k, t0:t0 + n], k == 0, k == 7,
                                hk + [('WGU', slot)], [('ps', gb)])
                    for k in range(8):
                        self.mm(PS[ub][:, :n], WGU[slot][:, k, 512 + j * 128:512 + (j + 1) * 128], self.HT[:, k, t0:t0 + n], k == 0, k == 7,
                                hk + [('WGU', slot)], [('ps', ub)])
                    self.act(sg[:, :n], PS[gb][:, :n], AF.Silu, [('ps', gb)], [('SG', j % 2)])
                    self.tt(sg[:, :n], sg[:, :n], cb[:, :n], ALU.mult, [('SG', j % 2), ('CB', ii % 2)], [('SG', j % 2)])
                    self.tt(ACTT[ii % 2][:, j, :n], sg[:, :n], PS[ub][:, :n], ALU.mult, [('SG', j % 2), ('ps', ub)], [('ACTT', ii % 2, j)])

            def stage_b(ii):
                e, ti = items[ii]
                t0, n = tiles[ti]
                slot = e % 2
                j_ = 2 if (l == 0 and ti == 0) else b
                xk = tk('X', t0, n)
                for c in range(8):
                    db = 5 + c % 3
                    for j in range(4):
                        self.mm(PS[db][:, :n], WD[slot][:, j, c * 128:(c + 1) * 128], ACTT[ii % 2][:, j, :n], j == 0, j == 3,
                                [('WD', slot), ('ACTT', ii % 2, j)], [('ps', db)])
                    self.stt(self.X[:, c, t0:t0 + n], PS[db][:, :n], self.MOD[:, l, 40 + c, j_:j_ + 1], self.X[:, c, t0:t0 + n], ALU.mult, ALU.add,
                             [('ps', db)] + xk, xk)
            stage_a(0)
            for ii in range(len(items)):
                if ii + 1 < len(items):
                    stage_a(ii + 1)
                stage_b(ii)
                e_, ti_ = items[ii]
                if ti_ == len(tiles) - 1 and e_ + 2 < 16:
                    load_expert(e_ + 2)
            p.barrier()

    def write_out(self, b, out):
        p, PS = self.p, self.PS
        with self.scope() as ph:
            OT = [self.sb(ph, "OT%d" % i, [128, 1024], F32) for i in range(2)]
            for ch in range(2, NCH):
                s = ch % 2
                for hf in range(2):
                    bank = (ch * 2 + hf) % 4
                    for kk in range(4):
                        k = hf * 4 + kk
                        self.tr(PS[bank][:, kk * 128:(kk + 1) * 128], self.X[:, k, ch * 128:(ch + 1) * 128], self.IDF[:], [('X', ch), 'IDF'], [('ps', bank)])
                    if hf == 0:
                        self.act(OT[s][:, 0:512], PS[bank][:], AF.Copy, [('ps', bank)], [('OT', s, 0)])
                    else:
                        self.cp(OT[s][:, 512:1024], PS[bank][:], [('ps', bank)], [('OT', s, 1)])
                p.dma('sp' if ch % 2 == 0 else 'act', lambda e, s=s, ch=ch: e.dma_start(out=out[b, (ch - 2) * 128:(ch - 1) * 128, :], in_=OT[s][:]),
                      reads=[('OT', s, 0), ('OT', s, 1)])
            p.barrier()

    def cis(self, R, MAG, ORE, OIM, I, F, G, S, key, neg_im=False):
        TWO_PI = 6.2831845
        MAGIC = 12582912.0
        k = lambda n: (key, n)
        self.ts(I, R, MAGIC, None, ALU.add, None, [k('R')], [k('I')])
        self.ts(I, I, MAGIC, None, ALU.subtract, None, [k('I')], [k('I')])
        self.tt(F, R, I, ALU.subtract, [k('R'), k('I')], [k('F')])
        self.act(S, F, AF.Sin, [k('F')], [k('S')], scale=TWO_PI)
        self.act(G, MAG, AF.Exp, [k('MAG'), k('G')], [k('G')])
        if neg_im:
            self.stt(OIM, G, -1.0, S, ALU.mult, ALU.mult, [k('G'), k('S')], [k('OIM')])
        else:
            self.tt(OIM, G, S, ALU.mult, [k('G'), k('S')], [k('OIM')])
        self.ts(F, F, 0.25, None, ALU.add, None, [k('F')], [k('F')])
        self.ts(I, F, MAGIC, None, ALU.add, None, [k('F')], [k('I')])
        self.ts(I, I, MAGIC, None, ALU.subtract, None, [k('I')], [k('I')])
        self.tt(F, F, I, ALU.subtract, [k('F'), k('I')], [k('F')])
        self.act(S, F, AF.Sin, [k('F')], [k('S')], scale=TWO_PI)
        self.tt(ORE, G, S, ALU.mult, [k('G'), k('S')], [k('ORE')])

    def s5(self, b):
        p, PS, nc = self.p, self.PS, self.nc
        w_in = self.od_w_in[0].rearrange("(k p) n -> p k n", p=128)
        order = [list(range(NCH)), [1, 0] + list(range(NCH - 1, 1, -1))]
        INV2PI = 1.0 / (2.0 * math.pi)
        with self.scope() as ph:
            P = lambda name, shape, dt=F32: self.sb(ph, name, shape, dt)
            UT = P("UT", [128, 4, NT], BF16); YT = self.MIXT[:, 0:4, :]
            self.S5_UT, self.S5_YT = UT, YT
            TRIB = P("TRIB", [128, 6, 128], BF16); MASK8 = P("MASK8", [128, 8]); POSC = P("POSC", [128, 4]); POSR = P("POSR", [128, 2, 128])
            TRIBN = P("TRIBN", [128, 2, 128], BF16)
            self.dmas('pool', [(TRIB[:], self.k_tri.rearrange("a p n -> p a n"))], writes=['TRIB'])
            self.ts(TRIBN[:, 0, :], TRIB[:, 0, :], -1.0, None, ALU.mult, None, ['TRIB'], ['TRIBN'])
            self.ts(TRIBN[:, 1, :], TRIB[:, 3, :], -1.0, None, ALU.mult, None, ['TRIB'], ['TRIBN'])
            self.dmas('sp', [(MASK8[:], self.k_mask8), (POSC[:], self.k_posc),
                         (POSR[:], self.k_posr)], writes=['MASK8', 'POSC', 'POSR'])
            with self.scope() as s1:
                WU = self.sb(s1, "WU", [128, 8, 512], BF16)
                self.dmas('pool', [(WU[:, i * 4:(i + 1) * 4, :], w_in[:, i * 4:(i + 1) * 4, 0:512]) for i in range(2)], writes=['WU'])
                cnt = 0
                for ti, (t0, n) in enumerate(TILES):
                    hk = tk('HT', t0, n)
                    for c in range(4):
                        bank = cnt % 4
                        cnt += 1
                        for k in range(8):
                            self.mm(PS[bank][:, :n], WU[:, k, c * 128:(c + 1) * 128], self.HT[:, k, t0:t0 + n], k == 0, k == 7, hk + ['WU'], [('ps', bank)])
                        self.act(UT[:, c, t0:t0 + n], PS[bank][:, :n], AF.Copy, [('ps', bank)], tk('UT', t0, n))
                p.barrier()
            for d in range(2):
                with self.scope() as sd:
                    D = lambda name, shape, dt=F32: self.sb(sd, name, shape, dt)
                    KR = D("KR", [128, 2048], BF16); KI = D("KI", [128, 2048], BF16)
                    QR = D("QR", [128, 16, 128], BF16); QI = D("QI", [128, 16, 128], BF16)
                    BDR = D("BDR", [128, 4, 512], BF16); BDI = D("BDI", [128, 4, 512], BF16)
                    CPR = D("CPR", [128, 16, 128], BF16); CPI = D("CPI", [128, 16, 128], BF16); CPRN = D("CPRN", [128, 16, 128], BF16)
                    with self.scope() as st_:
                        T = lambda name, shape, dt=F32: self.sb(st_, name, shape, dt)
                        AB = T("AB", [128, 512]); OM = T("OM", [128, 512]); LDT = T("LDT", [128, 32])
                        RR = T("RR", [128, 512]); MG = T("MG", [128, 512]); II = T("II", [128, 512])
                        FF = T("FF", [128, 512]); GG = T("GG", [128, 512]); SS = T("SS", [128, 512])
                        self.dmas('sp', [(LDT[:], self.s5_log_dt[0, d].partition_broadcast(128))], writes=['LDT'])
                        self.act(LDT[:], LDT[:], AF.Exp, ['LDT'], ['LDT'])
                        for q in range(4):
                            are_q = self.s5_a_re[0, d].rearrange("g n -> (g n)")[q * 512:(q + 1) * 512]
                            aim_q = self.s5_a_im[0, d].rearrange("g n -> (g n)")[q * 512:(q + 1) * 512]
                            self.dmas('sp', [(AB[:], are_q.partition_broadcast(128)),
                                         (OM[:], aim_q.partition_broadcast(128))],
                                  reads=[('kt', 'ORE'), ('kt', 'OIM')], writes=[('kt', 'ORE'), ('kt', 'OIM')])
                            dtb = LDT[:, q * 8:(q + 1) * 8].unsqueeze(2).to_broadcast([128, 8, 64])
                            v3 = lambda t: t[:].rearrange("p (g n) -> p g n", n=64)
                            self.tt(v3(AB), v3(AB), dtb, ALU.mult, [('kt', 'ORE'), 'LDT'], [('kt', 'ORE')])
                            self.tt(v3(OM), v3(OM), dtb, ALU.mult, [('kt', 'OIM'), 'LDT'], [('kt', 'OIM')])
                            self.ts(RR[:], OM[:], POSC[:, d:d + 1], INV2PI, ALU.mult, ALU.mult, [('kt', 'OIM'), 'POSC'], [('kt', 'R')])
                            self.ts(MG[:], AB[:], POSC[:, 2 + d:3 + d], None, ALU.mult, None, [('kt', 'ORE'), 'POSC'], [('kt', 'MAG')])
                            self.cis(RR[:], MG[:], AB[:], OM[:], II[:], FF[:], GG[:], SS[:], 'kt', neg_im=True)
                            self.cp(KR[:, q * 512:(q + 1) * 512], AB[:], [('kt', 'ORE')], ['KR'])
                            self.cp(KI[:, q * 512:(q + 1) * 512], OM[:], [('kt', 'OIM')], ['KI'])
                        p.barrier()
                    with self.scope() as st_:
                        T = lambda name, shape, dt=F32: self.sb(st_, name, shape, dt)
                        STG = T("STG", [128, 128]); PRM = T("PRM", [128, 32]); LD2 = T("LD2", [128, 16])
                        RHO = T("RHO", [128, 16]); OMT = T("OMT", [128, 16])
                        RR = T("RR", [128, 4, 128]); MG = T("MG", [128, 4, 128]); II = T("II", [128, 4, 128])
                        FF = T("FF", [128, 4, 128]); GG = T("GG", [128, 4, 128]); SS = T("SS", [128, 4, 128])
                        self.memset(STG[:], 0.0, ['STG'])
                        self.dmas('sp', [(STG[0:16, :], self.s5_a_re[0, d].rearrange("(pr g2) n -> pr (g2 n)", g2=2)),
                                     (STG[16:32, :], self.s5_a_im[0, d].rearrange("(pr g2) n -> pr (g2 n)", g2=2))],
                              reads=['STG'], writes=['STG'])
                        ld = self.s5_log_dt[0, d].rearrange("(pr g2) -> g2 pr", g2=2)
                        self.dmas('sp', [(LD2[g2 * 64:(g2 + 1) * 64, :], ld[g2].partition_broadcast(64))
                                     for g2 in range(2)], writes=['LD2'], slow=True)
                        self.tr(PS[0][:, 0:128], STG[:], self.IDF[:], ['STG', 'IDF'], [('ps', 0)])
                        self.cp(PRM[:], PS[0][:, 0:32], [('ps', 0)], ['PRM'])
                        self.act(LD2[:], LD2[:], AF.Exp, ['LD2'], ['LD2'])
                        self.tt(RHO[:], PRM[:, 0:16], LD2[:], ALU.mult, ['PRM', 'LD2'], ['RHO'])
                        self.stt(OMT[:], PRM[:, 16:32], INV2PI, LD2[:], ALU.mult, ALU.mult, ['PRM', 'LD2'], ['OMT'])
                        posb = POSR[:, d, :].unsqueeze(1).to_broadcast([128, 4, 128])
                        f2 = lambda t: t.rearrange("p a b -> p (a b)")
                        for q in range(4):
                            self.tt(RR[:], OMT[:, 4 * q:4 * q + 4].unsqueeze(2).to_broadcast([128, 4, 128]), posb, ALU.mult, ['OMT', 'POSR'], [('qt', 'R')])
                            self.tt(MG[:], RHO[:, 4 * q:4 * q + 4].unsqueeze(2).to_broadcast([128, 4, 128]), posb, ALU.mult, ['RHO', 'POSR'], [('qt', 'MAG')])
                            self.cis(f2(RR[:]), f2(MG[:]), f2(QR[:, 4 * q:4 * q + 4, :]), f2(QI[:, 4 * q:4 * q + 4, :]), f2(II[:]), f2(FF[:]), f2(GG[:]), f2(SS[:]), 'qt')
                        p.barrier()
                    with self.scope() as st_:
                        T = lambda name, shape, dt=F32: self.sb(st_, name, shape, dt)
                        STG = T("STG", [128, 64]); AT = T("AT", [64, 64]); DTB = T("DTB", [64, 32])
                        RHO = T("RHO", [64, 32]); OMT = T("OMT", [64, 32]); ABR = T("ABR", [64, 32]); ABI = T("ABI", [64, 32])
                        II = T("II", [64, 32]); FF = T("FF", [64, 32]); GG = T("GG", [64, 32]); SS = T("SS", [64, 32])
                        DEN = T("DEN", [64, 32]); ZR = T("ZR", [64, 32]); ZI = T("ZI", [64, 32]); TT1 = T("TT1", [64, 32]); TT2 = T("TT2", [64, 32])
                        BRE = T("BRE", [64, 32, 16]); BIM = T("BIM", [64, 32, 16]); BBR = T("BBR", [64, 32, 16]); BBI = T("BBI", [64, 32, 16]); TB3 = T("TB3", [64, 32, 16])
                        TRS = T("TRS", [128, 64]); CC = T("CC", [16, 2, 32, 64], BF16)
                        self.memset(STG[:], 0.0, ['STG'])
                        self.dmas('sp', [(STG[0:32, :], self.s5_a_re[0, d]),
                                     (STG[32:64, :], self.s5_a_im[0, d])], reads=['STG'], writes=['STG'])
                        self.dmas('sp', [(DTB[:], self.s5_log_dt[0, d].partition_broadcast(64)),
                                     (BRE[:], self.s5_b_re[0, d].rearrange("g n c -> n g c")),
                                     (BIM[:], self.s5_b_im[0, d].rearrange("g n c -> n g c"))],
                              writes=['DTB', 'BRE', 'BIM'])
                        self.dmas('pool', [(CC[:, 0], self.s5_c_re[0, d].rearrange("g c n -> c g n")),
                                       (CC[:, 1], self.s5_c_im[0, d].rearrange("g c n -> c g n"))], writes=['CC'])
                        self.tr(PS[0][0:64, 0:128], STG[:], self.IDF[:], ['STG', 'IDF'], [('ps', 0)])
                        self.cp(AT[:], PS[0][0:64, 0:64], [('ps', 0)], ['AT'])
                        self.act(DTB[:], DTB[:], AF.Exp, ['DTB'], ['DTB'])
                        self.tt(RHO[:], AT[:, 0:32], DTB[:], ALU.mult, ['AT', 'DTB'], [('zt', 'MAG')])
                        self.stt(OMT[:], AT[:, 32:64], INV2PI, DTB[:], ALU.mult, ALU.mult, ['AT', 'DTB'], [('zt', 'R')])
                        self.cis(OMT[:], RHO[:], ABR[:], ABI[:], II[:], FF[:], GG[:], SS[:], 'zt')
                        are, aim = AT[:, 0:32], AT[:, 32:64]
                        self.tt(DEN[:], are, are, ALU.mult, ['AT'], ['DEN'])
                        self.tt(TT1[:], aim, aim, ALU.mult, ['AT'], ['TT1'])
                        self.tt(DEN[:], DEN[:], TT1[:], ALU.add, ['DEN', 'TT1'], ['DEN'])
                        self.recip(DEN[:], DEN[:], ['DEN'], ['DEN'])
                        self.ts(ABR[:], ABR[:], -1.0, None, ALU.add, None, [('zt', 'ORE')], [('zt', 'ORE')])
                        self.tt(TT1[:], ABR[:], are, ALU.mult, [('zt', 'ORE'), 'AT'], ['TT1'])
                        self.tt(TT2[:], ABI[:], aim, ALU.mult, [('zt', 'OIM'), 'AT'], ['TT2'])
                        self.tt(TT1[:], TT1[:], TT2[:], ALU.add, ['TT1', 'TT2'], ['TT1'])
                        self.tt(ZR[:], TT1[:], DEN[:], ALU.mult, ['TT1', 'DEN'], ['ZR'])
                        self.tt(TT1[:], ABI[:], are, ALU.mult, [('zt', 'OIM'), 'AT'], ['TT1'])
                        self.tt(TT2[:], ABR[:], aim, ALU.mult, [('zt', 'ORE'), 'AT'], ['TT2'])
                        self.tt(TT1[:], TT1[:], TT2[:], ALU.subtract, ['TT1', 'TT2'], ['TT1'])
                        self.tt(ZI[:], TT1[:], DEN[:], ALU.mult, ['TT1', 'DEN'], ['ZI'])
                        zrb = ZR[:].unsqueeze(2).to_broadcast([64, 32, 16])
                        zib = ZI[:].unsqueeze(2).to_broadcast([64, 32, 16])
                        self.tt(BBR[:], BRE[:], zrb, ALU.mult, ['BRE', 'ZR'], ['BBR'])
                        self.tt(TB3[:], BIM[:], zib, ALU.mult, ['BIM', 'ZI'], ['TB3'])
                        self.tt(BBR[:], BBR[:], TB3[:], ALU.subtract, ['BBR', 'TB3'], ['BBR'])
                        self.tt(BBI[:], BIM[:], zrb, ALU.mult, ['BIM', 'ZR'], ['BBI'])
                        self.tt(TB3[:], BRE[:], zib, ALU.mult, ['BRE', 'ZI'], ['TB3'])
                        self.tt(BBI[:], BBI[:], TB3[:], ALU.add, ['BBI', 'TB3'], ['BBI'])
                        mk = MASK8[:].unsqueeze(2).to_broadcast([128, 8, 64])
                        for ri, (bb, bd) in enumerate([(BBR, BDR), (BBI, BDI)]):
                            for q in range(4):
                                bank = (ri * 4 + q) % 2
                                self.tr(PS[bank][:, 0:64], bb[:, q * 8:(q + 1) * 8, :].rearrange("p g c -> p (g c)"), self.IDF[0:64, 0:64],
                                        ['BBR', 'BBI', 'IDF'], [('ps', bank)])
                                self.cp(TRS[:], PS[bank][:, 0:64], [('ps', bank)], ['TRS'])
                                self.tt(bd[:, q, :].rearrange("p (g n) -> p g n", n=64), TRS[:].unsqueeze(1).to_broadcast([128, 8, 64]), mk, ALU.mult,
                                        ['TRS', 'MASK8'], [('BD', ri)])
                        self.memset(CPR[:], 0.0, [('CP', 0)], eng='pool')
                        self.memset(CPI[:], 0.0, [('CP', 1)], eng='pool')
                        self.memset(CPRN[:], 0.0, [('CP', 2)], eng='pool')
                        for ri, cp_ in enumerate([CPR, CPI]):
                            for g2 in range(2):
                                bank = 2 + (ri * 2 + g2) % 2
                                for pr in range(16):
                                    g = 2 * pr + g2
                                    self.tr(PS[bank][:].bitcast(BF16)[g2 * 64:(g2 + 1) * 64, pr * 16:(pr + 1) * 16], CC[:, ri, g, :], self.IDB[0:16, 0:16], ['CC', 'IDB'], [('ps', bank)])
                                for pq in range(4):
                                    src = PS[bank][:].bitcast(BF16)[g2 * 64:(g2 + 1) * 64, 0:256].rearrange("p (q r c) -> p q r c", q=4, r=4)[:, :, pq, :]
                                    c0 = (2 * pq + g2) * 16
                                    dst = cp_[g2 * 64:(g2 + 1) * 64, :, c0:c0 + 16].rearrange("p (q r) c -> p q r c", r=4)[:, :, pq, :]
                                    self.act(dst, src, AF.Copy, [('ps', bank)], [('CP', ri)], scale=(1.0 if ri == 0 else -1.0))
                                    if ri == 0:
                                        dstn = CPRN[g2 * 64:(g2 + 1) * 64, :, c0:c0 + 16].rearrange("p (q r) c -> p q r c", r=4)[:, :, pq, :]
                                        self.act(dstn, src, AF.Copy, [('ps', bank)], [('CP', 2)], scale=-1.0)
                        p.barrier()
                    if self.stop == 's5tab' and d == 0:
                        self.tap("KR", KR[:], [128, 2048], BF16, []); self.tap("KI", KI[:], [128, 2048], BF16, [])
                        self.tap("QR", QR[:], [128, 16, 128], F32, []); self.tap("QI", QI[:], [128, 16, 128], F32, [])
                        self.tap("BDR", BDR[:], [128, 4, 512], BF16, []); self.tap("BDI", BDI[:], [128, 4, 512], BF16, [])
                        self.tap("CPR", CPR[:], [128, 16, 128], BF16, []); self.tap("CPI", CPI[:], [128, 16, 128], BF16, [])
                        return
                    BUR = [D("BUR%d" % i, [128, 512], BF16) for i in range(3)]; BUI = [D("BUI%d" % i, [128, 512], BF16) for i in range(3)]
                    P1S = [[D("P1%d%d" % (j, i), [128, 512], BF16) for i in range(4)] for j in range(3)]
                    HR = [D("HR%d" % i, [128, 4, 128], BF16) for i in range(2)]; HI = [D("HI%d" % i, [128, 4, 128], BF16) for i in range(2)]
                    P2S = [[D("P2%d%d" % (j, i), [128, 4, 128], BF16) for i in range(4)] for j in range(2)]
                    H0S = [[D("H0S%d%d" % (i, q), [128, 2, 4]) for q in range(4)] for i in range(2)]
                    ZC = D("ZC", [128, 1])
                    self.memset(ZC[:], 0.0, ['ZC'])
                    tinc = 0 if d == 0 else 3
                    lastcol = 127 if d == 0 else 0
                    units = [(step, q) for step in range(NCH) for q in range(4)]

                    def stage_a(ui):
                        step, q = units[ui]
                        ch = order[d][step]
                        tsl = slice(ch * 128, (ch + 1) * 128)
                        s2 = ui % 3
                        ba, bb_ = (0, 1) if ui % 2 == 0 else (6, 7)
                        self.mm(PS[ba][:, :], UT[:, q, tsl], BDR[:, q, :], True, True, [('UT', ch), ('BD', 0)], [('ps', ba)])
                        self.mm(PS[bb_][:, :], UT[:, q, tsl], BDI[:, q, :], True, True, [('UT', ch), ('BD', 1)], [('ps', bb_)])
                        self.act(BUR[s2][:], PS[ba][:, :], AF.Copy, [('ps', ba)], [('BUR', s2)])
                        self.act(BUI[s2][:], PS[bb_][:, :], AF.Copy, [('ps', bb_)], [('BUI', s2)])
                        kr = KR[:, q * 512:(q + 1) * 512]
                        ki = KI[:, q * 512:(q + 1) * 512]
                        P1 = P1S[s2]
                        self.tt(P1[0][:], kr, BUR[s2][:], ALU.mult, ['KR', ('BUR', s2)], [('P1', s2, 0)])
                        self.tt(P1[2][:], kr, BUI[s2][:], ALU.mult, ['KR', ('BUI', s2)], [('P1', s2, 2)], eng='pool')
                        self.tt(P1[1][:], ki, BUI[s2][:], ALU.mult, ['KI', ('BUI', s2)], [('P1', s2, 1)])
                        self.tt(P1[3][:], ki, BUR[s2][:], ALU.mult, ['KI', ('BUR', s2)], [('P1', s2, 3)])

                    def stage_b(ui):
                        step, q = units[ui]
                        ch = order[d][step]
                        tsl = slice(ch * 128, (ch + 1) * 128)
                        s2 = ui % 2
                        s3 = ui % 3
                        par = step % 2
                        P2 = P2S[s2]
                        h0p = H0S[1 - par][q]
                        P1 = P1S[s3]
                        tneg = TRIBN[:, 0 if d == 0 else 1, :]
                        for pq in range(4):
                            cs_ = slice(pq * 128, (pq + 1) * 128)
                            self.mm(PS[2][:, cs_], P1[0][:, cs_], TRIB[:, tinc, :], pq == 0, False, [('P1', s3, 0), 'TRIB'], [('ps', 2)], skip_group_check=True)
                            self.mm(PS[2][:, cs_], P1[1][:, cs_], tneg, False, pq == 3, [('P1', s3, 1), 'TRIBN'], [('ps', 2)], skip_group_check=True)
                        for pq in range(4):
                            cs_ = slice(pq * 128, (pq + 1) * 128)
                            self.mm(PS[3][:, cs_], P1[2][:, cs_], TRIB[:, tinc, :], pq == 0, False, [('P1', s3, 2), 'TRIB'], [('ps', 3)], skip_group_check=True)
                            self.mm(PS[3][:, cs_], P1[3][:, cs_], TRIB[:, tinc, :], False, pq == 3, [('P1', s3, 3), 'TRIB'], [('ps', 3)], skip_group_check=True)
                        for pq in range(4):
                            if step == 0:
                                br, bi = ZC[:, 0:1], ZC[:, 0:1]
                                rk = ['ZC']
                            else:
                                br, bi = h0p[:, 0, pq:pq + 1], h0p[:, 1, pq:pq + 1]
                                rk = [('H0S', 1 - par, q)]
                            self.act(HR[s2][:, pq, :], PS[2][:, pq * 128:(pq + 1) * 128], AF.Identity, [('ps', 2)] + rk, [('HR', s2)], bias=br, scale=1.0)
                            self.act(HI[s2][:, pq, :], PS[3][:, pq * 128:(pq + 1) * 128], AF.Identity, [('ps', 3)] + rk, [('HI', s2)], bias=bi, scale=1.0)
                        qr = QR[:, 4 * q:4 * q + 4, :]
                        qi = QI[:, 4 * q:4 * q + 4, :]
                        self.tt(P2[0][:], qr, HR[s2][:], ALU.mult, ['Q', ('HR', s2)], [('P2', s2, 0)])
                        self.tt(P2[2][:], qr, HI[s2][:], ALU.mult, ['Q', ('HI', s2)], [('P2', s2, 2)], eng='pool')
                        self.tt(P2[1][:], qi, HI[s2][:], ALU.mult, ['Q', ('HI', s2)], [('P2', s2, 1)])
                        self.tt(P2[3][:], qi, HR[s2][:], ALU.mult, ['Q', ('HR', s2)], [('P2', s2, 3)], eng='pool')
                        self.tt(H0S[par][q][:, 0, :], P2[0][:, :, lastcol], P2[1][:, :, lastcol], ALU.subtract, [('P2', s2, 0), ('P2', s2, 1)], [('H0S', par, q)])
                        self.tt(H0S[par][q][:, 1, :], P2[2][:, :, lastcol], P2[3][:, :, lastcol], ALU.add, [('P2', s2, 2), ('P2', s2, 3)], [('H0S', par, q)])

                    def stage_c(ui):
                        step, q = units[ui]
                        ch = order[d][step]
                        tsl = slice(ch * 128, (ch + 1) * 128)
                        s2 = ui % 2
                        if ch >= 2:
                            yb_ = 4 + step % 2
                            for pq in range(4):
                                pr = 4 * q + pq
                                P2 = P2S[s2]
                                ys_ = PS[yb_][:, q * 128:(q + 1) * 128]
                                self.mm(ys_, CPR[:, pr, :], P2[0][:, pq, :], q == 0 and pq == 0, False, [('CP', 0), ('P2', s2, 0)], [('ps', yb_)], skip_group_check=True)
                                self.mm(ys_, CPRN[:, pr, :], P2[1][:, pq, :], False, False, [('CP', 2), ('P2', s2, 1)], [('ps', yb_)], skip_group_check=True)
                                self.mm(ys_, CPI[:, pr, :], P2[2][:, pq, :], False, False, [('CP', 1), ('P2', s2, 2)], [('ps', yb_)], skip_group_check=True)
                                self.mm(ys_, CPI[:, pr, :], P2[3][:, pq, :], False, q == 3 and pq == 3, [('CP', 1), ('P2', s2, 3)], [('ps', yb_)], skip_group_check=True)
                            if q == 3:
                                yv = YT[:, :, tsl]
                                pv = PS[yb_][:, :].rearrange("p (q t) -> p q t", t=128)
                                if d == 0:
                                    self.cp(yv, pv, [('ps', yb_)], [('YT', ch)])
                                else:
                                    self.tt(yv, pv, yv, ALU.add, [('ps', yb_), ('YT', ch)], [('YT', ch)])

                    stage_a(0)
                    stage_a(1)
                    for ui in range(len(units)):
                        if ui + 2 < len(units):
                            stage_a(ui + 2)
                        stage_b(ui)
                        if ui >= 1:
                            stage_c(ui - 1)
                    stage_c(len(units) - 1)
                    p.barrier()
            if self.stop == 's5':
                return
            with self.scope() as so:
                O = lambda name, shape, dt=F32: self.sb(so, name, shape, dt)
                GLW = O("GLW", [128, 4, 512], BF16); STG = O("STG", [128, 128]); PR1 = O("PR1", [128, 8])
                TT_ = [O("TTo%d" % i, [128, 512]) for i in range(2)]; T2_ = [O("T2o%d" % i, [128, 512]) for i in range(2)]
                GTt = O("GTt", [128, 4, 512], BF16); SGo = [O("SGo%d" % i, [128, 512], BF16) for i in range(2)]
                self.dmas('pool', [(GLW[:], self.s5_glu_w[0].rearrange("(k p) n -> p k n", p=128))], writes=['GLW'])
                self.memset(STG[:], 0.0, ['STG'])
                self.dmas('sp', [(STG[0:4, :], self.s5_d[0].rearrange("(k p) -> k p", p=128)),
                                 (STG[4:8, :], self.s5_glu_b[0].rearrange("(k p) -> k p", p=128))], reads=['STG'], writes=['STG'])
                self.tr(PS[0][:, 0:128], STG[:], self.IDF[:], ['STG', 'IDF'], [('ps', 0)])
                self.cp(PR1[:], PS[0][:, 0:8], [('ps', 0)], ['PR1'])
                for ti, (t0, n) in enumerate(TILES[1:]):
                    yk = tk('YT', t0, n)
                    for c in range(4):
                        s2 = c % 2
                        xg = TT_[s2]
                        self.stt(xg[:], UT[:, c, t0:t0 + n], PR1[:, c:c + 1], YT[:, c, t0:t0 + n], ALU.mult, ALU.add, tk('UT', t0, n) + yk + ['PR1'], [('TTo', s2)])
                        self.tt(T2_[s2][:], xg[:], xg[:], ALU.mult, [('TTo', s2)], [('T2o', s2)], eng='pool')
                        self.ts(T2_[s2][:], T2_[s2][:], 0.044715, 1.0, ALU.mult, ALU.add, [('T2o', s2)], [('T2o', s2)])
                        self.tt(T2_[s2][:], T2_[s2][:], xg[:], ALU.mult, [('T2o', s2), ('TTo', s2)], [('T2o', s2)], eng='pool')
                        self.act(T2_[s2][:], T2_[s2][:], AF.Sigmoid, [('T2o', s2)], [('T2o', s2)], scale=1.5957691)
                        self.tt(GTt[:, c, :], xg[:], T2_[s2][:], ALU.mult, [('TTo', s2), ('T2o', s2)], [('GTt', c)])
                    for c in range(4):
                        bank = 1 + c % 2
                        for k in range(4):
                            self.mm(PS[bank][:, :n], GLW[:, k, c * 128:(c + 1) * 128], GTt[:, k, :], k == 0, k == 3, ['GLW', ('GTt', k)], [('ps', bank)])
                        self.act(SGo[c % 2][:], PS[bank][:, :n], AF.Sigmoid, [('ps', bank), 'PR1'], [('SGo', c % 2)], bias=PR1[:, 4 + c:5 + c], scale=1.0)
                        self.tt(self.MIXT[:, c, t0:t0 + n], GTt[:, c, :], SGo[c % 2][:], ALU.mult, [('GTt', c), ('SGo', c % 2)], yk + tk('MIXT', t0, n))
                p.barrier()

    def ssd(self, b):
        p, PS, nc = self.p, self.PS, self.nc
        w_in = self.od_w_in[0].rearrange("(k p) n -> p k n", p=128)
        order = [list(range(NCH)), [1, 0] + list(range(NCH - 1, 1, -1))]
        with self.scope() as ph:
            P = lambda name, shape, dt=F32: self.sb(ph, name, shape, dt)
            XTOK = P("XTOK", [128, NCH, 768], BF16); YS = P("YS", [128, NCH, 512], BF16)
            DT = P("DTs", [128, NCH, 16]); DA = P("DAs", [128, NCH, 16]); LNDT = P("LNDT", [128, NCH, 16])
            GJ = P("GJs", [128, NCH, 2, 8]); BIASD = P("BIASD", [128, NCH, 2, 8]); WE = P("WEs", [128, NCH, 2, 8]); DEC = P("DECs", [128, NCH, 2, 8])
            PRM = P("PRMs", [128, 48]); DTB = P("DTBs", [128, 16]); AN = P("ANs", [128, 16]); DSK = P("DSK", [128, 8]); GN = P("GNs", [128, 512])
            ONEC = P("ONECs", [128, 1]); WDT = P("WDT", [128, 8, 16], BF16); TRIB = P("TRIBs", [128, 6, 128], BF16)
            markA = self.alo
            BCT = P("BCT", [128, 4, NT], BF16)
            self.memset(ONEC[:], 1.0, ['ONEC'])
            self.dmas('pool', [(WDT[:], w_in[:, :, 2048:2064]), (TRIB[:], self.k_tri.rearrange("a p n -> p a n"))], writes=['WDT', 'TRIB'])
            self.dmas('sp', [(DTB[:], self.ssd_dt_bias[0].rearrange("d h -> (d h)").partition_broadcast(128)),
                             (AN[:], self.ssd_a_log[0].rearrange("d h -> (d h)").partition_broadcast(128)),
                             (DSK[:], self.ssd_d[0].partition_broadcast(128)),
                             (GN[:], self.ssd_norm[0].partition_broadcast(128))], writes=['DTB', 'AN', 'DSK', 'GN'])
            self.act(AN[:], AN[:], AF.Exp, ['AN'], ['AN'])
            self.ts(AN[:], AN[:], -1.0, None, ALU.mult, None, ['AN'], ['AN'])
            with self.scope() as cs:
                C_ = lambda name, shape, dt=F32: self.sb(cs, name, shape, dt)
                STG = C_("STGc", [128, 128]); RAW = C_("RAW", [128, NT]); AC = C_("AC", [128, NT]); XFM = C_("XFM", [128, NT], BF16)
                WX = [C_("WX%d" % i, [128, 8, 128], BF16) for i in range(2)]
                self.memset(STG[:], 0.0, ['STG'])
                self.dmas('sp', [(STG[0:24, :], self.ssd_conv_w[0].rearrange("j (k p) -> (j k) p", p=128)),
                                 (STG[24:32, :], self.ssd_conv_b[0].rearrange("(k p) -> k p", p=128))], reads=['STG'], writes=['STG'])
                self.tr(PS[0][:, 0:128], STG[:], self.IDF[:], ['STG', 'IDF'], [('ps', 0)])
                self.cp(PRM[:, 0:32], PS[0][:, 0:32], [('ps', 0)], ['PRM'])
                for ch in range(NCH):
                    bank = 1 + ch % 2
                    hk = tk('HT', ch * 128, 128)
                    for k in range(8):
                        self.mm(PS[bank][:, 0:16], self.HT[:, k, ch * 128:(ch + 1) * 128], WDT[:, k, :], k == 0, k == 7, hk + ['WDT'], [('ps', bank)])
                    self.tt(DT[:, ch, :], PS[bank][:, 0:16], DTB[:], ALU.add, [('ps', bank), 'DTB'], [('DT', ch)])
                    self.act(DT[:, ch, :], DT[:, ch, :], AF.Exp, [('DT', ch)], [('DT', ch)])
                    self.act(DT[:, ch, :], DT[:, ch, :], AF.Ln, [('DT', ch)], [('DT', ch)], bias=ONEC[:, 0:1], scale=1.0)
                    self.act(LNDT[:, ch, :], DT[:, ch, :], AF.Ln, [('DT', ch)], [('LNDT', ch)])
                    self.tt(DA[:, ch, :], DT[:, ch, :], AN[:], ALU.mult, [('DT', ch), 'AN'], [('DA', ch)])
                    for d in range(2):
                        bk = 3 + d
                        rhs = DA[:, ch, d * 8:(d + 1) * 8]
                        self.mm(PS[bk][:, 0:8], self.TRI[:, 3 * d + 0, :], rhs, True, True, [('DA', ch), 'TRI'], [('ps', bk)])
                        self.mm(PS[bk][:, 8:16], self.TRI[:, 3 * d + 1, :], rhs, True, True, [('DA', ch), 'TRI'], [('ps', bk)])
                        self.mm(PS[bk][:, 16:24], self.ONESF[:], rhs, True, True, [('DA', ch), 'ONESF'], [('ps', bk)])
                        li = LNDT[:, ch, d * 8:(d + 1) * 8]
                        self.act(GJ[:, ch, d, :], PS[bk][:, 0:8], AF.Exp, [('ps', bk)], [('GJ', ch, d)])
                        self.tt(BIASD[:, ch, d, :], li, PS[bk][:, 0:8], ALU.subtract, [('ps', bk), ('LNDT', ch)], [('BIASD', ch, d)])
                        self.tt(WE[:, ch, d, :], li, PS[bk][:, 8:16], ALU.add, [('ps', bk), ('LNDT', ch)], [('WE', ch, d)])
                        self.act(WE[:, ch, d, :], WE[:, ch, d, :], AF.Exp, [('WE', ch, d)], [('WE', ch, d)])
                        self.act(DEC[:, ch, d, :], PS[bk][:, 16:24], AF.Exp, [('ps', bk)], [('DEC', ch, d)])
                for k8 in range(8):
                    wx = WX[k8 % 2]
                    self.dmas('pool', [(wx[:], w_in[:, :, 1024 + k8 * 128:1024 + (k8 + 1) * 128])], writes=[('WX', k8 % 2)])
                    for ti, (t0, n) in enumerate(TILES):
                        bank = 5 + ti % 3
                        hk = tk('HT', t0, n)
                        for k in range(8):
                            self.mm(PS[bank][:, :n], wx[:, k, :], self.HT[:, k, t0:t0 + n], k == 0, k == 7, hk + [('WX', k8 % 2)], [('ps', bank)])
                        self.act(RAW[:, t0:t0 + n], PS[bank][:, :n], AF.Copy, [('ps', bank)], ['RAW'])
                    w0, w1, w2, cb = PRM[:, k8:k8 + 1], PRM[:, 8 + k8:9 + k8], PRM[:, 16 + k8:17 + k8], PRM[:, 24 + k8:25 + k8]
                    self.act(AC[:], RAW[:], AF.Identity, ['RAW', 'PRM'], ['AC'], bias=cb, scale=w1)
                    for (a0, a1) in [(0, 256), (256, NT)]:
                        self.stt(AC[:, a0 + 1:a1], RAW[:, a0:a1 - 1], w0, AC[:, a0 + 1:a1], ALU.mult, ALU.add, ['RAW', 'AC', 'PRM'], ['AC'])
                        self.stt(AC[:, a0:a1 - 1], RAW[:, a0 + 1:a1], w2, AC[:, a0:a1 - 1], ALU.mult, ALU.add, ['RAW', 'AC', 'PRM'], ['AC'])
                    dstfm = XFM[:] if k8 < 4 else BCT[:, k8 - 4, :]
                    dkey = ['XFM'] if k8 < 4 else [('BCT', k8 - 4)]
                    self.act(dstfm, AC[:], AF.Silu, ['AC'], dkey)
                    if k8 < 6:
                        for c4 in range(0, NCH, 4):
                            nn = min(4, NCH - c4)
                            bank = (c4 // 4) % 2
                            pv = PS[bank][:].bitcast(BF16)
                            for i in range(nn):
                                ch = c4 + i
                                self.tr(pv[:, i * 128:(i + 1) * 128], dstfm[:, ch * 128:(ch + 1) * 128], self.IDB[:], dkey + ['IDB'], [('ps', bank)])
                            self.act(XTOK[:, c4:c4 + nn, k8 * 128:(k8 + 1) * 128], pv[:, 0:nn * 128].rearrange("p (a t) -> p a t", t=128), AF.Copy,
                                     [('ps', bank)], [('XTOK', c4 + i) for i in range(nn)])
                p.barrier()
            if self.stop == 'ssdprep':
                self.tap("XTOK", XTOK[:], [128, NCH, 768], BF16, []); self.tap("BCT", BCT[:], [128, 4, NT], BF16, [])
                self.tap("DTs", DT[:], [128, NCH, 16], F32, [])
                return
            STALL = P("STALL", [128, NCH, 2, 128], BF16)
            HS = [P("HSs%d" % d, [128, 8, 64]) for d in range(2)]; HSB = [P("HSB%d" % d, [128, 8, 64], BF16) for d in range(2)]
            LFB = [P("LFBs%d" % i, [128, 128]) for i in range(4)] * 2; DTm = [P("DTm%d" % i, [128, 128], BF16) for i in range(8)]
            PT = [P("PTs%d" % i, [128, 128], BF16) for i in range(32)]
            XW = [P("XWs0", [128, 8, 64], BF16)] * 2; TBs = [P("TBs0", [128, 8, 64])] * 2
            for ch in range(NCH):
                bank = ch % 2
                tsl = slice(ch * 128, (ch + 1) * 128)
                for g in range(2):
                    self.mm(PS[bank][:, g * 128:(g + 1) * 128], BCT[:, g, tsl], BCT[:, 2 + g, tsl], True, True, [('BCT', g), ('BCT', 2 + g)], [('ps', bank)])
                self.act(STALL[:, ch, :, :], PS[bank][:, 0:256].rearrange("p (g t) -> p g t", t=128), AF.Copy, [('ps', bank)], [('STALL', ch)])
            self.memset(YS[:], 0.0, [('YS', c) for c in range(NCH)])
            for d in range(2):
                self.memset(HS[d][:], 0.0, [('HS', d)])
                self.memset(HSB[d][:], 0.0, [('HSB', d)], eng='pool')
            def ssd_front(step):
                chs = [order[d][step] for d in range(2)]
                for rnd in range(2):
                    for d in range(2):
                        ch = chs[d]
                        if ch < 2:
                            continue
                        ya = 2 + d * 3
                        db = d
                        hs_ = list(range(4 * rnd, 4 * rnd + 4))
                        sl = lambda h: d * 4 + (h % 4)
                        for h in hs_:
                            self.act(LFB[sl(h)][:], self.ONESF[:], AF.Identity, [('DA', ch), 'ONESF'], [('LFB', sl(h) % 4)], scale=DA[:, ch, d * 8 + h:d * 8 + h + 1])
                        for h in hs_:
                            c0 = (h % 4) * 128
                            self.mm(PS[db][:, c0:c0 + 128], LFB[sl(h)][:], self.TRI[:, 3 * d + 0, :], True, False, [('LFB', sl(h) % 4), 'TRI'], [('ps', db)], skip_group_check=True)
                            self.mm(PS[db][:, c0:c0 + 128], self.IDF[:], self.TRI[:, 3 * d + 2, :], False, True, ['IDF', 'TRI'], [('ps', db)], skip_group_check=True)
                        for h in hs_:
                            c0 = (h % 4) * 128
                            self.act(DTm[sl(h)][:], PS[db][:, c0:c0 + 128], AF.Exp, [('ps', db), ('BIASD', ch, d)], [('DTm', sl(h))], bias=BIASD[:, ch, d, h:h + 1], scale=1.0)
                        for h in hs_:
                            pi = (step % 2) * 16 + d * 8 + h
                            self.tt(PT[pi][:], STALL[:, ch, h // 4, :], DTm[sl(h)][:], ALU.mult, [('STALL', ch), ('DTm', sl(h))], [('PT', d, pi)], eng='pool')

            def ssd_back(step):
                chs = [order[d][step] for d in range(2)]
                for d in range(2):
                    ch = chs[d]
                    tsl = slice(ch * 128, (ch + 1) * 128)
                    ya, yb, ub = 2 + d * 3, 3 + d * 3, 4 + d * 3
                    if ch >= 2:
                        for h in range(8):
                            pi = (step % 2) * 16 + d * 8 + h
                            self.mm(PS[ya][:, h * 64:(h + 1) * 64], PT[pi][:], XTOK[:, ch, h * 64:(h + 1) * 64], h == 0, h == 7, [('PT', d, pi), ('XTOK', ch)], [('ps', ya)],
                                    skip_group_check=True)
                        for g in range(2):
                            self.mm(PS[yb][:, g * 256:(g + 1) * 256], BCT[:, 2 + g, tsl], HSB[d][:, 4 * g:4 * g + 4, :].rearrange("p a c -> p (a c)"), True, True,
                                    [('BCT', 2 + g), ('HSB', d)], [('ps', yb)])
                    self.tt(XW[d][:], XTOK[:, ch, 0:512].rearrange("p (h c) -> p h c", c=64), WE[:, ch, d, :].unsqueeze(2).to_broadcast([128, 8, 64]), ALU.mult,
                            [('XTOK', ch), ('WE', ch, d)], [('XW', 0)])
                    for g in range(2):
                        self.mm(PS[ub][:, g * 256:(g + 1) * 256], XTOK[:, ch, 512 + g * 128:512 + (g + 1) * 128], XW[d][:, 4 * g:4 * g + 4, :].rearrange("p a c -> p (a c)"),
                                True, True, [('XTOK', ch), ('XW', 0)], [('ps', ub)])
                    self.tt(HS[d][:], HS[d][:], DEC[:, ch, d, :].unsqueeze(2).to_broadcast([128, 8, 64]), ALU.mult, [('HS', d), ('DEC', ch, d)], [('HS', d)])
                    self.tt(HS[d][:], PS[ub][:, :].rearrange("p (h c) -> p h c", c=64), HS[d][:], ALU.add, [('ps', ub), ('HS', d)], [('HS', d)])
                    self.act(HSB[d][:], HS[d][:], AF.Copy, [('HS', d)], [('HSB', d)])
                    if ch >= 2:
                        self.tt(TBs[d][:], PS[yb][:, :].rearrange("p (h c) -> p h c", c=64), GJ[:, ch, d, :].unsqueeze(2).to_broadcast([128, 8, 64]), ALU.mult,
                                [('ps', yb), ('GJ', ch, d)], [('TBs', 0)])
                        self.tt(TBs[d][:], PS[ya][:, :].rearrange("p (h c) -> p h c", c=64), TBs[d][:], ALU.add, [('ps', ya), ('TBs', 0)], [('TBs', 0)])
                        ysv = YS[:, ch, :].rearrange("p (h c) -> p h c", c=64)
                        self.tt(ysv, ysv, TBs[d][:], ALU.add, [('YS', ch), ('TBs', 0)], [('YS', ch)], eng='pool')

            ssd_front(0)
            for step in range(NCH):
                if step + 1 < NCH:
                    ssd_front(step + 1)
                ssd_back(step)
            p.barrier()
            if self.stop == 'ssdraw':
                self.tap("YS", YS[:], [128, NCH, 512], BF16, [])
                return
            self.alo = markA
            WZ = P("WZ", [128, 8, 512], BF16)
            YF = [P("YF%d" % i, [128, 512]) for i in range(2)]; SZ = [P("SZ%d" % i, [128, 512]) for i in range(2)]
            JKs = P("JKs", [128, 512]); SSQ = P("SSQs", [128, 2]); OB = [P("OBs%d" % i, [128, 512], BF16) for i in range(2)]
            self.dmas('pool', [(WZ[:, i * 4:(i + 1) * 4, :], w_in[:, i * 4:(i + 1) * 4, 512:1024]) for i in range(2)], writes=['WZ'])
            for ch in range(2, NCH):
                s2 = ch % 2
                zb = ch % 2
                tb = 2 + ch % 2
                hk = tk('HT', ch * 128, 128)
                for k in range(8):
                    self.mm(PS[zb][:, :], self.HT[:, k, ch * 128:(ch + 1) * 128], WZ[:, k, :], k == 0, k == 7, hk + ['WZ'], [('ps', zb)])
                self.act(SZ[s2][:], PS[zb][:, :], AF.Silu, [('ps', zb)], [('SZ', s2)])
                yf = YF[s2]
                self.tt(yf[:].rearrange("p (h c) -> p h c", c=64), XTOK[:, ch, 0:512].rearrange("p (h c) -> p h c", c=64),
                        DSK[:].unsqueeze(2).to_broadcast([128, 8, 64]), ALU.mult, [('XTOK', ch), 'DSK'], [('YF', s2)])
                self.tt(yf[:], yf[:], YS[:, ch, :], ALU.add, [('YF', s2), ('YS', ch)], [('YF', s2)])
                self.tt(yf[:], yf[:], SZ[s2][:], ALU.mult, [('YF', s2), ('SZ', s2)], [('YF', s2)])
                self.act(JKs[:], yf[:], AF.Square, [('YF', s2)], ['JKs'])
                self.p.op('dve', lambda e: e.reduce_sum(out=SSQ[:, 0:1], in_=JKs[:], axis=AX.X), ['JKs'], [('SSQ', 0)])
                self.act(SSQ[:, 0:1], SSQ[:, 0:1], AF.Sqrt, [('SSQ', 0)], [('SSQ', 0)], bias=self.EPSC[:, 0:1], scale=1.0 / 512)
                self.recip(SSQ[:, 1:2], SSQ[:, 0:1], [('SSQ', 0)], [('SSQ', 1)])
                self.stt(OB[s2][:], yf[:], SSQ[:, 1:2], GN[:], ALU.mult, ALU.mult, [('YF', s2), ('SSQ', 1), 'GN'], [('OB', s2)])
                pv = PS[tb][:].bitcast(BF16)
                for j in range(4):
                    self.tr(pv[:, j * 128:(j + 1) * 128], OB[s2][:, j * 128:(j + 1) * 128], self.IDB[:], [('OB', s2), 'IDB'], [('ps', tb)])
                self.act(self.MIXT[:, 4:8, ch * 128:(ch + 1) * 128], pv[:, 0:512].rearrange("p (a t) -> p a t", t=128), AF.Copy, [('ps', tb)], [('MIXT', ch)])
            p.barrier()

def make_consts():
    tri = np.zeros((6, 128, 128), np.float32)
    i = np.arange(128)
    tri[0] = (i[:, None] <= i[None, :])
    tri[1] = (i[:, None] > i[None, :])
    tri[2] = np.where(i[None, :] >= i[:, None], 0.0, -30000.0)
    tri[3] = (i[:, None] >= i[None, :])
    tri[4] = (i[:, None] < i[None, :])
    tri[5] = np.where(i[None, :] <= i[:, None], 0.0, -30000.0)
    t = np.arange(2048)
    row = (t // 64).astype(np.float32)
    col = (t % 64).astype(np.float32)
    inv = (1.0 / (np.float32(10000.0) ** (np.arange(16, dtype=np.float32) / np.float32(16)))).astype(np.float32)
    ar = row[:, None] * inv
    ac = col[:, None] * inv
    cosr, sinr, cosc, sinc = np.cos(ar), np.sin(ar), np.cos(ac), np.sin(ac)
    rope = np.zeros((2, 2048, 64), np.float32)
    rope[0] = np.concatenate([cosr, cosr, cosc, cosc], -1)
    rope[1] = np.concatenate([-sinr, sinr, -sinc, sinc], -1)
    sel = np.zeros((16, 16, 128), np.float32)
    for e in range(16):
        sel[e, e, :] = 1.0
    mask8 = (np.arange(128)[:, None] // 16 == np.arange(8)[None, :]).astype(np.float32)
    pidx = np.arange(128, dtype=np.float32)
    posc = np.stack([pidx + 1, 128 - pidx, -(pidx + 1), -(128 - pidx)], 1).astype(np.float32)
    posr = np.zeros((128, 2, 128), np.float32)
    posr[:, 0, :] = (pidx + 1)[None, :]
    posr[:, 1, :] = (128 - pidx)[None, :]
    return {"k_ident": np.eye(128, dtype=np.float32), "k_tri": tri, "k_rope": rope, "k_sel": sel,
            "k_mask8": mask8, "k_posc": posc, "k_posr": posr}


def make_in_maps(inputs, nb=NB, ncores=NCORES):
    consts = make_consts()
    maps = []
    for ci in range(ncores):
        m = {}
        for k, v in inputs.items():
            v = np.asarray(v)
            if k in ("x", "c", "ctx"):
                m[k] = np.ascontiguousarray(v[ci * nb:(ci + 1) * nb])
            else:
                m[k] = np.ascontiguousarray(v)
        m.update(consts)
        maps.append(m)
    return maps


def kernel(**inputs):
    bld = Builder()
    nc = bld.build()
    maps = make_in_maps(inputs)
    res = run_bass_kernel_spmd(nc, maps, core_ids=list(range(NCORES)))
    return np.concatenate([r["out"] for r in res.results], axis=0).astype(np.float32)
```
